# Optimizing a Trainium2 kernel written in Bass

```python
import math
import jax, jax.numpy as jnp
from jax import lax
import numpy as np

D_MODEL = 1024
BATCH = 4
SEQ = 8192
DEPTH = 4

CTX_LEN = 256
GRID_W = 64
N_MIXERS = 4
MIXER_DIFF = 0
MIXER_FOURIER = 1
MIXER_NEIGHBOR = 2
MIXER_MLA = 3
Q_BLOCK = 128
ROPE_BASE = 10000.0
NORM_EPS = 1e-6
ADA_CHUNKS = 6
DA_HEAD_DIM = 64
DA_HEADS = D_MODEL // (2 * DA_HEAD_DIM)
FN_GROUPS = 4
NA_HEADS = 16
NA_HEAD_DIM = D_MODEL // NA_HEADS
NA_WIN_ROWS = 8
NA_WIN_COLS = 16
MLA_HEADS = 16
MLA_NOPE_DIM = 64
MLA_ROPE_DIM = 32
MLA_V_DIM = 64
MLA_Q_RANK = 768
MLA_KV_RANK = 256
N_EXPERTS = 16
EXPERT_DIM = 2048
EC_CAPACITY_FACTOR = 2

kernel_name = "hybrid_diffusion_trunk_diffattn_fnet_natten_mla_ecmoe"


def _n_layers_of(kind):
    return len(range(kind, DEPTH, N_MIXERS))


def _rmsnorm(x, g):
    xf = x.astype(jnp.float32)
    y = xf * lax.rsqrt(jnp.mean(xf * xf, axis=-1, keepdims=True) + NORM_EPS)
    return (y * g.astype(jnp.float32)).astype(x.dtype)


def _axial_rope_tables(n_tok, rot_dim):
    t = jnp.arange(n_tok)
    rows = (t // GRID_W).astype(jnp.float32)
    cols = (t % GRID_W).astype(jnp.float32)
    n_freq = rot_dim // 4
    inv_freq = ROPE_BASE ** (-jnp.arange(n_freq, dtype=jnp.float32) / n_freq)
    ang = jnp.concatenate([rows[:, None] * inv_freq, cols[:, None] * inv_freq], axis=-1)
    return jnp.cos(ang), jnp.sin(ang)


def _apply_rope(t, cos, sin):
    half = t.shape[-1] // 2
    t1 = t[..., :half].astype(jnp.float32)
    t2 = t[..., half:].astype(jnp.float32)
    return jnp.concatenate([t1 * cos - t2 * sin, t2 * cos + t1 * sin], axis=-1).astype(t.dtype)


def _to_blocks(t):
    *lead, n, d = t.shape
    return jnp.moveaxis(t.reshape(*lead, n // Q_BLOCK, Q_BLOCK, d), -3, 0)


def _from_blocks(o):
    o = jnp.moveaxis(o, 0, -3)
    *lead, nb, qb, d = o.shape
    return o.reshape(*lead, nb * qb, d)


def _merge_heads(o):
    b, h, n, d = o.shape
    return o.transpose(0, 2, 1, 3).reshape(b, n, h * d)


def _softmax_f32(s):
    return jax.nn.softmax(s.astype(jnp.float32), axis=-1)


def _dense_attention(q, k, v, scale):
    p = _softmax_f32(jnp.einsum('bhqd,bhkd->bhqk', q, k) * scale)
    return jnp.einsum('bhqk,bhkd->bhqd', p.astype(v.dtype), v)


def _diff_attention(h_ctx, h_lat, w_qkv, w_o, lam_q1, lam_k1, lam_q2, lam_k2, subln, lambda_init, with_ctx_out):
    scale = DA_HEAD_DIM ** -0.5

    def proj(h):
        b, n, _ = h.shape
        q, k, v = jnp.split(h @ w_qkv, 3, axis=-1)
        q = q.reshape(b, n, DA_HEADS, 2, DA_HEAD_DIM).transpose(3, 0, 2, 1, 4)
        k = k.reshape(b, n, DA_HEADS, 2, DA_HEAD_DIM).transpose(3, 0, 2, 1, 4)
        v = v.reshape(b, n, DA_HEADS, 2 * DA_HEAD_DIM).transpose(0, 2, 1, 3)
        return q, k, v

    q_c, k_c, v_c = proj(h_ctx)
    q_l, k_l, v_l = proj(h_lat)
    cos, sin = _axial_rope_tables(h_lat.shape[1], DA_HEAD_DIM)
    q_l = _apply_rope(q_l, cos, sin)
    k_l = _apply_rope(k_l, cos, sin)
    lam = (jnp.exp(jnp.sum(lam_q1 * lam_k1)) - jnp.exp(jnp.sum(lam_q2 * lam_k2))).astype(jnp.float32) + lambda_init

    def core(q, k, v):
        p = _softmax_f32(jnp.einsum('ibhqd,ibhkd->ibhqk', q, k) * scale)
        return jnp.einsum('bhqk,bhkd->bhqd', (p[0] - lam * p[1]).astype(v.dtype), v)

    def finish(o):
        return _merge_heads(_rmsnorm(o, subln) * (1.0 - lambda_init)) @ w_o

    k_all = jnp.concatenate([k_c, k_l], axis=3)
    v_all = jnp.concatenate([v_c, v_l], axis=2)
    o_l = _from_blocks(lax.map(lambda qb: core(qb, k_all, v_all), _to_blocks(q_l)))
    y_ctx = finish(core(q_c, k_c, v_c)) if with_ctx_out else None
    return y_ctx, finish(o_l)


def _fourier_group_mix(h, w_o):
    b, n, d = h.shape
    hg = h.astype(jnp.float32).reshape(b, n, FN_GROUPS, d // FN_GROUPS).transpose(0, 2, 1, 3)
    y = jnp.fft.fft2(hg, norm="ortho").real
    return y.transpose(0, 2, 1, 3).reshape(b, n, d).astype(h.dtype) @ w_o


def _fourier_mixer(h_ctx, h_lat, w_o, with_ctx_out):
    y_ctx = _fourier_group_mix(h_ctx, w_o) if with_ctx_out else None
    return y_ctx, _fourier_group_mix(h_lat, w_o)


def _neighborhood_attention(h_ctx, h_lat, w_qkv, w_o, rpb, with_ctx_out):
    b, n, _ = h_lat.shape
    rows = n // GRID_W
    kr = min(NA_WIN_ROWS, rows)
    scale = NA_HEAD_DIM ** -0.5

    def proj(h):
        bh, nh, _ = h.shape
        heads = lambda t: t.reshape(bh, nh, NA_HEADS, NA_HEAD_DIM).transpose(0, 2, 1, 3)
        q, k, v = jnp.split(h @ w_qkv, 3, axis=-1)
        return heads(q), heads(k), heads(v)

    q_c, k_c, v_c = proj(h_ctx)
    q_l, k_l, v_l = proj(h_lat)
    grid = lambda t: t.reshape(b, NA_HEADS, rows, GRID_W, NA_HEAD_DIM)
    k_g, v_g = grid(k_l), grid(v_l)
    q_rows = jnp.moveaxis(grid(q_l), 2, 0)
    cols = np.arange(GRID_W)
    col_start = np.clip(cols - NA_WIN_COLS // 2, 0, GRID_W - NA_WIN_COLS)
    col_idx = col_start[:, None] + np.arange(NA_WIN_COLS)
    col_bias_idx = col_idx - cols[:, None] + NA_WIN_COLS - 1
    rpb_cols = rpb[:, :, col_bias_idx]

    def row_fn(args):
        r, q = args
        r0 = jnp.clip(r - kr // 2, 0, rows - kr)
        kb = lax.dynamic_slice_in_dim(k_g, r0, kr, axis=2)[:, :, :, col_idx]
        vb = lax.dynamic_slice_in_dim(v_g, r0, kr, axis=2)[:, :, :, col_idx]
        bias = rpb_cols[:, r0 + jnp.arange(kr) - r + NA_WIN_ROWS - 1].transpose(0, 2, 1, 3)
        s_win = jnp.einsum('bhqd,bhrqjd->bhqrj', q, kb).astype(jnp.float32) * scale + bias.astype(jnp.float32)[None]
        s_ctx = jnp.einsum('bhqd,bhkd->bhqk', q, k_c).astype(jnp.float32) * scale
        p = _softmax_f32(jnp.concatenate([s_win.reshape(b, NA_HEADS, GRID_W, kr * NA_WIN_COLS), s_ctx], axis=-1))
        p = p.astype(v_c.dtype)
        p_win = p[..., :kr * NA_WIN_COLS].reshape(b, NA_HEADS, GRID_W, kr, NA_WIN_COLS)
        p_ctx = p[..., kr * NA_WIN_COLS:]
        return jnp.einsum('bhqrj,bhrqjd->bhqd', p_win, vb) + jnp.einsum('bhqk,bhkd->bhqd', p_ctx, v_c)

    o_rows = lax.map(row_fn, (jnp.arange(rows), q_rows))
    o_l = jnp.moveaxis(o_rows, 0, 2).reshape(b, NA_HEADS, n, NA_HEAD_DIM)
    y_ctx = _merge_heads(_dense_attention(q_c, k_c, v_c, scale)) @ w_o if with_ctx_out else None
    return y_ctx, _merge_heads(o_l) @ w_o


def _mla(h_ctx, h_lat, w_dq, q_norm, w_uq, w_dkv, kv_norm, w_uk, w_uv, w_o, with_ctx_out):
    scale = (MLA_NOPE_DIM + MLA_ROPE_DIM) ** -0.5

    def proj(h):
        b, n, _ = h.shape
        q = (_rmsnorm(h @ w_dq, q_norm) @ w_uq).reshape(b, n, MLA_HEADS, MLA_NOPE_DIM + MLA_ROPE_DIM).transpose(0, 2, 1, 3)
        ckv = h @ w_dkv
        c_kv = _rmsnorm(ckv[..., :MLA_KV_RANK], kv_norm)
        k_rope = ckv[..., MLA_KV_RANK:]
        k_nope = (c_kv @ w_uk).reshape(b, n, MLA_HEADS, MLA_NOPE_DIM).transpose(0, 2, 1, 3)
        v = (c_kv @ w_uv).reshape(b, n, MLA_HEADS, MLA_V_DIM).transpose(0, 2, 1, 3)
        return q[..., :MLA_NOPE_DIM], q[..., MLA_NOPE_DIM:], k_nope, k_rope, v

    qn_c, qr_c, kn_c, kr_c, v_c = proj(h_ctx)
    qn_l, qr_l, kn_l, kr_l, v_l = proj(h_lat)
    cos, sin = _axial_rope_tables(h_lat.shape[1], MLA_ROPE_DIM)
    qr_l = _apply_rope(qr_l, cos, sin)
    kr_l = _apply_rope(kr_l, cos, sin)

    def core(qn, qr, kn, kr, v):
        s = jnp.einsum('bhqd,bhkd->bhqk', qn, kn) + jnp.einsum('bhqd,bkd->bhqk', qr, kr)
        p = _softmax_f32(s * scale)
        return jnp.einsum('bhqk,bhkd->bhqd', p.astype(v.dtype), v)

    kn_all = jnp.concatenate([kn_c, kn_l], axis=2)
    kr_all = jnp.concatenate([kr_c, kr_l], axis=1)
    v_all = jnp.concatenate([v_c, v_l], axis=2)
    o_l = _from_blocks(lax.map(lambda qs: core(qs[0], qs[1], kn_all, kr_all, v_all), (_to_blocks(qn_l), _to_blocks(qr_l))))
    y_ctx = _merge_heads(core(qn_c, qr_c, kn_c, kr_c, v_c)) @ w_o if with_ctx_out else None
    return y_ctx, _merge_heads(o_l) @ w_o


def _expert_choice_moe(h, w_router, w_gate, w_up, w_down):
    b, n, d = h.shape
    cap = EC_CAPACITY_FACTOR * n // N_EXPERTS
    aff = jax.nn.softmax((h @ w_router).astype(jnp.float32), axis=-1)
    gate, idx = lax.top_k(jnp.swapaxes(aff, 1, 2), cap)
    xs = jax.vmap(lambda hb, ib: hb[ib])(h, idx)
    a = jnp.einsum('becd,edf->becf', xs, w_gate)
    u = jnp.einsum('becd,edf->becf', xs, w_up)
    y = jnp.einsum('becf,efd->becd', jax.nn.silu(a) * u, w_down) * gate[..., None].astype(h.dtype)
    return jax.vmap(lambda ib, yb: jnp.zeros((n, d), yb.dtype).at[ib.reshape(-1)].add(yb.reshape(-1, d)))(idx, y)


def setup_inputs(seed: int = 0) -> dict:
    key = jax.random.key(seed)
    keys = iter(jax.random.split(key, 64))
    f32 = jnp.float32

    def normal(shape, std):
        return std * jax.random.normal(next(keys), shape, f32)

    def lin(shape, scale=1.0):
        return normal(shape, scale * shape[-2] ** -0.5)

    def gain(shape):
        return 1.0 + normal(shape, 0.05)

    d = D_MODEL
    n_a, n_b, n_c, n_d = (_n_layers_of(k) for k in range(N_MIXERS))
    return {
        "x": normal((BATCH, SEQ, d), 1.0),
        "c": normal((BATCH, d), 1.0),
        "ctx": normal((BATCH, CTX_LEN, d), 1.0),
        "c_ctx": normal((d,), 1.0),
        "ada_w": lin((DEPTH, d, ADA_CHUNKS * d), 0.5),
        "ada_b": normal((DEPTH, ADA_CHUNKS * d), 0.02),
        "norm_mix": gain((DEPTH, d)),
        "norm_ffn": gain((DEPTH, d)),
        "norm_final": gain((d,)),
        "da_w_qkv": lin((n_a, d, 3 * d)),
        "da_w_o": lin((n_a, d, d)),
        "da_lambda_q1": normal((n_a, DA_HEAD_DIM), 0.1),
        "da_lambda_k1": normal((n_a, DA_HEAD_DIM), 0.1),
        "da_lambda_q2": normal((n_a, DA_HEAD_DIM), 0.1),
        "da_lambda_k2": normal((n_a, DA_HEAD_DIM), 0.1),
        "da_subln": gain((n_a, 2 * DA_HEAD_DIM)),
        "fn_w_o": lin((n_b, d, d)),
        "na_w_qkv": lin((n_c, d, 3 * d)),
        "na_w_o": lin((n_c, d, d)),
        "na_rpb": normal((n_c, NA_HEADS, 2 * NA_WIN_ROWS - 1, 2 * NA_WIN_COLS - 1), 0.1),
        "mla_w_dq": lin((n_d, d, MLA_Q_RANK)),
        "mla_q_norm": gain((n_d, MLA_Q_RANK)),
        "mla_w_uq": lin((n_d, MLA_Q_RANK, MLA_HEADS * (MLA_NOPE_DIM + MLA_ROPE_DIM))),
        "mla_w_dkv": lin((n_d, d, MLA_KV_RANK + MLA_ROPE_DIM)),
        "mla_kv_norm": gain((n_d, MLA_KV_RANK)),
        "mla_w_uk": lin((n_d, MLA_KV_RANK, MLA_HEADS * MLA_NOPE_DIM)),
        "mla_w_uv": lin((n_d, MLA_KV_RANK, MLA_HEADS * MLA_V_DIM)),
        "mla_w_o": lin((n_d, MLA_HEADS * MLA_V_DIM, d)),
        "moe_w_router": lin((DEPTH, d, N_EXPERTS)),
        "moe_w_gate": lin((DEPTH, N_EXPERTS, d, EXPERT_DIM)),
        "moe_w_up": lin((DEPTH, N_EXPERTS, d, EXPERT_DIM)),
        "moe_w_down": lin((DEPTH, N_EXPERTS, EXPERT_DIM, d)),
    }


def reference(x, c, ctx, c_ctx, ada_w, ada_b, norm_mix, norm_ffn, norm_final,
              da_w_qkv, da_w_o, da_lambda_q1, da_lambda_k1, da_lambda_q2, da_lambda_k2, da_subln,
              fn_w_o, na_w_qkv, na_w_o, na_rpb,
              mla_w_dq, mla_q_norm, mla_w_uq, mla_w_dkv, mla_kv_norm, mla_w_uk, mla_w_uv, mla_w_o,
              moe_w_router, moe_w_gate, moe_w_up, moe_w_down):
    for i in range(DEPTH):
        kind, j = i % N_MIXERS, i // N_MIXERS
        keep_ctx = i < DEPTH - 1
        mod = jax.nn.silu(c) @ ada_w[i] + ada_b[i]
        mod_c = jax.nn.silu(c_ctx) @ ada_w[i] + ada_b[i]
        sh1, sc1, g1, sh2, sc2, g2 = jnp.split(mod[:, None, :], ADA_CHUNKS, axis=-1)
        csh1, csc1, cg1, csh2, csc2, cg2 = jnp.split(mod_c, ADA_CHUNKS, axis=-1)
        h_lat = _rmsnorm(x, norm_mix[i]) * (1.0 + sc1) + sh1
        h_ctx = _rmsnorm(ctx, norm_mix[i]) * (1.0 + csc1) + csh1
        if kind == MIXER_DIFF:
            lambda_init = 0.8 - 0.6 * math.exp(-0.3 * i)
            y_ctx, y_lat = _diff_attention(h_ctx, h_lat, da_w_qkv[j], da_w_o[j], da_lambda_q1[j], da_lambda_k1[j],
                                           da_lambda_q2[j], da_lambda_k2[j], da_subln[j], lambda_init, keep_ctx)
        elif kind == MIXER_FOURIER:
            y_ctx, y_lat = _fourier_mixer(h_ctx, h_lat, fn_w_o[j], keep_ctx)
        elif kind == MIXER_NEIGHBOR:
            y_ctx, y_lat = _neighborhood_attention(h_ctx, h_lat, na_w_qkv[j], na_w_o[j], na_rpb[j], keep_ctx)
        else:
            y_ctx, y_lat = _mla(h_ctx, h_lat, mla_w_dq[j], mla_q_norm[j], mla_w_uq[j], mla_w_dkv[j], mla_kv_norm[j],
                                mla_w_uk[j], mla_w_uv[j], mla_w_o[j], keep_ctx)
        x = x + g1 * y_lat
        h_lat = _rmsnorm(x, norm_ffn[i]) * (1.0 + sc2) + sh2
        x = x + g2 * _expert_choice_moe(h_lat, moe_w_router[i], moe_w_gate[i], moe_w_up[i], moe_w_down[i])
        if keep_ctx:
            ctx = ctx + cg1 * y_ctx
            h_ctx = _rmsnorm(ctx, norm_ffn[i]) * (1.0 + csc2) + csh2
            ctx = ctx + cg2 * _expert_choice_moe(h_ctx, moe_w_router[i], moe_w_gate[i], moe_w_up[i], moe_w_down[i])
    return _rmsnorm(x, norm_final)
```

```python
import math
from contextlib import ExitStack
import numpy as np
import concourse.bass as bass
import concourse.mybir as mybir
from concourse.bass_utils import run_bass_kernel_spmd

F32 = mybir.dt.float32
BF16 = mybir.dt.bfloat16
I32 = mybir.dt.int32
AF = mybir.ActivationFunctionType
ALU = mybir.AluOpType
AX = mybir.AxisListType

D = 1024
NL = 8192
NC_ = 256
NT = NL + NC_
NTILE = NT // 128
DUMMY = NT
EPS = 1e-6
NEXP = 16
EDIM = 2048
CAP_L = 1024
CAP_C = 32
SLOTS = 1152
GRID_W = 64

ENGS = ["pe", "act", "dve", "pool", "sp"]


class Buf:
    __slots__ = ("w", "r")

    def __init__(self):
        self.w = None
        self.r = {}


def bufs(n):
    return [Buf() for _ in range(n)]


class Lazy:
    def __init__(self, f):
        self.f = f


class _Rec:
    def __init__(self):
        self.calls = []

    def __getattr__(self, name):
        def f(*args, **kw):
            self.calls.append((name, args, kw))
            return self
        return f


def _replay(E, call):
    name, args, kw = call
    args = [a.f() if isinstance(a, Lazy) else a for a in args]
    kw = {k_: (v.f() if isinstance(v, Lazy) else v) for k_, v in kw.items()}
    return getattr(E, name)(*args, **kw)


def _record(fn):
    r = _Rec()
    fn(r)
    assert len(r.calls) == 1, r.calls
    return r.calls[0]


class Sched:
    def __init__(self, nc, es, n_dma=None):
        self.nc = nc
        self.es = es
        self.ckeys = []
        self.prog = {e: [] for e in ENGS}
        self.cnt = {e: 0 for e in ENGS}
        self.waited = {e: {} for e in ENGS}
        self.sem = {}
        for e in ["pe", "act", "dve", "pool"]:
            self.sem[e] = es.enter_context(nc.semaphore("s_" + e))
        n_dma = n_dma or {"sp": 16, "pool": 16, "act": 4}
        self.dkeys = {}
        self.dnext = {}
        self.dval = {}
        for q, n in n_dma.items():
            ks = []
            for i in range(n):
                k = "d_%s_%d" % (q, i)
                self.sem[k] = es.enter_context(nc.semaphore(k))
                self.dval[k] = 0
                ks.append(k)
            self.dkeys[q] = ks
            self.dnext[q] = 0

    def _deps(self, reads, writes):
        deps = {}

        def add(k, v):
            if deps.get(k, 0) < v:
                deps[k] = v
        for b in reads:
            if b.w is not None:
                add(*b.w)
        for b in writes:
            if b.w is not None:
                add(*b.w)
            for k, v in b.r.items():
                add(k, v)
        return deps

    def _waits(self, eng, deps):
        for k, v in deps.items():
            if eng == "pe" and k == "pe":
                continue
            if self.waited[eng].get(k, 0) >= v:
                continue
            self.waited[eng][k] = v
            sem = self.sem[k]
            self.prog[eng].append(lambda E, sem=sem, v=v: E.wait_ge(sem, v))

    def _mark(self, ev, reads, writes):
        k, v = ev
        for b in reads:
            if b.r.get(k, 0) < v:
                b.r[k] = v
        for b in writes:
            b.w = ev
            b.r = {}

    def op(self, eng, fn, reads=(), writes=(), track=True):
        self._waits(eng, self._deps(reads, writes))
        if track:
            self.cnt[eng] += 1
            v = self.cnt[eng]
            sem = self.sem[eng]
            call = _record(fn)
            self.prog[eng].append(lambda E, call=call, sem=sem: _replay(E, call).then_inc(sem, 1))
        else:
            v = self.cnt[eng] + 1
            call = _record(fn)
            self.prog[eng].append(lambda E, call=call: _replay(E, call))
        self._mark((eng, v), reads, writes)

    def dma(self, q, fn, reads=(), writes=()):
        ks = self.dkeys[q]
        key = ks[self.dnext[q] % len(ks)]
        self.dnext[q] += 1
        deps = self._deps(reads, writes)
        if self.dval[key] > 0 and deps.get(key, 0) < self.dval[key]:
            deps[key] = self.dval[key]
        self._waits(q, deps)
        self.dval[key] += 16
        v = self.dval[key]
        sem = self.sem[key]
        call = _record(fn)
        self.prog[q].append(lambda E, call=call, sem=sem: _replay(E, call).then_inc(sem, 16))
        self._mark((key, v), reads, writes)

    def coll(self, fn, reads=(), writes=()):
        key = "cc_%d" % len(self.ckeys)
        self.ckeys.append(key)
        self.sem[key] = self.es.enter_context(self.nc.semaphore(key))
        self._waits("pool", self._deps(reads, writes))
        sem = self.sem[key]
        call = _record(fn)
        self.prog["pool"].append(lambda E, call=call, sem=sem: _replay(E, call).then_inc(sem, 1))
        self.dval[key] = 1
        self._mark((key, 1), reads, writes)

    def raw(self, eng, fn):
        self.prog[eng].append(lambda E, fn=fn: fn(E))

    def barrier(self):
        allev = {}
        for e in ["pe", "act", "dve", "pool"]:
            if self.cnt[e] > 0:
                allev[e] = self.cnt[e]
        for k, v in self.dval.items():
            if v > 0:
                allev[k] = v
        for e in ENGS:
            d = dict(allev)
            self._waits_all(e, d)

    def _waits_all(self, eng, deps):
        for k, v in deps.items():
            if self.waited[eng].get(k, 0) >= v:
                continue
            self.waited[eng][k] = v
            sem = self.sem[k]
            self.prog[eng].append(lambda E, sem=sem, v=v: E.wait_ge(sem, v))

    def emit(self):
        nc = self.nc
        with nc.Block() as block:
            @block.tensor
            def _(E):
                for f in self.prog["pe"]:
                    f(E)

            @block.scalar
            def _(E):
                for f in self.prog["act"]:
                    f(E)

            @block.vector
            def _(E):
                for f in self.prog["dve"]:
                    f(E)

            @block.gpsimd
            def _(E):
                for f in self.prog["pool"]:
                    f(E)

            @block.sync
            def _(E):
                for f in self.prog["sp"]:
                    f(E)


class K:
    pass


LAST_INPUT_NAMES = []


class _Stop(Exception):
    pass


def build(n_layers=4, dbg=False, stop=None, gather=True):
    nc = bass.Bass("TRN2", target_bir_lowering=False)
    del LAST_INPUT_NAMES[:]
    es = ExitStack()
    k = K()
    k.nc = nc
    k.dbg = dbg
    k.uid = 0
    k.stop = stop

    def chk(name):
        if stop == name:
            raise _Stop()
    k.chk = chk
    s = Sched(nc, es)
    k.s = s

    def din(name, shape, dt=F32):
        LAST_INPUT_NAMES.append(name)
        return nc.dram_tensor(name, list(shape), dt, kind="ExternalInput").ap()

    def dscr(name, shape, dt=F32):
        return nc.dram_tensor(name, list(shape), dt).ap()

    k.din = din
    k.dscr = dscr
    I = {}
    IB = {}

    def gin(name, rows, cols):
        assert rows % 8 == 0
        if not gather:
            I[name] = din(name, [rows, cols])
            IB[name] = Buf()
            return
        ext = din(name, [rows // 8, cols])
        loc = dscr(name + "_l", [rows // 8, cols])
        full = dscr(name + "_g", [rows, cols])
        lb, fb = Buf(), Buf()
        s.dma("pool", lambda E: E.dma_start(out=loc[:, :], in_=ext[:, :]), [], [lb])
        s.coll(lambda E: E.collective_compute("AllGather", ALU.bypass, replica_groups=[list(range(8))], ins=[loc[:, :]], outs=[full[:, :]]), [lb], [fb])
        I[name] = full
        IB[name] = fb

    k.gin = gin
    I["xin"] = din("xin", [NT, D])
    I["ccT"] = din("ccT", [128, 8, 2])
    I["ada_b"] = din("ada_b", [4, 6 * D])
    I["norm_mix"] = din("norm_mix", [4, D])
    I["norm_ffn"] = din("norm_ffn", [4, D])
    I["norm_final"] = din("norm_final", [D])
    I["ident"] = din("ident", [128, 128])
    I["tokid"] = din("tokid", [128, NTILE])
    I["metainit"] = din("metainit", [NEXP * SLOTS, 2])
    I["ebase"] = din("ebase", [NEXP, 2])
    I["moe_wr"] = din("moe_wr", [4, D, NEXP])
    I["da_lam"] = din("da_lam", [4, 64])
    I["da_subln"] = din("da_subln", [128, 1])
    for l in range(4):
        gin("ada_w%d" % l, D, 6 * D)
    gin("da_w", D, 3 * D)
    gin("da_wo", D, D)
    gin("rope0c", 128, NT)
    gin("rope0s", 128, NT)
    I["mla_qnorm"] = din("mla_qnorm", [768])
    I["mla_kvnorm"] = din("mla_kvnorm", [256])
    if n_layers >= 2:
        gin("fn_wo", D, D)
        gin("fn_cos", 128, NL)
        gin("fn_sin", 128, NL)
        I["fn_f64"] = din("fn_f64", [64, 192])
        I["fn_cc"] = din("fn_cc", [256, 256])
        I["fn_sc"] = din("fn_sc", [256, 256])
        I["fn_nsc"] = din("fn_nsc", [256, 256])
    if n_layers >= 3:
        gin("na_w", D, 3 * D)
        gin("na_wo", D, D)
        gin("na_bias", NEXP * 128, 2048)
    if n_layers >= 4:
        gin("mla_wdq", D, 768)
        gin("mla_wuq", 768, 1536)
        gin("mla_wdkv", D, 288)
        gin("mla_wuk", 256, D)
        gin("mla_wuv", 256, D)
        gin("mla_wo", D, D)
        gin("rope3c", 96, NT)
        gin("rope3s", 96, NT)
    for l in range(n_layers):
        gin("moe_wg%d" % l, NEXP * D, EDIM)
        gin("moe_wu%d" % l, NEXP * D, EDIM)
        gin("moe_wd%d" % l, NEXP * EDIM, D)
    k.IB = IB
    k.I = I
    out = nc.dram_tensor("out", [NL, D], F32, kind="ExternalOutput").ap()
    k.out = out
    if dbg:
        k.dbg_x = nc.dram_tensor("dbg_x", [NT, D], F32, kind="ExternalOutput").ap()
        k.dbg_t = {}
        for nm, shp in [("qT", [1536, NT]), ("kT", [D, NT]), ("vv", [NT, D]), ("aT", [D, NT])]:
            k.dbg_t[nm] = nc.dram_tensor("dbg_" + nm, shp, BF16, kind="ExternalOutput").ap()
    k.xres = dscr("xres", [NT + 1, D])
    k.xres_b = bufs(NTILE + 1)
    k.modv = dscr("modv", [4, 6, 2, D])
    k.modv_b = Buf()
    k.h2tab = dscr("h2tab", [NT + 1, D], BF16)
    k.h2tab_b = bufs(NTILE + 1)
    k.meta = dscr("meta", [NEXP * SLOTS, 2])
    k.meta_b = bufs(NEXP)
    k.qT = dscr("qT", [1536, NT], BF16)
    k.krT = dscr("krT", [32, NT], BF16)
    k.krT_b = bufs(17)
    k.kT = dscr("kT", [D, NT], BF16)
    k.vv = dscr("vv", [NT, D], BF16)
    k.aT = dscr("aT", [D, NT], BF16)
    NG = 17
    k.NG = NG
    k.qT_b = bufs(NG)
    k.kT_b = bufs(NG)
    k.vv_b = bufs(NG)
    k.aT_b = bufs(NG)

    def sb(name, shape, dt=F32, stack=es):
        k.uid += 1
        return stack.enter_context(nc.sbuf_tensor("sb%d_%s" % (k.uid, name), list(shape), dt))

    def ps(name, shape, dt=F32, stack=es):
        k.uid += 1
        return stack.enter_context(nc.psum_tensor("ps%d_%s" % (k.uid, name), list(shape), dt))

    k.sb = sb
    k.ps = ps
    k.ident = sb("ident", [128, 128])
    k.ident_b = Buf()
    k.identb = sb("identb", [128, 128], BF16)
    k.identb_b = Buf()
    k.ones_f = sb("ones_f", [128, 128])
    k.ones_b = sb("ones_b", [128, 128], BF16)
    k.ones_bb = Buf()
    k.tokid = sb("tokid", [128, NTILE])
    k.tokid_b = Buf()
    s.dma("sp", lambda E: E.dma_start(out=k.ident[:], in_=I["ident"][:, :]), [], [k.ident_b])
    s.dma("sp", lambda E: E.dma_start(out=k.tokid[:], in_=I["tokid"][:, :]), [], [k.tokid_b])
    s.op("dve", lambda E: E.tensor_copy(out=k.identb[:], in_=k.ident[:]), [k.ident_b], [k.identb_b])
    s.op("dve", lambda E: E.memset(k.ones_f[:], 1.0), [], [k.ones_bb])
    s.op("dve", lambda E: E.memset(k.ones_b[:], 1.0), [], [k.ones_bb])
    k.epsc = sb("epsc", [128, 1])
    k.epsc_b = Buf()
    s.op("dve", lambda E: E.memset(k.epsc[:], float(EPS)), [], [k.epsc_b])

    for t in range(NTILE):
        s.dma("sp", lambda E, t=t: E.dma_start(out=k.xres[t * 128:(t + 1) * 128, :], in_=I["xin"][t * 128:(t + 1) * 128, :]),
              [], [k.xres_b[t]])

    try:
        chk("init")
        phase_mod(k)
        if stop is not None and stop.startswith("mod"):
            raise _Stop()
        for l in range(n_layers):
            kind = l % 4
            if kind == 0:
                mixer_diff(k, l)
            elif kind == 1:
                mixer_fourier(k, l, keep_ctx=(l < 3))
            elif kind == 2:
                mixer_na(k, l, keep_ctx=(l < 3))
            elif kind == 3:
                mixer_mla(k, l, keep_ctx=(l < 3))
            chk("mixer%d" % l)
            post_mixer_and_moe(k, l, keep_ctx=(l < 3))
            chk("layer%d" % l)
    except _Stop:
        pass
    if dbg:
        s.barrier()
        for nm, src in [("qT", k.qT), ("kT", k.kT), ("vv", k.vv), ("aT", k.aT)]:
            rows = src.shape[0]
            for r0 in range(0, rows, 128):
                r1 = min(rows, r0 + 128)
                s.dma("sp", lambda E, nm=nm, src=src, r0=r0, r1=r1: E.dma_start(out=k.dbg_t[nm][r0:r1, :], in_=src[r0:r1, :]), [], [])
        for t in range(NTILE):
            s.dma("sp", lambda E, t=t: E.dma_start(out=k.dbg_x[t * 128:(t + 1) * 128, :], in_=k.xres[t * 128:(t + 1) * 128, :]),
                  [k.xres_b[t]], [])
    final_norm(k)
    s.barrier()
    s.emit()
    es.close()
    return nc


def phase_mod(k):
    nc, s, I = k.nc, k.s, k.I
    with ExitStack() as st:
        cc = k.sb("m_cc", [128, 8, 2], stack=st)
        sil = k.sb("m_sil", [128, 8, 2], stack=st)
        cc_b, sil_b = Buf(), Buf()
        wt = [k.sb("m_w%d" % i, [128, 8, 512], stack=st) for i in range(2)]
        wt_b = bufs(2)
        mrow = k.sb("m_row", [2, 6 * D], stack=st)
        mrow_b = Buf()
        adab = k.sb("m_adab", [2, 6 * D], stack=st)
        adab_b = Buf()
        nw = k.sb("m_nw", [2, 2, D], stack=st)
        nw_b = Buf()
        aa = k.sb("m_aa", [2, 2, D], stack=st)
        aa_b = Buf()
        pp = [k.ps("m_ps%d" % i, [2, 512], stack=st) for i in range(2)]
        pp_b = bufs(2)
        s.dma("sp", lambda E: E.dma_start(out=cc[:], in_=I["ccT"][:, :, :]), [], [cc_b])
        s.op("act", lambda E: E.activation(out=sil[:], in_=cc[:], func=AF.Silu), [cc_b], [sil_b])
        if k.stop == "mod_silu":
            s.barrier()
            return
        it = 0
        for l in range(4):
            s.dma("sp", lambda E, l=l: E.dma_start(out=adab[:], in_=I["ada_b"][l:l + 1, :].broadcast_to([2, 6 * D])), [], [adab_b])
            s.dma("sp", lambda E, l=l: E.dma_start(out=nw[:, 0, :], in_=I["norm_mix"][l:l + 1, :].broadcast_to([2, D])), [], [nw_b])
            s.dma("sp", lambda E, l=l: E.dma_start(out=nw[:, 1, :], in_=I["norm_ffn"][l:l + 1, :].broadcast_to([2, D])), [], [nw_b])
            for nb in range(12):
                w = wt[it % 2]
                wb = wt_b[it % 2]
                p = pp[it % 2]
                pb = pp_b[it % 2]
                it += 1
                s.dma("sp", lambda E, l=l, nb=nb, w=w: E.dma_start(
                    out=w[:], in_=I["ada_w%d" % l][:, nb * 512:(nb + 1) * 512].rearrange("(c p) n -> p c n", p=128)), [k.IB["ada_w%d" % l]], [wb])
                for c in range(8):
                    s.op("pe", lambda E, c=c, w=w, p=p: E.matmul(p[:], lhsT=sil[:, c, :], rhs=w[:, c, :], start=(c == 0), stop=(c == 7)),
                         [sil_b, wb], [pb], track=(c == 7))
                s.op("dve", lambda E, nb=nb, p=p: E.tensor_tensor(out=mrow[:, nb * 512:(nb + 1) * 512], in0=p[:], in1=adab[:, nb * 512:(nb + 1) * 512], op=ALU.add),
                     [pb, adab_b], [mrow_b])
                if k.stop == "mod_mm":
                    s.barrier()
                    return
            s.op("dve", lambda E: E.scalar_tensor_tensor(out=aa[:, 0, :], in0=mrow[:, D:2 * D], scalar=1.0, in1=nw[:, 0, :], op0=ALU.add, op1=ALU.mult),
                 [mrow_b, nw_b], [aa_b])
            s.op("dve", lambda E: E.scalar_tensor_tensor(out=aa[:, 1, :], in0=mrow[:, 4 * D:5 * D], scalar=1.0, in1=nw[:, 1, :], op0=ALU.add, op1=ALU.mult),
                 [mrow_b, nw_b], [aa_b])
            srcs = [aa[:, 0, :], mrow[:, 0:D], mrow[:, 2 * D:3 * D], aa[:, 1, :], mrow[:, 3 * D:4 * D], mrow[:, 5 * D:6 * D]]
            for v in range(6):
                s.dma("sp", lambda E, l=l, v=v, src=srcs[v]: E.dma_start(out=k.modv[l, v, :, :], in_=src), [mrow_b, aa_b], [k.modv_b])
            if k.stop == "mod_l0":
                s.barrier()
                return
        s.barrier()


def load_bcast(k, q, dst, dst_b, l, v, cond):
    k.s.dma(q, lambda E: E.dma_start(out=dst, in_=k.modv[l, v, cond:cond + 1, :].broadcast_to([128, D])), [k.modv_b], [dst_b])


def load_weight_bf16(k, st, name, src, src_b, C, ncols, blk=512, swap_cols=0, swap_unit=64):
    s = k.s
    dst = k.sb(name, [128, C, ncols + swap_cols], BF16, stack=st)
    dst_b = Buf()
    if getattr(st, "_wstg", None) is None:
        st._wstg = ([k.sb(name + "_st%d" % i, [128, 4096], stack=st) for i in range(2)], bufs(2), [0])
    stg_full, stg_b, stg_ctr = st._wstg
    stg = [t[:, 0:C * blk].rearrange("p (c n) -> p c n", c=C) for t in stg_full]
    nb = ncols // blk
    hu = swap_unit // 2
    for b in range(nb):
        t = stg[stg_ctr[0] % 2]
        tb = stg_b[stg_ctr[0] % 2]
        stg_ctr[0] += 1
        s.dma("sp", lambda E, b=b, t=t: E.dma_start(out=t, in_=src[:, b * blk:(b + 1) * blk].rearrange("(c p) n -> p c n", p=128)), [src_b], [tb])
        eng = ["pool", "dve"][b % 2]
        s.op(eng, lambda E, b=b, t=t: E.tensor_copy(out=dst[:, :, b * blk:(b + 1) * blk], in_=t), [tb], [dst_b])
        if (b + 1) * blk <= swap_cols:
            for c in range(C):
                for half in range(2):
                    eng2 = ["dve", "pool"][(c + half) % 2]
                    s.op(eng2, lambda E, b=b, t=t, c=c, half=half: E.tensor_copy(
                        out=dst[:, c, ncols + b * blk:ncols + (b + 1) * blk].rearrange("p (u two h) -> p u two h", two=2, h=hu)[:, :, half, :],
                        in_=t[:, c, :].rearrange("p (u two h) -> p u two h", two=2, h=hu)[:, :, 1 - half, :]), [tb], [dst_b])
    return dst, dst_b


def norm_tile(k, xt, xt_b, ht, ht_b, Ab, Ab_b, Bb, Bb_b, scr, scr_b):
    s = k.s
    junk, stat = scr
    s.op("act", lambda E: E.activation(out=junk[:], in_=xt[:], func=AF.Square, accum_out=stat[:, 0:1]), [xt_b], [scr_b])
    s.op("act", lambda E: E.activation(out=stat[:, 1:2], in_=stat[:, 0:1], func=AF.Sqrt, scale=float(1.0 / D), bias=k.epsc[:, 0:1]), [scr_b, k.epsc_b], [scr_b])
    s.op("dve", lambda E: E.reciprocal(out=stat[:, 1:2], in_=stat[:, 1:2]), [scr_b], [scr_b])
    s.op("dve", lambda E: E.scalar_tensor_tensor(out=ht[:], in0=xt[:], scalar=stat[:, 1:2], in1=Ab[:], op0=ALU.mult, op1=ALU.mult),
         [xt_b, scr_b, Ab_b], [ht_b])
    s.op("pool", lambda E: E.tensor_tensor(out=ht[:], in0=ht[:], in1=Bb[:], op=ALU.add), [ht_b, Bb_b], [ht_b])


def transpose_tile(k, ht, ht_b, tp, tp_b, hT, hT_b, col0, ncol=128, nchunk=8, evac=("act", "dve")):
    s = k.s
    for half in range((nchunk + 3) // 4):
        p = tp[half % len(tp)]
        pb = tp_b[half % len(tp)]
        cs = list(range(half * 4, min(nchunk, half * 4 + 4)))
        for j, c in enumerate(cs):
            s.op("pe", lambda E, c=c, j=j, p=p: E.transpose(out=p[:, j * 128:j * 128 + ncol], in_=ht[0:ncol, c * 128:(c + 1) * 128], identity=k.ident[0:ncol, 0:ncol]),
                 [ht_b, k.ident_b], [pb], track=(j == len(cs) - 1))
        eng = evac[half % len(evac)]
        n = len(cs)
        if eng == "act":
            s.op("act", lambda E, p=p, cs=cs, n=n: E.activation(out=hT[:, cs[0]:cs[0] + n, col0:col0 + ncol], in_=p[:, 0:n * 128].rearrange("p (c t) -> p c t", c=n)[:, :, 0:ncol], func=AF.Copy),
                 [pb], [hT_b])
        else:
            s.op("dve", lambda E, p=p, cs=cs, n=n: E.tensor_copy(out=hT[:, cs[0]:cs[0] + n, col0:col0 + ncol], in_=p[:, 0:n * 128].rearrange("p (c t) -> p c t", c=n)[:, :, 0:ncol]),
                 [pb], [hT_b])


def group_range(g):
    t0 = g * 512
    w = min(512, NT - t0)
    return t0, w


def mixer_diff(k, l):
    nc, s, I = k.nc, k.s, k.I
    lam_init = 0.8 - 0.6 * math.exp(-0.3 * l)
    with ExitStack() as st:
        W, W_b = load_weight_bf16(k, st, "d_w", I["da_w"], k.IB["da_w"], 8, 3 * D, swap_cols=2 * D, swap_unit=64)
        Ab = [k.sb("d_Ab%d" % c, [128, D], stack=st) for c in range(2)]
        Bb = [k.sb("d_Bb%d" % c, [128, D], stack=st) for c in range(2)]
        Ab_b, Bb_b = bufs(2), bufs(2)
        for c in range(2):
            load_bcast(k, "sp", Ab[c][:], Ab_b[c], l, 0, c)
            load_bcast(k, "sp", Bb[c][:], Bb_b[c], l, 1, c)
        xt = [k.sb("d_xt%d" % i, [128, D], stack=st) for i in range(2)]
        xt_b = bufs(2)
        ht = [k.sb("d_ht%d" % i, [128, D], stack=st) for i in range(2)]
        ht_b = bufs(2)
        junk = k.sb("d_junk", [128, D], stack=st)
        stat = [k.sb("d_stat%d" % i, [128, 2], stack=st) for i in range(2)]
        scr_b = bufs(2)
        hT = [k.sb("d_hT%d" % i, [128, 8, 512], BF16, stack=st) for i in range(2)]
        hT_b = bufs(2)
        rc = [k.sb("d_rc%d" % i, [128, 512], stack=st) for i in range(2)]
        rs = [k.sb("d_rs%d" % i, [128, 512], stack=st) for i in range(2)]
        rc_b, rs_b = bufs(2), bufs(2)
        t1 = [k.sb("d_t1%d" % i, [128, 512], stack=st) for i in range(2)]
        t2 = [k.sb("d_t2%d" % i, [128, 512], stack=st) for i in range(2)]
        t1_b, t2_b = bufs(2), bufs(2)
        qo = [k.sb("d_qo%d" % i, [128, 512], BF16, stack=st) for i in range(4)]
        qo_b = bufs(4)
        vo = [k.sb("d_vo%d" % i, [128, D], BF16, stack=st) for i in range(2)]
        vo_b = bufs(2)
        tp = [k.ps("d_tp%d" % i, [128, 512], stack=st) for i in range(2)]
        tp_b = bufs(2)
        pq = [k.ps("d_pq%d" % i, [128, 512], stack=st) for i in range(4)]
        pq_b = bufs(4)
        pv = [k.ps("d_pv%d" % i, [128, 512], stack=st) for i in range(2)]
        pv_b = bufs(2)
        ti = 0
        oi = 0
        vi = 0
        for g in range(k.NG):
            t0, w = group_range(g)
            hTg, hTg_b = hT[g % 2], hT_b[g % 2]
            s.dma("sp", lambda E, g=g, t0=t0, w=w: E.dma_start(out=rc[g % 2][:, 0:w], in_=I["rope0c"][:, t0:t0 + w]), [k.IB["rope0c"]], [rc_b[g % 2]])
            s.dma("sp", lambda E, g=g, t0=t0, w=w: E.dma_start(out=rs[g % 2][:, 0:w], in_=I["rope0s"][:, t0:t0 + w]), [k.IB["rope0s"]], [rs_b[g % 2]])
            for j in range(w // 128):
                t = (t0 // 128) + j
                cond = 0 if t < 64 else 1
                x_, x_b = xt[ti % 2], xt_b[ti % 2]
                h_, h_b = ht[ti % 2], ht_b[ti % 2]
                s.dma("sp", lambda E, t=t, x_=x_: E.dma_start(out=x_[:], in_=k.xres[t * 128:(t + 1) * 128, :]), [k.xres_b[t]], [x_b])
                norm_tile(k, x_, x_b, h_, h_b, Ab[cond], Ab_b[cond], Bb[cond], Bb_b[cond], (junk, stat[ti % 2]), scr_b[ti % 2])
                transpose_tile(k, h_, h_b, tp, tp_b, hTg, hTg_b, j * 128)
                ti += 1
            for o in range(16):
                pa, pa_b = pq[(2 * o) % 4], pq_b[(2 * o) % 4]
                pb_, pb_b = pq[(2 * o + 1) % 4], pq_b[(2 * o + 1) % 4]
                for c in range(8):
                    s.op("pe", lambda E, c=c, o=o, pa=pa: E.matmul(pa[:, 0:w], lhsT=W[:, c, o * 128:(o + 1) * 128], rhs=hTg[:, c, 0:w], start=(c == 0), stop=(c == 7)),
                         [W_b, hTg_b], [pa_b], track=(c == 7))
                for c in range(8):
                    s.op("pe", lambda E, c=c, o=o, pb_=pb_: E.matmul(pb_[:, 0:w], lhsT=W[:, c, 3 * D + o * 128:3 * D + (o + 1) * 128], rhs=hTg[:, c, 0:w], start=(c == 0), stop=(c == 7)),
                         [W_b, hTg_b], [pb_b], track=(c == 7))
                a1, a1_b = t1[o % 2], t1_b[o % 2]
                a2, a2_b = t2[o % 2], t2_b[o % 2]
                q_, q_b = qo[oi % 4], qo_b[oi % 4]
                oi += 1
                s.op("dve", lambda E, pa=pa, a1=a1: E.tensor_tensor(out=a1[:, 0:w], in0=pa[:, 0:w], in1=rc[g % 2][:, 0:w], op=ALU.mult), [pa_b, rc_b[g % 2]], [a1_b])
                s.op("dve", lambda E, pb_=pb_, a2=a2: E.tensor_tensor(out=a2[:, 0:w], in0=pb_[:, 0:w], in1=rs[g % 2][:, 0:w], op=ALU.mult), [pb_b, rs_b[g % 2]], [a2_b])
                s.op("pool", lambda E, a1=a1, a2=a2, q_=q_: E.tensor_tensor(out=q_[:, 0:w], in0=a1[:, 0:w], in1=a2[:, 0:w], op=ALU.add), [a1_b, a2_b], [q_b])
                dstT = k.qT if o < 8 else k.kT
                dst_b = (k.qT_b if o < 8 else k.kT_b)[g]
                oo = o % 8
                s.dma("pool", lambda E, dstT=dstT, oo=oo, q_=q_: E.dma_start(out=dstT[oo * 128:(oo + 1) * 128, t0:t0 + w], in_=q_[:, 0:w]), [q_b], [dst_b])
            for j in range(w // 128):
                v_, v_b = vo[vi % 2], vo_b[vi % 2]
                vi += 1
                for nb in range(2):
                    p, p_b = pv[nb], pv_b[nb]
                    for c in range(8):
                        s.op("pe", lambda E, c=c, nb=nb, p=p, j=j: E.matmul(p[:], lhsT=hTg[:, c, j * 128:(j + 1) * 128], rhs=W[:, c, 2 * D + nb * 512:2 * D + (nb + 1) * 512], start=(c == 0), stop=(c == 7)),
                             [W_b, hTg_b], [p_b], track=(c == 7))
                    s.op("act", lambda E, nb=nb, p=p, v_=v_: E.activation(out=v_[:, nb * 512:(nb + 1) * 512], in_=p[:], func=AF.Copy), [p_b], [v_b])
                t = (t0 // 128) + j
                s.dma("pool", lambda E, t=t, v_=v_: E.dma_start(out=k.vv[t * 128:(t + 1) * 128, :], in_=v_[:]), [v_b], [k.vv_b[g]])
        s.barrier()
    k.chk("proj%d" % l)
    with ExitStack() as st:
        lamt = k.sb("a_lamt", [128, 4, 64], stack=st)
        lamt_b = Buf()
        lamp = k.sb("a_lamp", [128, 2, 64], stack=st)
        lamv = k.sb("a_lamv", [128, 4], stack=st)
        lam_b = Buf()
        gcol = k.sb("a_gcol", [128, 1], stack=st)
        gcol_b = Buf()
        s.dma("sp", lambda E: E.dma_start(out=lamt[:].rearrange("p a b -> p (a b)"), in_=I["da_lam"].rearrange("a b -> (a b)").unsqueeze(0).broadcast_to([128, 256])), [], [lamt_b])
        s.dma("sp", lambda E: E.dma_start(out=gcol[:], in_=I["da_subln"][:, :]), [], [gcol_b])
        s.op("dve", lambda E: E.tensor_tensor(out=lamp[:, 0, :], in0=lamt[:, 0, :], in1=lamt[:, 1, :], op=ALU.mult), [lamt_b], [lam_b])
        s.op("dve", lambda E: E.tensor_tensor(out=lamp[:, 1, :], in0=lamt[:, 2, :], in1=lamt[:, 3, :], op=ALU.mult), [lam_b, lamt_b], [lam_b])
        s.op("dve", lambda E: E.tensor_reduce(out=lamv[:, 0:2], in_=lamp[:], axis=AX.X, op=ALU.add), [lam_b], [lam_b])
        s.op("act", lambda E: E.activation(out=lamv[:, 0:2], in_=lamv[:, 0:2], func=AF.Exp), [lam_b], [lam_b])
        s.op("dve", lambda E: E.tensor_tensor(out=lamv[:, 2:3], in0=lamv[:, 1:2], in1=lamv[:, 0:1], op=ALU.subtract), [lam_b], [lam_b])
        s.op("dve", lambda E: E.tensor_scalar(out=lamv[:, 2:3], in0=lamv[:, 2:3], scalar1=float(-lam_init), scalar2=None, op0=ALU.add), [lam_b], [lam_b])
        s.op("dve", lambda E: E.tensor_scalar(out=gcol[:], in0=gcol[:], scalar1=float(1.0 - lam_init), scalar2=None, op0=ALU.mult), [gcol_b], [gcol_b])
        neg_lam = lamv[:, 2:3]

        def post(h, qb, w, O, O_b, Z, Z_b, wk):
            (r0, o0, o1, sq, on, pss, wk_b, pss_b) = wk
            s.op("dve", lambda E: E.reciprocal(out=r0[:, 0:w], in_=Z[0][:, 0:w]), [Z_b[0]], [wk_b])
            s.op("dve", lambda E: E.tensor_tensor(out=o0[:, 0:w], in0=O[0][:, 0:w], in1=r0[:, 0:w], op=ALU.mult), [O_b[0], wk_b], [wk_b])
            s.op("dve", lambda E: E.reciprocal(out=r0[:, 0:w], in_=Z[1][:, 0:w]), [Z_b[1], wk_b], [wk_b])
            s.op("dve", lambda E: E.tensor_tensor(out=o1[:, 0:w], in0=O[1][:, 0:w], in1=r0[:, 0:w], op=ALU.mult), [O_b[1], wk_b], [wk_b])
            s.op("dve", lambda E: E.scalar_tensor_tensor(out=o0[:, 0:w], in0=o1[:, 0:w], scalar=neg_lam, in1=o0[:, 0:w], op0=ALU.mult, op1=ALU.add), [wk_b, lam_b], [wk_b])
            s.op("pool", lambda E: E.tensor_tensor(out=sq[:, 0:w], in0=o0[:, 0:w], in1=o0[:, 0:w], op=ALU.mult), [wk_b], [wk_b])
            s.op("pe", lambda E: E.matmul(pss[:, 0:w], lhsT=k.ones_f[:], rhs=sq[:, 0:w], start=True, stop=True), [wk_b, k.ones_bb], [pss_b])
            s.op("act", lambda E: E.activation(out=r0[:, 0:w], in_=pss[:, 0:w], func=AF.Sqrt, scale=float(1.0 / 128.0), bias=k.epsc[:, 0:1]), [pss_b, wk_b, k.epsc_b], [wk_b])
            s.op("dve", lambda E: E.reciprocal(out=r0[:, 0:w], in_=r0[:, 0:w]), [wk_b], [wk_b])
            s.op("dve", lambda E: E.scalar_tensor_tensor(out=on[:, 0:w], in0=o0[:, 0:w], scalar=gcol[:, 0:1], in1=r0[:, 0:w], op0=ALU.mult, op1=ALU.mult), [wk_b, gcol_b], [wk_b])
            return on

        attention(k, st, n_heads=8, maps=2, kp=64, dv=128, scale=0.125, post=post)
        s.barrier()


def attention(k, st, n_heads, maps, kp, dv, scale, post, krows=None, qrows=None, extra=None, with_ctx_q=True):
    nc, s = k.nc, k.s
    KP = maps * kp if maps > 1 else kp
    krows = krows or KP
    qrows = qrows or KP
    KT = [k.sb("a_KT%d" % i, [128, NT], BF16, stack=st) for i in range(2)]
    KT_b = bufs(2)
    V = [k.sb("a_V%d" % i, [128, NTILE, dv], BF16, stack=st) for i in range(2)]
    V_b = bufs(2)
    Q = [k.sb("a_Q%d" % i, [128, 512], BF16, stack=st) for i in range(2)]
    Q_b = bufs(2)
    P = [k.sb("a_P%d" % i, [128, 512], BF16, stack=st) for i in range(3)]
    P_b = bufs(3)
    r0 = k.sb("a_r0", [128, 512], stack=st)
    o0 = k.sb("a_o0", [128, 512], stack=st)
    o1 = k.sb("a_o1", [128, 512], stack=st)
    sq = k.sb("a_sq", [128, 512], stack=st)
    on = [k.sb("a_on%d" % i, [128, 512], BF16, stack=st) for i in range(2)]
    wk_b = Buf()
    S = [k.ps("a_S%d" % i, [128, 512], stack=st) for i in range(2)]
    S_b = bufs(2)
    O = [k.ps("a_O%d" % i, [128, 512], stack=st) for i in range(2)]
    O_b = bufs(2)
    Z = [k.ps("a_Z%d" % i, [128, 512], stack=st) for i in range(2)]
    Z_b = bufs(2)
    pss = k.ps("a_pss", [128, 512], stack=st)
    pss_b = Buf()
    qblocks = [(g,) + group_range(g) for g in range(k.NG if with_ctx_q else 16)]
    si = 0
    pi = 0
    qi = 0
    oi = 0
    for h in range(n_heads):
        KTh, KTh_b = KT[h % 2], KT_b[h % 2]
        Vh, Vh_b = V[h % 2], V_b[h % 2]
        s.dma("sp", lambda E, h=h, KTh=KTh: E.dma_start(out=KTh[0:krows, :], in_=k.kT[h * krows:(h + 1) * krows, :]), list(k.kT_b), [KTh_b])
        if extra is not None:
            ex_ap, ex_b, ex_rows = extra
            s.dma("sp", lambda E, KTh=KTh: E.dma_start(out=KTh[krows:krows + ex_rows, :], in_=ex_ap[:, :]), list(ex_b), [KTh_b])
        for half in range(2):
            s.dma("sp", lambda E, h=h, Vh=Vh, half=half: E.dma_start(out=Vh[:, half * 33:(half + 1) * 33, :], in_=k.vv[half * 33 * 128:(half + 1) * 33 * 128, h * dv:(h + 1) * dv].rearrange("(t p) d -> p t d", p=128)),
                  list(k.vv_b), [Vh_b])
        for (g, t0, w) in qblocks:
            Qb, Qb_b = Q[qi % 2], Q_b[qi % 2]
            qi += 1
            s.dma("sp", lambda E, h=h, t0=t0, w=w, Qb=Qb: E.dma_start(out=Qb[0:qrows, 0:w], in_=k.qT[h * qrows:(h + 1) * qrows, t0:t0 + w]), [k.qT_b[g]], [Qb_b])
            ktiles = list(range(NTILE)) if t0 < NL else [64, 65]
            for m in range(maps):
                p0 = m * kp
                for i, kt in enumerate(ktiles):
                    Sx, Sx_b = S[si % 2], S_b[si % 2]
                    si += 1
                    Px, Px_b = P[pi % 3], P_b[pi % 3]
                    pi += 1
                    s.op("pe", lambda E, Sx=Sx, kt=kt, p0=p0, KTh=KTh, Qb=Qb, w=w: E.matmul(Sx[:, 0:w], lhsT=KTh[p0:p0 + kp, kt * 128:(kt + 1) * 128], rhs=Qb[p0:p0 + kp, 0:w], start=True, stop=True),
                         [KTh_b, Qb_b], [Sx_b])
                    s.op("act", lambda E, Sx=Sx, Px=Px, w=w: E.activation(out=Px[:, 0:w], in_=Sx[:, 0:w], func=AF.Exp, scale=float(scale)), [Sx_b], [Px_b])
                    last = (i == len(ktiles) - 1)
                    s.op("pe", lambda E, m=m, kt=kt, Px=Px, Vh=Vh, w=w, i=i, last=last: E.matmul(O[m][0:dv, 0:w], lhsT=Vh[:, kt, :], rhs=Px[:, 0:w], start=(i == 0), stop=last),
                         [Vh_b, Px_b], [O_b[m]], track=last)
                    s.op("pe", lambda E, m=m, Px=Px, w=w, i=i, last=last: E.matmul(Z[m][0:dv, 0:w], lhsT=k.ones_b[:, 0:dv], rhs=Px[:, 0:w], start=(i == 0), stop=last),
                         [k.ones_bb, Px_b], [Z_b[m]], track=True)
            onx = on[oi % 2]
            oi += 1
            res = post(h, g, w, O, O_b, Z, Z_b, (r0, o0, o1, sq, onx, pss, wk_b, pss_b))
            s.dma("pool", lambda E, h=h, t0=t0, w=w, res=res: E.dma_start(out=k.aT[h * dv:(h + 1) * dv, t0:t0 + w], in_=res[0:dv, 0:w]), [wk_b], [k.aT_b[g]])


class HTP:
    def __init__(self, k, st, l, pfx, v0=0):
        self.k = k
        sb, ps = k.sb, k.ps
        self.Ab = [sb(pfx + "_Ab%d" % c, [128, D], stack=st) for c in range(2)]
        self.Bb = [sb(pfx + "_Bb%d" % c, [128, D], stack=st) for c in range(2)]
        self.Ab_b, self.Bb_b = bufs(2), bufs(2)
        for c in range(2):
            load_bcast(k, "sp", self.Ab[c][:], self.Ab_b[c], l, v0, c)
            load_bcast(k, "sp", self.Bb[c][:], self.Bb_b[c], l, v0 + 1, c)
        self.xt = [sb(pfx + "_xt%d" % i, [128, D], stack=st) for i in range(2)]
        self.xt_b = bufs(2)
        self.ht = [sb(pfx + "_ht%d" % i, [128, D], stack=st) for i in range(2)]
        self.ht_b = bufs(2)
        self.junk = sb(pfx + "_junk", [128, D], stack=st)
        self.stat = [sb(pfx + "_stat%d" % i, [128, 2], stack=st) for i in range(2)]
        self.scr_b = bufs(2)
        self.hT = [sb(pfx + "_hT%d" % i, [128, 8, 512], BF16, stack=st) for i in range(2)]
        self.hT_b = bufs(2)
        self.tp = [ps(pfx + "_tp%d" % i, [128, 512], stack=st) for i in range(2)]
        self.tp_b = bufs(2)
        self.ti = 0

    def tile(self, t):
        k, s = self.k, self.k.s
        i = self.ti % 2
        self.ti += 1
        cond = 0 if t < 64 else 1
        x_, x_b, h_, h_b = self.xt[i], self.xt_b[i], self.ht[i], self.ht_b[i]
        s.dma("sp", lambda E: E.dma_start(out=x_[:], in_=k.xres[t * 128:(t + 1) * 128, :]), [k.xres_b[t]], [x_b])
        norm_tile(k, x_, x_b, h_, h_b, self.Ab[cond], self.Ab_b[cond], self.Bb[cond], self.Bb_b[cond], (self.junk, self.stat[i]), self.scr_b[i])
        return h_, h_b

    def group(self, g):
        k = self.k
        t0, w = group_range(g)
        hTg, hTg_b = self.hT[g % 2], self.hT_b[g % 2]
        for j in range(w // 128):
            h_, h_b = self.tile(t0 // 128 + j)
            transpose_tile(k, h_, h_b, self.tp, self.tp_b, hTg, hTg_b, j * 128)
        return hTg, hTg_b, t0, w


def rms_rows(k, src_list, n, gain, gain_b, dst, dst_b, wk, wk_b):
    s = k.s
    junk, st4 = wk
    for i, (ap, b, wd) in enumerate(src_list):
        s.op("act", lambda E, ap=ap, i=i, wd=wd: E.activation(out=junk[:, 0:wd], in_=ap, func=AF.Square, accum_out=st4[:, i:i + 1]), [b], [wk_b])
    if len(src_list) == 2:
        s.op("dve", lambda E: E.tensor_tensor(out=st4[:, 0:1], in0=st4[:, 0:1], in1=st4[:, 1:2], op=ALU.add), [wk_b], [wk_b])
    s.op("act", lambda E: E.activation(out=st4[:, 2:3], in_=st4[:, 0:1], func=AF.Sqrt, scale=float(1.0 / n), bias=k.epsc[:, 0:1]), [wk_b, k.epsc_b], [wk_b])
    s.op("dve", lambda E: E.reciprocal(out=st4[:, 2:3], in_=st4[:, 2:3]), [wk_b], [wk_b])
    c0 = 0
    for (ap, b, wd) in src_list:
        s.op("dve", lambda E, ap=ap, c0=c0, wd=wd: E.scalar_tensor_tensor(out=dst[:, c0:c0 + wd], in0=ap, scalar=st4[:, 2:3], in1=gain[:, c0:c0 + wd], op0=ALU.mult, op1=ALU.mult),
             [b, wk_b, gain_b], [dst_b])
        c0 += wd


def mixer_mla(k, l, keep_ctx):
    nc, s, I, IB = k.nc, k.s, k.I, k.IB
    with ExitStack() as st:
        Wdq, Wdq_b = load_weight_bf16(k, st, "m_wdq", I["mla_wdq"], IB["mla_wdq"], 8, 768, blk=256)
        Wdkv, Wdkv_b = load_weight_bf16(k, st, "m_wdkv", I["mla_wdkv"], IB["mla_wdkv"], 8, 288, blk=288, swap_cols=288, swap_unit=32)
        Wuq, Wuq_b = load_weight_bf16(k, st, "m_wuq", I["mla_wuq"], IB["mla_wuq"], 6, 1536, blk=512, swap_cols=1536, swap_unit=32)
        Wuk, Wuk_b = load_weight_bf16(k, st, "m_wuk", I["mla_wuk"], IB["mla_wuk"], 2, 1024, blk=512)
        Wuv, Wuv_b = load_weight_bf16(k, st, "m_wuv", I["mla_wuv"], IB["mla_wuv"], 2, 1024, blk=512)
        qnb = k.sb("m_qnb", [128, 768], stack=st)
        kvnb = k.sb("m_kvnb", [128, 256], stack=st)
        qnb_b, kvnb_b = Buf(), Buf()
        s.dma("sp", lambda E: E.dma_start(out=qnb[:], in_=I["mla_qnorm"].unsqueeze(0).broadcast_to([128, 768])), [], [qnb_b])
        s.dma("sp", lambda E: E.dma_start(out=kvnb[:], in_=I["mla_kvnorm"].unsqueeze(0).broadcast_to([128, 256])), [], [kvnb_b])
        htp = HTP(k, st, l, "m")
        qn = [k.sb("m_qn%d" % i, [128, 768], stack=st) for i in range(2)]
        qn_b = bufs(2)
        cn = [k.sb("m_cn%d" % i, [128, 256], stack=st) for i in range(2)]
        cn_b = bufs(2)
        junk = htp.junk
        st4 = [k.sb("m_st4%d" % i, [128, 4], stack=st) for i in range(2)]
        st4_b = bufs(2)
        qlT = [k.sb("m_qlT%d" % i, [128, 6, 512], BF16, stack=st) for i in range(2)]
        qlT_b = bufs(2)
        ckT = [k.sb("m_ckT%d" % i, [128, 2, 512], BF16, stack=st) for i in range(2)]
        ckT_b = bufs(2)
        rc = [k.sb("m_rc0", [96, 512], stack=st)] * 2
        rs = [k.sb("m_rs0", [96, 512], stack=st)] * 2
        rkc = [k.sb("m_rkc0", [32, 512], stack=st)] * 2
        rks = [k.sb("m_rks0", [32, 512], stack=st)] * 2
        rt_b = [Buf()] * 2
        t1 = [k.sb("m_t1%d" % i, [128, 512], stack=st) for i in range(2)]
        t2 = [k.sb("m_t2%d" % i, [128, 512], stack=st) for i in range(2)]
        t1_b, t2_b = bufs(2), bufs(2)
        ob = [k.sb("m_ob%d" % i, [128, 512], BF16, stack=st) for i in range(4)]
        ob_b = bufs(4)
        vo = [k.sb("m_vo0", [128, D], BF16, stack=st)] * 2
        vo_b = [Buf()] * 2
        pA0 = k.ps("m_pA0", [128, 512], stack=st)
        pA1 = k.ps("m_pA1", [128, 512], stack=st)
        pC = k.ps("m_pC", [128, 512], stack=st)
        pE = [k.ps("m_pE%d" % i, [128, 512], stack=st) for i in range(2)]
        pF = k.ps("m_pF", [128, 512], stack=st)
        pA0_b, pA1_b, pC_b, pF_b = Buf(), Buf(), Buf(), Buf()
        pE_b = bufs(2)
        oi = 0
        vi = 0
        ti = 0
        for g in range(k.NG):
            hTg, hTg_b, t0, w = htp.group(g)
            qlTg, qlTg_b = qlT[g % 2], qlT_b[g % 2]
            ckTg, ckTg_b = ckT[g % 2], ckT_b[g % 2]
            s.dma("sp", lambda E, g=g, t0=t0, w=w: E.dma_start(out=rc[g % 2][:, 0:w], in_=I["rope3c"][:, t0:t0 + w]), [IB["rope3c"]], [rt_b[g % 2]])
            s.dma("sp", lambda E, g=g, t0=t0, w=w: E.dma_start(out=rs[g % 2][:, 0:w], in_=I["rope3s"][:, t0:t0 + w]), [IB["rope3s"]], [rt_b[g % 2]])
            s.dma("sp", lambda E, g=g, t0=t0, w=w: E.dma_start(out=rkc[g % 2][:, 0:w], in_=I["rope3c"][64:96, t0:t0 + w]), [IB["rope3c"]], [rt_b[g % 2]])
            s.dma("sp", lambda E, g=g, t0=t0, w=w: E.dma_start(out=rks[g % 2][:, 0:w], in_=I["rope3s"][64:96, t0:t0 + w]), [IB["rope3s"]], [rt_b[g % 2]])
            for j in range(w // 128):
                i2 = ti % 2
                ti += 1
                for c in range(8):
                    s.op("pe", lambda E, c=c, j=j: E.matmul(pA0[:], lhsT=hTg[:, c, j * 128:(j + 1) * 128], rhs=Wdq[:, c, 0:512], start=(c == 0), stop=(c == 7)), [hTg_b, Wdq_b], [pA0_b], track=(c == 7))
                for c in range(8):
                    s.op("pe", lambda E, c=c, j=j: E.matmul(pA1[:, 0:256], lhsT=hTg[:, c, j * 128:(j + 1) * 128], rhs=Wdq[:, c, 512:768], start=(c == 0), stop=(c == 7)), [hTg_b, Wdq_b], [pA1_b], track=(c == 7))
                for c in range(8):
                    s.op("pe", lambda E, c=c, j=j: E.matmul(pC[:, 0:288], lhsT=hTg[:, c, j * 128:(j + 1) * 128], rhs=Wdkv[:, c, 0:288], start=(c == 0), stop=(c == 7)), [hTg_b, Wdkv_b], [pC_b], track=(c == 7))
                rms_rows(k, [(pA0[:], pA0_b, 512), (pA1[:, 0:256], pA1_b, 256)], 768, qnb, qnb_b, qn[i2], qn_b[i2], (junk, st4[i2]), st4_b[i2])
                transpose_tile(k, qn[i2], qn_b[i2], htp.tp, htp.tp_b, qlTg, qlTg_b, j * 128, nchunk=6)
                rms_rows(k, [(pC[:, 0:256], pC_b, 256)], 256, kvnb, kvnb_b, cn[i2], cn_b[i2], (junk, st4[i2]), st4_b[i2])
                transpose_tile(k, cn[i2], cn_b[i2], htp.tp, htp.tp_b, ckTg, ckTg_b, j * 128, nchunk=2)
            for c in range(8):
                s.op("pe", lambda E, c=c: E.matmul(pE[0][0:32, 0:w], lhsT=Wdkv[:, c, 256:288], rhs=hTg[:, c, 0:w], start=(c == 0), stop=(c == 7)), [hTg_b, Wdkv_b], [pE_b[0]], track=(c == 7))
            for c in range(8):
                s.op("pe", lambda E, c=c: E.matmul(pE[1][0:32, 0:w], lhsT=Wdkv[:, c, 288 + 256:288 + 288], rhs=hTg[:, c, 0:w], start=(c == 0), stop=(c == 7)), [hTg_b, Wdkv_b], [pE_b[1]], track=(c == 7))
            o_, o_b = ob[oi % 4], ob_b[oi % 4]
            oi += 1
            s.op("dve", lambda E: E.tensor_tensor(out=t1[0][0:32, 0:w], in0=pE[0][0:32, 0:w], in1=rkc[g % 2][:, 0:w], op=ALU.mult), [pE_b[0], rt_b[g % 2]], [t1_b[0]])
            s.op("dve", lambda E: E.tensor_tensor(out=t2[0][0:32, 0:w], in0=pE[1][0:32, 0:w], in1=rks[g % 2][:, 0:w], op=ALU.mult), [pE_b[1], rt_b[g % 2]], [t2_b[0]])
            s.op("pool", lambda E, o_=o_: E.tensor_tensor(out=o_[0:32, 0:w], in0=t1[0][0:32, 0:w], in1=t2[0][0:32, 0:w], op=ALU.add), [t1_b[0], t2_b[0]], [o_b])
            s.dma("pool", lambda E, o_=o_: E.dma_start(out=k.krT[:, t0:t0 + w], in_=o_[0:32, 0:w]), [o_b], [k.krT_b[g]])
            for hp in range(8):
                for c in range(2):
                    s.op("pe", lambda E, c=c, hp=hp: E.matmul(pF[:, 0:w], lhsT=Wuk[:, c, hp * 128:(hp + 1) * 128], rhs=ckTg[:, c, 0:w], start=(c == 0), stop=(c == 1)), [ckTg_b, Wuk_b], [pF_b], track=(c == 1))
                o_, o_b = ob[oi % 4], ob_b[oi % 4]
                oi += 1
                s.op("act", lambda E, o_=o_: E.activation(out=o_[:, 0:w], in_=pF[:, 0:w], func=AF.Copy), [pF_b], [o_b])
                s.dma("pool", lambda E, o_=o_, hp=hp: E.dma_start(out=k.kT[hp * 128:(hp + 1) * 128, t0:t0 + w], in_=o_[:, 0:w]), [o_b], [k.kT_b[g]])
            for h in range(16):
                for c in range(6):
                    s.op("pe", lambda E, c=c, h=h: E.matmul(pE[0][0:96, 0:w], lhsT=Wuq[:, c, h * 96:(h + 1) * 96], rhs=qlTg[:, c, 0:w], start=(c == 0), stop=(c == 5)), [qlTg_b, Wuq_b], [pE_b[0]], track=(c == 5))
                for c in range(6):
                    s.op("pe", lambda E, c=c, h=h: E.matmul(pE[1][0:96, 0:w], lhsT=Wuq[:, c, 1536 + h * 96:1536 + (h + 1) * 96], rhs=qlTg[:, c, 0:w], start=(c == 0), stop=(c == 5)), [qlTg_b, Wuq_b], [pE_b[1]], track=(c == 5))
                a1, a1_b = t1[h % 2], t1_b[h % 2]
                a2, a2_b = t2[h % 2], t2_b[h % 2]
                o_, o_b = ob[oi % 4], ob_b[oi % 4]
                oi += 1
                s.op("dve", lambda E, a1=a1: E.tensor_tensor(out=a1[0:96, 0:w], in0=pE[0][0:96, 0:w], in1=rc[g % 2][:, 0:w], op=ALU.mult), [pE_b[0], rt_b[g % 2]], [a1_b])
                s.op("dve", lambda E, a2=a2: E.tensor_tensor(out=a2[0:96, 0:w], in0=pE[1][0:96, 0:w], in1=rs[g % 2][:, 0:w], op=ALU.mult), [pE_b[1], rt_b[g % 2]], [a2_b])
                s.op("pool", lambda E, a1=a1, a2=a2, o_=o_: E.tensor_tensor(out=o_[0:96, 0:w], in0=a1[0:96, 0:w], in1=a2[0:96, 0:w], op=ALU.add), [a1_b, a2_b], [o_b])
                s.dma("pool", lambda E, o_=o_, h=h: E.dma_start(out=k.qT[h * 96:(h + 1) * 96, t0:t0 + w], in_=o_[0:96, 0:w]), [o_b], [k.qT_b[g]])
            for j in range(w // 128):
                v_, v_b = vo[vi % 2], vo_b[vi % 2]
                vi += 1
                for nb in range(2):
                    for c in range(2):
                        s.op("pe", lambda E, c=c, nb=nb, j=j: E.matmul(pF[:], lhsT=ckTg[:, c, j * 128:(j + 1) * 128], rhs=Wuv[:, c, nb * 512:(nb + 1) * 512], start=(c == 0), stop=(c == 1)), [ckTg_b, Wuv_b], [pF_b], track=(c == 1))
                    s.op("act", lambda E, nb=nb, v_=v_: E.activation(out=v_[:, nb * 512:(nb + 1) * 512], in_=pF[:], func=AF.Copy), [pF_b], [v_b])
                t = (t0 // 128) + j
                s.dma("pool", lambda E, t=t, v_=v_: E.dma_start(out=k.vv[t * 128:(t + 1) * 128, :], in_=v_[:]), [v_b], [k.vv_b[g]])
        s.barrier()
    with ExitStack() as st:
        def post(h, qb, w, O, O_b, Z, Z_b, wk):
            (r0, o0, o1, sq, on, pss, wk_b, pss_b) = wk
            s.op("dve", lambda E: E.reciprocal(out=r0[0:64, 0:w], in_=Z[0][0:64, 0:w]), [Z_b[0]], [wk_b])
            s.op("dve", lambda E: E.tensor_tensor(out=on[0:64, 0:w], in0=O[0][0:64, 0:w], in1=r0[0:64, 0:w], op=ALU.mult), [O_b[0], wk_b], [wk_b])
            return on
        attention(k, st, n_heads=16, maps=1, kp=96, dv=64, scale=float(96 ** -0.5), post=post, krows=64, qrows=96, extra=(k.krT, k.krT_b, 32), with_ctx_q=keep_ctx)
        s.barrier()


def qkv_plain(k, st, l, pfx, wname, q_scale):
    s, I, IB = k.s, k.I, k.IB
    W, W_b = load_weight_bf16(k, st, pfx + "_w", I[wname], IB[wname], 8, 3 * D)
    htp = HTP(k, st, l, pfx)
    qo = [k.sb(pfx + "_qo%d" % i, [128, 512], BF16, stack=st) for i in range(4)]
    qo_b = bufs(4)
    vo = [k.sb(pfx + "_vo%d" % i, [128, D], BF16, stack=st) for i in range(2)]
    vo_b = bufs(2)
    pq = [k.ps(pfx + "_pq%d" % i, [128, 512], stack=st) for i in range(2)]
    pq_b = bufs(2)
    pv = [k.ps(pfx + "_pv%d" % i, [128, 512], stack=st) for i in range(2)]
    pv_b = bufs(2)
    oi = 0
    vi = 0
    for g in range(k.NG):
        hTg, hTg_b, t0, w = htp.group(g)
        for o in range(16):
            p, p_b = pq[o % 2], pq_b[o % 2]
            for c in range(8):
                s.op("pe", lambda E, c=c, o=o, p=p: E.matmul(p[:, 0:w], lhsT=W[:, c, o * 128:(o + 1) * 128], rhs=hTg[:, c, 0:w], start=(c == 0), stop=(c == 7)), [W_b, hTg_b], [p_b], track=(c == 7))
            q_, q_b = qo[oi % 4], qo_b[oi % 4]
            oi += 1
            if o < 8:
                s.op("act", lambda E, p=p, q_=q_: E.activation(out=q_[:, 0:w], in_=p[:, 0:w], func=AF.Copy, scale=float(q_scale)), [p_b], [q_b])
            else:
                s.op("dve", lambda E, p=p, q_=q_: E.tensor_copy(out=q_[:, 0:w], in_=p[:, 0:w]), [p_b], [q_b])
            dstT = k.qT if o < 8 else k.kT
            dst_b = (k.qT_b if o < 8 else k.kT_b)[g]
            oo = o % 8
            s.dma("pool", lambda E, dstT=dstT, oo=oo, q_=q_: E.dma_start(out=dstT[oo * 128:(oo + 1) * 128, t0:t0 + w], in_=q_[:, 0:w]), [q_b], [dst_b])
        for j in range(w // 128):
            v_, v_b = vo[vi % 2], vo_b[vi % 2]
            vi += 1
            for nb in range(2):
                p, p_b = pv[nb], pv_b[nb]
                for c in range(8):
                    s.op("pe", lambda E, c=c, nb=nb, p=p, j=j: E.matmul(p[:], lhsT=hTg[:, c, j * 128:(j + 1) * 128], rhs=W[:, c, 2 * D + nb * 512:2 * D + (nb + 1) * 512], start=(c == 0), stop=(c == 7)), [W_b, hTg_b], [p_b], track=(c == 7))
                s.op("act", lambda E, nb=nb, p=p, v_=v_: E.activation(out=v_[:, nb * 512:(nb + 1) * 512], in_=p[:], func=AF.Copy), [p_b], [v_b])
            t = (t0 // 128) + j
            s.dma("pool", lambda E, t=t, v_=v_: E.dma_start(out=k.vv[t * 128:(t + 1) * 128, :], in_=v_[:]), [v_b], [k.vv_b[g]])


def mixer_na(k, l, keep_ctx):
    s, I, IB = k.s, k.I, k.IB
    with ExitStack() as st:
        qkv_plain(k, st, l, "n", "na_w", 0.125)
        s.barrier()
    with ExitStack() as st:
        KT = [k.sb("n_KT%d" % i, [64, NT], BF16, stack=st) for i in range(2)]
        QT = [k.sb("n_QT%d" % i, [64, NT], BF16, stack=st) for i in range(2)]
        Ve = [k.sb("n_Ve%d" % i, [128, 66, 64], BF16, stack=st) for i in range(2)]
        Vo = [k.sb("n_Vo%d" % i, [128, 65, 64], BF16, stack=st) for i in range(2)]
        hb_b = bufs(2)
        bf = [k.sb("n_bf%d" % i, [128, 2048], stack=st) for i in range(2)]
        bf_b = bufs(2)
        bb = [k.sb("n_bb%d" % i, [128, 8, 4, 64], BF16, stack=st) for i in range(2)]
        bb_b = bufs(2)
        P = [k.sb("n_P%d" % i, [128, 512], BF16, stack=st) for i in range(3)]
        P_b = bufs(3)
        r0t = k.sb("n_r0", [64, 512], stack=st)
        on = [k.sb("n_on%d" % i, [64, 512], BF16, stack=st) for i in range(2)]
        on_b = bufs(2)
        r0_b = Buf()
        S = [k.ps("n_S%d" % i, [128, 512], stack=st) for i in range(2)]
        S_b = bufs(2)
        O = [k.ps("n_O%d" % i, [64, 512], stack=st) for i in range(2)]
        O_b = bufs(2)
        Z = [k.ps("n_Z%d" % i, [64, 512], stack=st) for i in range(2)]
        Z_b = bufs(2)
        si = 0
        pi = 0
        oi = 0
        for h in range(16):
            i2 = h % 2
            KTh, QTh, Veh, Voh, hb = KT[i2], QT[i2], Ve[i2], Vo[i2], hb_b[i2]
            s.dma("sp", lambda E, h=h, KTh=KTh: E.dma_start(out=KTh[:, :], in_=k.kT[h * 64:(h + 1) * 64, :]), list(k.kT_b), [hb])
            s.dma("sp", lambda E, h=h, QTh=QTh: E.dma_start(out=QTh[:, :], in_=k.qT[h * 64:(h + 1) * 64, :]), list(k.qT_b), [hb])
            for half in range(2):
                s.dma("sp", lambda E, h=h, Veh=Veh, half=half: E.dma_start(out=Veh[:, half * 33:(half + 1) * 33, :], in_=k.vv[half * 33 * 128:(half + 1) * 33 * 128, h * 64:(h + 1) * 64].rearrange("(t p) d -> p t d", p=128)), list(k.vv_b), [hb])
            s.dma("sp", lambda E, h=h, Voh=Voh: E.dma_start(out=Voh[:, 0:33, :], in_=k.vv[64:64 + 33 * 128, h * 64:(h + 1) * 64].rearrange("(t p) d -> p t d", p=128)), list(k.vv_b), [hb])
            s.dma("sp", lambda E, h=h, Voh=Voh: E.dma_start(out=Voh[:, 33:65, :], in_=k.vv[64 + 33 * 128:64 + 65 * 128, h * 64:(h + 1) * 64].rearrange("(t p) d -> p t d", p=128)), list(k.vv_b), [hb])
            s.dma("sp", lambda E, h=h: E.dma_start(out=bf[i2][:], in_=I["na_bias"][h * 128:(h + 1) * 128, :]), [IB["na_bias"]], [bf_b[i2]])
            s.op("pool", lambda E: E.tensor_copy(out=bb[i2][:].rearrange("p a b c -> p (a b c)"), in_=bf[i2][:]), [bf_b[i2]], [bb_b[i2]])
            blocks = [("lat", rg) for rg in range(16)] + ([("ctx", 0)] if keep_ctx else [])
            for (kind, rg) in blocks:
                Ox, Ox_b, Zx, Zx_b = O[oi % 2], O_b[oi % 2], Z[oi % 2], Z_b[oi % 2]
                onx, onx_b = on[oi % 2], on_b[oi % 2]
                oi += 1
                if kind == "lat":
                    items = []
                    for i in range(8):
                        r = rg * 8 + i
                        r0 = min(max(r - 4, 0), 120)
                        items.append((r, r0, r - r0))
                    for i, (r, r0, v) in enumerate(items):
                        Sx, Sx_b = S[si % 2], S_b[si % 2]
                        si += 1
                        Px, Px_b = P[pi % 3], P_b[pi % 3]
                        pi += 1
                        for kt in range(4):
                            tok0 = r0 * 64 + kt * 128
                            s.op("pe", lambda E, Sx=Sx, kt=kt, tok0=tok0, r=r: E.matmul(Sx[:, kt * 64:(kt + 1) * 64], lhsT=KTh[:, tok0:tok0 + 128], rhs=QTh[:, r * 64:(r + 1) * 64], start=True, stop=False), [hb], [Sx_b], track=False)
                            s.op("pe", lambda E, Sx=Sx, kt=kt, v=v: E.matmul(Sx[:, kt * 64:(kt + 1) * 64], lhsT=k.identb[:], rhs=bb[i2][:, v, kt, :], start=False, stop=True), [k.identb_b, bb_b[i2]], [Sx_b], track=False)
                        for c in range(2):
                            s.op("pe", lambda E, Sx=Sx, c=c, r=r: E.matmul(Sx[:, (4 + c) * 64:(5 + c) * 64], lhsT=KTh[:, NL + c * 128:NL + (c + 1) * 128], rhs=QTh[:, r * 64:(r + 1) * 64], start=True, stop=True), [hb], [Sx_b], track=(c == 1))
                        s.op("act", lambda E, Sx=Sx, Px=Px: E.activation(out=Px[:, 0:384], in_=Sx[:, 0:384], func=AF.Exp), [Sx_b], [Px_b])
                        for kt in range(6):
                            if kt < 4:
                                vt = Veh[:, r0 // 2 + kt, :] if r0 % 2 == 0 else Voh[:, (r0 - 1) // 2 + kt, :]
                            else:
                                vt = Veh[:, 64 + (kt - 4), :]
                            s.op("pe", lambda E, vt=vt, Px=Px, kt=kt, i=i, Ox=Ox: E.matmul(Ox[:, i * 64:(i + 1) * 64], lhsT=vt, rhs=Px[:, kt * 64:(kt + 1) * 64], start=(kt == 0), stop=(kt == 5)), [hb, Px_b], [Ox_b], track=False)
                            s.op("pe", lambda E, Px=Px, kt=kt, i=i, Zx=Zx: E.matmul(Zx[:, i * 64:(i + 1) * 64], lhsT=k.ones_b[:, 0:64], rhs=Px[:, kt * 64:(kt + 1) * 64], start=(kt == 0), stop=(kt == 5)), [k.ones_bb, Px_b], [Zx_b], track=(kt == 5))
                    w = 512
                    c0 = rg * 512
                else:
                    Sx, Sx_b = S[si % 2], S_b[si % 2]
                    si += 1
                    Px, Px_b = P[pi % 3], P_b[pi % 3]
                    pi += 1
                    for c in range(2):
                        s.op("pe", lambda E, Sx=Sx, c=c: E.matmul(Sx[:, c * 256:(c + 1) * 256], lhsT=KTh[:, NL + c * 128:NL + (c + 1) * 128], rhs=QTh[:, NL:NT], start=True, stop=True), [hb], [Sx_b], track=(c == 1))
                    s.op("act", lambda E, Sx=Sx, Px=Px: E.activation(out=Px[:, :], in_=Sx[:, :], func=AF.Exp), [Sx_b], [Px_b])
                    for c in range(2):
                        s.op("pe", lambda E, Px=Px, c=c, Ox=Ox: E.matmul(Ox[:, 0:256], lhsT=Veh[:, 64 + c, :], rhs=Px[:, c * 256:(c + 1) * 256], start=(c == 0), stop=(c == 1)), [hb, Px_b], [Ox_b], track=False)
                        s.op("pe", lambda E, Px=Px, c=c, Zx=Zx: E.matmul(Zx[:, 0:256], lhsT=k.ones_b[:, 0:64], rhs=Px[:, c * 256:(c + 1) * 256], start=(c == 0), stop=(c == 1)), [k.ones_bb, Px_b], [Zx_b], track=(c == 1))
                    w = 256
                    c0 = NL
                s.op("dve", lambda E, Zx=Zx, w=w: E.reciprocal(out=r0t[:, 0:w], in_=Zx[:, 0:w]), [Zx_b], [r0_b])
                s.op("dve", lambda E, Ox=Ox, onx=onx, w=w: E.tensor_tensor(out=onx[:, 0:w], in0=Ox[:, 0:w], in1=r0t[:, 0:w], op=ALU.mult), [Ox_b, r0_b], [onx_b])
                g0 = c0 // 512
                s.dma("pool", lambda E, h=h, c0=c0, w=w, onx=onx: E.dma_start(out=k.aT[h * 64:(h + 1) * 64, c0:c0 + w], in_=onx[:, 0:w]), [onx_b], [k.aT_b[g0]])
        s.barrier()


def mixer_fourier(k, l, keep_ctx):
    s, I, IB = k.s, k.I, k.IB
    with ExitStack() as st:
        htp = HTP(k, st, l, "f")
        hb = [k.sb("f_hb%d" % i, [128, D], BF16, stack=st) for i in range(2)]
        hb_b = bufs(2)
        for t in range(NTILE):
            h_, h_b = htp.tile(t)
            s.op("act", lambda E, h_=h_, t=t: E.activation(out=hb[t % 2][:], in_=h_[:], func=AF.Copy), [h_b], [hb_b[t % 2]])
            s.dma("pool", lambda E, t=t: E.dma_start(out=k.h2tab[t * 128:(t + 1) * 128, :], in_=hb[t % 2][:]), [hb_b[t % 2]], [k.h2tab_b[t]])
        s.barrier()
    with ExitStack() as st:
        stg = k.sb("f_stg", [128, 2048], stack=st)
        stg_b = Buf()
        COS = k.sb("f_cos", [128, 64, 128], BF16, stack=st)
        SIN = k.sb("f_sin", [128, 64, 128], BF16, stack=st)
        F64 = k.sb("f_f64", [64, 4, 48], BF16, stack=st)
        CC = k.sb("f_cc", [128, 2, 256], BF16, stack=st)
        SC = k.sb("f_sc", [128, 2, 256], BF16, stack=st)
        NSC = k.sb("f_nsc", [128, 2, 256], BF16, stack=st)
        tab_b = Buf()
        for (dst, name) in [(COS, "fn_cos"), (SIN, "fn_sin")]:
            for q in range(4):
                s.dma("sp", lambda E, name=name, q=q: E.dma_start(out=stg[:], in_=I[name][:, q * 2048:(q + 1) * 2048]), [IB[name]], [stg_b])
                s.op("dve", lambda E, dst=dst, q=q: E.tensor_copy(out=dst[:].rearrange("p a b -> p (a b)")[:, q * 2048:(q + 1) * 2048], in_=stg[:]), [stg_b], [tab_b])
        s.dma("sp", lambda E: E.dma_start(out=stg[0:64, 0:192], in_=I["fn_f64"][:, :]), [], [stg_b])
        s.op("dve", lambda E: E.tensor_copy(out=F64[:].rearrange("p a b -> p (a b)"), in_=stg[0:64, 0:192]), [stg_b], [tab_b])
        for (dst, name) in [(CC, "fn_cc"), (SC, "fn_sc"), (NSC, "fn_nsc")]:
            s.dma("sp", lambda E, name=name: E.dma_start(out=stg[:, 0:512].rearrange("p (a b) -> p a b", a=2), in_=I[name].rearrange("(a p) l -> p a l", p=128)), [], [stg_b])
            s.op("dve", lambda E, dst=dst: E.tensor_copy(out=dst[:].rearrange("p a b -> p (a b)"), in_=stg[:, 0:512]), [stg_b], [tab_b])
        Xs = k.sb("f_Xs", [64, 128, 128], BF16, stack=st)
        Xs_b = Buf()
        A = k.sb("f_A", [128, 48, 128], BF16, stack=st)
        A_b = Buf()
        GTr = k.sb("f_GTr", [128, 2, NL], BF16, stack=st)
        GTi = k.sb("f_GTi", [128, 2, NL], BF16, stack=st)
        GT_b = Buf()
        hc = k.sb("f_hc", [128, 2, 256], BF16, stack=st)
        hc_b = Buf()
        yo = [k.sb("f_yo%d" % i, [128, 512], BF16, stack=st) for i in range(2)]
        yo_b = bufs(2)
        PA = [k.ps("f_PA%d" % i, [128, 512], stack=st) for i in range(2)]
        PA_b = bufs(2)
        PG = [k.ps("f_PG%d" % i, [128, 512], stack=st) for i in range(4)]
        PG_b = bufs(4)
        PY = [k.ps("f_PY%d" % i, [128, 512], stack=st) for i in range(2)]
        PY_b = bufs(2)
        ai = 0
        gi = 0
        yi = 0

        def channel_dft(gq, k0, kw, nk, scale, col0):
            nonlocal yi
            for lc in range(2):
                for kb in range(nk):
                    p, p_b = PY[yi % 2], PY_b[yi % 2]
                    y_, y_b = yo[yi % 2], yo_b[yi % 2]
                    yi += 1
                    ka = k0 + kb * kw
                    n = 0
                    for cc in range(2):
                        for (T, Gx) in [(CC, GTr), (SC, GTi)]:
                            s.op("pe", lambda E, T=T, Gx=Gx, cc=cc, lc=lc, ka=ka, p=p, n=n: E.matmul(p[:, 0:kw], lhsT=T[:, cc, lc * 128:(lc + 1) * 128], rhs=Gx[:, cc, ka:ka + kw], start=(n == 0), stop=(n == 3)), [tab_b, GT_b], [p_b], track=(n == 3))
                            n += 1
                    s.op("act", lambda E, p=p, y_=y_: E.activation(out=y_[:, 0:kw], in_=p[:, 0:kw], func=AF.Copy, scale=float(scale)), [p_b], [y_b])
                    g0 = (col0 + ka - k0) // 512
                    s.dma("pool", lambda E, y_=y_, lc=lc, ka=ka: E.dma_start(out=k.aT[gq * 256 + lc * 128:gq * 256 + (lc + 1) * 128, col0 + ka - k0:col0 + ka - k0 + kw], in_=y_[:, 0:kw]), [y_b], [k.aT_b[g0]])

        for gq in range(4):
            for cc in range(2):
                col = gq * 256 + cc * 128
                s.dma("sp", lambda E, col=col: E.dma_start(out=Xs[:, :, :], in_=k.h2tab[0:NL, col:col + 128].rearrange("(a b) c -> a b c", b=128)), list(k.h2tab_b), [Xs_b])
                for kb in range(4):
                    for c8 in range(16):
                        p, p_b = PA[ai % 2], PA_b[ai % 2]
                        for ci in range(8):
                            c = c8 * 8 + ci
                            s.op("pe", lambda E, p=p, ci=ci, c=c, kb=kb: E.matmul(p[:, ci * 48:(ci + 1) * 48], lhsT=Xs[:, :, c], rhs=F64[:, kb, :], start=True, stop=True), [Xs_b, tab_b], [p_b], track=(ci == 7))
                        eng = ["act", "dve"][ai % 2]
                        ai += 1
                        if eng == "act":
                            s.op("act", lambda E, p=p, c8=c8: E.activation(out=A[:, :, c8 * 8:(c8 + 1) * 8], in_=p[:, 0:384].rearrange("p (c j) -> p j c", j=48), func=AF.Copy), [p_b], [A_b])
                        else:
                            s.op("dve", lambda E, p=p, c8=c8: E.tensor_copy(out=A[:, :, c8 * 8:(c8 + 1) * 8], in_=p[:, 0:384].rearrange("p (c j) -> p j c", j=48)), [p_b], [A_b])
                    for quad in range(4):
                        pr, pr_b = PG[gi % 4], PG_b[gi % 4]
                        pim, pim_b = PG[(gi + 1) % 4], PG_b[(gi + 1) % 4]
                        gi += 2
                        for q in range(4):
                            k1l = quad * 4 + q
                            k1 = kb * 16 + k1l
                            s.op("pe", lambda E, pr=pr, q=q, k1l=k1l, k1=k1: E.matmul(pr[:, q * 128:(q + 1) * 128], lhsT=A[:, k1l, :], rhs=COS[:, k1, :], start=True, stop=False), [A_b, tab_b], [pr_b], track=False)
                            s.op("pe", lambda E, pr=pr, q=q, k1l=k1l, k1=k1: E.matmul(pr[:, q * 128:(q + 1) * 128], lhsT=A[:, 16 + k1l, :], rhs=SIN[:, k1, :], start=False, stop=True), [A_b, tab_b], [pr_b], track=(q == 3))
                            s.op("pe", lambda E, pim=pim, q=q, k1l=k1l, k1=k1: E.matmul(pim[:, q * 128:(q + 1) * 128], lhsT=A[:, 16 + k1l, :], rhs=COS[:, k1, :], start=True, stop=False), [A_b, tab_b], [pim_b], track=False)
                            s.op("pe", lambda E, pim=pim, q=q, k1l=k1l, k1=k1: E.matmul(pim[:, q * 128:(q + 1) * 128], lhsT=A[:, 32 + k1l, :], rhs=SIN[:, k1, :], start=False, stop=True), [A_b, tab_b], [pim_b], track=(q == 3))
                        k10 = kb * 16 + quad * 4
                        s.op("act", lambda E, pr=pr, cc=cc, k10=k10: E.activation(out=GTr[:, cc, :].rearrange("p (b a) -> p a b", a=64)[:, k10:k10 + 4, :], in_=pr[:].rearrange("p (q b) -> p q b", q=4), func=AF.Copy), [pr_b], [GT_b])
                        s.op("dve", lambda E, pim=pim, cc=cc, k10=k10: E.tensor_copy(out=GTi[:, cc, :].rearrange("p (b a) -> p a b", a=64)[:, k10:k10 + 4, :], in_=pim[:].rearrange("p (q b) -> p q b", q=4)), [pim_b], [GT_b])
            channel_dft(gq, 0, 512, 16, 1.0 / math.sqrt(NL * 256.0), 0)
            if keep_ctx:
                s.dma("sp", lambda E, gq=gq: E.dma_start(out=hc[:, :, :], in_=k.h2tab[NL:NT, gq * 256:(gq + 1) * 256].rearrange("(t p) c -> p t c", p=128)), list(k.h2tab_b), [hc_b])
                for cc in range(2):
                    pr, pr_b = PG[gi % 4], PG_b[gi % 4]
                    pim, pim_b = PG[(gi + 1) % 4], PG_b[(gi + 1) % 4]
                    gi += 2
                    for t in range(2):
                        s.op("pe", lambda E, pr=pr, t=t, cc=cc: E.matmul(pr[:, 0:256], lhsT=hc[:, t, cc * 128:(cc + 1) * 128], rhs=CC[:, t, :], start=(t == 0), stop=(t == 1)), [hc_b, tab_b], [pr_b], track=(t == 1))
                    for t in range(2):
                        s.op("pe", lambda E, pim=pim, t=t, cc=cc: E.matmul(pim[:, 0:256], lhsT=hc[:, t, cc * 128:(cc + 1) * 128], rhs=NSC[:, t, :], start=(t == 0), stop=(t == 1)), [hc_b, tab_b], [pim_b], track=(t == 1))
                    s.op("act", lambda E, pr=pr, cc=cc: E.activation(out=GTr[:, cc, 0:256], in_=pr[:, 0:256], func=AF.Copy), [pr_b], [GT_b])
                    s.op("dve", lambda E, pim=pim, cc=cc: E.tensor_copy(out=GTi[:, cc, 0:256], in_=pim[:, 0:256]), [pim_b], [GT_b])
                channel_dft(gq, 0, 256, 1, 1.0 / 256.0, NL)
        s.barrier()


def post_mixer_and_moe(k, l, keep_ctx):
    nc, s, I = k.nc, k.s, k.I
    wo_name = {0: "da_wo", 1: "fn_wo", 2: "na_wo", 3: "mla_wo"}[l % 4]
    wo_src, wo_src_b = I[wo_name], k.IB[wo_name]
    ntile = NTILE if keep_ctx else 64
    ngrp = k.NG if keep_ctx else 16
    conds = [0, 1] if keep_ctx else [0]
    es2 = ExitStack()
    aff = k.sb("r_aff", [128, NTILE, NEXP], stack=es2)
    aff_b = Buf()
    posi = k.sb("r_posi", [128, NTILE, NEXP], I32, stack=es2)
    posi_b = Buf()
    G2b = [k.sb("r_G2b%d" % c, [128, D], stack=es2) for c in range(2)]
    G2b_b = bufs(2)
    for c in conds:
        load_bcast(k, "sp", G2b[c][:], G2b_b[c], l, 5, c)
    with ExitStack() as st:
        Wo, Wo_b = load_weight_bf16(k, st, "o_w", wo_src, wo_src_b, 8, D)
        wr = k.sb("o_wr", [128, 8, NEXP], stack=st)
        wr_b = Buf()
        s.dma("sp", lambda E: E.dma_start(out=wr[:], in_=I["moe_wr"][l].rearrange("(c p) e -> p c e", p=128)), [], [wr_b])
        G1b = [k.sb("o_G1b%d" % c, [128, D], stack=st) for c in range(2)]
        A2b = [k.sb("o_A2b%d" % c, [128, D], stack=st) for c in range(2)]
        B2b = [k.sb("o_B2b%d" % c, [128, D], stack=st) for c in range(2)]
        G1b_b, A2b_b, B2b_b = bufs(2), bufs(2), bufs(2)
        for c in conds:
            load_bcast(k, "sp", G1b[c][:], G1b_b[c], l, 2, c)
            load_bcast(k, "sp", A2b[c][:], A2b_b[c], l, 3, c)
            load_bcast(k, "sp", B2b[c][:], B2b_b[c], l, 4, c)
        aTs = [k.sb("o_aT%d" % i, [128, 8, 512], BF16, stack=st) for i in range(2)]
        aTs_b = bufs(2)
        xt = [k.sb("o_xt%d" % i, [128, D], stack=st) for i in range(2)]
        xt_b = bufs(2)
        ht = [k.sb("o_ht%d" % i, [128, D], stack=st) for i in range(2)]
        ht_b = bufs(2)
        hb = [k.sb("o_hb%d" % i, [128, D], BF16, stack=st) for i in range(2)]
        hb_b = bufs(2)
        junk = k.sb("o_junk", [128, D], stack=st)
        stat = [k.sb("o_stat%d" % i, [128, 2], stack=st) for i in range(2)]
        scr_b = bufs(2)
        hT = [k.sb("o_hT%d" % i, [128, 8, 128], stack=st) for i in range(2)]
        hT_b = bufs(2)
        lg = k.sb("o_lg", [128, NEXP], stack=st)
        lgs = k.sb("o_lgs", [128, 2], stack=st)
        lg_b = Buf()
        py = [k.ps("o_py%d" % i, [128, 512], stack=st) for i in range(2)]
        py_b = bufs(2)
        tp = [k.ps("o_tp%d" % i, [128, 512], stack=st) for i in range(2)]
        tp_b = bufs(2)
        pl = k.ps("o_pl", [128, NEXP], stack=st)
        pl_b = Buf()
        ti = 0
        for g in range(ngrp):
            t0, w = group_range(g)
            a_, a_b = aTs[g % 2], aTs_b[g % 2]
            s.dma("sp", lambda E, a_=a_, t0=t0, w=w: E.dma_start(out=a_[:, :, 0:w], in_=k.aT[:, t0:t0 + w].rearrange("(c p) t -> p c t", p=128)), [k.aT_b[g]], [a_b])
            for j in range(w // 128):
                t = t0 // 128 + j
                cond = 0 if t < 64 else 1
                x_, x_b = xt[ti % 2], xt_b[ti % 2]
                h_, h_b = ht[ti % 2], ht_b[ti % 2]
                hb_, hb_bb = hb[ti % 2], hb_b[ti % 2]
                hT_, hT_bb = hT[ti % 2], hT_b[ti % 2]
                s.dma("sp", lambda E, t=t, x_=x_: E.dma_start(out=x_[:], in_=k.xres[t * 128:(t + 1) * 128, :]), [k.xres_b[t]], [x_b])
                for nb in range(2):
                    p, p_b = py[nb], py_b[nb]
                    for c in range(8):
                        s.op("pe", lambda E, c=c, nb=nb, p=p, j=j, a_=a_: E.matmul(p[:], lhsT=a_[:, c, j * 128:(j + 1) * 128], rhs=Wo[:, c, nb * 512:(nb + 1) * 512], start=(c == 0), stop=(c == 7)),
                             [a_b, Wo_b], [p_b], track=(c == 7))
                    s.op("dve", lambda E, nb=nb, p=p, h_=h_, cond=cond: E.tensor_tensor(out=h_[:, nb * 512:(nb + 1) * 512], in0=p[:], in1=G1b[cond][:, nb * 512:(nb + 1) * 512], op=ALU.mult),
                         [p_b, G1b_b[cond]], [h_b])
                s.op("pool", lambda E, x_=x_, h_=h_: E.tensor_tensor(out=x_[:], in0=x_[:], in1=h_[:], op=ALU.add), [x_b, h_b], [x_b])
                s.dma("pool", lambda E, t=t, x_=x_: E.dma_start(out=k.xres[t * 128:(t + 1) * 128, :], in_=x_[:]), [x_b], [k.xres_b[t]])
                norm_tile(k, x_, x_b, h_, h_b, A2b[cond], A2b_b[cond], B2b[cond], B2b_b[cond], (junk, stat[ti % 2]), scr_b[ti % 2])
                s.op("act", lambda E, h_=h_, hb_=hb_: E.activation(out=hb_[:], in_=h_[:], func=AF.Copy), [h_b], [hb_bb])
                s.dma("pool", lambda E, t=t, hb_=hb_: E.dma_start(out=k.h2tab[t * 128:(t + 1) * 128, :], in_=hb_[:]), [hb_bb], [k.h2tab_b[t]])
                transpose_tile_f32(k, h_, h_b, tp, tp_b, hT_, hT_bb)
                for c in range(8):
                    s.op("pe", lambda E, c=c, hT_=hT_: E.matmul(pl[:], lhsT=hT_[:, c, :], rhs=wr[:, c, :], start=(c == 0), stop=(c == 7)),
                         [hT_bb, wr_b], [pl_b], track=(c == 7))
                s.op("act", lambda E: E.activation(out=lg[:], in_=pl[:], func=AF.Exp, accum_out=lgs[:, 0:1]), [pl_b], [lg_b])
                s.op("dve", lambda E: E.reciprocal(out=lgs[:, 1:2], in_=lgs[:, 0:1]), [lg_b], [lg_b])
                s.op("dve", lambda E, t=t: E.tensor_scalar(out=aff[:, t, :], in0=lg[:], scalar1=lgs[:, 1:2], scalar2=None, op0=ALU.mult), [lg_b], [aff_b])
                ti += 1
        s.barrier()
    if k.stop == "postA%d" % l:
        es2.close()
        raise _Stop()
    routing(k, l, keep_ctx, aff, aff_b, posi, posi_b)
    if k.stop == "route%d" % l:
        es2.close()
        raise _Stop()
    experts(k, l, keep_ctx, G2b, G2b_b)
    es2.close()
    s.barrier()


def transpose_tile_f32(k, ht, ht_b, tp, tp_b, hT, hT_b):
    s = k.s
    for half in range(2):
        p, pb = tp[half], tp_b[half]
        for j in range(4):
            c = half * 4 + j
            s.op("pe", lambda E, c=c, j=j, p=p: E.transpose(out=p[:, j * 128:(j + 1) * 128], in_=ht[:, c * 128:(c + 1) * 128], identity=k.ident[:]),
                 [ht_b, k.ident_b], [pb], track=(j == 3))
        if half == 0:
            s.op("act", lambda E, p=p: E.activation(out=hT[:, 0:4, :], in_=p[:].rearrange("p (c t) -> p c t", c=4), func=AF.Copy), [pb], [hT_b])
        else:
            s.op("dve", lambda E, p=p: E.tensor_copy(out=hT[:, 4:8, :], in_=p[:].rearrange("p (c t) -> p c t", c=4)), [pb], [hT_b])


def routing(k, l, keep_ctx, aff, aff_b, posi, posi_b):
    nc, s, I = k.nc, k.s, k.I
    BIG = float(2 ** 20)
    with ExitStack() as st:
        affT = k.sb("g_affT", [NEXP, NT], stack=st)
        affT_b = Buf()
        msk = k.sb("g_msk", [NEXP, NT], stack=st)
        msk_b = Buf()
        cum = k.sb("g_cum", [NEXP, NT], stack=st)
        cum_b = Buf()
        onesr = k.sb("g_ones", [NEXP, NL], stack=st)
        onesr_b = Buf()
        sv = k.sb("g_sv", [NEXP, 8], stack=st)
        sv_b = Buf()
        posf = k.sb("g_posf", [128, NTILE, NEXP], stack=st)
        posf_b = Buf()
        metas = k.sb("g_metas", [128, NTILE, NEXP, 2], stack=st)
        metas_b = Buf()
        tp = [k.ps("g_tp%d" % i, [128, 512], stack=st) for i in range(2)]
        tp_b = bufs(2)
        s.op("pool", lambda E: E.memset(onesr[:], 1.0), [], [onesr_b])
        ebase = k.sb("g_ebase", [NEXP, 2], stack=st)
        ebase_b = Buf()
        s.dma("sp", lambda E: E.dma_start(out=ebase[:], in_=I["ebase"][:, :]), [], [ebase_b])
        for e in range(NEXP):
            s.dma("sp", lambda E, e=e: E.dma_start(out=k.meta[e * SLOTS:(e + 1) * SLOTS, :], in_=I["metainit"][e * SLOTS:(e + 1) * SLOTS, :]), [], [k.meta_b[e]])
        ntile = NTILE if keep_ctx else 64
        for t4 in range(0, ntile, 4):
            p, pb = tp[(t4 // 4) % 2], tp_b[(t4 // 4) % 2]
            n = min(4, ntile - t4)
            for j in range(n):
                s.op("pe", lambda E, t4=t4, j=j, p=p: E.transpose(out=p[0:NEXP, j * 128:(j + 1) * 128], in_=aff[:, t4 + j, :], identity=k.ident[:]),
                     [aff_b, k.ident_b], [pb], track=(j == n - 1))
            s.op("act", lambda E, t4=t4, n=n, p=p: E.activation(out=affT[:, t4 * 128:(t4 + n) * 128], in_=p[0:NEXP, 0:n * 128], func=AF.Copy), [pb], [affT_b])
        segs = [(0, NL, CAP_L, 0, 0)]
        if keep_ctx:
            segs.append((NL, NT, CAP_C, 4, CAP_L))
        for (a, b, cap, so, base) in segs:
            lo, mid, cntv, stp = (sv[:, so + i:so + i + 1] for i in range(4))
            s.op("dve", lambda E, lo=lo: E.memset(lo, 0.0), [], [sv_b])
            for it in range(30):
                wstep = float(2.0 ** -(it + 1))
                s.op("dve", lambda E, lo=lo, mid=mid, wstep=wstep: E.tensor_scalar(out=mid, in0=lo, scalar1=wstep, scalar2=None, op0=ALU.add), [sv_b], [sv_b])
                s.op("dve", lambda E, mid=mid, cntv=cntv, a=a, b=b: E.tensor_scalar(out=msk[:, a:b], in0=affT[:, a:b], scalar1=mid, scalar2=0.0, op0=ALU.is_ge, op1=ALU.add, accum_out=cntv),
                     [affT_b, sv_b], [msk_b, sv_b])
                s.op("dve", lambda E, cntv=cntv, stp=stp, cap=cap, wstep=wstep: E.tensor_scalar(out=stp, in0=cntv, scalar1=float(cap), scalar2=wstep, op0=ALU.is_ge, op1=ALU.mult), [sv_b], [sv_b])
                s.op("dve", lambda E, lo=lo, stp=stp: E.tensor_tensor(out=lo, in0=lo, in1=stp, op=ALU.add), [sv_b], [sv_b])
            s.op("dve", lambda E, lo=lo, a=a, b=b: E.tensor_scalar(out=msk[:, a:b], in0=affT[:, a:b], scalar1=lo, scalar2=None, op0=ALU.is_ge), [affT_b, sv_b, msk_b], [msk_b])
            s.op("dve", lambda E, a=a, b=b: E.tensor_tensor_scan(out=cum[:, a:b], data0=onesr[:, 0:b - a], data1=msk[:, a:b], initial=0.0, op0=ALU.mult, op1=ALU.add),
                 [msk_b, onesr_b], [cum_b])
            col = 0 if base == 0 else 1
            s.op("dve", lambda E, a=a, b=b, cap=cap: E.scalar_tensor_tensor(out=msk[:, a:b], in0=cum[:, a:b], scalar=float(cap), in1=msk[:, a:b], op0=ALU.is_le, op1=ALU.mult), [msk_b, cum_b], [msk_b])
            s.op("dve", lambda E, a=a, b=b, col=col: E.scalar_tensor_tensor(out=cum[:, a:b], in0=cum[:, a:b], scalar=ebase[:, col:col + 1], in1=msk[:, a:b], op0=ALU.add, op1=ALU.mult), [msk_b, cum_b, ebase_b], [cum_b])
            s.op("dve", lambda E, a=a, b=b: E.tensor_scalar(out=cum[:, a:b], in0=cum[:, a:b], scalar1=BIG, scalar2=None, op0=ALU.add), [cum_b], [cum_b])
        for t4 in range(0, ntile, 4):
            p, pb = tp[(t4 // 4) % 2], tp_b[(t4 // 4) % 2]
            n = min(4, ntile - t4)
            for j in range(n):
                s.op("pe", lambda E, t4=t4, j=j, p=p: E.transpose(out=p[:, j * NEXP:(j + 1) * NEXP], in_=cum[:, (t4 + j) * 128:(t4 + j + 1) * 128], identity=k.ident[0:NEXP, 0:NEXP]),
                     [cum_b, k.ident_b], [pb], track=(j == n - 1))
            s.op("act", lambda E, t4=t4, n=n, p=p: E.activation(out=posf[:, t4:t4 + n, :], in_=p[:, 0:n * NEXP].rearrange("p (t e) -> p t e", t=n), func=AF.Copy), [pb], [posf_b])
        s.op("dve", lambda E: E.tensor_copy(out=posi[:, 0:ntile, :], in_=posf[:, 0:ntile, :]), [posf_b], [posi_b])
        s.op("pool", lambda E: E.tensor_copy(out=metas[:, 0:ntile, :, 0], in_=k.tokid[:, 0:ntile].unsqueeze(2).broadcast_to([128, ntile, NEXP])), [k.tokid_b], [metas_b])
        s.op("pool", lambda E: E.tensor_copy(out=metas[:, 0:ntile, :, 1], in_=aff[:, 0:ntile, :]), [aff_b, metas_b], [metas_b])
        regs = {}

        def mkregs(E):
            regs["l"] = E.alloc_register("bnd_l%d" % l)
            E.reg_mov(regs["l"], NEXP * SLOTS - 1)
        s.raw("pool", mkregs)
        for t in range(ntile):
            rk = "l"
            for e in range(NEXP):
                s.dma("pool", lambda E, t=t, e=e, rk=rk: E.indirect_dma_start(
                    out=k.meta[:, :], out_offset=bass.IndirectOffsetOnAxis(ap=posi[:, t, e:e + 1], axis=0),
                    in_=metas[:, t, e, :], in_offset=None, bounds_check=Lazy(lambda: regs["l"]), oob_is_err=False),
                    [posi_b, metas_b], [k.meta_b[e]])

        def freeregs(E):
            E.free_register(regs["l"])
        s.raw("pool", freeregs)
        s.barrier()


def experts(k, l, keep_ctx, G2b, G2b_b):
    nc, s, I = k.nc, k.s, k.I
    nst = 9 if keep_ctx else 8
    nsl = nst * 128
    groups = [(0, 512), (512, 512)] + ([(1024, 128)] if keep_ctx else [])
    with ExitStack() as st:
        mt = [k.sb("e_mt%d" % i, [128, 9, 2], stack=st) for i in range(2)]
        mt_b = bufs(2)
        idx = [k.sb("e_idx%d" % i, [128, 9], I32, stack=st) for i in range(2)]
        idx_b = bufs(2)
        xs = [k.sb("e_xs%d" % i, [128, D], BF16, stack=st) for i in range(3)]
        xs_b = bufs(3)
        xsT = k.sb("e_xsT", [128, 8, SLOTS], BF16, stack=st)
        xsT_b = Buf()
        aT = k.sb("e_aT", [128, 16, SLOTS], BF16, stack=st)
        aT_b = Buf()
        wstg = [k.sb("e_ws%d" % i, [128, 8, 512], stack=st) for i in range(2)]
        wstg_b = bufs(2)
        wbf = [k.sb("e_wb%d" % i, [128, 8, 512], BF16, stack=st) for i in range(4)]
        wbf_b = bufs(4)
        sg = [k.sb("e_sg%d" % i, [128, 512], stack=st) for i in range(2)]
        sg_b = bufs(2)
        yo = [k.sb("e_yo%d" % i, [128, D], stack=st) for i in range(2)]
        yo_b = bufs(2)
        tpb = [k.ps("e_tp%d" % i, [128, 512], BF16, stack=st) for i in range(2)]
        tpb_b = bufs(2)
        pg = [k.ps("e_pg%d" % i, [128, 512], stack=st) for i in range(2)]
        pg_b = bufs(2)
        pu = [k.ps("e_pu%d" % i, [128, 512], stack=st) for i in range(2)]
        pu_b = bufs(2)
        pyy = [k.ps("e_py%d" % i, [128, 512], stack=st) for i in range(2)]
        pyy_b = bufs(2)
        regs = {}

        def mkregs(E):
            regs["b"] = E.alloc_register("bnd_x%d" % l)
            E.reg_mov(regs["b"], NT)
        s.raw("pool", mkregs)
        wi = [0]
        ci = [0]

        def load_w(src_ap, src_b, kind):
            i = wi[0]
            wi[0] += 1
            stg, stg_b = wstg[i % 2], wstg_b[i % 2]
            wb, wb_b = wbf[i % 4], wbf_b[i % 4]
            if kind == "col":
                s.dma("sp", lambda E: E.dma_start(out=stg[:], in_=src_ap.rearrange("(c p) n -> p c n", p=128)), [src_b], [stg_b])
            else:
                s.dma("sp", lambda E: E.dma_start(out=stg[:].rearrange("p a b -> p (a b)").rearrange("p (c n) -> p c n", c=4), in_=src_ap.rearrange("(c p) n -> p c n", p=128)), [src_b], [stg_b])
            eng = ["pool", "dve", "act"][ci[0] % 3]
            ci[0] += 1
            if eng == "act":
                s.op("act", lambda E: E.activation(out=wb[:], in_=stg[:], func=AF.Copy), [stg_b], [wb_b])
            else:
                s.op(eng, lambda E: E.tensor_copy(out=wb[:], in_=stg[:]), [stg_b], [wb_b])
            return wb, wb_b

        xi = 0
        gi = 0
        yi = 0
        for e in range(NEXP):
            m_, m_b = mt[e % 2], mt_b[e % 2]
            ix, ix_b = idx[e % 2], idx_b[e % 2]
            s.dma("sp", lambda E, e=e, m_=m_: E.dma_start(out=m_[:, 0:nst, :], in_=k.meta[e * SLOTS:e * SLOTS + nsl, :].rearrange("(t p) c -> p t c", p=128)), [k.meta_b[e]], [m_b])
            s.op("dve", lambda E, m_=m_, ix=ix: E.tensor_copy(out=ix[:, 0:nst], in_=m_[:, 0:nst, 0]), [m_b], [ix_b])
            for stl in range(nst):
                x_, x_b = xs[xi % 3], xs_b[xi % 3]
                p, pb = tpb[xi % 2], tpb_b[xi % 2]
                xi += 1
                s.dma("pool", lambda E, stl=stl, x_=x_, ix=ix: E.indirect_dma_start(
                    out=x_[:, :], out_offset=None, in_=k.h2tab[:, :], in_offset=bass.IndirectOffsetOnAxis(ap=ix[:, stl:stl + 1], axis=0),
                    bounds_check=Lazy(lambda: regs["b"]), oob_is_err=False), list(k.h2tab_b) + [ix_b], [x_b])
                for half in range(2):
                    for j in range(4):
                        c = half * 4 + j
                        s.op("pe", lambda E, c=c, j=j, p=p, x_=x_: E.transpose(out=p[:, j * 128:(j + 1) * 128], in_=x_[:, c * 128:(c + 1) * 128], identity=k.identb[:]),
                             [x_b, k.identb_b], [pb], track=(j == 3))
                    if half == 0:
                        s.op("act", lambda E, p=p, stl=stl: E.activation(out=xsT[:, 0:4, stl * 128:(stl + 1) * 128], in_=p[:].rearrange("p (c t) -> p c t", c=4), func=AF.Copy), [pb], [xsT_b])
                    else:
                        s.op("dve", lambda E, p=p, stl=stl: E.tensor_copy(out=xsT[:, 4:8, stl * 128:(stl + 1) * 128], in_=p[:].rearrange("p (c t) -> p c t", c=4)), [pb], [xsT_b])
            for fb in range(4):
                wg, wg_b = load_w(I["moe_wg%d" % l][e * D:(e + 1) * D, fb * 512:(fb + 1) * 512], k.IB["moe_wg%d" % l], "col")
                wu, wu_b = load_w(I["moe_wu%d" % l][e * D:(e + 1) * D, fb * 512:(fb + 1) * 512], k.IB["moe_wu%d" % l], "col")
                for f4 in range(4):
                    f = fb * 4 + f4
                    for (s0, sw) in groups:
                        g_, g_b = pg[gi % 2], pg_b[gi % 2]
                        u_, u_b = pu[gi % 2], pu_b[gi % 2]
                        sg_, sg_bb = sg[gi % 2], sg_b[gi % 2]
                        gi += 1
                        for c in range(8):
                            s.op("pe", lambda E, c=c, f4=f4, g_=g_, wg=wg, s0=s0, sw=sw: E.matmul(g_[:, 0:sw], lhsT=wg[:, c, f4 * 128:(f4 + 1) * 128], rhs=xsT[:, c, s0:s0 + sw], start=(c == 0), stop=(c == 7)),
                                 [wg_b, xsT_b], [g_b], track=(c == 7))
                        for c in range(8):
                            s.op("pe", lambda E, c=c, f4=f4, u_=u_, wu=wu, s0=s0, sw=sw: E.matmul(u_[:, 0:sw], lhsT=wu[:, c, f4 * 128:(f4 + 1) * 128], rhs=xsT[:, c, s0:s0 + sw], start=(c == 0), stop=(c == 7)),
                                 [wu_b, xsT_b], [u_b], track=(c == 7))
                        s.op("act", lambda E, g_=g_, sg_=sg_, sw=sw: E.activation(out=sg_[:, 0:sw], in_=g_[:, 0:sw], func=AF.Silu), [g_b], [sg_bb])
                        s.op("dve", lambda E, u_=u_, sg_=sg_, f=f, s0=s0, sw=sw: E.tensor_tensor(out=aT[:, f, s0:s0 + sw], in0=u_[:, 0:sw], in1=sg_[:, 0:sw], op=ALU.mult), [u_b, sg_bb], [aT_b])
            wds = []
            for rb in range(4):
                wds.append(load_w(I["moe_wd%d" % l][e * EDIM + rb * 512:e * EDIM + (rb + 1) * 512, :], k.IB["moe_wd%d" % l], "row"))
            for stl in range(nst):
                cond = 0 if stl < 8 else 1
                y_, y_b = yo[yi % 2], yo_b[yi % 2]
                yi += 1
                for nb in range(2):
                    p, p_b = pyy[nb], pyy_b[nb]
                    for f in range(16):
                        wd, wd_b = wds[f // 4]
                        wdv = wd[:].rearrange("p a b -> p (a b)").rearrange("p (c n) -> p c n", c=4)
                        s.op("pe", lambda E, f=f, nb=nb, p=p, wdv=wdv, stl=stl: E.matmul(p[:], lhsT=aT[:, f, stl * 128:(stl + 1) * 128], rhs=wdv[:, f % 4, nb * 512:(nb + 1) * 512], start=(f == 0), stop=(f == 15)),
                             [aT_b, wd_b], [p_b], track=(f == 15))
                    s.op("dve", lambda E, nb=nb, p=p, y_=y_, m_=m_, stl=stl, cond=cond: E.scalar_tensor_tensor(out=y_[:, nb * 512:(nb + 1) * 512], in0=p[:], scalar=m_[:, stl, 1:2], in1=G2b[cond][:, nb * 512:(nb + 1) * 512], op0=ALU.mult, op1=ALU.mult),
                         [p_b, m_b, G2b_b[cond]], [y_b])
                s.dma("pool", lambda E, y_=y_, ix=ix, stl=stl: E.indirect_dma_start(
                    out=k.xres[:, :], out_offset=bass.IndirectOffsetOnAxis(ap=ix[:, stl:stl + 1], axis=0), in_=y_[:, :], in_offset=None,
                    bounds_check=Lazy(lambda: regs["b"]), oob_is_err=True, compute_op=ALU.add), [y_b, ix_b], list(k.xres_b))

        def freeregs(E):
            E.free_register(regs["b"])
        s.raw("pool", freeregs)
        s.barrier()


def final_norm(k):
    nc, s, I = k.nc, k.s, k.I
    with ExitStack() as st:
        nf = k.sb("f_nf", [128, D], stack=st)
        nf_b = Buf()
        s.dma("sp", lambda E: E.dma_start(out=nf[:], in_=I["norm_final"].unsqueeze(0).broadcast_to([128, D])), [], [nf_b])
        xt = [k.sb("f_xt%d" % i, [128, D], stack=st) for i in range(2)]
        xt_b = bufs(2)
        ot = [k.sb("f_ot%d" % i, [128, D], stack=st) for i in range(2)]
        ot_b = bufs(2)
        junk = k.sb("f_junk", [128, D], stack=st)
        stat = [k.sb("f_stat%d" % i, [128, 2], stack=st) for i in range(2)]
        scr_b = bufs(2)
        k.out_b = Buf()
        for t in range(64):
            x_, x_b = xt[t % 2], xt_b[t % 2]
            o_, o_b = ot[t % 2], ot_b[t % 2]
            sc = stat[t % 2]
            sb_ = scr_b[t % 2]
            s.dma("sp", lambda E, t=t, x_=x_: E.dma_start(out=x_[:], in_=k.xres[t * 128:(t + 1) * 128, :]), [k.xres_b[t]], [x_b])
            s.op("act", lambda E, x_=x_, sc=sc: E.activation(out=junk[:], in_=x_[:], func=AF.Square, accum_out=sc[:, 0:1]), [x_b], [sb_])
            s.op("act", lambda E, sc=sc: E.activation(out=sc[:, 1:2], in_=sc[:, 0:1], func=AF.Sqrt, scale=float(1.0 / D), bias=k.epsc[:, 0:1]), [sb_, k.epsc_b], [sb_])
            s.op("dve", lambda E, sc=sc: E.reciprocal(out=sc[:, 1:2], in_=sc[:, 1:2]), [sb_], [sb_])
            s.op("dve", lambda E, x_=x_, o_=o_, sc=sc: E.scalar_tensor_tensor(out=o_[:], in0=x_[:], scalar=sc[:, 1:2], in1=nf[:], op0=ALU.mult, op1=ALU.mult), [x_b, sb_, nf_b], [o_b])
            s.dma("pool", lambda E, t=t, o_=o_: E.dma_start(out=k.out[t * 128:(t + 1) * 128, :], in_=o_[:]), [o_b], [k.out_b])


def _rope_tables_T(rot_dim, reps_rows):
    t = np.arange(NL)
    rows = (t // GRID_W).astype(np.float32)
    cols = (t % GRID_W).astype(np.float32)
    n_freq = rot_dim // 4
    inv_freq = (np.float32(10000.0) ** (-np.arange(n_freq, dtype=np.float32) / np.float32(n_freq))).astype(np.float32)
    ang = np.concatenate([rows[:, None] * inv_freq, cols[:, None] * inv_freq], axis=-1).astype(np.float32)
    cos = np.cos(ang).astype(np.float32)
    sin = np.sin(ang).astype(np.float32)
    half = rot_dim // 2
    cT = np.ones((rot_dim, NT), np.float32)
    sT = np.zeros((rot_dim, NT), np.float32)
    cT[:half, :NL] = cos.T
    cT[half:, :NL] = cos.T
    sT[:half, :NL] = -sin.T
    sT[half:, :NL] = sin.T
    return cT, sT


def _swap_halves_cols(w, unit):
    din, dout = w.shape
    w4 = w.reshape(din, dout // unit, 2, unit // 2)
    return np.ascontiguousarray(w4[:, :, ::-1, :]).reshape(din, dout)


def _na_bias(rpb):
    NEG = np.float32(-30000.0)
    qc = np.arange(64)
    cs = np.clip(qc - 8, 0, 48)
    kc = np.arange(64)
    valid = (kc[:, None] >= cs[None, :]) & (kc[:, None] < cs[None, :] + 16)
    colidx = np.clip(kc[:, None] - qc[None, :] + 15, 0, 30)
    out = np.full((16, 128, 8, 4, 64), NEG, np.float32)
    for v in range(8):
        for kt in range(4):
            for half in range(2):
                kr = 2 * kt + half
                ridx = kr - v + 7
                vals = rpb[:, ridx][:, colidx]
                out[:, half * 64:(half + 1) * 64, v, kt, :] = np.where(valid[None], vals, NEG)
    return out


def _shard(a2d, r):
    n = a2d.shape[0] // 8
    return a2d[r * n:(r + 1) * n]


def make_shared(inp):
    f = lambda a: np.ascontiguousarray(np.asarray(a, dtype=np.float32))
    S = {}
    S["ada_b"] = f(inp["ada_b"])
    S["norm_mix"] = f(inp["norm_mix"])
    S["norm_ffn"] = f(inp["norm_ffn"])
    S["norm_final"] = f(inp["norm_final"])
    S["ident"] = np.eye(128, dtype=np.float32)
    S["tokid"] = f((np.arange(NTILE)[None, :] * 128 + np.arange(128)[:, None]))
    mi = np.zeros((NEXP * SLOTS, 2), np.float32)
    mi[:, 0] = DUMMY
    S["metainit"] = mi
    BIG = float(2 ** 20)
    eb = np.zeros((NEXP, 2), np.float32)
    eb[:, 0] = np.arange(NEXP) * SLOTS - 1 - BIG
    eb[:, 1] = np.arange(NEXP) * SLOTS + CAP_L - 1 - BIG
    S["ebase"] = eb
    S["moe_wr"] = f(inp["moe_w_router"])
    S["da_lam"] = f(np.stack([inp["da_lambda_q1"][0], inp["da_lambda_k1"][0], inp["da_lambda_q2"][0], inp["da_lambda_k2"][0]]))
    S["da_subln"] = f(np.asarray(inp["da_subln"][0]).reshape(128, 1))
    G = {}
    for l in range(4):
        G["ada_w%d" % l] = f(inp["ada_w"][l])
        G["moe_wg%d" % l] = f(inp["moe_w_gate"][l]).reshape(NEXP * D, EDIM)
        G["moe_wu%d" % l] = f(inp["moe_w_up"][l]).reshape(NEXP * D, EDIM)
        G["moe_wd%d" % l] = f(inp["moe_w_down"][l]).reshape(NEXP * EDIM, D)
    G["da_w"] = f(inp["da_w_qkv"][0])
    G["da_wo"] = f(inp["da_w_o"][0])
    G["fn_wo"] = f(inp["fn_w_o"][0])
    n2 = np.arange(128, dtype=np.float64)[:, None, None]
    k1 = np.arange(64, dtype=np.float64)[None, :, None]
    k2 = np.arange(128, dtype=np.float64)[None, None, :]
    th = 2.0 * np.pi * n2 * (k1 + 64.0 * k2) / 8192.0
    G["fn_cos"] = f(np.cos(th).reshape(128, 8192))
    G["fn_sin"] = f(np.sin(th).reshape(128, 8192))
    n1 = np.arange(64, dtype=np.float64)[:, None]
    kk = np.arange(64, dtype=np.float64)[None, :]
    c64 = np.cos(2.0 * np.pi * n1 * kk / 64.0)
    s64 = np.sin(2.0 * np.pi * n1 * kk / 64.0)
    f64t = np.zeros((64, 4, 48))
    for kb in range(4):
        f64t[:, kb, 0:16] = c64[:, kb * 16:(kb + 1) * 16]
        f64t[:, kb, 16:32] = -s64[:, kb * 16:(kb + 1) * 16]
        f64t[:, kb, 32:48] = -c64[:, kb * 16:(kb + 1) * 16]
    S["fn_f64"] = f(f64t.reshape(64, 192))
    cc_ = np.arange(256, dtype=np.float64)
    th2 = 2.0 * np.pi * cc_[:, None] * cc_[None, :] / 256.0
    S["fn_cc"] = f(np.cos(th2))
    S["fn_sc"] = f(np.sin(th2))
    S["fn_nsc"] = f(-np.sin(th2))
    G["na_w"] = f(inp["na_w_qkv"][0])
    G["na_wo"] = f(inp["na_w_o"][0])
    G["na_bias"] = _na_bias(np.asarray(inp["na_rpb"][0], np.float32)).reshape(NEXP * 128, 2048)
    S["mla_qnorm"] = f(inp["mla_q_norm"][0])
    S["mla_kvnorm"] = f(inp["mla_kv_norm"][0])
    G["mla_wdq"] = f(inp["mla_w_dq"][0])
    G["mla_wuq"] = f(inp["mla_w_uq"][0])
    G["mla_wdkv"] = f(inp["mla_w_dkv"][0])
    G["mla_wuk"] = f(inp["mla_w_uk"][0])
    G["mla_wuv"] = f(inp["mla_w_uv"][0])
    G["mla_wo"] = f(inp["mla_w_o"][0])
    c3, s3 = _rope_tables_T(32, None)
    G["rope3c"] = f(np.concatenate([np.ones((64, NT), np.float32), c3], axis=0))
    G["rope3s"] = f(np.concatenate([np.zeros((64, NT), np.float32), s3], axis=0))
    c0, s0 = _rope_tables_T(64, None)
    G["rope0c"] = f(np.concatenate([c0, c0], axis=0))
    G["rope0s"] = f(np.concatenate([s0, s0], axis=0))
    return S, G


def make_core_inputs(inp, S, G, core):
    b = core // 2
    m = dict(S)
    m["xin"] = np.ascontiguousarray(np.concatenate([np.asarray(inp["x"][b], np.float32), np.asarray(inp["ctx"][b], np.float32)], axis=0))
    cc = np.stack([np.asarray(inp["c"][b], np.float32), np.asarray(inp["c_ctx"], np.float32)], axis=-1)
    m["ccT"] = np.ascontiguousarray(cc.reshape(8, 128, 2).transpose(1, 0, 2))
    for name, a in G.items():
        m[name] = _shard(a, core)
    return m


_NC_CACHE = {}


def kernel(**inputs):
    if "nc" not in _NC_CACHE:
        _NC_CACHE["nc"] = build(n_layers=4, dbg=False, gather=False)
    nc = _NC_CACHE["nc"]
    S, G = make_shared(inputs)
    names = set(LAST_INPUT_NAMES)
    in_maps = []
    for b in range(4):
        m = make_core_inputs(inputs, S, G, 2 * b)
        m.update(G)
        in_maps.append({n: v for n, v in m.items() if n in names})
    res = run_bass_kernel_spmd(nc, in_maps, core_ids=list(range(4)))
    out = np.stack([np.asarray(res.results[b]["out"], dtype=np.float32) for b in range(4)], axis=0)
    return out
```

```python
import math
from contextlib import ExitStack
import numpy as np
import concourse.bass as bass
import concourse.mybir as mybir
from concourse.bass_utils import run_bass_kernel_spmd

F32 = mybir.dt.float32
BF16 = mybir.dt.bfloat16
I32 = mybir.dt.int32
AF = mybir.ActivationFunctionType
ALU = mybir.AluOpType
AX = mybir.AxisListType

D = 1024
NL = 8192
NC_ = 256
NT = NL + NC_
NTILE = NT // 128
DUMMY = NT
EPS = 1e-6
NEXP = 16
EDIM = 2048
CAP_L = 1024
CAP_C = 32
SLOTS = 1152
GRID_W = 64

ENGS = ["pe", "act", "dve", "pool", "sp"]


class Buf:
    __slots__ = ("w", "r")

    def __init__(self):
        self.w = None
        self.r = {}


def bufs(n):
    return [Buf() for _ in range(n)]


class Lazy:
    def __init__(self, f):
        self.f = f


class _Rec:
    def __init__(self):
        self.calls = []

    def __getattr__(self, name):
        def f(*args, **kw):
            self.calls.append((name, args, kw))
            return self
        return f


def _replay(E, call):
    name, args, kw = call
    args = [a.f() if isinstance(a, Lazy) else a for a in args]
    kw = {k_: (v.f() if isinstance(v, Lazy) else v) for k_, v in kw.items()}
    return getattr(E, name)(*args, **kw)


def _record(fn):
    r = _Rec()
    fn(r)
    assert len(r.calls) == 1, r.calls
    return r.calls[0]


class Sched:
    def __init__(self, nc, es, n_dma=None):
        self.nc = nc
        self.es = es
        self.ckeys = []
        self.prog = {e: [] for e in ENGS}
        self.cnt = {e: 0 for e in ENGS}
        self.waited = {e: {} for e in ENGS}
        self.sem = {}
        for e in ["pe", "act", "dve", "pool"]:
            self.sem[e] = es.enter_context(nc.semaphore("s_" + e))
        n_dma = n_dma or {"sp": 16, "pool": 16, "act": 4}
        self.dkeys = {}
        self.dnext = {}
        self.dval = {}
        for q, n in n_dma.items():
            ks = []
            for i in range(n):
                k = "d_%s_%d" % (q, i)
                self.sem[k] = es.enter_context(nc.semaphore(k))
                self.dval[k] = 0
                ks.append(k)
            self.dkeys[q] = ks
            self.dnext[q] = 0

    def _deps(self, reads, writes):
        deps = {}

        def add(k, v):
            if deps.get(k, 0) < v:
                deps[k] = v
        for b in reads:
            if b.w is not None:
                add(*b.w)
        for b in writes:
            if b.w is not None:
                add(*b.w)
            for k, v in b.r.items():
                add(k, v)
        return deps

    def _waits(self, eng, deps):
        for k, v in deps.items():
            if eng == "pe" and k == "pe":
                continue
            if self.waited[eng].get(k, 0) >= v:
                continue
            self.waited[eng][k] = v
            sem = self.sem[k]
            self.prog[eng].append(lambda E, sem=sem, v=v: E.wait_ge(sem, v))

    def _mark(self, ev, reads, writes):
        k, v = ev
        for b in reads:
            if b.r.get(k, 0) < v:
                b.r[k] = v
        for b in writes:
            b.w = ev
            b.r = {}

    def op(self, eng, fn, reads=(), writes=(), track=True):
        self._waits(eng, self._deps(reads, writes))
        if track:
            self.cnt[eng] += 1
            v = self.cnt[eng]
            sem = self.sem[eng]
            call = _record(fn)
            self.prog[eng].append(lambda E, call=call, sem=sem: _replay(E, call).then_inc(sem, 1))
        else:
            v = self.cnt[eng] + 1
            call = _record(fn)
            self.prog[eng].append(lambda E, call=call: _replay(E, call))
        self._mark((eng, v), reads, writes)

    def dma(self, q, fn, reads=(), writes=()):
        ks = self.dkeys[q]
        key = ks[self.dnext[q] % len(ks)]
        self.dnext[q] += 1
        deps = self._deps(reads, writes)
        if self.dval[key] > 0 and deps.get(key, 0) < self.dval[key]:
            deps[key] = self.dval[key]
        self._waits(q, deps)
        self.dval[key] += 16
        v = self.dval[key]
        sem = self.sem[key]
        call = _record(fn)
        self.prog[q].append(lambda E, call=call, sem=sem: _replay(E, call).then_inc(sem, 16))
        self._mark((key, v), reads, writes)

    def coll(self, fn, reads=(), writes=()):
        key = "cc_%d" % len(self.ckeys)
        self.ckeys.append(key)
        self.sem[key] = self.es.enter_context(self.nc.semaphore(key))
        self._waits("pool", self._deps(reads, writes))
        sem = self.sem[key]
        call = _record(fn)
        self.prog["pool"].append(lambda E, call=call, sem=sem: _replay(E, call).then_inc(sem, 1))
        self.dval[key] = 1
        self._mark((key, 1), reads, writes)

    def raw(self, eng, fn):
        self.prog[eng].append(lambda E, fn=fn: fn(E))

    def barrier(self):
        allev = {}
        for e in ["pe", "act", "dve", "pool"]:
            if self.cnt[e] > 0:
                allev[e] = self.cnt[e]
        for k, v in self.dval.items():
            if v > 0:
                allev[k] = v
        for e in ENGS:
            d = dict(allev)
            self._waits_all(e, d)

    def _waits_all(self, eng, deps):
        for k, v in deps.items():
            if self.waited[eng].get(k, 0) >= v:
                continue
            self.waited[eng][k] = v
            sem = self.sem[k]
            self.prog[eng].append(lambda E, sem=sem, v=v: E.wait_ge(sem, v))

    def emit(self):
        nc = self.nc
        with nc.Block() as block:
            @block.tensor
            def _(E):
                for f in self.prog["pe"]:
                    f(E)

            @block.scalar
            def _(E):
                for f in self.prog["act"]:
                    f(E)

            @block.vector
            def _(E):
                for f in self.prog["dve"]:
                    f(E)

            @block.gpsimd
            def _(E):
                for f in self.prog["pool"]:
                    f(E)

            @block.sync
            def _(E):
                for f in self.prog["sp"]:
                    f(E)


class K:
    pass


LAST_INPUT_NAMES = []


class _Stop(Exception):
    pass


def build(n_layers=4, dbg=False, stop=None, gather=True):
    nc = bass.Bass("TRN2", target_bir_lowering=False)
    del LAST_INPUT_NAMES[:]
    es = ExitStack()
    k = K()
    k.nc = nc
    k.dbg = dbg
    k.uid = 0
    k.stop = stop

    def chk(name):
        if stop == name:
            raise _Stop()
    k.chk = chk
    s = Sched(nc, es)
    k.s = s

    def din(name, shape, dt=F32):
        LAST_INPUT_NAMES.append(name)
        return nc.dram_tensor(name, list(shape), dt, kind="ExternalInput").ap()

    def dscr(name, shape, dt=F32):
        return nc.dram_tensor(name, list(shape), dt).ap()

    k.din = din
    k.dscr = dscr
    I = {}
    IB = {}

    def gin(name, rows, cols):
        assert rows % 8 == 0
        if not gather:
            I[name] = din(name, [rows, cols])
            IB[name] = Buf()
            return
        ext = din(name, [rows // 8, cols])
        loc = dscr(name + "_l", [rows // 8, cols])
        full = dscr(name + "_g", [rows, cols])
        lb, fb = Buf(), Buf()
        s.dma("pool", lambda E: E.dma_start(out=loc[:, :], in_=ext[:, :]), [], [lb])
        s.coll(lambda E: E.collective_compute("AllGather", ALU.bypass, replica_groups=[list(range(8))], ins=[loc[:, :]], outs=[full[:, :]]), [lb], [fb])
        I[name] = full
        IB[name] = fb

    k.gin = gin
    I["xin"] = din("xin", [NT, D])
    I["ccT"] = din("ccT", [128, 8, 2])
    I["ada_b"] = din("ada_b", [4, 6 * D])
    I["norm_mix"] = din("norm_mix", [4, D])
    I["norm_ffn"] = din("norm_ffn", [4, D])
    I["norm_final"] = din("norm_final", [D])
    I["ident"] = din("ident", [128, 128])
    I["tokid"] = din("tokid", [128, NTILE])
    I["metainit"] = din("metainit", [NEXP * SLOTS, 2])
    I["ebase"] = din("ebase", [NEXP, 2])
    I["moe_wr"] = din("moe_wr", [4, D, NEXP])
    I["da_lam"] = din("da_lam", [4, 64])
    I["da_subln"] = din("da_subln", [128, 1])
    for l in range(4):
        gin("ada_w%d" % l, D, 6 * D)
    gin("da_w", D, 3 * D)
    gin("da_wo", D, D)
    gin("rope0c", 128, NT)
    gin("rope0s", 128, NT)
    I["mla_qnorm"] = din("mla_qnorm", [768])
    I["mla_kvnorm"] = din("mla_kvnorm", [256])
    if n_layers >= 2:
        gin("fn_wo", D, D)
        gin("fn_cos", 128, NL)
        gin("fn_sin", 128, NL)
        I["fn_f64"] = din("fn_f64", [64, 192])
        I["fn_cc"] = din("fn_cc", [256, 256])
        I["fn_sc"] = din("fn_sc", [256, 256])
        I["fn_nsc"] = din("fn_nsc", [256, 256])
    if n_layers >= 3:
        gin("na_w", D, 3 * D)
        gin("na_wo", D, D)
        gin("na_bias", NEXP * 128, 2048)
    if n_layers >= 4:
        gin("mla_wdq", D, 768)
        gin("mla_wuq", 768, 1536)
        gin("mla_wdkv", D, 288)
        gin("mla_wuk", 256, D)
        gin("mla_wuv", 256, D)
        gin("mla_wo", D, D)
        gin("rope3c", 96, NT)
        gin("rope3s", 96, NT)
    for l in range(n_layers):
        gin("moe_wg%d" % l, NEXP * D, EDIM)
        gin("moe_wu%d" % l, NEXP * D, EDIM)
        gin("moe_wd%d" % l, NEXP * EDIM, D)
    k.IB = IB
    k.I = I
    out = nc.dram_tensor("out", [NL, D], F32, kind="ExternalOutput").ap()
    k.out = out
    if dbg:
        k.dbg_x = nc.dram_tensor("dbg_x", [NT, D], F32, kind="ExternalOutput").ap()
        k.dbg_t = {}
        for nm, shp in [("qT", [1536, NT]), ("kT", [D, NT]), ("vv", [NT, D]), ("aT", [D, NT])]:
            k.dbg_t[nm] = nc.dram_tensor("dbg_" + nm, shp, BF16, kind="ExternalOutput").ap()
    k.xres = dscr("xres", [NT + 1, D])
    k.xres_b = bufs(NTILE + 1)
    k.modv = dscr("modv", [4, 6, 2, D])
    k.modv_b = Buf()
    k.h2tab = dscr("h2tab", [NT + 1, D], BF16)
    k.h2tab_b = bufs(NTILE + 1)
    k.meta = dscr("meta", [NEXP * SLOTS, 2])
    k.meta_b = bufs(NEXP)
    k.qT = dscr("qT", [1536, NT], BF16)
    k.krT = dscr("krT", [32, NT], BF16)
    k.krT_b = bufs(17)
    k.kT = dscr("kT", [D, NT], BF16)
    k.vv = dscr("vv", [NT, D], BF16)
    k.aT = dscr("aT", [D, NT], BF16)
    NG = 17
    k.NG = NG
    k.qT_b = bufs(NG)
    k.kT_b = bufs(NG)
    k.vv_b = bufs(NG)
    k.aT_b = bufs(NG)

    def sb(name, shape, dt=F32, stack=es):
        k.uid += 1
        return stack.enter_context(nc.sbuf_tensor("sb%d_%s" % (k.uid, name), list(shape), dt))

    def ps(name, shape, dt=F32, stack=es):
        k.uid += 1
        return stack.enter_context(nc.psum_tensor("ps%d_%s" % (k.uid, name), list(shape), dt))

    k.sb = sb
    k.ps = ps
    k.ident = sb("ident", [128, 128])
    k.ident_b = Buf()
    k.identb = sb("identb", [128, 128], BF16)
    k.identb_b = Buf()
    k.ones_f = sb("ones_f", [128, 128])
    k.ones_b = sb("ones_b", [128, 128], BF16)
    k.ones_bb = Buf()
    k.tokid = sb("tokid", [128, NTILE])
    k.tokid_b = Buf()
    s.dma("sp", lambda E: E.dma_start(out=k.ident[:], in_=I["ident"][:, :]), [], [k.ident_b])
    s.dma("sp", lambda E: E.dma_start(out=k.tokid[:], in_=I["tokid"][:, :]), [], [k.tokid_b])
    s.op("dve", lambda E: E.tensor_copy(out=k.identb[:], in_=k.ident[:]), [k.ident_b], [k.identb_b])
    s.op("dve", lambda E: E.memset(k.ones_f[:], 1.0), [], [k.ones_bb])
    s.op("dve", lambda E: E.memset(k.ones_b[:], 1.0), [], [k.ones_bb])
    k.epsc = sb("epsc", [128, 1])
    k.epsc_b = Buf()
    s.op("dve", lambda E: E.memset(k.epsc[:], float(EPS)), [], [k.epsc_b])

    for t in range(NTILE):
        s.dma("sp", lambda E, t=t: E.dma_start(out=k.xres[t * 128:(t + 1) * 128, :], in_=I["xin"][t * 128:(t + 1) * 128, :]),
              [], [k.xres_b[t]])

    try:
        chk("init")
        phase_mod(k)
        if stop is not None and stop.startswith("mod"):
            raise _Stop()
        for l in range(n_layers):
            kind = l % 4
            if kind == 0:
                mixer_diff(k, l)
            elif kind == 1:
                mixer_fourier(k, l, keep_ctx=(l < 3))
            elif kind == 2:
                mixer_na(k, l, keep_ctx=(l < 3))
            elif kind == 3:
                mixer_mla(k, l, keep_ctx=(l < 3))
            chk("mixer%d" % l)
            post_mixer_and_moe(k, l, keep_ctx=(l < 3))
            chk("layer%d" % l)
    except _Stop:
        pass
    if dbg:
        s.barrier()
        for nm, src in [("qT", k.qT), ("kT", k.kT), ("vv", k.vv), ("aT", k.aT)]:
            rows = src.shape[0]
            for r0 in range(0, rows, 128):
                r1 = min(rows, r0 + 128)
                s.dma("sp", lambda E, nm=nm, src=src, r0=r0, r1=r1: E.dma_start(out=k.dbg_t[nm][r0:r1, :], in_=src[r0:r1, :]), [], [])
        for t in range(NTILE):
            s.dma("sp", lambda E, t=t: E.dma_start(out=k.dbg_x[t * 128:(t + 1) * 128, :], in_=k.xres[t * 128:(t + 1) * 128, :]),
                  [k.xres_b[t]], [])
    final_norm(k)
    s.barrier()
    s.emit()
    es.close()
    return nc


def phase_mod(k):
    nc, s, I = k.nc, k.s, k.I
    with ExitStack() as st:
        cc = k.sb("m_cc", [128, 8, 2], stack=st)
        sil = k.sb("m_sil", [128, 8, 2], stack=st)
        cc_b, sil_b = Buf(), Buf()
        wt = [k.sb("m_w%d" % i, [128, 8, 512], stack=st) for i in range(2)]
        wt_b = bufs(2)
        mrow = k.sb("m_row", [2, 6 * D], stack=st)
        mrow_b = Buf()
        adab = k.sb("m_adab", [2, 6 * D], stack=st)
        adab_b = Buf()
        nw = k.sb("m_nw", [2, 2, D], stack=st)
        nw_b = Buf()
        aa = k.sb("m_aa", [2, 2, D], stack=st)
        aa_b = Buf()
        pp = [k.ps("m_ps%d" % i, [2, 512], stack=st) for i in range(2)]
        pp_b = bufs(2)
        s.dma("sp", lambda E: E.dma_start(out=cc[:], in_=I["ccT"][:, :, :]), [], [cc_b])
        s.op("act", lambda E: E.activation(out=sil[:], in_=cc[:], func=AF.Silu), [cc_b], [sil_b])
        if k.stop == "mod_silu":
            s.barrier()
            return
        it = 0
        for l in range(4):
            s.dma("sp", lambda E, l=l: E.dma_start(out=adab[:], in_=I["ada_b"][l:l + 1, :].broadcast_to([2, 6 * D])), [], [adab_b])
            s.dma("sp", lambda E, l=l: E.dma_start(out=nw[:, 0, :], in_=I["norm_mix"][l:l + 1, :].broadcast_to([2, D])), [], [nw_b])
            s.dma("sp", lambda E, l=l: E.dma_start(out=nw[:, 1, :], in_=I["norm_ffn"][l:l + 1, :].broadcast_to([2, D])), [], [nw_b])
            for nb in range(12):
                w = wt[it % 2]
                wb = wt_b[it % 2]
                p = pp[it % 2]
                pb = pp_b[it % 2]
                it += 1
                s.dma("sp", lambda E, l=l, nb=nb, w=w: E.dma_start(
                    out=w[:], in_=I["ada_w%d" % l][:, nb * 512:(nb + 1) * 512].rearrange("(c p) n -> p c n", p=128)), [k.IB["ada_w%d" % l]], [wb])
                for c in range(8):
                    s.op("pe", lambda E, c=c, w=w, p=p: E.matmul(p[:], lhsT=sil[:, c, :], rhs=w[:, c, :], start=(c == 0), stop=(c == 7)),
                         [sil_b, wb], [pb], track=(c == 7))
                s.op("dve", lambda E, nb=nb, p=p: E.tensor_tensor(out=mrow[:, nb * 512:(nb + 1) * 512], in0=p[:], in1=adab[:, nb * 512:(nb + 1) * 512], op=ALU.add),
                     [pb, adab_b], [mrow_b])
                if k.stop == "mod_mm":
                    s.barrier()
                    return
            s.op("dve", lambda E: E.scalar_tensor_tensor(out=aa[:, 0, :], in0=mrow[:, D:2 * D], scalar=1.0, in1=nw[:, 0, :], op0=ALU.add, op1=ALU.mult),
                 [mrow_b, nw_b], [aa_b])
            s.op("dve", lambda E: E.scalar_tensor_tensor(out=aa[:, 1, :], in0=mrow[:, 4 * D:5 * D], scalar=1.0, in1=nw[:, 1, :], op0=ALU.add, op1=ALU.mult),
                 [mrow_b, nw_b], [aa_b])
            srcs = [aa[:, 0, :], mrow[:, 0:D], mrow[:, 2 * D:3 * D], aa[:, 1, :], mrow[:, 3 * D:4 * D], mrow[:, 5 * D:6 * D]]
            for v in range(6):
                s.dma("sp", lambda E, l=l, v=v, src=srcs[v]: E.dma_start(out=k.modv[l, v, :, :], in_=src), [mrow_b, aa_b], [k.modv_b])
            if k.stop == "mod_l0":
                s.barrier()
                return
        s.barrier()


def load_bcast(k, q, dst, dst_b, l, v, cond):
    k.s.dma(q, lambda E: E.dma_start(out=dst, in_=k.modv[l, v, cond:cond + 1, :].broadcast_to([128, D])), [k.modv_b], [dst_b])


def load_weight_bf16(k, st, name, src, src_b, C, ncols, blk=512, swap_cols=0, swap_unit=64):
    s = k.s
    dst = k.sb(name, [128, C, ncols + swap_cols], BF16, stack=st)
    dst_b = Buf()
    if getattr(st, "_wstg", None) is None:
        st._wstg = ([k.sb(name + "_st%d" % i, [128, 4096], stack=st) for i in range(2)], bufs(2), [0])
    stg_full, stg_b, stg_ctr = st._wstg
    stg = [t[:, 0:C * blk].rearrange("p (c n) -> p c n", c=C) for t in stg_full]
    nb = ncols // blk
    hu = swap_unit // 2
    for b in range(nb):
        t = stg[stg_ctr[0] % 2]
        tb = stg_b[stg_ctr[0] % 2]
        stg_ctr[0] += 1
        s.dma("sp", lambda E, b=b, t=t: E.dma_start(out=t, in_=src[:, b * blk:(b + 1) * blk].rearrange("(c p) n -> p c n", p=128)), [src_b], [tb])
        eng = ["pool", "dve"][b % 2]
        s.op(eng, lambda E, b=b, t=t: E.tensor_copy(out=dst[:, :, b * blk:(b + 1) * blk], in_=t), [tb], [dst_b])
        if (b + 1) * blk <= swap_cols:
            for c in range(C):
                for half in range(2):
                    eng2 = ["dve", "pool"][(c + half) % 2]
                    s.op(eng2, lambda E, b=b, t=t, c=c, half=half: E.tensor_copy(
                        out=dst[:, c, ncols + b * blk:ncols + (b + 1) * blk].rearrange("p (u two h) -> p u two h", two=2, h=hu)[:, :, half, :],
                        in_=t[:, c, :].rearrange("p (u two h) -> p u two h", two=2, h=hu)[:, :, 1 - half, :]), [tb], [dst_b])
    return dst, dst_b


def norm_tile(k, xt, xt_b, ht, ht_b, Ab, Ab_b, Bb, Bb_b, scr, scr_b):
    s = k.s
    junk, stat = scr
    s.op("act", lambda E: E.activation(out=junk[:], in_=xt[:], func=AF.Square, accum_out=stat[:, 0:1]), [xt_b], [scr_b])
    s.op("act", lambda E: E.activation(out=stat[:, 1:2], in_=stat[:, 0:1], func=AF.Sqrt, scale=float(1.0 / D), bias=k.epsc[:, 0:1]), [scr_b, k.epsc_b], [scr_b])
    s.op("dve", lambda E: E.reciprocal(out=stat[:, 1:2], in_=stat[:, 1:2]), [scr_b], [scr_b])
    s.op("dve", lambda E: E.scalar_tensor_tensor(out=ht[:], in0=xt[:], scalar=stat[:, 1:2], in1=Ab[:], op0=ALU.mult, op1=ALU.mult),
         [xt_b, scr_b, Ab_b], [ht_b])
    s.op("pool", lambda E: E.tensor_tensor(out=ht[:], in0=ht[:], in1=Bb[:], op=ALU.add), [ht_b, Bb_b], [ht_b])


def transpose_tile(k, ht, ht_b, tp, tp_b, hT, hT_b, col0, ncol=128, nchunk=8, evac=("act", "dve")):
    s = k.s
    for half in range((nchunk + 3) // 4):
        p = tp[half % len(tp)]
        pb = tp_b[half % len(tp)]
        cs = list(range(half * 4, min(nchunk, half * 4 + 4)))
        for j, c in enumerate(cs):
            s.op("pe", lambda E, c=c, j=j, p=p: E.transpose(out=p[:, j * 128:j * 128 + ncol], in_=ht[0:ncol, c * 128:(c + 1) * 128], identity=k.ident[0:ncol, 0:ncol]),
                 [ht_b, k.ident_b], [pb], track=(j == len(cs) - 1))
        eng = evac[half % len(evac)]
        n = len(cs)
        if eng == "act":
            s.op("act", lambda E, p=p, cs=cs, n=n: E.activation(out=hT[:, cs[0]:cs[0] + n, col0:col0 + ncol], in_=p[:, 0:n * 128].rearrange("p (c t) -> p c t", c=n)[:, :, 0:ncol], func=AF.Copy),
                 [pb], [hT_b])
        else:
            s.op("dve", lambda E, p=p, cs=cs, n=n: E.tensor_copy(out=hT[:, cs[0]:cs[0] + n, col0:col0 + ncol], in_=p[:, 0:n * 128].rearrange("p (c t) -> p c t", c=n)[:, :, 0:ncol]),
                 [pb], [hT_b])


def group_range(g):
    t0 = g * 512
    w = min(512, NT - t0)
    return t0, w


def mixer_diff(k, l):
    nc, s, I = k.nc, k.s, k.I
    lam_init = 0.8 - 0.6 * math.exp(-0.3 * l)
    with ExitStack() as st:
        W, W_b = load_weight_bf16(k, st, "d_w", I["da_w"], k.IB["da_w"], 8, 3 * D, swap_cols=2 * D, swap_unit=64)
        Ab = [k.sb("d_Ab%d" % c, [128, D], stack=st) for c in range(2)]
        Bb = [k.sb("d_Bb%d" % c, [128, D], stack=st) for c in range(2)]
        Ab_b, Bb_b = bufs(2), bufs(2)
        for c in range(2):
            load_bcast(k, "sp", Ab[c][:], Ab_b[c], l, 0, c)
            load_bcast(k, "sp", Bb[c][:], Bb_b[c], l, 1, c)
        xt = [k.sb("d_xt%d" % i, [128, D], stack=st) for i in range(2)]
        xt_b = bufs(2)
        ht = [k.sb("d_ht%d" % i, [128, D], stack=st) for i in range(2)]
        ht_b = bufs(2)
        junk = k.sb("d_junk", [128, D], stack=st)
        stat = [k.sb("d_stat%d" % i, [128, 2], stack=st) for i in range(2)]
        scr_b = bufs(2)
        hT = [k.sb("d_hT%d" % i, [128, 8, 512], BF16, stack=st) for i in range(2)]
        hT_b = bufs(2)
        rc = [k.sb("d_rc%d" % i, [128, 512], stack=st) for i in range(2)]
        rs = [k.sb("d_rs%d" % i, [128, 512], stack=st) for i in range(2)]
        rc_b, rs_b = bufs(2), bufs(2)
        t1 = [k.sb("d_t1%d" % i, [128, 512], stack=st) for i in range(2)]
        t2 = [k.sb("d_t2%d" % i, [128, 512], stack=st) for i in range(2)]
        t1_b, t2_b = bufs(2), bufs(2)
        qo = [k.sb("d_qo%d" % i, [128, 512], BF16, stack=st) for i in range(4)]
        qo_b = bufs(4)
        vo = [k.sb("d_vo%d" % i, [128, D], BF16, stack=st) for i in range(2)]
        vo_b = bufs(2)
        tp = [k.ps("d_tp%d" % i, [128, 512], stack=st) for i in range(2)]
        tp_b = bufs(2)
        pq = [k.ps("d_pq%d" % i, [128, 512], stack=st) for i in range(4)]
        pq_b = bufs(4)
        pv = [k.ps("d_pv%d" % i, [128, 512], stack=st) for i in range(2)]
        pv_b = bufs(2)
        ti = 0
        oi = 0
        vi = 0
        for g in range(k.NG):
            t0, w = group_range(g)
            hTg, hTg_b = hT[g % 2], hT_b[g % 2]
            s.dma("sp", lambda E, g=g, t0=t0, w=w: E.dma_start(out=rc[g % 2][:, 0:w], in_=I["rope0c"][:, t0:t0 + w]), [k.IB["rope0c"]], [rc_b[g % 2]])
            s.dma("sp", lambda E, g=g, t0=t0, w=w: E.dma_start(out=rs[g % 2][:, 0:w], in_=I["rope0s"][:, t0:t0 + w]), [k.IB["rope0s"]], [rs_b[g % 2]])
            for j in range(w // 128):
                t = (t0 // 128) + j
                cond = 0 if t < 64 else 1
                x_, x_b = xt[ti % 2], xt_b[ti % 2]
                h_, h_b = ht[ti % 2], ht_b[ti % 2]
                s.dma("sp", lambda E, t=t, x_=x_: E.dma_start(out=x_[:], in_=k.xres[t * 128:(t + 1) * 128, :]), [k.xres_b[t]], [x_b])
                norm_tile(k, x_, x_b, h_, h_b, Ab[cond], Ab_b[cond], Bb[cond], Bb_b[cond], (junk, stat[ti % 2]), scr_b[ti % 2])
                transpose_tile(k, h_, h_b, tp, tp_b, hTg, hTg_b, j * 128)
                ti += 1
            for o in range(16):
                pa, pa_b = pq[(2 * o) % 4], pq_b[(2 * o) % 4]
                pb_, pb_b = pq[(2 * o + 1) % 4], pq_b[(2 * o + 1) % 4]
                for c in range(8):
                    s.op("pe", lambda E, c=c, o=o, pa=pa: E.matmul(pa[:, 0:w], lhsT=W[:, c, o * 128:(o + 1) * 128], rhs=hTg[:, c, 0:w], start=(c == 0), stop=(c == 7)),
                         [W_b, hTg_b], [pa_b], track=(c == 7))
                for c in range(8):
                    s.op("pe", lambda E, c=c, o=o, pb_=pb_: E.matmul(pb_[:, 0:w], lhsT=W[:, c, 3 * D + o * 128:3 * D + (o + 1) * 128], rhs=hTg[:, c, 0:w], start=(c == 0), stop=(c == 7)),
                         [W_b, hTg_b], [pb_b], track=(c == 7))
                a1, a1_b = t1[o % 2], t1_b[o % 2]
                a2, a2_b = t2[o % 2], t2_b[o % 2]
                q_, q_b = qo[oi % 4], qo_b[oi % 4]
                oi += 1
                s.op("dve", lambda E, pa=pa, a1=a1: E.tensor_tensor(out=a1[:, 0:w], in0=pa[:, 0:w], in1=rc[g % 2][:, 0:w], op=ALU.mult), [pa_b, rc_b[g % 2]], [a1_b])
                s.op("dve", lambda E, pb_=pb_, a2=a2: E.tensor_tensor(out=a2[:, 0:w], in0=pb_[:, 0:w], in1=rs[g % 2][:, 0:w], op=ALU.mult), [pb_b, rs_b[g % 2]], [a2_b])
                s.op("pool", lambda E, a1=a1, a2=a2, q_=q_: E.tensor_tensor(out=q_[:, 0:w], in0=a1[:, 0:w], in1=a2[:, 0:w], op=ALU.add), [a1_b, a2_b], [q_b])
                dstT = k.qT if o < 8 else k.kT
                dst_b = (k.qT_b if o < 8 else k.kT_b)[g]
                oo = o % 8
                s.dma("pool", lambda E, dstT=dstT, oo=oo, q_=q_: E.dma_start(out=dstT[oo * 128:(oo + 1) * 128, t0:t0 + w], in_=q_[:, 0:w]), [q_b], [dst_b])
            for j in range(w // 128):
                v_, v_b = vo[vi % 2], vo_b[vi % 2]
                vi += 1
                for nb in range(2):
                    p, p_b = pv[nb], pv_b[nb]
                    for c in range(8):
                        s.op("pe", lambda E, c=c, nb=nb, p=p, j=j: E.matmul(p[:], lhsT=hTg[:, c, j * 128:(j + 1) * 128], rhs=W[:, c, 2 * D + nb * 512:2 * D + (nb + 1) * 512], start=(c == 0), stop=(c == 7)),
                             [W_b, hTg_b], [p_b], track=(c == 7))
                    s.op("act", lambda E, nb=nb, p=p, v_=v_: E.activation(out=v_[:, nb * 512:(nb + 1) * 512], in_=p[:], func=AF.Copy), [p_b], [v_b])
                t = (t0 // 128) + j
                s.dma("pool", lambda E, t=t, v_=v_: E.dma_start(out=k.vv[t * 128:(t + 1) * 128, :], in_=v_[:]), [v_b], [k.vv_b[g]])
        s.barrier()
    k.chk("proj%d" % l)
    with ExitStack() as st:
        lamt = k.sb("a_lamt", [128, 4, 64], stack=st)
        lamt_b = Buf()
        lamp = k.sb("a_lamp", [128, 2, 64], stack=st)
        lamv = k.sb("a_lamv", [128, 4], stack=st)
        lam_b = Buf()
        gcol = k.sb("a_gcol", [128, 1], stack=st)
        gcol_b = Buf()
        s.dma("sp", lambda E: E.dma_start(out=lamt[:].rearrange("p a b -> p (a b)"), in_=I["da_lam"].rearrange("a b -> (a b)").unsqueeze(0).broadcast_to([128, 256])), [], [lamt_b])
        s.dma("sp", lambda E: E.dma_start(out=gcol[:], in_=I["da_subln"][:, :]), [], [gcol_b])
        s.op("dve", lambda E: E.tensor_tensor(out=lamp[:, 0, :], in0=lamt[:, 0, :], in1=lamt[:, 1, :], op=ALU.mult), [lamt_b], [lam_b])
        s.op("dve", lambda E: E.tensor_tensor(out=lamp[:, 1, :], in0=lamt[:, 2, :], in1=lamt[:, 3, :], op=ALU.mult), [lam_b, lamt_b], [lam_b])
        s.op("dve", lambda E: E.tensor_reduce(out=lamv[:, 0:2], in_=lamp[:], axis=AX.X, op=ALU.add), [lam_b], [lam_b])
        s.op("act", lambda E: E.activation(out=lamv[:, 0:2], in_=lamv[:, 0:2], func=AF.Exp), [lam_b], [lam_b])
        s.op("dve", lambda E: E.tensor_tensor(out=lamv[:, 2:3], in0=lamv[:, 1:2], in1=lamv[:, 0:1], op=ALU.subtract), [lam_b], [lam_b])
        s.op("dve", lambda E: E.tensor_scalar(out=lamv[:, 2:3], in0=lamv[:, 2:3], scalar1=float(-lam_init), scalar2=None, op0=ALU.add), [lam_b], [lam_b])
        s.op("dve", lambda E: E.tensor_scalar(out=gcol[:], in0=gcol[:], scalar1=float(1.0 - lam_init), scalar2=None, op0=ALU.mult), [gcol_b], [gcol_b])
        neg_lam = lamv[:, 2:3]

        def post(h, qb, w, O, O_b, Z, Z_b, wk):
            (r0, o0, o1, sq, on, pss, wk_b, pss_b) = wk
            s.op("dve", lambda E: E.reciprocal(out=r0[:, 0:w], in_=Z[0][:, 0:w]), [Z_b[0]], [wk_b])
            s.op("dve", lambda E: E.tensor_tensor(out=o0[:, 0:w], in0=O[0][:, 0:w], in1=r0[:, 0:w], op=ALU.mult), [O_b[0], wk_b], [wk_b])
            s.op("dve", lambda E: E.reciprocal(out=r0[:, 0:w], in_=Z[1][:, 0:w]), [Z_b[1], wk_b], [wk_b])
            s.op("dve", lambda E: E.tensor_tensor(out=o1[:, 0:w], in0=O[1][:, 0:w], in1=r0[:, 0:w], op=ALU.mult), [O_b[1], wk_b], [wk_b])
            s.op("dve", lambda E: E.scalar_tensor_tensor(out=o0[:, 0:w], in0=o1[:, 0:w], scalar=neg_lam, in1=o0[:, 0:w], op0=ALU.mult, op1=ALU.add), [wk_b, lam_b], [wk_b])
            s.op("pool", lambda E: E.tensor_tensor(out=sq[:, 0:w], in0=o0[:, 0:w], in1=o0[:, 0:w], op=ALU.mult), [wk_b], [wk_b])
            s.op("pe", lambda E: E.matmul(pss[:, 0:w], lhsT=k.ones_f[:], rhs=sq[:, 0:w], start=True, stop=True), [wk_b, k.ones_bb], [pss_b])
            s.op("act", lambda E: E.activation(out=r0[:, 0:w], in_=pss[:, 0:w], func=AF.Sqrt, scale=float(1.0 / 128.0), bias=k.epsc[:, 0:1]), [pss_b, wk_b, k.epsc_b], [wk_b])
            s.op("dve", lambda E: E.reciprocal(out=r0[:, 0:w], in_=r0[:, 0:w]), [wk_b], [wk_b])
            s.op("dve", lambda E: E.scalar_tensor_tensor(out=on[:, 0:w], in0=o0[:, 0:w], scalar=gcol[:, 0:1], in1=r0[:, 0:w], op0=ALU.mult, op1=ALU.mult), [wk_b, gcol_b], [wk_b])
            return on

        attention(k, st, n_heads=8, maps=2, kp=64, dv=128, scale=0.125, post=post)
        s.barrier()


def attention(k, st, n_heads, maps, kp, dv, scale, post, krows=None, qrows=None, extra=None, with_ctx_q=True):
    nc, s = k.nc, k.s
    KP = maps * kp if maps > 1 else kp
    krows = krows or KP
    qrows = qrows or KP
    KT = [k.sb("a_KT%d" % i, [128, NT], BF16, stack=st) for i in range(2)]
    KT_b = bufs(2)
    V = [k.sb("a_V%d" % i, [128, NTILE, dv], BF16, stack=st) for i in range(2)]
    V_b = bufs(2)
    Q = [k.sb("a_Q%d" % i, [128, 512], BF16, stack=st) for i in range(2)]
    Q_b = bufs(2)
    P = [k.sb("a_P%d" % i, [128, 2, 512], BF16, stack=st) for i in range(3)]
    P_b = bufs(3)
    acc = [k.sb("a_acc%d" % i, [128, 512], stack=st) for i in range(2)]
    acc_b = bufs(2)
    accb = [k.sb("a_accb%d" % i, [128, 512], BF16, stack=st) for i in range(2)]
    accb_b = bufs(2)
    r0 = k.sb("a_r0", [128, 512], stack=st)
    o0 = k.sb("a_o0", [128, 512], stack=st)
    o1 = k.sb("a_o1", [128, 512], stack=st)
    sq = k.sb("a_sq", [128, 512], stack=st)
    on = [k.sb("a_on%d" % i, [128, 512], BF16, stack=st) for i in range(2)]
    wk_b = Buf()
    S = [k.ps("a_S%d" % i, [128, 1024], stack=st) for i in range(2)]
    S_b = bufs(2)
    O = [k.ps("a_O%d" % i, [128, 512], stack=st) for i in range(2)]
    O_b = bufs(2)
    Z = [k.ps("a_Z%d" % i, [128, 512], stack=st) for i in range(2)]
    Z_b = bufs(2)
    qblocks = [(g,) + group_range(g) for g in range(k.NG if with_ctx_q else 16)]
    si = 0
    pi = 0
    qi = 0
    oi = 0
    ai = 0
    for h in range(n_heads):
        KTh, KTh_b = KT[h % 2], KT_b[h % 2]
        Vh, Vh_b = V[h % 2], V_b[h % 2]
        s.dma("sp", lambda E: E.dma_start(out=KTh[0:krows, :], in_=k.kT[h * krows:(h + 1) * krows, :]), list(k.kT_b), [KTh_b])
        if extra is not None:
            ex_ap, ex_b, ex_rows = extra
            s.dma("sp", lambda E: E.dma_start(out=KTh[krows:krows + ex_rows, :], in_=ex_ap[:, :]), list(ex_b), [KTh_b])
        for half in range(2):
            s.dma("sp", lambda E: E.dma_start(out=Vh[:, half * 33:(half + 1) * 33, :], in_=k.vv[half * 33 * 128:(half + 1) * 33 * 128, h * dv:(h + 1) * dv].rearrange("(t p) d -> p t d", p=128)),
                  list(k.vv_b), [Vh_b])
        for (g, t0, w) in qblocks:
            Qb, Qb_b = Q[qi % 2], Q_b[qi % 2]
            qi += 1
            s.dma("sp", lambda E: E.dma_start(out=Qb[0:qrows, 0:w], in_=k.qT[h * qrows:(h + 1) * qrows, t0:t0 + w]), [k.qT_b[g]], [Qb_b])
            ktiles = list(range(NTILE)) if t0 < NL else [64, 65]
            pairs = [(ktiles[2 * i], ktiles[2 * i + 1]) for i in range(len(ktiles) // 2)]
            npair = len(pairs)
            for m in range(maps):
                p0 = m * kp
                ac, ac_b = acc[ai % 2], acc_b[ai % 2]
                acb, acb_b = accb[ai % 2], accb_b[ai % 2]
                ai += 1
                slots = []

                def emit_S(p):
                    nonlocal si
                    Sx, Sx_b = S[si % 2], S_b[si % 2]
                    si += 1
                    for j in range(2):
                        kt = pairs[p][j]
                        s.op("pe", lambda E: E.matmul(Sx[:, j * 512:j * 512 + w], lhsT=KTh[p0:p0 + kp, kt * 128:(kt + 1) * 128], rhs=Qb[p0:p0 + kp, 0:w], start=True, stop=True),
                             [KTh_b, Qb_b], [Sx_b], track=(j == 1))
                    slots.append((Sx, Sx_b))

                emit_S(0)
                for p in range(npair):
                    if p + 1 < npair:
                        emit_S(p + 1)
                    Sx, Sx_b = slots[p]
                    Px, Px_b = P[pi % 3], P_b[pi % 3]
                    pi += 1
                    s.op("act", lambda E: E.activation(out=Px[:, :, 0:w], in_=Sx[:].rearrange("p (a b) -> p a b", a=2)[:, :, 0:w], func=AF.Exp, scale=float(scale)), [Sx_b], [Px_b])
                    if p == 0:
                        s.op("dve", lambda E: E.tensor_copy(out=ac[:, 0:w], in_=Px[:, 1, 0:w]), [Px_b], [ac_b])
                    else:
                        s.op("dve", lambda E: E.tensor_tensor(out=ac[:, 0:w], in0=ac[:, 0:w], in1=Px[:, 1, 0:w], op=ALU.add), [Px_b, ac_b], [ac_b])
                    for j in range(2):
                        kt = pairs[p][j]
                        s.op("pe", lambda E: E.matmul(O[m][0:dv, 0:w], lhsT=Vh[:, kt, :], rhs=Px[:, j, 0:w], start=(p == 0 and j == 0), stop=(p == npair - 1 and j == 1)),
                             [Vh_b, Px_b], [O_b[m]], track=(p == npair - 1 and j == 1))
                    s.op("pe", lambda E: E.matmul(Z[m][0:dv, 0:w], lhsT=k.ones_b[:, 0:dv], rhs=Px[:, 0, 0:w], start=(p == 0), stop=False),
                         [k.ones_bb, Px_b], [Z_b[m]], track=True)
                s.op("dve", lambda E: E.tensor_copy(out=acb[:, 0:w], in_=ac[:, 0:w]), [ac_b], [acb_b])
                s.op("pe", lambda E: E.matmul(Z[m][0:dv, 0:w], lhsT=k.ones_b[:, 0:dv], rhs=acb[:, 0:w], start=False, stop=True), [k.ones_bb, acb_b], [Z_b[m]], track=True)
            onx = on[oi % 2]
            oi += 1
            res = post(h, g, w, O, O_b, Z, Z_b, (r0, o0, o1, sq, onx, S[0], wk_b, S_b[0]))
            s.dma("pool", lambda E: E.dma_start(out=k.aT[h * dv:(h + 1) * dv, t0:t0 + w], in_=res[0:dv, 0:w]), [wk_b], [k.aT_b[g]])


class HTP:
    def __init__(self, k, st, l, pfx, v0=0):
        self.k = k
        sb, ps = k.sb, k.ps
        self.Ab = [sb(pfx + "_Ab%d" % c, [128, D], stack=st) for c in range(2)]
        self.Bb = [sb(pfx + "_Bb%d" % c, [128, D], stack=st) for c in range(2)]
        self.Ab_b, self.Bb_b = bufs(2), bufs(2)
        for c in range(2):
            load_bcast(k, "sp", self.Ab[c][:], self.Ab_b[c], l, v0, c)
            load_bcast(k, "sp", self.Bb[c][:], self.Bb_b[c], l, v0 + 1, c)
        self.xt = [sb(pfx + "_xt%d" % i, [128, D], stack=st) for i in range(2)]
        self.xt_b = bufs(2)
        self.ht = [sb(pfx + "_ht%d" % i, [128, D], stack=st) for i in range(2)]
        self.ht_b = bufs(2)
        self.junk = sb(pfx + "_junk", [128, D], stack=st)
        self.stat = [sb(pfx + "_stat%d" % i, [128, 2], stack=st) for i in range(2)]
        self.scr_b = bufs(2)
        self.hT = [sb(pfx + "_hT%d" % i, [128, 8, 512], BF16, stack=st) for i in range(2)]
        self.hT_b = bufs(2)
        self.tp = [ps(pfx + "_tp%d" % i, [128, 512], stack=st) for i in range(2)]
        self.tp_b = bufs(2)
        self.ti = 0

    def tile(self, t):
        k, s = self.k, self.k.s
        i = self.ti % 2
        self.ti += 1
        cond = 0 if t < 64 else 1
        x_, x_b, h_, h_b = self.xt[i], self.xt_b[i], self.ht[i], self.ht_b[i]
        s.dma("sp", lambda E: E.dma_start(out=x_[:], in_=k.xres[t * 128:(t + 1) * 128, :]), [k.xres_b[t]], [x_b])
        norm_tile(k, x_, x_b, h_, h_b, self.Ab[cond], self.Ab_b[cond], self.Bb[cond], self.Bb_b[cond], (self.junk, self.stat[i]), self.scr_b[i])
        return h_, h_b

    def group(self, g):
        k = self.k
        t0, w = group_range(g)
        hTg, hTg_b = self.hT[g % 2], self.hT_b[g % 2]
        for j in range(w // 128):
            h_, h_b = self.tile(t0 // 128 + j)
            transpose_tile(k, h_, h_b, self.tp, self.tp_b, hTg, hTg_b, j * 128)
        return hTg, hTg_b, t0, w


def rms_rows(k, src_list, n, gain, gain_b, dst, dst_b, wk, wk_b):
    s = k.s
    junk, st4 = wk
    for i, (ap, b, wd) in enumerate(src_list):
        s.op("act", lambda E, ap=ap, i=i, wd=wd: E.activation(out=junk[:, 0:wd], in_=ap, func=AF.Square, accum_out=st4[:, i:i + 1]), [b], [wk_b])
    if len(src_list) == 2:
        s.op("dve", lambda E: E.tensor_tensor(out=st4[:, 0:1], in0=st4[:, 0:1], in1=st4[:, 1:2], op=ALU.add), [wk_b], [wk_b])
    s.op("act", lambda E: E.activation(out=st4[:, 2:3], in_=st4[:, 0:1], func=AF.Sqrt, scale=float(1.0 / n), bias=k.epsc[:, 0:1]), [wk_b, k.epsc_b], [wk_b])
    s.op("dve", lambda E: E.reciprocal(out=st4[:, 2:3], in_=st4[:, 2:3]), [wk_b], [wk_b])
    c0 = 0
    for (ap, b, wd) in src_list:
        s.op("dve", lambda E, ap=ap, c0=c0, wd=wd: E.scalar_tensor_tensor(out=dst[:, c0:c0 + wd], in0=ap, scalar=st4[:, 2:3], in1=gain[:, c0:c0 + wd], op0=ALU.mult, op1=ALU.mult),
             [b, wk_b, gain_b], [dst_b])
        c0 += wd


def mixer_mla(k, l, keep_ctx):
    nc, s, I, IB = k.nc, k.s, k.I, k.IB
    with ExitStack() as st:
        Wdq, Wdq_b = load_weight_bf16(k, st, "m_wdq", I["mla_wdq"], IB["mla_wdq"], 8, 768, blk=256)
        Wdkv, Wdkv_b = load_weight_bf16(k, st, "m_wdkv", I["mla_wdkv"], IB["mla_wdkv"], 8, 288, blk=288, swap_cols=288, swap_unit=32)
        Wuq, Wuq_b = load_weight_bf16(k, st, "m_wuq", I["mla_wuq"], IB["mla_wuq"], 6, 1536, blk=512, swap_cols=1536, swap_unit=32)
        Wuk, Wuk_b = load_weight_bf16(k, st, "m_wuk", I["mla_wuk"], IB["mla_wuk"], 2, 1024, blk=512)
        Wuv, Wuv_b = load_weight_bf16(k, st, "m_wuv", I["mla_wuv"], IB["mla_wuv"], 2, 1024, blk=512)
        qnb = k.sb("m_qnb", [128, 768], stack=st)
        kvnb = k.sb("m_kvnb", [128, 256], stack=st)
        qnb_b, kvnb_b = Buf(), Buf()
        s.dma("sp", lambda E: E.dma_start(out=qnb[:], in_=I["mla_qnorm"].unsqueeze(0).broadcast_to([128, 768])), [], [qnb_b])
        s.dma("sp", lambda E: E.dma_start(out=kvnb[:], in_=I["mla_kvnorm"].unsqueeze(0).broadcast_to([128, 256])), [], [kvnb_b])
        htp = HTP(k, st, l, "m")
        qn = [k.sb("m_qn%d" % i, [128, 768], stack=st) for i in range(2)]
        qn_b = bufs(2)
        cn = [k.sb("m_cn%d" % i, [128, 256], stack=st) for i in range(2)]
        cn_b = bufs(2)
        junk = htp.junk
        st4 = [k.sb("m_st4%d" % i, [128, 4], stack=st) for i in range(2)]
        st4_b = bufs(2)
        qlT = [k.sb("m_qlT%d" % i, [128, 6, 512], BF16, stack=st) for i in range(2)]
        qlT_b = bufs(2)
        ckT = [k.sb("m_ckT%d" % i, [128, 2, 512], BF16, stack=st) for i in range(2)]
        ckT_b = bufs(2)
        rc = [k.sb("m_rc0", [96, 512], stack=st)] * 2
        rs = [k.sb("m_rs0", [96, 512], stack=st)] * 2
        rkc = [k.sb("m_rkc0", [32, 512], stack=st)] * 2
        rks = [k.sb("m_rks0", [32, 512], stack=st)] * 2
        rt_b = [Buf()] * 2
        t1 = [k.sb("m_t1%d" % i, [128, 512], stack=st) for i in range(2)]
        t2 = [k.sb("m_t2%d" % i, [128, 512], stack=st) for i in range(2)]
        t1_b, t2_b = bufs(2), bufs(2)
        ob = [k.sb("m_ob%d" % i, [128, 512], BF16, stack=st) for i in range(4)]
        ob_b = bufs(4)
        vo = [k.sb("m_vo0", [128, D], BF16, stack=st)] * 2
        vo_b = [Buf()] * 2
        pA0 = k.ps("m_pA0", [128, 512], stack=st)
        pA1 = k.ps("m_pA1", [128, 512], stack=st)
        pC = k.ps("m_pC", [128, 512], stack=st)
        pE = [k.ps("m_pE%d" % i, [128, 512], stack=st) for i in range(2)]
        pF = k.ps("m_pF", [128, 512], stack=st)
        pA0_b, pA1_b, pC_b, pF_b = Buf(), Buf(), Buf(), Buf()
        pE_b = bufs(2)
        oi = 0
        vi = 0
        ti = 0
        for g in range(k.NG):
            hTg, hTg_b, t0, w = htp.group(g)
            qlTg, qlTg_b = qlT[g % 2], qlT_b[g % 2]
            ckTg, ckTg_b = ckT[g % 2], ckT_b[g % 2]
            s.dma("sp", lambda E, g=g, t0=t0, w=w: E.dma_start(out=rc[g % 2][:, 0:w], in_=I["rope3c"][:, t0:t0 + w]), [IB["rope3c"]], [rt_b[g % 2]])
            s.dma("sp", lambda E, g=g, t0=t0, w=w: E.dma_start(out=rs[g % 2][:, 0:w], in_=I["rope3s"][:, t0:t0 + w]), [IB["rope3s"]], [rt_b[g % 2]])
            s.dma("sp", lambda E, g=g, t0=t0, w=w: E.dma_start(out=rkc[g % 2][:, 0:w], in_=I["rope3c"][64:96, t0:t0 + w]), [IB["rope3c"]], [rt_b[g % 2]])
            s.dma("sp", lambda E, g=g, t0=t0, w=w: E.dma_start(out=rks[g % 2][:, 0:w], in_=I["rope3s"][64:96, t0:t0 + w]), [IB["rope3s"]], [rt_b[g % 2]])
            for j in range(w // 128):
                i2 = ti % 2
                ti += 1
                for c in range(8):
                    s.op("pe", lambda E, c=c, j=j: E.matmul(pA0[:], lhsT=hTg[:, c, j * 128:(j + 1) * 128], rhs=Wdq[:, c, 0:512], start=(c == 0), stop=(c == 7)), [hTg_b, Wdq_b], [pA0_b], track=(c == 7))
                for c in range(8):
                    s.op("pe", lambda E, c=c, j=j: E.matmul(pA1[:, 0:256], lhsT=hTg[:, c, j * 128:(j + 1) * 128], rhs=Wdq[:, c, 512:768], start=(c == 0), stop=(c == 7)), [hTg_b, Wdq_b], [pA1_b], track=(c == 7))
                for c in range(8):
                    s.op("pe", lambda E, c=c, j=j: E.matmul(pC[:, 0:288], lhsT=hTg[:, c, j * 128:(j + 1) * 128], rhs=Wdkv[:, c, 0:288], start=(c == 0), stop=(c == 7)), [hTg_b, Wdkv_b], [pC_b], track=(c == 7))
                rms_rows(k, [(pA0[:], pA0_b, 512), (pA1[:, 0:256], pA1_b, 256)], 768, qnb, qnb_b, qn[i2], qn_b[i2], (junk, st4[i2]), st4_b[i2])
                transpose_tile(k, qn[i2], qn_b[i2], htp.tp, htp.tp_b, qlTg, qlTg_b, j * 128, nchunk=6)
                rms_rows(k, [(pC[:, 0:256], pC_b, 256)], 256, kvnb, kvnb_b, cn[i2], cn_b[i2], (junk, st4[i2]), st4_b[i2])
                transpose_tile(k, cn[i2], cn_b[i2], htp.tp, htp.tp_b, ckTg, ckTg_b, j * 128, nchunk=2)
            for c in range(8):
                s.op("pe", lambda E, c=c: E.matmul(pE[0][0:32, 0:w], lhsT=Wdkv[:, c, 256:288], rhs=hTg[:, c, 0:w], start=(c == 0), stop=(c == 7)), [hTg_b, Wdkv_b], [pE_b[0]], track=(c == 7))
            for c in range(8):
                s.op("pe", lambda E, c=c: E.matmul(pE[1][0:32, 0:w], lhsT=Wdkv[:, c, 288 + 256:288 + 288], rhs=hTg[:, c, 0:w], start=(c == 0), stop=(c == 7)), [hTg_b, Wdkv_b], [pE_b[1]], track=(c == 7))
            o_, o_b = ob[oi % 4], ob_b[oi % 4]
            oi += 1
            s.op("dve", lambda E: E.tensor_tensor(out=t1[0][0:32, 0:w], in0=pE[0][0:32, 0:w], in1=rkc[g % 2][:, 0:w], op=ALU.mult), [pE_b[0], rt_b[g % 2]], [t1_b[0]])
            s.op("dve", lambda E: E.tensor_tensor(out=t2[0][0:32, 0:w], in0=pE[1][0:32, 0:w], in1=rks[g % 2][:, 0:w], op=ALU.mult), [pE_b[1], rt_b[g % 2]], [t2_b[0]])
            s.op("pool", lambda E, o_=o_: E.tensor_tensor(out=o_[0:32, 0:w], in0=t1[0][0:32, 0:w], in1=t2[0][0:32, 0:w], op=ALU.add), [t1_b[0], t2_b[0]], [o_b])
            s.dma("pool", lambda E, o_=o_: E.dma_start(out=k.krT[:, t0:t0 + w], in_=o_[0:32, 0:w]), [o_b], [k.krT_b[g]])
            for hp in range(8):
                for c in range(2):
                    s.op("pe", lambda E, c=c, hp=hp: E.matmul(pF[:, 0:w], lhsT=Wuk[:, c, hp * 128:(hp + 1) * 128], rhs=ckTg[:, c, 0:w], start=(c == 0), stop=(c == 1)), [ckTg_b, Wuk_b], [pF_b], track=(c == 1))
                o_, o_b = ob[oi % 4], ob_b[oi % 4]
                oi += 1
                s.op("act", lambda E, o_=o_: E.activation(out=o_[:, 0:w], in_=pF[:, 0:w], func=AF.Copy), [pF_b], [o_b])
                s.dma("pool", lambda E, o_=o_, hp=hp: E.dma_start(out=k.kT[hp * 128:(hp + 1) * 128, t0:t0 + w], in_=o_[:, 0:w]), [o_b], [k.kT_b[g]])
            for h in range(16):
                for c in range(6):
                    s.op("pe", lambda E, c=c, h=h: E.matmul(pE[0][0:96, 0:w], lhsT=Wuq[:, c, h * 96:(h + 1) * 96], rhs=qlTg[:, c, 0:w], start=(c == 0), stop=(c == 5)), [qlTg_b, Wuq_b], [pE_b[0]], track=(c == 5))
                for c in range(6):
                    s.op("pe", lambda E, c=c, h=h: E.matmul(pE[1][0:96, 0:w], lhsT=Wuq[:, c, 1536 + h * 96:1536 + (h + 1) * 96], rhs=qlTg[:, c, 0:w], start=(c == 0), stop=(c == 5)), [qlTg_b, Wuq_b], [pE_b[1]], track=(c == 5))
                a1, a1_b = t1[h % 2], t1_b[h % 2]
                a2, a2_b = t2[h % 2], t2_b[h % 2]
                o_, o_b = ob[oi % 4], ob_b[oi % 4]
                oi += 1
                s.op("dve", lambda E, a1=a1: E.tensor_tensor(out=a1[0:96, 0:w], in0=pE[0][0:96, 0:w], in1=rc[g % 2][:, 0:w], op=ALU.mult), [pE_b[0], rt_b[g % 2]], [a1_b])
                s.op("dve", lambda E, a2=a2: E.tensor_tensor(out=a2[0:96, 0:w], in0=pE[1][0:96, 0:w], in1=rs[g % 2][:, 0:w], op=ALU.mult), [pE_b[1], rt_b[g % 2]], [a2_b])
                s.op("pool", lambda E, a1=a1, a2=a2, o_=o_: E.tensor_tensor(out=o_[0:96, 0:w], in0=a1[0:96, 0:w], in1=a2[0:96, 0:w], op=ALU.add), [a1_b, a2_b], [o_b])
                s.dma("pool", lambda E, o_=o_, h=h: E.dma_start(out=k.qT[h * 96:(h + 1) * 96, t0:t0 + w], in_=o_[0:96, 0:w]), [o_b], [k.qT_b[g]])
            for j in range(w // 128):
                v_, v_b = vo[vi % 2], vo_b[vi % 2]
                vi += 1
                for nb in range(2):
                    for c in range(2):
                        s.op("pe", lambda E, c=c, nb=nb, j=j: E.matmul(pF[:], lhsT=ckTg[:, c, j * 128:(j + 1) * 128], rhs=Wuv[:, c, nb * 512:(nb + 1) * 512], start=(c == 0), stop=(c == 1)), [ckTg_b, Wuv_b], [pF_b], track=(c == 1))
                    s.op("act", lambda E, nb=nb, v_=v_: E.activation(out=v_[:, nb * 512:(nb + 1) * 512], in_=pF[:], func=AF.Copy), [pF_b], [v_b])
                t = (t0 // 128) + j
                s.dma("pool", lambda E, t=t, v_=v_: E.dma_start(out=k.vv[t * 128:(t + 1) * 128, :], in_=v_[:]), [v_b], [k.vv_b[g]])
        s.barrier()
    with ExitStack() as st:
        def post(h, qb, w, O, O_b, Z, Z_b, wk):
            (r0, o0, o1, sq, on, pss, wk_b, pss_b) = wk
            s.op("dve", lambda E: E.reciprocal(out=r0[0:64, 0:w], in_=Z[0][0:64, 0:w]), [Z_b[0]], [wk_b])
            s.op("dve", lambda E: E.tensor_tensor(out=on[0:64, 0:w], in0=O[0][0:64, 0:w], in1=r0[0:64, 0:w], op=ALU.mult), [O_b[0], wk_b], [wk_b])
            return on
        attention(k, st, n_heads=16, maps=1, kp=96, dv=64, scale=float(96 ** -0.5), post=post, krows=64, qrows=96, extra=(k.krT, k.krT_b, 32), with_ctx_q=keep_ctx)
        s.barrier()


def qkv_plain(k, st, l, pfx, wname, q_scale):
    s, I, IB = k.s, k.I, k.IB
    W, W_b = load_weight_bf16(k, st, pfx + "_w", I[wname], IB[wname], 8, 3 * D)
    htp = HTP(k, st, l, pfx)
    qo = [k.sb(pfx + "_qo%d" % i, [128, 512], BF16, stack=st) for i in range(4)]
    qo_b = bufs(4)
    vo = [k.sb(pfx + "_vo%d" % i, [128, D], BF16, stack=st) for i in range(2)]
    vo_b = bufs(2)
    pq = [k.ps(pfx + "_pq%d" % i, [128, 512], stack=st) for i in range(2)]
    pq_b = bufs(2)
    pv = [k.ps(pfx + "_pv%d" % i, [128, 512], stack=st) for i in range(2)]
    pv_b = bufs(2)
    oi = 0
    vi = 0
    for g in range(k.NG):
        hTg, hTg_b, t0, w = htp.group(g)
        for o in range(16):
            p, p_b = pq[o % 2], pq_b[o % 2]
            for c in range(8):
                s.op("pe", lambda E, c=c, o=o, p=p: E.matmul(p[:, 0:w], lhsT=W[:, c, o * 128:(o + 1) * 128], rhs=hTg[:, c, 0:w], start=(c == 0), stop=(c == 7)), [W_b, hTg_b], [p_b], track=(c == 7))
            q_, q_b = qo[oi % 4], qo_b[oi % 4]
            oi += 1
            if o < 8:
                s.op("act", lambda E, p=p, q_=q_: E.activation(out=q_[:, 0:w], in_=p[:, 0:w], func=AF.Copy, scale=float(q_scale)), [p_b], [q_b])
            else:
                s.op("dve", lambda E, p=p, q_=q_: E.tensor_copy(out=q_[:, 0:w], in_=p[:, 0:w]), [p_b], [q_b])
            dstT = k.qT if o < 8 else k.kT
            dst_b = (k.qT_b if o < 8 else k.kT_b)[g]
            oo = o % 8
            s.dma("pool", lambda E, dstT=dstT, oo=oo, q_=q_: E.dma_start(out=dstT[oo * 128:(oo + 1) * 128, t0:t0 + w], in_=q_[:, 0:w]), [q_b], [dst_b])
        for j in range(w // 128):
            v_, v_b = vo[vi % 2], vo_b[vi % 2]
            vi += 1
            for nb in range(2):
                p, p_b = pv[nb], pv_b[nb]
                for c in range(8):
                    s.op("pe", lambda E, c=c, nb=nb, p=p, j=j: E.matmul(p[:], lhsT=hTg[:, c, j * 128:(j + 1) * 128], rhs=W[:, c, 2 * D + nb * 512:2 * D + (nb + 1) * 512], start=(c == 0), stop=(c == 7)), [W_b, hTg_b], [p_b], track=(c == 7))
                s.op("act", lambda E, nb=nb, p=p, v_=v_: E.activation(out=v_[:, nb * 512:(nb + 1) * 512], in_=p[:], func=AF.Copy), [p_b], [v_b])
            t = (t0 // 128) + j
            s.dma("pool", lambda E, t=t, v_=v_: E.dma_start(out=k.vv[t * 128:(t + 1) * 128, :], in_=v_[:]), [v_b], [k.vv_b[g]])


def mixer_na(k, l, keep_ctx):
    s, I, IB = k.s, k.I, k.IB
    with ExitStack() as st:
        qkv_plain(k, st, l, "n", "na_w", 0.125)
        s.barrier()
    with ExitStack() as st:
        KT = [k.sb("n_KT%d" % i, [64, NT], BF16, stack=st) for i in range(2)]
        QT = [k.sb("n_QT%d" % i, [64, NT], BF16, stack=st) for i in range(2)]
        Ve = [k.sb("n_Ve%d" % i, [128, 66, 64], BF16, stack=st) for i in range(2)]
        Vo = [k.sb("n_Vo%d" % i, [128, 65, 64], BF16, stack=st) for i in range(2)]
        hb_b = bufs(2)
        bf = [k.sb("n_bf%d" % i, [128, 2048], stack=st) for i in range(2)]
        bf_b = bufs(2)
        bb = [k.sb("n_bb%d" % i, [128, 8, 4, 64], BF16, stack=st) for i in range(2)]
        bb_b = bufs(2)
        P = [k.sb("n_P%d" % i, [128, 512], BF16, stack=st) for i in range(3)]
        P_b = bufs(3)
        r0t = k.sb("n_r0", [64, 512], stack=st)
        on = [k.sb("n_on%d" % i, [64, 512], BF16, stack=st) for i in range(2)]
        on_b = bufs(2)
        r0_b = Buf()
        S = [k.ps("n_S%d" % i, [128, 512], stack=st) for i in range(2)]
        S_b = bufs(2)
        O = [k.ps("n_O%d" % i, [64, 512], stack=st) for i in range(2)]
        O_b = bufs(2)
        Z = [k.ps("n_Z%d" % i, [64, 512], stack=st) for i in range(2)]
        Z_b = bufs(2)
        si = 0
        pi = 0
        oi = 0
        for h in range(16):
            i2 = h % 2
            KTh, QTh, Veh, Voh, hb = KT[i2], QT[i2], Ve[i2], Vo[i2], hb_b[i2]
            s.dma("sp", lambda E, h=h, KTh=KTh: E.dma_start(out=KTh[:, :], in_=k.kT[h * 64:(h + 1) * 64, :]), list(k.kT_b), [hb])
            s.dma("sp", lambda E, h=h, QTh=QTh: E.dma_start(out=QTh[:, :], in_=k.qT[h * 64:(h + 1) * 64, :]), list(k.qT_b), [hb])
            for half in range(2):
                s.dma("sp", lambda E, h=h, Veh=Veh, half=half: E.dma_start(out=Veh[:, half * 33:(half + 1) * 33, :], in_=k.vv[half * 33 * 128:(half + 1) * 33 * 128, h * 64:(h + 1) * 64].rearrange("(t p) d -> p t d", p=128)), list(k.vv_b), [hb])
            s.dma("sp", lambda E, h=h, Voh=Voh: E.dma_start(out=Voh[:, 0:33, :], in_=k.vv[64:64 + 33 * 128, h * 64:(h + 1) * 64].rearrange("(t p) d -> p t d", p=128)), list(k.vv_b), [hb])
            s.dma("sp", lambda E, h=h, Voh=Voh: E.dma_start(out=Voh[:, 33:65, :], in_=k.vv[64 + 33 * 128:64 + 65 * 128, h * 64:(h + 1) * 64].rearrange("(t p) d -> p t d", p=128)), list(k.vv_b), [hb])
            s.dma("sp", lambda E, h=h: E.dma_start(out=bf[i2][:], in_=I["na_bias"][h * 128:(h + 1) * 128, :]), [IB["na_bias"]], [bf_b[i2]])
            s.op("pool", lambda E: E.tensor_copy(out=bb[i2][:].rearrange("p a b c -> p (a b c)"), in_=bf[i2][:]), [bf_b[i2]], [bb_b[i2]])
            blocks = [("lat", rg) for rg in range(16)] + ([("ctx", 0)] if keep_ctx else [])
            for (kind, rg) in blocks:
                Ox, Ox_b, Zx, Zx_b = O[oi % 2], O_b[oi % 2], Z[oi % 2], Z_b[oi % 2]
                onx, onx_b = on[oi % 2], on_b[oi % 2]
                oi += 1
                if kind == "lat":
                    items = []
                    for i in range(8):
                        r = rg * 8 + i
                        r0 = min(max(r - 4, 0), 120)
                        items.append((r, r0, r - r0))
                    for i, (r, r0, v) in enumerate(items):
                        Sx, Sx_b = S[si % 2], S_b[si % 2]
                        si += 1
                        Px, Px_b = P[pi % 3], P_b[pi % 3]
                        pi += 1
                        for kt in range(4):
                            tok0 = r0 * 64 + kt * 128
                            s.op("pe", lambda E, Sx=Sx, kt=kt, tok0=tok0, r=r: E.matmul(Sx[:, kt * 64:(kt + 1) * 64], lhsT=KTh[:, tok0:tok0 + 128], rhs=QTh[:, r * 64:(r + 1) * 64], start=True, stop=False), [hb], [Sx_b], track=False)
                            s.op("pe", lambda E, Sx=Sx, kt=kt, v=v: E.matmul(Sx[:, kt * 64:(kt + 1) * 64], lhsT=k.identb[:], rhs=bb[i2][:, v, kt, :], start=False, stop=True), [k.identb_b, bb_b[i2]], [Sx_b], track=False)
                        for c in range(2):
                            s.op("pe", lambda E, Sx=Sx, c=c, r=r: E.matmul(Sx[:, (4 + c) * 64:(5 + c) * 64], lhsT=KTh[:, NL + c * 128:NL + (c + 1) * 128], rhs=QTh[:, r * 64:(r + 1) * 64], start=True, stop=True), [hb], [Sx_b], track=(c == 1))
                        s.op("act", lambda E, Sx=Sx, Px=Px: E.activation(out=Px[:, 0:384], in_=Sx[:, 0:384], func=AF.Exp), [Sx_b], [Px_b])
                        for kt in range(6):
                            if kt < 4:
                                vt = Veh[:, r0 // 2 + kt, :] if r0 % 2 == 0 else Voh[:, (r0 - 1) // 2 + kt, :]
                            else:
                                vt = Veh[:, 64 + (kt - 4), :]
                            s.op("pe", lambda E, vt=vt, Px=Px, kt=kt, i=i, Ox=Ox: E.matmul(Ox[:, i * 64:(i + 1) * 64], lhsT=vt, rhs=Px[:, kt * 64:(kt + 1) * 64], start=(kt == 0), stop=(kt == 5)), [hb, Px_b], [Ox_b], track=False)
                            s.op("pe", lambda E, Px=Px, kt=kt, i=i, Zx=Zx: E.matmul(Zx[:, i * 64:(i + 1) * 64], lhsT=k.ones_b[:, 0:64], rhs=Px[:, kt * 64:(kt + 1) * 64], start=(kt == 0), stop=(kt == 5)), [k.ones_bb, Px_b], [Zx_b], track=(kt == 5))
                    w = 512
                    c0 = rg * 512
                else:
                    Sx, Sx_b = S[si % 2], S_b[si % 2]
                    si += 1
                    Px, Px_b = P[pi % 3], P_b[pi % 3]
                    pi += 1
                    for c in range(2):
                        s.op("pe", lambda E, Sx=Sx, c=c: E.matmul(Sx[:, c * 256:(c + 1) * 256], lhsT=KTh[:, NL + c * 128:NL + (c + 1) * 128], rhs=QTh[:, NL:NT], start=True, stop=True), [hb], [Sx_b], track=(c == 1))
                    s.op("act", lambda E, Sx=Sx, Px=Px: E.activation(out=Px[:, :], in_=Sx[:, :], func=AF.Exp), [Sx_b], [Px_b])
                    for c in range(2):
                        s.op("pe", lambda E, Px=Px, c=c, Ox=Ox: E.matmul(Ox[:, 0:256], lhsT=Veh[:, 64 + c, :], rhs=Px[:, c * 256:(c + 1) * 256], start=(c == 0), stop=(c == 1)), [hb, Px_b], [Ox_b], track=False)
                        s.op("pe", lambda E, Px=Px, c=c, Zx=Zx: E.matmul(Zx[:, 0:256], lhsT=k.ones_b[:, 0:64], rhs=Px[:, c * 256:(c + 1) * 256], start=(c == 0), stop=(c == 1)), [k.ones_bb, Px_b], [Zx_b], track=(c == 1))
                    w = 256
                    c0 = NL
                s.op("dve", lambda E, Zx=Zx, w=w: E.reciprocal(out=r0t[:, 0:w], in_=Zx[:, 0:w]), [Zx_b], [r0_b])
                s.op("dve", lambda E, Ox=Ox, onx=onx, w=w: E.tensor_tensor(out=onx[:, 0:w], in0=Ox[:, 0:w], in1=r0t[:, 0:w], op=ALU.mult), [Ox_b, r0_b], [onx_b])
                g0 = c0 // 512
                s.dma("pool", lambda E, h=h, c0=c0, w=w, onx=onx: E.dma_start(out=k.aT[h * 64:(h + 1) * 64, c0:c0 + w], in_=onx[:, 0:w]), [onx_b], [k.aT_b[g0]])
        s.barrier()


def mixer_fourier(k, l, keep_ctx):
    s, I, IB = k.s, k.I, k.IB
    with ExitStack() as st:
        htp = HTP(k, st, l, "f")
        hb = [k.sb("f_hb%d" % i, [128, D], BF16, stack=st) for i in range(2)]
        hb_b = bufs(2)
        for t in range(NTILE):
            h_, h_b = htp.tile(t)
            s.op("act", lambda E, h_=h_, t=t: E.activation(out=hb[t % 2][:], in_=h_[:], func=AF.Copy), [h_b], [hb_b[t % 2]])
            s.dma("pool", lambda E, t=t: E.dma_start(out=k.h2tab[t * 128:(t + 1) * 128, :], in_=hb[t % 2][:]), [hb_b[t % 2]], [k.h2tab_b[t]])
        s.barrier()
    with ExitStack() as st:
        stg = k.sb("f_stg", [128, 2048], stack=st)
        stg_b = Buf()
        COS = k.sb("f_cos", [128, 64, 128], BF16, stack=st)
        SIN = k.sb("f_sin", [128, 64, 128], BF16, stack=st)
        F64 = k.sb("f_f64", [64, 4, 48], BF16, stack=st)
        CC = k.sb("f_cc", [128, 2, 256], BF16, stack=st)
        SC = k.sb("f_sc", [128, 2, 256], BF16, stack=st)
        NSC = k.sb("f_nsc", [128, 2, 256], BF16, stack=st)
        tab_b = Buf()
        for (dst, name) in [(COS, "fn_cos"), (SIN, "fn_sin")]:
            for q in range(4):
                s.dma("sp", lambda E, name=name, q=q: E.dma_start(out=stg[:], in_=I[name][:, q * 2048:(q + 1) * 2048]), [IB[name]], [stg_b])
                s.op("dve", lambda E, dst=dst, q=q: E.tensor_copy(out=dst[:].rearrange("p a b -> p (a b)")[:, q * 2048:(q + 1) * 2048], in_=stg[:]), [stg_b], [tab_b])
        s.dma("sp", lambda E: E.dma_start(out=stg[0:64, 0:192], in_=I["fn_f64"][:, :]), [], [stg_b])
        s.op("dve", lambda E: E.tensor_copy(out=F64[:].rearrange("p a b -> p (a b)"), in_=stg[0:64, 0:192]), [stg_b], [tab_b])
        for (dst, name) in [(CC, "fn_cc"), (SC, "fn_sc"), (NSC, "fn_nsc")]:
            s.dma("sp", lambda E, name=name: E.dma_start(out=stg[:, 0:512].rearrange("p (a b) -> p a b", a=2), in_=I[name].rearrange("(a p) l -> p a l", p=128)), [], [stg_b])
            s.op("dve", lambda E, dst=dst: E.tensor_copy(out=dst[:].rearrange("p a b -> p (a b)"), in_=stg[:, 0:512]), [stg_b], [tab_b])
        Xs = k.sb("f_Xs", [64, 128, 128], BF16, stack=st)
        Xs_b = Buf()
        A = k.sb("f_A", [128, 48, 128], BF16, stack=st)
        A_b = Buf()
        GTr = k.sb("f_GTr", [128, 2, NL], BF16, stack=st)
        GTi = k.sb("f_GTi", [128, 2, NL], BF16, stack=st)
        GT_b = Buf()
        hc = k.sb("f_hc", [128, 2, 256], BF16, stack=st)
        hc_b = Buf()
        yo = [k.sb("f_yo%d" % i, [128, 512], BF16, stack=st) for i in range(2)]
        yo_b = bufs(2)
        PA = [k.ps("f_PA%d" % i, [128, 512], stack=st) for i in range(2)]
        PA_b = bufs(2)
        PG = [k.ps("f_PG%d" % i, [128, 512], stack=st) for i in range(4)]
        PG_b = bufs(4)
        PY = [k.ps("f_PY%d" % i, [128, 512], stack=st) for i in range(2)]
        PY_b = bufs(2)
        ai = 0
        gi = 0
        yi = 0

        def channel_dft(gq, k0, kw, nk, scale, col0):
            nonlocal yi
            for lc in range(2):
                for kb in range(nk):
                    p, p_b = PY[yi % 2], PY_b[yi % 2]
                    y_, y_b = yo[yi % 2], yo_b[yi % 2]
                    yi += 1
                    ka = k0 + kb * kw
                    n = 0
                    for cc in range(2):
                        for (T, Gx) in [(CC, GTr), (SC, GTi)]:
                            s.op("pe", lambda E, T=T, Gx=Gx, cc=cc, lc=lc, ka=ka, p=p, n=n: E.matmul(p[:, 0:kw], lhsT=T[:, cc, lc * 128:(lc + 1) * 128], rhs=Gx[:, cc, ka:ka + kw], start=(n == 0), stop=(n == 3)), [tab_b, GT_b], [p_b], track=(n == 3))
                            n += 1
                    s.op("act", lambda E, p=p, y_=y_: E.activation(out=y_[:, 0:kw], in_=p[:, 0:kw], func=AF.Copy, scale=float(scale)), [p_b], [y_b])
                    g0 = (col0 + ka - k0) // 512
                    s.dma("pool", lambda E, y_=y_, lc=lc, ka=ka: E.dma_start(out=k.aT[gq * 256 + lc * 128:gq * 256 + (lc + 1) * 128, col0 + ka - k0:col0 + ka - k0 + kw], in_=y_[:, 0:kw]), [y_b], [k.aT_b[g0]])

        for gq in range(4):
            for cc in range(2):
                col = gq * 256 + cc * 128
                s.dma("sp", lambda E, col=col: E.dma_start(out=Xs[:, :, :], in_=k.h2tab[0:NL, col:col + 128].rearrange("(a b) c -> a b c", b=128)), list(k.h2tab_b), [Xs_b])
                for kb in range(4):
                    for c8 in range(16):
                        p, p_b = PA[ai % 2], PA_b[ai % 2]
                        for ci in range(8):
                            c = c8 * 8 + ci
                            s.op("pe", lambda E, p=p, ci=ci, c=c, kb=kb: E.matmul(p[:, ci * 48:(ci + 1) * 48], lhsT=Xs[:, :, c], rhs=F64[:, kb, :], start=True, stop=True), [Xs_b, tab_b], [p_b], track=(ci == 7))
                        eng = ["act", "dve"][ai % 2]
                        ai += 1
                        if eng == "act":
                            s.op("act", lambda E, p=p, c8=c8: E.activation(out=A[:, :, c8 * 8:(c8 + 1) * 8], in_=p[:, 0:384].rearrange("p (c j) -> p j c", j=48), func=AF.Copy), [p_b], [A_b])
                        else:
                            s.op("dve", lambda E, p=p, c8=c8: E.tensor_copy(out=A[:, :, c8 * 8:(c8 + 1) * 8], in_=p[:, 0:384].rearrange("p (c j) -> p j c", j=48)), [p_b], [A_b])
                    for quad in range(4):
                        pr, pr_b = PG[gi % 4], PG_b[gi % 4]
                        pim, pim_b = PG[(gi + 1) % 4], PG_b[(gi + 1) % 4]
                        gi += 2
                        for q in range(4):
                            k1l = quad * 4 + q
                            k1 = kb * 16 + k1l
                            s.op("pe", lambda E, pr=pr, q=q, k1l=k1l, k1=k1: E.matmul(pr[:, q * 128:(q + 1) * 128], lhsT=A[:, k1l, :], rhs=COS[:, k1, :], start=True, stop=False), [A_b, tab_b], [pr_b], track=False)
                            s.op("pe", lambda E, pr=pr, q=q, k1l=k1l, k1=k1: E.matmul(pr[:, q * 128:(q + 1) * 128], lhsT=A[:, 16 + k1l, :], rhs=SIN[:, k1, :], start=False, stop=True), [A_b, tab_b], [pr_b], track=(q == 3))
                            s.op("pe", lambda E, pim=pim, q=q, k1l=k1l, k1=k1: E.matmul(pim[:, q * 128:(q + 1) * 128], lhsT=A[:, 16 + k1l, :], rhs=COS[:, k1, :], start=True, stop=False), [A_b, tab_b], [pim_b], track=False)
                            s.op("pe", lambda E, pim=pim, q=q, k1l=k1l, k1=k1: E.matmul(pim[:, q * 128:(q + 1) * 128], lhsT=A[:, 32 + k1l, :], rhs=SIN[:, k1, :], start=False, stop=True), [A_b, tab_b], [pim_b], track=(q == 3))
                        k10 = kb * 16 + quad * 4
                        s.op("act", lambda E, pr=pr, cc=cc, k10=k10: E.activation(out=GTr[:, cc, :].rearrange("p (b a) -> p a b", a=64)[:, k10:k10 + 4, :], in_=pr[:].rearrange("p (q b) -> p q b", q=4), func=AF.Copy), [pr_b], [GT_b])
                        s.op("dve", lambda E, pim=pim, cc=cc, k10=k10: E.tensor_copy(out=GTi[:, cc, :].rearrange("p (b a) -> p a b", a=64)[:, k10:k10 + 4, :], in_=pim[:].rearrange("p (q b) -> p q b", q=4)), [pim_b], [GT_b])
            channel_dft(gq, 0, 512, 16, 1.0 / math.sqrt(NL * 256.0), 0)
            if keep_ctx:
                s.dma("sp", lambda E, gq=gq: E.dma_start(out=hc[:, :, :], in_=k.h2tab[NL:NT, gq * 256:(gq + 1) * 256].rearrange("(t p) c -> p t c", p=128)), list(k.h2tab_b), [hc_b])
                for cc in range(2):
                    pr, pr_b = PG[gi % 4], PG_b[gi % 4]
                    pim, pim_b = PG[(gi + 1) % 4], PG_b[(gi + 1) % 4]
                    gi += 2
                    for t in range(2):
                        s.op("pe", lambda E, pr=pr, t=t, cc=cc: E.matmul(pr[:, 0:256], lhsT=hc[:, t, cc * 128:(cc + 1) * 128], rhs=CC[:, t, :], start=(t == 0), stop=(t == 1)), [hc_b, tab_b], [pr_b], track=(t == 1))
                    for t in range(2):
                        s.op("pe", lambda E, pim=pim, t=t, cc=cc: E.matmul(pim[:, 0:256], lhsT=hc[:, t, cc * 128:(cc + 1) * 128], rhs=NSC[:, t, :], start=(t == 0), stop=(t == 1)), [hc_b, tab_b], [pim_b], track=(t == 1))
                    s.op("act", lambda E, pr=pr, cc=cc: E.activation(out=GTr[:, cc, 0:256], in_=pr[:, 0:256], func=AF.Copy), [pr_b], [GT_b])
                    s.op("dve", lambda E, pim=pim, cc=cc: E.tensor_copy(out=GTi[:, cc, 0:256], in_=pim[:, 0:256]), [pim_b], [GT_b])
                channel_dft(gq, 0, 256, 1, 1.0 / 256.0, NL)
        s.barrier()


def post_mixer_and_moe(k, l, keep_ctx):
    nc, s, I = k.nc, k.s, k.I
    wo_name = {0: "da_wo", 1: "fn_wo", 2: "na_wo", 3: "mla_wo"}[l % 4]
    wo_src, wo_src_b = I[wo_name], k.IB[wo_name]
    ntile = NTILE if keep_ctx else 64
    ngrp = k.NG if keep_ctx else 16
    conds = [0, 1] if keep_ctx else [0]
    es2 = ExitStack()
    aff = k.sb("r_aff", [128, NTILE, NEXP], stack=es2)
    aff_b = Buf()
    posi = k.sb("r_posi", [128, NTILE, NEXP], I32, stack=es2)
    posi_b = Buf()
    G2b = [k.sb("r_G2b%d" % c, [128, D], stack=es2) for c in range(2)]
    G2b_b = bufs(2)
    for c in conds:
        load_bcast(k, "sp", G2b[c][:], G2b_b[c], l, 5, c)
    with ExitStack() as st:
        Wo, Wo_b = load_weight_bf16(k, st, "o_w", wo_src, wo_src_b, 8, D)
        wr = k.sb("o_wr", [128, 8, NEXP], stack=st)
        wr_b = Buf()
        s.dma("sp", lambda E: E.dma_start(out=wr[:], in_=I["moe_wr"][l].rearrange("(c p) e -> p c e", p=128)), [], [wr_b])
        G1b = [k.sb("o_G1b%d" % c, [128, D], stack=st) for c in range(2)]
        A2b = [k.sb("o_A2b%d" % c, [128, D], stack=st) for c in range(2)]
        B2b = [k.sb("o_B2b%d" % c, [128, D], stack=st) for c in range(2)]
        G1b_b, A2b_b, B2b_b = bufs(2), bufs(2), bufs(2)
        for c in conds:
            load_bcast(k, "sp", G1b[c][:], G1b_b[c], l, 2, c)
            load_bcast(k, "sp", A2b[c][:], A2b_b[c], l, 3, c)
            load_bcast(k, "sp", B2b[c][:], B2b_b[c], l, 4, c)
        aTs = [k.sb("o_aT%d" % i, [128, 8, 512], BF16, stack=st) for i in range(2)]
        aTs_b = bufs(2)
        xt = [k.sb("o_xt%d" % i, [128, D], stack=st) for i in range(2)]
        xt_b = bufs(2)
        ht = [k.sb("o_ht%d" % i, [128, D], stack=st) for i in range(2)]
        ht_b = bufs(2)
        hb = [k.sb("o_hb%d" % i, [128, D], BF16, stack=st) for i in range(2)]
        hb_b = bufs(2)
        junk = k.sb("o_junk", [128, D], stack=st)
        stat = [k.sb("o_stat%d" % i, [128, 2], stack=st) for i in range(2)]
        scr_b = bufs(2)
        hT = [k.sb("o_hT%d" % i, [128, 8, 128], stack=st) for i in range(2)]
        hT_b = bufs(2)
        lg = k.sb("o_lg", [128, NEXP], stack=st)
        lgs = k.sb("o_lgs", [128, 2], stack=st)
        lg_b = Buf()
        py = [k.ps("o_py%d" % i, [128, 512], stack=st) for i in range(2)]
        py_b = bufs(2)
        tp = [k.ps("o_tp%d" % i, [128, 512], stack=st) for i in range(2)]
        tp_b = bufs(2)
        pl = k.ps("o_pl", [128, NEXP], stack=st)
        pl_b = Buf()
        ti = 0
        for g in range(ngrp):
            t0, w = group_range(g)
            a_, a_b = aTs[g % 2], aTs_b[g % 2]
            s.dma("sp", lambda E, a_=a_, t0=t0, w=w: E.dma_start(out=a_[:, :, 0:w], in_=k.aT[:, t0:t0 + w].rearrange("(c p) t -> p c t", p=128)), [k.aT_b[g]], [a_b])
            for j in range(w // 128):
                t = t0 // 128 + j
                cond = 0 if t < 64 else 1
                x_, x_b = xt[ti % 2], xt_b[ti % 2]
                h_, h_b = ht[ti % 2], ht_b[ti % 2]
                hb_, hb_bb = hb[ti % 2], hb_b[ti % 2]
                hT_, hT_bb = hT[ti % 2], hT_b[ti % 2]
                s.dma("sp", lambda E, t=t, x_=x_: E.dma_start(out=x_[:], in_=k.xres[t * 128:(t + 1) * 128, :]), [k.xres_b[t]], [x_b])
                for nb in range(2):
                    p, p_b = py[nb], py_b[nb]
                    for c in range(8):
                        s.op("pe", lambda E, c=c, nb=nb, p=p, j=j, a_=a_: E.matmul(p[:], lhsT=a_[:, c, j * 128:(j + 1) * 128], rhs=Wo[:, c, nb * 512:(nb + 1) * 512], start=(c == 0), stop=(c == 7)),
                             [a_b, Wo_b], [p_b], track=(c == 7))
                    s.op("dve", lambda E, nb=nb, p=p, h_=h_, cond=cond: E.tensor_tensor(out=h_[:, nb * 512:(nb + 1) * 512], in0=p[:], in1=G1b[cond][:, nb * 512:(nb + 1) * 512], op=ALU.mult),
                         [p_b, G1b_b[cond]], [h_b])
                s.op("pool", lambda E, x_=x_, h_=h_: E.tensor_tensor(out=x_[:], in0=x_[:], in1=h_[:], op=ALU.add), [x_b, h_b], [x_b])
                s.dma("pool", lambda E, t=t, x_=x_: E.dma_start(out=k.xres[t * 128:(t + 1) * 128, :], in_=x_[:]), [x_b], [k.xres_b[t]])
                norm_tile(k, x_, x_b, h_, h_b, A2b[cond], A2b_b[cond], B2b[cond], B2b_b[cond], (junk, stat[ti % 2]), scr_b[ti % 2])
                s.op("act", lambda E, h_=h_, hb_=hb_: E.activation(out=hb_[:], in_=h_[:], func=AF.Copy), [h_b], [hb_bb])
                s.dma("pool", lambda E, t=t, hb_=hb_: E.dma_start(out=k.h2tab[t * 128:(t + 1) * 128, :], in_=hb_[:]), [hb_bb], [k.h2tab_b[t]])
                transpose_tile_f32(k, h_, h_b, tp, tp_b, hT_, hT_bb)
                for c in range(8):
                    s.op("pe", lambda E, c=c, hT_=hT_: E.matmul(pl[:], lhsT=hT_[:, c, :], rhs=wr[:, c, :], start=(c == 0), stop=(c == 7)),
                         [hT_bb, wr_b], [pl_b], track=(c == 7))
                s.op("act", lambda E: E.activation(out=lg[:], in_=pl[:], func=AF.Exp, accum_out=lgs[:, 0:1]), [pl_b], [lg_b])
                s.op("dve", lambda E: E.reciprocal(out=lgs[:, 1:2], in_=lgs[:, 0:1]), [lg_b], [lg_b])
                s.op("dve", lambda E, t=t: E.tensor_scalar(out=aff[:, t, :], in0=lg[:], scalar1=lgs[:, 1:2], scalar2=None, op0=ALU.mult), [lg_b], [aff_b])
                ti += 1
        s.barrier()
    if k.stop == "postA%d" % l:
        es2.close()
        raise _Stop()
    routing(k, l, keep_ctx, aff, aff_b, posi, posi_b)
    if k.stop == "route%d" % l:
        es2.close()
        raise _Stop()
    experts(k, l, keep_ctx, G2b, G2b_b)
    es2.close()
    s.barrier()


def transpose_tile_f32(k, ht, ht_b, tp, tp_b, hT, hT_b):
    s = k.s
    for half in range(2):
        p, pb = tp[half], tp_b[half]
        for j in range(4):
            c = half * 4 + j
            s.op("pe", lambda E, c=c, j=j, p=p: E.transpose(out=p[:, j * 128:(j + 1) * 128], in_=ht[:, c * 128:(c + 1) * 128], identity=k.ident[:]),
                 [ht_b, k.ident_b], [pb], track=(j == 3))
        if half == 0:
            s.op("act", lambda E, p=p: E.activation(out=hT[:, 0:4, :], in_=p[:].rearrange("p (c t) -> p c t", c=4), func=AF.Copy), [pb], [hT_b])
        else:
            s.op("dve", lambda E, p=p: E.tensor_copy(out=hT[:, 4:8, :], in_=p[:].rearrange("p (c t) -> p c t", c=4)), [pb], [hT_b])


def routing(k, l, keep_ctx, aff, aff_b, posi, posi_b):
    nc, s, I = k.nc, k.s, k.I
    BIG = float(2 ** 20)
    with ExitStack() as st:
        affT = k.sb("g_affT", [NEXP, NT], stack=st)
        affT_b = Buf()
        msk = k.sb("g_msk", [NEXP, NT], stack=st)
        msk_b = Buf()
        cum = k.sb("g_cum", [NEXP, NT], stack=st)
        cum_b = Buf()
        onesr = k.sb("g_ones", [NEXP, NL], stack=st)
        onesr_b = Buf()
        sv = k.sb("g_sv", [NEXP, 8], stack=st)
        sv_b = Buf()
        posf = k.sb("g_posf", [128, NTILE, NEXP], stack=st)
        posf_b = Buf()
        metas = k.sb("g_metas", [128, NTILE, NEXP, 2], stack=st)
        metas_b = Buf()
        tp = [k.ps("g_tp%d" % i, [128, 512], stack=st) for i in range(2)]
        tp_b = bufs(2)
        s.op("pool", lambda E: E.memset(onesr[:], 1.0), [], [onesr_b])
        ebase = k.sb("g_ebase", [NEXP, 2], stack=st)
        ebase_b = Buf()
        s.dma("sp", lambda E: E.dma_start(out=ebase[:], in_=I["ebase"][:, :]), [], [ebase_b])
        for e in range(NEXP):
            s.dma("sp", lambda E, e=e: E.dma_start(out=k.meta[e * SLOTS:(e + 1) * SLOTS, :], in_=I["metainit"][e * SLOTS:(e + 1) * SLOTS, :]), [], [k.meta_b[e]])
        ntile = NTILE if keep_ctx else 64
        for t4 in range(0, ntile, 4):
            p, pb = tp[(t4 // 4) % 2], tp_b[(t4 // 4) % 2]
            n = min(4, ntile - t4)
            for j in range(n):
                s.op("pe", lambda E, t4=t4, j=j, p=p: E.transpose(out=p[0:NEXP, j * 128:(j + 1) * 128], in_=aff[:, t4 + j, :], identity=k.ident[:]),
                     [aff_b, k.ident_b], [pb], track=(j == n - 1))
            s.op("act", lambda E, t4=t4, n=n, p=p: E.activation(out=affT[:, t4 * 128:(t4 + n) * 128], in_=p[0:NEXP, 0:n * 128], func=AF.Copy), [pb], [affT_b])
        segs = [(0, NL, CAP_L, 0, 0)]
        if keep_ctx:
            segs.append((NL, NT, CAP_C, 4, CAP_L))
        for (a, b, cap, so, base) in segs:
            lo, mid, cntv, stp = (sv[:, so + i:so + i + 1] for i in range(4))
            s.op("dve", lambda E, lo=lo: E.memset(lo, 0.0), [], [sv_b])
            for it in range(30):
                wstep = float(2.0 ** -(it + 1))
                s.op("dve", lambda E, lo=lo, mid=mid, wstep=wstep: E.tensor_scalar(out=mid, in0=lo, scalar1=wstep, scalar2=None, op0=ALU.add), [sv_b], [sv_b])
                s.op("dve", lambda E, mid=mid, cntv=cntv, a=a, b=b: E.tensor_scalar(out=msk[:, a:b], in0=affT[:, a:b], scalar1=mid, scalar2=0.0, op0=ALU.is_ge, op1=ALU.add, accum_out=cntv),
                     [affT_b, sv_b], [msk_b, sv_b])
                s.op("dve", lambda E, cntv=cntv, stp=stp, cap=cap, wstep=wstep: E.tensor_scalar(out=stp, in0=cntv, scalar1=float(cap), scalar2=wstep, op0=ALU.is_ge, op1=ALU.mult), [sv_b], [sv_b])
                s.op("dve", lambda E, lo=lo, stp=stp: E.tensor_tensor(out=lo, in0=lo, in1=stp, op=ALU.add), [sv_b], [sv_b])
            s.op("dve", lambda E, lo=lo, a=a, b=b: E.tensor_scalar(out=msk[:, a:b], in0=affT[:, a:b], scalar1=lo, scalar2=None, op0=ALU.is_ge), [affT_b, sv_b, msk_b], [msk_b])
            s.op("dve", lambda E, a=a, b=b: E.tensor_tensor_scan(out=cum[:, a:b], data0=onesr[:, 0:b - a], data1=msk[:, a:b], initial=0.0, op0=ALU.mult, op1=ALU.add),
                 [msk_b, onesr_b], [cum_b])
            col = 0 if base == 0 else 1
            s.op("dve", lambda E, a=a, b=b, cap=cap: E.scalar_tensor_tensor(out=msk[:, a:b], in0=cum[:, a:b], scalar=float(cap), in1=msk[:, a:b], op0=ALU.is_le, op1=ALU.mult), [msk_b, cum_b], [msk_b])
            s.op("dve", lambda E, a=a, b=b, col=col: E.scalar_tensor_tensor(out=cum[:, a:b], in0=cum[:, a:b], scalar=ebase[:, col:col + 1], in1=msk[:, a:b], op0=ALU.add, op1=ALU.mult), [msk_b, cum_b, ebase_b], [cum_b])
            s.op("dve", lambda E, a=a, b=b: E.tensor_scalar(out=cum[:, a:b], in0=cum[:, a:b], scalar1=BIG, scalar2=None, op0=ALU.add), [cum_b], [cum_b])
        for t4 in range(0, ntile, 4):
            p, pb = tp[(t4 // 4) % 2], tp_b[(t4 // 4) % 2]
            n = min(4, ntile - t4)
            for j in range(n):
                s.op("pe", lambda E, t4=t4, j=j, p=p: E.transpose(out=p[:, j * NEXP:(j + 1) * NEXP], in_=cum[:, (t4 + j) * 128:(t4 + j + 1) * 128], identity=k.ident[0:NEXP, 0:NEXP]),
                     [cum_b, k.ident_b], [pb], track=(j == n - 1))
            s.op("act", lambda E, t4=t4, n=n, p=p: E.activation(out=posf[:, t4:t4 + n, :], in_=p[:, 0:n * NEXP].rearrange("p (t e) -> p t e", t=n), func=AF.Copy), [pb], [posf_b])
        s.op("dve", lambda E: E.tensor_copy(out=posi[:, 0:ntile, :], in_=posf[:, 0:ntile, :]), [posf_b], [posi_b])
        s.op("pool", lambda E: E.tensor_copy(out=metas[:, 0:ntile, :, 0], in_=k.tokid[:, 0:ntile].unsqueeze(2).broadcast_to([128, ntile, NEXP])), [k.tokid_b], [metas_b])
        s.op("pool", lambda E: E.tensor_copy(out=metas[:, 0:ntile, :, 1], in_=aff[:, 0:ntile, :]), [aff_b, metas_b], [metas_b])
        regs = {}

        def mkregs(E):
            regs["l"] = E.alloc_register("bnd_l%d" % l)
            E.reg_mov(regs["l"], NEXP * SLOTS - 1)
        s.raw("pool", mkregs)
        for t in range(ntile):
            rk = "l"
            for e in range(NEXP):
                s.dma("pool", lambda E, t=t, e=e, rk=rk: E.indirect_dma_start(
                    out=k.meta[:, :], out_offset=bass.IndirectOffsetOnAxis(ap=posi[:, t, e:e + 1], axis=0),
                    in_=metas[:, t, e, :], in_offset=None, bounds_check=Lazy(lambda: regs["l"]), oob_is_err=False),
                    [posi_b, metas_b], [k.meta_b[e]])

        def freeregs(E):
            E.free_register(regs["l"])
        s.raw("pool", freeregs)
        s.barrier()


def experts(k, l, keep_ctx, G2b, G2b_b):
    nc, s, I = k.nc, k.s, k.I
    nst = 9 if keep_ctx else 8
    nsl = nst * 128
    groups = [(0, 512), (512, 512)] + ([(1024, 128)] if keep_ctx else [])
    with ExitStack() as st:
        mt = [k.sb("e_mt%d" % i, [128, 9, 2], stack=st) for i in range(2)]
        mt_b = bufs(2)
        idx = [k.sb("e_idx%d" % i, [128, 9], I32, stack=st) for i in range(2)]
        idx_b = bufs(2)
        xs = [k.sb("e_xs%d" % i, [128, D], BF16, stack=st) for i in range(3)]
        xs_b = bufs(3)
        xsT = k.sb("e_xsT", [128, 8, SLOTS], BF16, stack=st)
        xsT_b = Buf()
        aT = k.sb("e_aT", [128, 16, SLOTS], BF16, stack=st)
        aT_b = Buf()
        wstg = [k.sb("e_ws%d" % i, [128, 8, 512], stack=st) for i in range(2)]
        wstg_b = bufs(2)
        wbf = [k.sb("e_wb%d" % i, [128, 8, 512], BF16, stack=st) for i in range(4)]
        wbf_b = bufs(4)
        sg = [k.sb("e_sg%d" % i, [128, 512], stack=st) for i in range(2)]
        sg_b = bufs(2)
        yo = [k.sb("e_yo%d" % i, [128, D], stack=st) for i in range(2)]
        yo_b = bufs(2)
        tpb = [k.ps("e_tp%d" % i, [128, 512], BF16, stack=st) for i in range(2)]
        tpb_b = bufs(2)
        pg = [k.ps("e_pg%d" % i, [128, 512], stack=st) for i in range(2)]
        pg_b = bufs(2)
        pu = [k.ps("e_pu%d" % i, [128, 512], stack=st) for i in range(2)]
        pu_b = bufs(2)
        pyy = [k.ps("e_py%d" % i, [128, 512], stack=st) for i in range(2)]
        pyy_b = bufs(2)
        regs = {}

        def mkregs(E):
            regs["b"] = E.alloc_register("bnd_x%d" % l)
            E.reg_mov(regs["b"], NT)
        s.raw("pool", mkregs)
        wi = [0]
        ci = [0]

        def load_w(src_ap, src_b, kind):
            i = wi[0]
            wi[0] += 1
            stg, stg_b = wstg[i % 2], wstg_b[i % 2]
            wb, wb_b = wbf[i % 4], wbf_b[i % 4]
            if kind == "col":
                s.dma("sp", lambda E: E.dma_start(out=stg[:], in_=src_ap.rearrange("(c p) n -> p c n", p=128)), [src_b], [stg_b])
            else:
                s.dma("sp", lambda E: E.dma_start(out=stg[:].rearrange("p a b -> p (a b)").rearrange("p (c n) -> p c n", c=4), in_=src_ap.rearrange("(c p) n -> p c n", p=128)), [src_b], [stg_b])
            eng = ["pool", "dve", "act"][ci[0] % 3]
            ci[0] += 1
            if eng == "act":
                s.op("act", lambda E: E.activation(out=wb[:], in_=stg[:], func=AF.Copy), [stg_b], [wb_b])
            else:
                s.op(eng, lambda E: E.tensor_copy(out=wb[:], in_=stg[:]), [stg_b], [wb_b])
            return wb, wb_b

        xi = 0
        gi = 0
        yi = 0
        for e in range(NEXP):
            m_, m_b = mt[e % 2], mt_b[e % 2]
            ix, ix_b = idx[e % 2], idx_b[e % 2]
            s.dma("sp", lambda E, e=e, m_=m_: E.dma_start(out=m_[:, 0:nst, :], in_=k.meta[e * SLOTS:e * SLOTS + nsl, :].rearrange("(t p) c -> p t c", p=128)), [k.meta_b[e]], [m_b])
            s.op("dve", lambda E, m_=m_, ix=ix: E.tensor_copy(out=ix[:, 0:nst], in_=m_[:, 0:nst, 0]), [m_b], [ix_b])
            for stl in range(nst):
                x_, x_b = xs[xi % 3], xs_b[xi % 3]
                p, pb = tpb[xi % 2], tpb_b[xi % 2]
                xi += 1
                s.dma("pool", lambda E, stl=stl, x_=x_, ix=ix: E.indirect_dma_start(
                    out=x_[:, :], out_offset=None, in_=k.h2tab[:, :], in_offset=bass.IndirectOffsetOnAxis(ap=ix[:, stl:stl + 1], axis=0),
                    bounds_check=Lazy(lambda: regs["b"]), oob_is_err=False), list(k.h2tab_b) + [ix_b], [x_b])
                for half in range(2):
                    for j in range(4):
                        c = half * 4 + j
                        s.op("pe", lambda E, c=c, j=j, p=p, x_=x_: E.transpose(out=p[:, j * 128:(j + 1) * 128], in_=x_[:, c * 128:(c + 1) * 128], identity=k.identb[:]),
                             [x_b, k.identb_b], [pb], track=(j == 3))
                    if half == 0:
                        s.op("act", lambda E, p=p, stl=stl: E.activation(out=xsT[:, 0:4, stl * 128:(stl + 1) * 128], in_=p[:].rearrange("p (c t) -> p c t", c=4), func=AF.Copy), [pb], [xsT_b])
                    else:
                        s.op("dve", lambda E, p=p, stl=stl: E.tensor_copy(out=xsT[:, 4:8, stl * 128:(stl + 1) * 128], in_=p[:].rearrange("p (c t) -> p c t", c=4)), [pb], [xsT_b])
            for fb in range(4):
                wg, wg_b = load_w(I["moe_wg%d" % l][e * D:(e + 1) * D, fb * 512:(fb + 1) * 512], k.IB["moe_wg%d" % l], "col")
                wu, wu_b = load_w(I["moe_wu%d" % l][e * D:(e + 1) * D, fb * 512:(fb + 1) * 512], k.IB["moe_wu%d" % l], "col")
                for f4 in range(4):
                    f = fb * 4 + f4
                    for (s0, sw) in groups:
                        g_, g_b = pg[gi % 2], pg_b[gi % 2]
                        u_, u_b = pu[gi % 2], pu_b[gi % 2]
                        sg_, sg_bb = sg[gi % 2], sg_b[gi % 2]
                        gi += 1
                        for c in range(8):
                            s.op("pe", lambda E, c=c, f4=f4, g_=g_, wg=wg, s0=s0, sw=sw: E.matmul(g_[:, 0:sw], lhsT=wg[:, c, f4 * 128:(f4 + 1) * 128], rhs=xsT[:, c, s0:s0 + sw], start=(c == 0), stop=(c == 7)),
                                 [wg_b, xsT_b], [g_b], track=(c == 7))
                        for c in range(8):
                            s.op("pe", lambda E, c=c, f4=f4, u_=u_, wu=wu, s0=s0, sw=sw: E.matmul(u_[:, 0:sw], lhsT=wu[:, c, f4 * 128:(f4 + 1) * 128], rhs=xsT[:, c, s0:s0 + sw], start=(c == 0), stop=(c == 7)),
                                 [wu_b, xsT_b], [u_b], track=(c == 7))
                        s.op("act", lambda E, g_=g_, sg_=sg_, sw=sw: E.activation(out=sg_[:, 0:sw], in_=g_[:, 0:sw], func=AF.Silu), [g_b], [sg_bb])
                        s.op("dve", lambda E, u_=u_, sg_=sg_, f=f, s0=s0, sw=sw: E.tensor_tensor(out=aT[:, f, s0:s0 + sw], in0=u_[:, 0:sw], in1=sg_[:, 0:sw], op=ALU.mult), [u_b, sg_bb], [aT_b])
            wds = []
            for rb in range(4):
                wds.append(load_w(I["moe_wd%d" % l][e * EDIM + rb * 512:e * EDIM + (rb + 1) * 512, :], k.IB["moe_wd%d" % l], "row"))
            for stl in range(nst):
                cond = 0 if stl < 8 else 1
                y_, y_b = yo[yi % 2], yo_b[yi % 2]
                yi += 1
                for nb in range(2):
                    p, p_b = pyy[nb], pyy_b[nb]
                    for f in range(16):
                        wd, wd_b = wds[f // 4]
                        wdv = wd[:].rearrange("p a b -> p (a b)").rearrange("p (c n) -> p c n", c=4)
                        s.op("pe", lambda E, f=f, nb=nb, p=p, wdv=wdv, stl=stl: E.matmul(p[:], lhsT=aT[:, f, stl * 128:(stl + 1) * 128], rhs=wdv[:, f % 4, nb * 512:(nb + 1) * 512], start=(f == 0), stop=(f == 15)),
                             [aT_b, wd_b], [p_b], track=(f == 15))
                    s.op("dve", lambda E, nb=nb, p=p, y_=y_, m_=m_, stl=stl, cond=cond: E.scalar_tensor_tensor(out=y_[:, nb * 512:(nb + 1) * 512], in0=p[:], scalar=m_[:, stl, 1:2], in1=G2b[cond][:, nb * 512:(nb + 1) * 512], op0=ALU.mult, op1=ALU.mult),
                         [p_b, m_b, G2b_b[cond]], [y_b])
                s.dma("pool", lambda E, y_=y_, ix=ix, stl=stl: E.indirect_dma_start(
                    out=k.xres[:, :], out_offset=bass.IndirectOffsetOnAxis(ap=ix[:, stl:stl + 1], axis=0), in_=y_[:, :], in_offset=None,
                    bounds_check=Lazy(lambda: regs["b"]), oob_is_err=True, compute_op=ALU.add), [y_b, ix_b], list(k.xres_b))

        def freeregs(E):
            E.free_register(regs["b"])
        s.raw("pool", freeregs)
        s.barrier()


def final_norm(k):
    nc, s, I = k.nc, k.s, k.I
    with ExitStack() as st:
        nf = k.sb("f_nf", [128, D], stack=st)
        nf_b = Buf()
        s.dma("sp", lambda E: E.dma_start(out=nf[:], in_=I["norm_final"].unsqueeze(0).broadcast_to([128, D])), [], [nf_b])
        xt = [k.sb("f_xt%d" % i, [128, D], stack=st) for i in range(2)]
        xt_b = bufs(2)
        ot = [k.sb("f_ot%d" % i, [128, D], stack=st) for i in range(2)]
        ot_b = bufs(2)
        junk = k.sb("f_junk", [128, D], stack=st)
        stat = [k.sb("f_stat%d" % i, [128, 2], stack=st) for i in range(2)]
        scr_b = bufs(2)
        k.out_b = Buf()
        for t in range(64):
            x_, x_b = xt[t % 2], xt_b[t % 2]
            o_, o_b = ot[t % 2], ot_b[t % 2]
            sc = stat[t % 2]
            sb_ = scr_b[t % 2]
            s.dma("sp", lambda E, t=t, x_=x_: E.dma_start(out=x_[:], in_=k.xres[t * 128:(t + 1) * 128, :]), [k.xres_b[t]], [x_b])
            s.op("act", lambda E, x_=x_, sc=sc: E.activation(out=junk[:], in_=x_[:], func=AF.Square, accum_out=sc[:, 0:1]), [x_b], [sb_])
            s.op("act", lambda E, sc=sc: E.activation(out=sc[:, 1:2], in_=sc[:, 0:1], func=AF.Sqrt, scale=float(1.0 / D), bias=k.epsc[:, 0:1]), [sb_, k.epsc_b], [sb_])
            s.op("dve", lambda E, sc=sc: E.reciprocal(out=sc[:, 1:2], in_=sc[:, 1:2]), [sb_], [sb_])
            s.op("dve", lambda E, x_=x_, o_=o_, sc=sc: E.scalar_tensor_tensor(out=o_[:], in0=x_[:], scalar=sc[:, 1:2], in1=nf[:], op0=ALU.mult, op1=ALU.mult), [x_b, sb_, nf_b], [o_b])
            s.dma("pool", lambda E, t=t, o_=o_: E.dma_start(out=k.out[t * 128:(t + 1) * 128, :], in_=o_[:]), [o_b], [k.out_b])


def _rope_tables_T(rot_dim, reps_rows):
    t = np.arange(NL)
    rows = (t // GRID_W).astype(np.float32)
    cols = (t % GRID_W).astype(np.float32)
    n_freq = rot_dim // 4
    inv_freq = (np.float32(10000.0) ** (-np.arange(n_freq, dtype=np.float32) / np.float32(n_freq))).astype(np.float32)
    ang = np.concatenate([rows[:, None] * inv_freq, cols[:, None] * inv_freq], axis=-1).astype(np.float32)
    cos = np.cos(ang).astype(np.float32)
    sin = np.sin(ang).astype(np.float32)
    half = rot_dim // 2
    cT = np.ones((rot_dim, NT), np.float32)
    sT = np.zeros((rot_dim, NT), np.float32)
    cT[:half, :NL] = cos.T
    cT[half:, :NL] = cos.T
    sT[:half, :NL] = -sin.T
    sT[half:, :NL] = sin.T
    return cT, sT


def _swap_halves_cols(w, unit):
    din, dout = w.shape
    w4 = w.reshape(din, dout // unit, 2, unit // 2)
    return np.ascontiguousarray(w4[:, :, ::-1, :]).reshape(din, dout)


def _na_bias(rpb):
    NEG = np.float32(-30000.0)
    qc = np.arange(64)
    cs = np.clip(qc - 8, 0, 48)
    kc = np.arange(64)
    valid = (kc[:, None] >= cs[None, :]) & (kc[:, None] < cs[None, :] + 16)
    colidx = np.clip(kc[:, None] - qc[None, :] + 15, 0, 30)
    out = np.full((16, 128, 8, 4, 64), NEG, np.float32)
    for v in range(8):
        for kt in range(4):
            for half in range(2):
                kr = 2 * kt + half
                ridx = kr - v + 7
                vals = rpb[:, ridx][:, colidx]
                out[:, half * 64:(half + 1) * 64, v, kt, :] = np.where(valid[None], vals, NEG)
    return out


def _shard(a2d, r):
    n = a2d.shape[0] // 8
    return a2d[r * n:(r + 1) * n]


def make_shared(inp):
    f = lambda a: np.ascontiguousarray(np.asarray(a, dtype=np.float32))
    S = {}
    S["ada_b"] = f(inp["ada_b"])
    S["norm_mix"] = f(inp["norm_mix"])
    S["norm_ffn"] = f(inp["norm_ffn"])
    S["norm_final"] = f(inp["norm_final"])
    S["ident"] = np.eye(128, dtype=np.float32)
    S["tokid"] = f((np.arange(NTILE)[None, :] * 128 + np.arange(128)[:, None]))
    mi = np.zeros((NEXP * SLOTS, 2), np.float32)
    mi[:, 0] = DUMMY
    S["metainit"] = mi
    BIG = float(2 ** 20)
    eb = np.zeros((NEXP, 2), np.float32)
    eb[:, 0] = np.arange(NEXP) * SLOTS - 1 - BIG
    eb[:, 1] = np.arange(NEXP) * SLOTS + CAP_L - 1 - BIG
    S["ebase"] = eb
    S["moe_wr"] = f(inp["moe_w_router"])
    S["da_lam"] = f(np.stack([inp["da_lambda_q1"][0], inp["da_lambda_k1"][0], inp["da_lambda_q2"][0], inp["da_lambda_k2"][0]]))
    S["da_subln"] = f(np.asarray(inp["da_subln"][0]).reshape(128, 1))
    G = {}
    for l in range(4):
        G["ada_w%d" % l] = f(inp["ada_w"][l])
        G["moe_wg%d" % l] = f(inp["moe_w_gate"][l]).reshape(NEXP * D, EDIM)
        G["moe_wu%d" % l] = f(inp["moe_w_up"][l]).reshape(NEXP * D, EDIM)
        G["moe_wd%d" % l] = f(inp["moe_w_down"][l]).reshape(NEXP * EDIM, D)
    G["da_w"] = f(inp["da_w_qkv"][0])
    G["da_wo"] = f(inp["da_w_o"][0])
    G["fn_wo"] = f(inp["fn_w_o"][0])
    n2 = np.arange(128, dtype=np.float64)[:, None, None]
    k1 = np.arange(64, dtype=np.float64)[None, :, None]
    k2 = np.arange(128, dtype=np.float64)[None, None, :]
    th = 2.0 * np.pi * n2 * (k1 + 64.0 * k2) / 8192.0
    G["fn_cos"] = f(np.cos(th).reshape(128, 8192))
    G["fn_sin"] = f(np.sin(th).reshape(128, 8192))
    n1 = np.arange(64, dtype=np.float64)[:, None]
    kk = np.arange(64, dtype=np.float64)[None, :]
    c64 = np.cos(2.0 * np.pi * n1 * kk / 64.0)
    s64 = np.sin(2.0 * np.pi * n1 * kk / 64.0)
    f64t = np.zeros((64, 4, 48))
    for kb in range(4):
        f64t[:, kb, 0:16] = c64[:, kb * 16:(kb + 1) * 16]
        f64t[:, kb, 16:32] = -s64[:, kb * 16:(kb + 1) * 16]
        f64t[:, kb, 32:48] = -c64[:, kb * 16:(kb + 1) * 16]
    S["fn_f64"] = f(f64t.reshape(64, 192))
    cc_ = np.arange(256, dtype=np.float64)
    th2 = 2.0 * np.pi * cc_[:, None] * cc_[None, :] / 256.0
    S["fn_cc"] = f(np.cos(th2))
    S["fn_sc"] = f(np.sin(th2))
    S["fn_nsc"] = f(-np.sin(th2))
    G["na_w"] = f(inp["na_w_qkv"][0])
    G["na_wo"] = f(inp["na_w_o"][0])
    G["na_bias"] = _na_bias(np.asarray(inp["na_rpb"][0], np.float32)).reshape(NEXP * 128, 2048)
    S["mla_qnorm"] = f(inp["mla_q_norm"][0])
    S["mla_kvnorm"] = f(inp["mla_kv_norm"][0])
    G["mla_wdq"] = f(inp["mla_w_dq"][0])
    G["mla_wuq"] = f(inp["mla_w_uq"][0])
    G["mla_wdkv"] = f(inp["mla_w_dkv"][0])
    G["mla_wuk"] = f(inp["mla_w_uk"][0])
    G["mla_wuv"] = f(inp["mla_w_uv"][0])
    G["mla_wo"] = f(inp["mla_w_o"][0])
    c3, s3 = _rope_tables_T(32, None)
    G["rope3c"] = f(np.concatenate([np.ones((64, NT), np.float32), c3], axis=0))
    G["rope3s"] = f(np.concatenate([np.zeros((64, NT), np.float32), s3], axis=0))
    c0, s0 = _rope_tables_T(64, None)
    G["rope0c"] = f(np.concatenate([c0, c0], axis=0))
    G["rope0s"] = f(np.concatenate([s0, s0], axis=0))
    return S, G


def make_core_inputs(inp, S, G, core):
    b = core // 2
    m = dict(S)
    m["xin"] = np.ascontiguousarray(np.concatenate([np.asarray(inp["x"][b], np.float32), np.asarray(inp["ctx"][b], np.float32)], axis=0))
    cc = np.stack([np.asarray(inp["c"][b], np.float32), np.asarray(inp["c_ctx"], np.float32)], axis=-1)
    m["ccT"] = np.ascontiguousarray(cc.reshape(8, 128, 2).transpose(1, 0, 2))
    for name, a in G.items():
        m[name] = _shard(a, core)
    return m


_NC_CACHE = {}


def kernel(**inputs):
    if "nc" not in _NC_CACHE:
        _NC_CACHE["nc"] = build(n_layers=4, dbg=False, gather=False)
    nc = _NC_CACHE["nc"]
    S, G = make_shared(inputs)
    names = set(LAST_INPUT_NAMES)
    in_maps = []
    for b in range(4):
        m = make_core_inputs(inputs, S, G, 2 * b)
        m.update(G)
        in_maps.append({n: v for n, v in m.items() if n in names})
    res = run_bass_kernel_spmd(nc, in_maps, core_ids=list(range(4)))
    out = np.stack([np.asarray(res.results[b]["out"], dtype=np.float32) for b in range(4)], axis=0)
    return out
```

```python
import math
from contextlib import ExitStack
import numpy as np
import concourse.bass as bass
import concourse.mybir as mybir
from concourse.bass_utils import run_bass_kernel_spmd

F32 = mybir.dt.float32
BF16 = mybir.dt.bfloat16
I32 = mybir.dt.int32
AF = mybir.ActivationFunctionType
ALU = mybir.AluOpType
AX = mybir.AxisListType

D = 1024
NL = 8192
NC_ = 256
NT = NL + NC_
NTILE = NT // 128
DUMMY = NT
EPS = 1e-6
NEXP = 16
EDIM = 2048
CAP_L = 1024
CAP_C = 32
SLOTS = 1152
GRID_W = 64

ENGS = ["pe", "act", "dve", "pool", "sp"]


class Buf:
    __slots__ = ("w", "r")

    def __init__(self):
        self.w = None
        self.r = {}


def bufs(n):
    return [Buf() for _ in range(n)]


class Lazy:
    def __init__(self, f):
        self.f = f


class _Rec:
    def __init__(self):
        self.calls = []

    def __getattr__(self, name):
        def f(*args, **kw):
            self.calls.append((name, args, kw))
            return self
        return f


def _replay(E, call):
    name, args, kw = call
    args = [a.f() if isinstance(a, Lazy) else a for a in args]
    kw = {k_: (v.f() if isinstance(v, Lazy) else v) for k_, v in kw.items()}
    return getattr(E, name)(*args, **kw)


def _record(fn):
    r = _Rec()
    fn(r)
    assert len(r.calls) == 1, r.calls
    return r.calls[0]


class Sched:
    def __init__(self, nc, es, n_dma=None):
        self.nc = nc
        self.es = es
        self.ckeys = []
        self.prog = {e: [] for e in ENGS}
        self.cnt = {e: 0 for e in ENGS}
        self.waited = {e: {} for e in ENGS}
        self.sem = {}
        for e in ["pe", "act", "dve", "pool"]:
            self.sem[e] = es.enter_context(nc.semaphore("s_" + e))
        n_dma = n_dma or {"sp": 16, "pool": 16, "act": 4}
        self.dkeys = {}
        self.dnext = {}
        self.dval = {}
        for q, n in n_dma.items():
            ks = []
            for i in range(n):
                k = "d_%s_%d" % (q, i)
                self.sem[k] = es.enter_context(nc.semaphore(k))
                self.dval[k] = 0
                ks.append(k)
            self.dkeys[q] = ks
            self.dnext[q] = 0

    def _deps(self, reads, writes):
        deps = {}

        def add(k, v):
            if deps.get(k, 0) < v:
                deps[k] = v
        for b in reads:
            if b.w is not None:
                add(*b.w)
        for b in writes:
            if b.w is not None:
                add(*b.w)
            for k, v in b.r.items():
                add(k, v)
        return deps

    def _waits(self, eng, deps):
        for k, v in deps.items():
            if eng == "pe" and k == "pe":
                continue
            if self.waited[eng].get(k, 0) >= v:
                continue
            self.waited[eng][k] = v
            sem = self.sem[k]
            self.prog[eng].append(lambda E, sem=sem, v=v: E.wait_ge(sem, v))

    def _mark(self, ev, reads, writes):
        k, v = ev
        for b in reads:
            if b.r.get(k, 0) < v:
                b.r[k] = v
        for b in writes:
            b.w = ev
            b.r = {}

    def op(self, eng, fn, reads=(), writes=(), track=True):
        self._waits(eng, self._deps(reads, writes))
        if track:
            self.cnt[eng] += 1
            v = self.cnt[eng]
            sem = self.sem[eng]
            call = _record(fn)
            self.prog[eng].append(lambda E, call=call, sem=sem: _replay(E, call).then_inc(sem, 1))
        else:
            v = self.cnt[eng] + 1
            call = _record(fn)
            self.prog[eng].append(lambda E, call=call: _replay(E, call))
        self._mark((eng, v), reads, writes)

    def dma(self, q, fn, reads=(), writes=()):
        ks = self.dkeys[q]
        key = ks[self.dnext[q] % len(ks)]
        self.dnext[q] += 1
        deps = self._deps(reads, writes)
        if self.dval[key] > 0 and deps.get(key, 0) < self.dval[key]:
            deps[key] = self.dval[key]
        self._waits(q, deps)
        self.dval[key] += 16
        v = self.dval[key]
        sem = self.sem[key]
        call = _record(fn)
        self.prog[q].append(lambda E, call=call, sem=sem: _replay(E, call).then_inc(sem, 16))
        self._mark((key, v), reads, writes)

    def coll(self, fn, reads=(), writes=()):
        key = "cc_%d" % len(self.ckeys)
        self.ckeys.append(key)
        self.sem[key] = self.es.enter_context(self.nc.semaphore(key))
        self._waits("pool", self._deps(reads, writes))
        sem = self.sem[key]
        call = _record(fn)
        self.prog["pool"].append(lambda E, call=call, sem=sem: _replay(E, call).then_inc(sem, 1))
        self.dval[key] = 1
        self._mark((key, 1), reads, writes)

    def raw(self, eng, fn):
        self.prog[eng].append(lambda E, fn=fn: fn(E))

    def barrier(self):
        allev = {}
        for e in ["pe", "act", "dve", "pool"]:
            if self.cnt[e] > 0:
                allev[e] = self.cnt[e]
        for k, v in self.dval.items():
            if v > 0:
                allev[k] = v
        for e in ENGS:
            d = dict(allev)
            self._waits_all(e, d)

    def _waits_all(self, eng, deps):
        for k, v in deps.items():
            if self.waited[eng].get(k, 0) >= v:
                continue
            self.waited[eng][k] = v
            sem = self.sem[k]
            self.prog[eng].append(lambda E, sem=sem, v=v: E.wait_ge(sem, v))

    def emit(self):
        nc = self.nc
        with nc.Block() as block:
            @block.tensor
            def _(E):
                for f in self.prog["pe"]:
                    f(E)

            @block.scalar
            def _(E):
                for f in self.prog["act"]:
                    f(E)

            @block.vector
            def _(E):
                for f in self.prog["dve"]:
                    f(E)

            @block.gpsimd
            def _(E):
                for f in self.prog["pool"]:
                    f(E)

            @block.sync
            def _(E):
                for f in self.prog["sp"]:
                    f(E)


class K:
    pass


LAST_INPUT_NAMES = []


class _Stop(Exception):
    pass


def build(n_layers=4, dbg=False, stop=None, gather=True):
    nc = bass.Bass("TRN2", target_bir_lowering=False)
    del LAST_INPUT_NAMES[:]
    es = ExitStack()
    k = K()
    k.nc = nc
    k.dbg = dbg
    k.uid = 0
    k.stop = stop

    def chk(name):
        if stop == name:
            raise _Stop()
    k.chk = chk
    s = Sched(nc, es)
    k.s = s

    def din(name, shape, dt=F32):
        LAST_INPUT_NAMES.append(name)
        return nc.dram_tensor(name, list(shape), dt, kind="ExternalInput").ap()

    def dscr(name, shape, dt=F32):
        return nc.dram_tensor(name, list(shape), dt).ap()

    k.din = din
    k.dscr = dscr
    I = {}
    IB = {}

    def gin(name, rows, cols):
        assert rows % 8 == 0
        if not gather:
            I[name] = din(name, [rows, cols])
            IB[name] = Buf()
            return
        ext = din(name, [rows // 8, cols])
        loc = dscr(name + "_l", [rows // 8, cols])
        full = dscr(name + "_g", [rows, cols])
        lb, fb = Buf(), Buf()
        s.dma("pool", lambda E: E.dma_start(out=loc[:, :], in_=ext[:, :]), [], [lb])
        s.coll(lambda E: E.collective_compute("AllGather", ALU.bypass, replica_groups=[list(range(8))], ins=[loc[:, :]], outs=[full[:, :]]), [lb], [fb])
        I[name] = full
        IB[name] = fb

    k.gin = gin
    I["xin"] = din("xin", [NT, D])
    I["ccT"] = din("ccT", [128, 8, 2])
    I["ada_b"] = din("ada_b", [4, 6 * D])
    I["norm_mix"] = din("norm_mix", [4, D])
    I["norm_ffn"] = din("norm_ffn", [4, D])
    I["norm_final"] = din("norm_final", [D])
    I["ident"] = din("ident", [128, 128])
    I["tokid"] = din("tokid", [128, NTILE])
    I["metainit"] = din("metainit", [NEXP * SLOTS, 2])
    I["ebase"] = din("ebase", [NEXP, 2])
    I["moe_wr"] = din("moe_wr", [4, D, NEXP])
    I["da_lam"] = din("da_lam", [4, 64])
    I["da_subln"] = din("da_subln", [128, 1])
    for l in range(4):
        gin("ada_w%d" % l, D, 6 * D)
    gin("da_w", D, 3 * D)
    gin("da_wo", D, D)
    gin("rope0c", 128, NT)
    gin("rope0s", 128, NT)
    I["mla_qnorm"] = din("mla_qnorm", [768])
    I["mla_kvnorm"] = din("mla_kvnorm", [256])
    if n_layers >= 2:
        gin("fn_wo", D, D)
        gin("fn_cos", 128, NL)
        gin("fn_sin", 128, NL)
        I["fn_f64"] = din("fn_f64", [64, 192])
        I["fn_cc"] = din("fn_cc", [256, 256])
        I["fn_sc"] = din("fn_sc", [256, 256])
        I["fn_nsc"] = din("fn_nsc", [256, 256])
    if n_layers >= 3:
        gin("na_w", D, 3 * D)
        gin("na_wo", D, D)
        gin("na_bias", NEXP * 128, 2048)
    if n_layers >= 4:
        gin("mla_wdq", D, 768)
        gin("mla_wuq", 768, 1536)
        gin("mla_wdkv", D, 288)
        gin("mla_wuk", 256, D)
        gin("mla_wuv", 256, D)
        gin("mla_wo", D, D)
        gin("rope3c", 96, NT)
        gin("rope3s", 96, NT)
    for l in range(n_layers):
        gin("moe_wg%d" % l, NEXP * D, EDIM)
        gin("moe_wu%d" % l, NEXP * D, EDIM)
        gin("moe_wd%d" % l, NEXP * EDIM, D)
    k.IB = IB
    k.I = I
    out = nc.dram_tensor("out", [NL, D], F32, kind="ExternalOutput").ap()
    k.out = out
    if dbg:
        k.dbg_x = nc.dram_tensor("dbg_x", [NT, D], F32, kind="ExternalOutput").ap()
        k.dbg_t = {}
        for nm, shp in [("qT", [1536, NT]), ("kT", [D, NT]), ("vv", [NT, D]), ("aT", [D, NT])]:
            k.dbg_t[nm] = nc.dram_tensor("dbg_" + nm, shp, BF16, kind="ExternalOutput").ap()
    k.xres = dscr("xres", [NT + 1, D])
    k.xres_b = bufs(NTILE + 1)
    k.modv = dscr("modv", [4, 6, 2, D])
    k.modv_b = Buf()
    k.h2tab = dscr("h2tab", [NT + 1, D], BF16)
    k.h2tab_b = bufs(NTILE + 1)
    k.meta = dscr("meta", [NEXP * SLOTS, 2])
    k.meta_b = bufs(NEXP)
    k.qT = dscr("qT", [1536, NT], BF16)
    k.krT = dscr("krT", [32, NT], BF16)
    k.krT_b = bufs(17)
    k.kT = dscr("kT", [D, NT], BF16)
    k.vv = dscr("vv", [NT, D], BF16)
    k.aT = dscr("aT", [D, NT], BF16)
    NG = 17
    k.NG = NG
    k.qT_b = bufs(NG)
    k.kT_b = bufs(NG)
    k.vv_b = bufs(NG)
    k.aT_b = bufs(NG)

    def sb(name, shape, dt=F32, stack=es):
        k.uid += 1
        return stack.enter_context(nc.sbuf_tensor("sb%d_%s" % (k.uid, name), list(shape), dt))

    def ps(name, shape, dt=F32, stack=es):
        k.uid += 1
        return stack.enter_context(nc.psum_tensor("ps%d_%s" % (k.uid, name), list(shape), dt))

    k.sb = sb
    k.ps = ps
    k.ident = sb("ident", [128, 128])
    k.ident_b = Buf()
    k.identb = sb("identb", [128, 128], BF16)
    k.identb_b = Buf()
    k.ones_f = sb("ones_f", [128, 128])
    k.ones_b = sb("ones_b", [128, 128], BF16)
    k.ones_bb = Buf()
    k.tokid = sb("tokid", [128, NTILE])
    k.tokid_b = Buf()
    s.dma("sp", lambda E: E.dma_start(out=k.ident[:], in_=I["ident"][:, :]), [], [k.ident_b])
    s.dma("sp", lambda E: E.dma_start(out=k.tokid[:], in_=I["tokid"][:, :]), [], [k.tokid_b])
    s.op("dve", lambda E: E.tensor_copy(out=k.identb[:], in_=k.ident[:]), [k.ident_b], [k.identb_b])
    s.op("dve", lambda E: E.memset(k.ones_f[:], 1.0), [], [k.ones_bb])
    s.op("dve", lambda E: E.memset(k.ones_b[:], 1.0), [], [k.ones_bb])
    k.epsc = sb("epsc", [128, 1])
    k.epsc_b = Buf()
    s.op("dve", lambda E: E.memset(k.epsc[:], float(EPS)), [], [k.epsc_b])

    for t in range(NTILE):
        s.dma("sp", lambda E, t=t: E.dma_start(out=k.xres[t * 128:(t + 1) * 128, :], in_=I["xin"][t * 128:(t + 1) * 128, :]),
              [], [k.xres_b[t]])

    try:
        chk("init")
        phase_mod(k)
        if stop is not None and stop.startswith("mod"):
            raise _Stop()
        for l in range(n_layers):
            kind = l % 4
            if kind == 0:
                mixer_diff(k, l)
            elif kind == 1:
                mixer_fourier(k, l, keep_ctx=(l < 3))
            elif kind == 2:
                mixer_na(k, l, keep_ctx=(l < 3))
            elif kind == 3:
                mixer_mla(k, l, keep_ctx=(l < 3))
            chk("mixer%d" % l)
            post_mixer_and_moe(k, l, keep_ctx=(l < 3))
            chk("layer%d" % l)
    except _Stop:
        pass
    if dbg:
        s.barrier()
        for nm, src in [("qT", k.qT), ("kT", k.kT), ("vv", k.vv), ("aT", k.aT)]:
            rows = src.shape[0]
            for r0 in range(0, rows, 128):
                r1 = min(rows, r0 + 128)
                s.dma("sp", lambda E, nm=nm, src=src, r0=r0, r1=r1: E.dma_start(out=k.dbg_t[nm][r0:r1, :], in_=src[r0:r1, :]), [], [])
        for t in range(NTILE):
            s.dma("sp", lambda E, t=t: E.dma_start(out=k.dbg_x[t * 128:(t + 1) * 128, :], in_=k.xres[t * 128:(t + 1) * 128, :]),
                  [k.xres_b[t]], [])
    final_norm(k)
    s.barrier()
    s.emit()
    es.close()
    return nc


def phase_mod(k):
    nc, s, I = k.nc, k.s, k.I
    with ExitStack() as st:
        cc = k.sb("m_cc", [128, 8, 2], stack=st)
        sil = k.sb("m_sil", [128, 8, 2], stack=st)
        cc_b, sil_b = Buf(), Buf()
        wt = [k.sb("m_w%d" % i, [128, 8, 512], stack=st) for i in range(2)]
        wt_b = bufs(2)
        mrow = k.sb("m_row", [2, 6 * D], stack=st)
        mrow_b = Buf()
        adab = k.sb("m_adab", [2, 6 * D], stack=st)
        adab_b = Buf()
        nw = k.sb("m_nw", [2, 2, D], stack=st)
        nw_b = Buf()
        aa = k.sb("m_aa", [2, 2, D], stack=st)
        aa_b = Buf()
        pp = [k.ps("m_ps%d" % i, [2, 512], stack=st) for i in range(2)]
        pp_b = bufs(2)
        s.dma("sp", lambda E: E.dma_start(out=cc[:], in_=I["ccT"][:, :, :]), [], [cc_b])
        s.op("act", lambda E: E.activation(out=sil[:], in_=cc[:], func=AF.Silu), [cc_b], [sil_b])
        if k.stop == "mod_silu":
            s.barrier()
            return
        it = 0
        for l in range(4):
            s.dma("sp", lambda E, l=l: E.dma_start(out=adab[:], in_=I["ada_b"][l:l + 1, :].broadcast_to([2, 6 * D])), [], [adab_b])
            s.dma("sp", lambda E, l=l: E.dma_start(out=nw[:, 0, :], in_=I["norm_mix"][l:l + 1, :].broadcast_to([2, D])), [], [nw_b])
            s.dma("sp", lambda E, l=l: E.dma_start(out=nw[:, 1, :], in_=I["norm_ffn"][l:l + 1, :].broadcast_to([2, D])), [], [nw_b])
            for nb in range(12):
                w = wt[it % 2]
                wb = wt_b[it % 2]
                p = pp[it % 2]
                pb = pp_b[it % 2]
                it += 1
                s.dma("sp", lambda E, l=l, nb=nb, w=w: E.dma_start(
                    out=w[:], in_=I["ada_w%d" % l][:, nb * 512:(nb + 1) * 512].rearrange("(c p) n -> p c n", p=128)), [k.IB["ada_w%d" % l]], [wb])
                for c in range(8):
                    s.op("pe", lambda E, c=c, w=w, p=p: E.matmul(p[:], lhsT=sil[:, c, :], rhs=w[:, c, :], start=(c == 0), stop=(c == 7)),
                         [sil_b, wb], [pb], track=(c == 7))
                s.op("dve", lambda E, nb=nb, p=p: E.tensor_tensor(out=mrow[:, nb * 512:(nb + 1) * 512], in0=p[:], in1=adab[:, nb * 512:(nb + 1) * 512], op=ALU.add),
                     [pb, adab_b], [mrow_b])
                if k.stop == "mod_mm":
                    s.barrier()
                    return
            s.op("dve", lambda E: E.scalar_tensor_tensor(out=aa[:, 0, :], in0=mrow[:, D:2 * D], scalar=1.0, in1=nw[:, 0, :], op0=ALU.add, op1=ALU.mult),
                 [mrow_b, nw_b], [aa_b])
            s.op("dve", lambda E: E.scalar_tensor_tensor(out=aa[:, 1, :], in0=mrow[:, 4 * D:5 * D], scalar=1.0, in1=nw[:, 1, :], op0=ALU.add, op1=ALU.mult),
                 [mrow_b, nw_b], [aa_b])
            srcs = [aa[:, 0, :], mrow[:, 0:D], mrow[:, 2 * D:3 * D], aa[:, 1, :], mrow[:, 3 * D:4 * D], mrow[:, 5 * D:6 * D]]
            for v in range(6):
                s.dma("sp", lambda E, l=l, v=v, src=srcs[v]: E.dma_start(out=k.modv[l, v, :, :], in_=src), [mrow_b, aa_b], [k.modv_b])
            if k.stop == "mod_l0":
                s.barrier()
                return
        s.barrier()


def load_bcast(k, q, dst, dst_b, l, v, cond):
    k.s.dma(q, lambda E: E.dma_start(out=dst, in_=k.modv[l, v, cond:cond + 1, :].broadcast_to([128, D])), [k.modv_b], [dst_b])


def load_weight_bf16(k, st, name, src, src_b, C, ncols, blk=512, swap_cols=0, swap_unit=64):
    s = k.s
    dst = k.sb(name, [128, C, ncols + swap_cols], BF16, stack=st)
    dst_b = Buf()
    if getattr(st, "_wstg", None) is None:
        st._wstg = ([k.sb(name + "_st%d" % i, [128, 4096], stack=st) for i in range(2)], bufs(2), [0])
    stg_full, stg_b, stg_ctr = st._wstg
    stg = [t[:, 0:C * blk].rearrange("p (c n) -> p c n", c=C) for t in stg_full]
    nb = ncols // blk
    hu = swap_unit // 2
    for b in range(nb):
        t = stg[stg_ctr[0] % 2]
        tb = stg_b[stg_ctr[0] % 2]
        stg_ctr[0] += 1
        s.dma("sp", lambda E, b=b, t=t: E.dma_start(out=t, in_=src[:, b * blk:(b + 1) * blk].rearrange("(c p) n -> p c n", p=128)), [src_b], [tb])
        eng = ["pool", "dve"][b % 2]
        s.op(eng, lambda E, b=b, t=t: E.tensor_copy(out=dst[:, :, b * blk:(b + 1) * blk], in_=t), [tb], [dst_b])
        if (b + 1) * blk <= swap_cols:
            for c in range(C):
                for half in range(2):
                    eng2 = ["dve", "pool"][(c + half) % 2]
                    s.op(eng2, lambda E, b=b, t=t, c=c, half=half: E.tensor_copy(
                        out=dst[:, c, ncols + b * blk:ncols + (b + 1) * blk].rearrange("p (u two h) -> p u two h", two=2, h=hu)[:, :, half, :],
                        in_=t[:, c, :].rearrange("p (u two h) -> p u two h", two=2, h=hu)[:, :, 1 - half, :]), [tb], [dst_b])
    return dst, dst_b


def norm_tile(k, xt, xt_b, ht, ht_b, Ab, Ab_b, Bb, Bb_b, scr, scr_b):
    s = k.s
    junk, stat = scr
    s.op("act", lambda E: E.activation(out=junk[:], in_=xt[:], func=AF.Square, accum_out=stat[:, 0:1]), [xt_b], [scr_b])
    s.op("act", lambda E: E.activation(out=stat[:, 1:2], in_=stat[:, 0:1], func=AF.Ln, scale=float(1.0 / D), bias=k.epsc[:, 0:1]), [scr_b, k.epsc_b], [scr_b])
    s.op("act", lambda E: E.activation(out=stat[:, 1:2], in_=stat[:, 1:2], func=AF.Exp, scale=-0.5), [scr_b], [scr_b])
    s.op("dve", lambda E: E.scalar_tensor_tensor(out=ht[:], in0=xt[:], scalar=stat[:, 1:2], in1=Ab[:], op0=ALU.mult, op1=ALU.mult),
         [xt_b, scr_b, Ab_b], [ht_b])
    s.op("pool", lambda E: E.tensor_tensor(out=ht[:], in0=ht[:], in1=Bb[:], op=ALU.add), [ht_b, Bb_b], [ht_b])


def transpose_tile(k, ht, ht_b, tp, tp_b, hT, hT_b, col0, ncol=128, nchunk=8, evac=("act", "dve")):
    s = k.s
    for half in range((nchunk + 3) // 4):
        p = tp[half % len(tp)]
        pb = tp_b[half % len(tp)]
        cs = list(range(half * 4, min(nchunk, half * 4 + 4)))
        for j, c in enumerate(cs):
            s.op("pe", lambda E, c=c, j=j, p=p: E.transpose(out=p[:, j * 128:j * 128 + ncol], in_=ht[0:ncol, c * 128:(c + 1) * 128], identity=k.ident[0:ncol, 0:ncol]),
                 [ht_b, k.ident_b], [pb], track=(j == len(cs) - 1))
        eng = evac[half % len(evac)]
        n = len(cs)
        if eng == "act":
            s.op("act", lambda E, p=p, cs=cs, n=n: E.activation(out=hT[:, cs[0]:cs[0] + n, col0:col0 + ncol], in_=p[:, 0:n * 128].rearrange("p (c t) -> p c t", c=n)[:, :, 0:ncol], func=AF.Copy),
                 [pb], [hT_b])
        else:
            s.op("dve", lambda E, p=p, cs=cs, n=n: E.tensor_copy(out=hT[:, cs[0]:cs[0] + n, col0:col0 + ncol], in_=p[:, 0:n * 128].rearrange("p (c t) -> p c t", c=n)[:, :, 0:ncol]),
                 [pb], [hT_b])


def group_range(g):
    t0 = g * 512
    w = min(512, NT - t0)
    return t0, w


def mixer_diff(k, l):
    nc, s, I = k.nc, k.s, k.I
    lam_init = 0.8 - 0.6 * math.exp(-0.3 * l)
    with ExitStack() as st:
        W, W_b = load_weight_bf16(k, st, "d_w", I["da_w"], k.IB["da_w"], 8, 3 * D, swap_cols=2 * D, swap_unit=64)
        Ab = [k.sb("d_Ab%d" % c, [128, D], stack=st) for c in range(2)]
        Bb = [k.sb("d_Bb%d" % c, [128, D], stack=st) for c in range(2)]
        Ab_b, Bb_b = bufs(2), bufs(2)
        for c in range(2):
            load_bcast(k, "sp", Ab[c][:], Ab_b[c], l, 0, c)
            load_bcast(k, "sp", Bb[c][:], Bb_b[c], l, 1, c)
        xt = [k.sb("d_xt%d" % i, [128, D], stack=st) for i in range(2)]
        xt_b = bufs(2)
        ht = [k.sb("d_ht%d" % i, [128, D], stack=st) for i in range(2)]
        ht_b = bufs(2)
        junk = k.sb("d_junk", [128, D], stack=st)
        stat = [k.sb("d_stat%d" % i, [128, 2], stack=st) for i in range(2)]
        scr_b = bufs(2)
        hT = [k.sb("d_hT%d" % i, [128, 8, 512], BF16, stack=st) for i in range(2)]
        hT_b = bufs(2)
        rc = [k.sb("d_rc%d" % i, [128, 512], stack=st) for i in range(2)]
        rs = [k.sb("d_rs%d" % i, [128, 512], stack=st) for i in range(2)]
        rc_b, rs_b = bufs(2), bufs(2)
        t1 = [k.sb("d_t1%d" % i, [128, 512], stack=st) for i in range(2)]
        t2 = [k.sb("d_t2%d" % i, [128, 512], stack=st) for i in range(2)]
        t1_b, t2_b = bufs(2), bufs(2)
        qo = [k.sb("d_qo%d" % i, [128, 512], BF16, stack=st) for i in range(4)]
        qo_b = bufs(4)
        vo = [k.sb("d_vo%d" % i, [128, D], BF16, stack=st) for i in range(2)]
        vo_b = bufs(2)
        tp = [k.ps("d_tp%d" % i, [128, 512], stack=st) for i in range(2)]
        tp_b = bufs(2)
        pq = [k.ps("d_pq%d" % i, [128, 512], stack=st) for i in range(4)]
        pq_b = bufs(4)
        pv = [k.ps("d_pv%d" % i, [128, 512], stack=st) for i in range(2)]
        pv_b = bufs(2)
        ti = 0
        oi = 0
        vi = 0
        for g in range(k.NG):
            t0, w = group_range(g)
            hTg, hTg_b = hT[g % 2], hT_b[g % 2]
            s.dma("sp", lambda E, g=g, t0=t0, w=w: E.dma_start(out=rc[g % 2][:, 0:w], in_=I["rope0c"][:, t0:t0 + w]), [k.IB["rope0c"]], [rc_b[g % 2]])
            s.dma("sp", lambda E, g=g, t0=t0, w=w: E.dma_start(out=rs[g % 2][:, 0:w], in_=I["rope0s"][:, t0:t0 + w]), [k.IB["rope0s"]], [rs_b[g % 2]])
            for j in range(w // 128):
                t = (t0 // 128) + j
                cond = 0 if t < 64 else 1
                x_, x_b = xt[ti % 2], xt_b[ti % 2]
                h_, h_b = ht[ti % 2], ht_b[ti % 2]
                s.dma("sp", lambda E, t=t, x_=x_: E.dma_start(out=x_[:], in_=k.xres[t * 128:(t + 1) * 128, :]), [k.xres_b[t]], [x_b])
                norm_tile(k, x_, x_b, h_, h_b, Ab[cond], Ab_b[cond], Bb[cond], Bb_b[cond], (junk, stat[ti % 2]), scr_b[ti % 2])
                transpose_tile(k, h_, h_b, tp, tp_b, hTg, hTg_b, j * 128)
                ti += 1
            for o in range(16):
                pa, pa_b = pq[(2 * o) % 4], pq_b[(2 * o) % 4]
                pb_, pb_b = pq[(2 * o + 1) % 4], pq_b[(2 * o + 1) % 4]
                for c in range(8):
                    s.op("pe", lambda E, c=c, o=o, pa=pa: E.matmul(pa[:, 0:w], lhsT=W[:, c, o * 128:(o + 1) * 128], rhs=hTg[:, c, 0:w], start=(c == 0), stop=(c == 7)),
                         [W_b, hTg_b], [pa_b], track=(c == 7))
                for c in range(8):
                    s.op("pe", lambda E, c=c, o=o, pb_=pb_: E.matmul(pb_[:, 0:w], lhsT=W[:, c, 3 * D + o * 128:3 * D + (o + 1) * 128], rhs=hTg[:, c, 0:w], start=(c == 0), stop=(c == 7)),
                         [W_b, hTg_b], [pb_b], track=(c == 7))
                a1, a1_b = t1[o % 2], t1_b[o % 2]
                a2, a2_b = t2[o % 2], t2_b[o % 2]
                q_, q_b = qo[oi % 4], qo_b[oi % 4]
                oi += 1
                s.op("dve", lambda E, pa=pa, a1=a1: E.tensor_tensor(out=a1[:, 0:w], in0=pa[:, 0:w], in1=rc[g % 2][:, 0:w], op=ALU.mult), [pa_b, rc_b[g % 2]], [a1_b])
                s.op("dve", lambda E, pb_=pb_, a2=a2: E.tensor_tensor(out=a2[:, 0:w], in0=pb_[:, 0:w], in1=rs[g % 2][:, 0:w], op=ALU.mult), [pb_b, rs_b[g % 2]], [a2_b])
                s.op("pool", lambda E, a1=a1, a2=a2, q_=q_: E.tensor_tensor(out=q_[:, 0:w], in0=a1[:, 0:w], in1=a2[:, 0:w], op=ALU.add), [a1_b, a2_b], [q_b])
                dstT = k.qT if o < 8 else k.kT
                dst_b = (k.qT_b if o < 8 else k.kT_b)[g]
                oo = o % 8
                s.dma("pool", lambda E, dstT=dstT, oo=oo, q_=q_: E.dma_start(out=dstT[oo * 128:(oo + 1) * 128, t0:t0 + w], in_=q_[:, 0:w]), [q_b], [dst_b])
            for j in range(w // 128):
                v_, v_b = vo[vi % 2], vo_b[vi % 2]
                vi += 1
                for nb in range(2):
                    p, p_b = pv[nb], pv_b[nb]
                    for c in range(8):
                        s.op("pe", lambda E, c=c, nb=nb, p=p, j=j: E.matmul(p[:], lhsT=hTg[:, c, j * 128:(j + 1) * 128], rhs=W[:, c, 2 * D + nb * 512:2 * D + (nb + 1) * 512], start=(c == 0), stop=(c == 7)),
                             [W_b, hTg_b], [p_b], track=(c == 7))
                    s.op("act", lambda E, nb=nb, p=p, v_=v_: E.activation(out=v_[:, nb * 512:(nb + 1) * 512], in_=p[:], func=AF.Copy), [p_b], [v_b])
                t = (t0 // 128) + j
                s.dma("pool", lambda E, t=t, v_=v_: E.dma_start(out=k.vv[t * 128:(t + 1) * 128, :], in_=v_[:]), [v_b], [k.vv_b[g]])
        s.barrier()
    k.chk("proj%d" % l)
    with ExitStack() as st:
        lamt = k.sb("a_lamt", [128, 4, 64], stack=st)
        lamt_b = Buf()
        lamp = k.sb("a_lamp", [128, 2, 64], stack=st)
        lamv = k.sb("a_lamv", [128, 4], stack=st)
        lam_b = Buf()
        gcol = k.sb("a_gcol", [128, 1], stack=st)
        gcol_b = Buf()
        s.dma("sp", lambda E: E.dma_start(out=lamt[:].rearrange("p a b -> p (a b)"), in_=I["da_lam"].rearrange("a b -> (a b)").unsqueeze(0).broadcast_to([128, 256])), [], [lamt_b])
        s.dma("sp", lambda E: E.dma_start(out=gcol[:], in_=I["da_subln"][:, :]), [], [gcol_b])
        s.op("dve", lambda E: E.tensor_tensor(out=lamp[:, 0, :], in0=lamt[:, 0, :], in1=lamt[:, 1, :], op=ALU.mult), [lamt_b], [lam_b])
        s.op("dve", lambda E: E.tensor_tensor(out=lamp[:, 1, :], in0=lamt[:, 2, :], in1=lamt[:, 3, :], op=ALU.mult), [lam_b, lamt_b], [lam_b])
        s.op("dve", lambda E: E.tensor_reduce(out=lamv[:, 0:2], in_=lamp[:], axis=AX.X, op=ALU.add), [lam_b], [lam_b])
        s.op("act", lambda E: E.activation(out=lamv[:, 0:2], in_=lamv[:, 0:2], func=AF.Exp), [lam_b], [lam_b])
        s.op("dve", lambda E: E.tensor_tensor(out=lamv[:, 2:3], in0=lamv[:, 1:2], in1=lamv[:, 0:1], op=ALU.subtract), [lam_b], [lam_b])
        s.op("dve", lambda E: E.tensor_scalar(out=lamv[:, 2:3], in0=lamv[:, 2:3], scalar1=float(-lam_init), scalar2=None, op0=ALU.add), [lam_b], [lam_b])
        s.op("dve", lambda E: E.tensor_scalar(out=gcol[:], in0=gcol[:], scalar1=float(1.0 - lam_init), scalar2=None, op0=ALU.mult), [gcol_b], [gcol_b])
        neg_lam = lamv[:, 2:3]

        def post(h, qb, w, O, O_b, Z, Z_b, wk):
            (r0, o0, o1, sq, on, pss, wk_b, pss_b) = wk
            s.op("dve", lambda E: E.reciprocal(out=r0[:, 0:w], in_=Z[0][:, 0:w]), [Z_b[0]], [wk_b])
            s.op("dve", lambda E: E.tensor_tensor(out=o0[:, 0:w], in0=O[0][:, 0:w], in1=r0[:, 0:w], op=ALU.mult), [O_b[0], wk_b], [wk_b])
            s.op("dve", lambda E: E.reciprocal(out=r0[:, 0:w], in_=Z[1][:, 0:w]), [Z_b[1], wk_b], [wk_b])
            s.op("dve", lambda E: E.tensor_tensor(out=o1[:, 0:w], in0=O[1][:, 0:w], in1=r0[:, 0:w], op=ALU.mult), [O_b[1], wk_b], [wk_b])
            s.op("dve", lambda E: E.scalar_tensor_tensor(out=o0[:, 0:w], in0=o1[:, 0:w], scalar=neg_lam, in1=o0[:, 0:w], op0=ALU.mult, op1=ALU.add), [wk_b, lam_b], [wk_b])
            s.op("pool", lambda E: E.tensor_tensor(out=sq[:, 0:w], in0=o0[:, 0:w], in1=o0[:, 0:w], op=ALU.mult), [wk_b], [wk_b])
            s.op("pe", lambda E: E.matmul(pss[:, 0:w], lhsT=k.ones_f[:], rhs=sq[:, 0:w], start=True, stop=True), [wk_b, k.ones_bb], [pss_b])
            s.op("act", lambda E: E.activation(out=r0[:, 0:w], in_=pss[:, 0:w], func=AF.Ln, scale=float(1.0 / 128.0), bias=k.epsc[:, 0:1]), [pss_b, wk_b, k.epsc_b], [wk_b])
            s.op("act", lambda E: E.activation(out=r0[:, 0:w], in_=r0[:, 0:w], func=AF.Exp, scale=-0.5), [wk_b], [wk_b])
            s.op("dve", lambda E: E.scalar_tensor_tensor(out=on[:, 0:w], in0=o0[:, 0:w], scalar=gcol[:, 0:1], in1=r0[:, 0:w], op0=ALU.mult, op1=ALU.mult), [wk_b, gcol_b], [wk_b])
            return on

        attention(k, st, n_heads=8, maps=2, kp=64, dv=128, scale=0.125, post=post)
        s.barrier()


def attention(k, st, n_heads, maps, kp, dv, scale, post, krows=None, qrows=None, extra=None, with_ctx_q=True, aug=False):
    nc, s = k.nc, k.s
    KP = maps * kp if maps > 1 else kp
    krows = krows or KP
    qrows = qrows or KP
    KT = [k.sb("a_KT%d" % i, [128, NT], BF16, stack=st) for i in range(2)]
    KT_b = bufs(2)
    dva = dv + 1 if aug else dv
    V = [k.sb("a_V%d" % i, [128, NTILE, dva], BF16, stack=st) for i in range(2)]
    V_b = bufs(2)
    if aug:
        for i in range(2):
            s.op("pool", lambda E: E.memset(V[i][:, :, dv:dv + 1], 1.0), [], [V_b[i]])
    Q = [k.sb("a_Q%d" % i, [128, 512], BF16, stack=st) for i in range(2)]
    Q_b = bufs(2)
    P = [k.sb("a_P%d" % i, [128, 2, 512], BF16, stack=st) for i in range(3)]
    P_b = bufs(3)
    acc = [k.sb("a_acc%d" % i, [128, 512], stack=st) for i in range(2)]
    acc_b = bufs(2)
    accb = [k.sb("a_accb%d" % i, [128, 512], BF16, stack=st) for i in range(2)]
    accb_b = bufs(2)
    r0 = k.sb("a_r0", [128, 512], stack=st)
    o0 = k.sb("a_o0", [128, 512], stack=st)
    o1 = k.sb("a_o1", [128, 512], stack=st)
    sq = k.sb("a_sq", [128, 512], stack=st)
    on = [k.sb("a_on%d" % i, [128, 512], BF16, stack=st) for i in range(2)]
    wk_b = Buf()
    S = [k.ps("a_S%d" % i, [128, 1024], stack=st) for i in range(2)]
    S_b = bufs(2)
    O = [k.ps("a_O%d" % i, [128, 512], stack=st) for i in range(2)]
    O_b = bufs(2)
    Z = [k.ps("a_Z%d" % i, [128, 512], stack=st) for i in range(2)]
    Z_b = bufs(2)
    qblocks = [(g,) + group_range(g) for g in range(k.NG if with_ctx_q else 16)]
    si = 0
    pi = 0
    qi = 0
    oi = 0
    ai = 0
    for h in range(n_heads):
        KTh, KTh_b = KT[h % 2], KT_b[h % 2]
        Vh, Vh_b = V[h % 2], V_b[h % 2]
        s.dma("sp", lambda E: E.dma_start(out=KTh[0:krows, :], in_=k.kT[h * krows:(h + 1) * krows, :]), list(k.kT_b), [KTh_b])
        if extra is not None:
            ex_ap, ex_b, ex_rows = extra
            s.dma("sp", lambda E: E.dma_start(out=KTh[krows:krows + ex_rows, :], in_=ex_ap[:, :]), list(ex_b), [KTh_b])
        for half in range(2):
            s.dma("sp", lambda E: E.dma_start(out=Vh[:, half * 33:(half + 1) * 33, 0:dv], in_=k.vv[half * 33 * 128:(half + 1) * 33 * 128, h * dv:(h + 1) * dv].rearrange("(t p) d -> p t d", p=128)),
                  list(k.vv_b), [Vh_b])
        for (g, t0, w) in qblocks:
            Qb, Qb_b = Q[qi % 2], Q_b[qi % 2]
            qi += 1
            s.dma("sp", lambda E: E.dma_start(out=Qb[0:qrows, 0:w], in_=k.qT[h * qrows:(h + 1) * qrows, t0:t0 + w]), [k.qT_b[g]], [Qb_b])
            ktiles = list(range(NTILE)) if t0 < NL else [64, 65]
            pairs = [(ktiles[2 * i], ktiles[2 * i + 1]) for i in range(len(ktiles) // 2)]
            npair = len(pairs)
            for m in range(maps):
                p0 = m * kp
                ac, ac_b = acc[ai % 2], acc_b[ai % 2]
                acb, acb_b = accb[ai % 2], accb_b[ai % 2]
                ai += 1
                slots = []

                def emit_S(p):
                    nonlocal si
                    Sx, Sx_b = S[si % 2], S_b[si % 2]
                    si += 1
                    for j in range(2):
                        kt = pairs[p][j]
                        s.op("pe", lambda E: E.matmul(Sx[:, j * 512:j * 512 + w], lhsT=KTh[p0:p0 + kp, kt * 128:(kt + 1) * 128], rhs=Qb[p0:p0 + kp, 0:w], start=True, stop=True),
                             [KTh_b, Qb_b], [Sx_b], track=(j == 1))
                    slots.append((Sx, Sx_b))

                emit_S(0)
                for p in range(npair):
                    if p + 1 < npair:
                        emit_S(p + 1)
                    Sx, Sx_b = slots[p]
                    Px, Px_b = P[pi % 3], P_b[pi % 3]
                    pi += 1
                    s.op("act", lambda E: E.activation(out=Px[:, :, 0:w], in_=Sx[:].rearrange("p (a b) -> p a b", a=2)[:, :, 0:w], func=AF.Exp, scale=float(scale)), [Sx_b], [Px_b])
                    if not aug:
                        if p == 0:
                            s.op("dve", lambda E: E.tensor_copy(out=ac[:, 0:w], in_=Px[:, 1, 0:w]), [Px_b], [ac_b])
                        else:
                            s.op("dve", lambda E: E.tensor_tensor(out=ac[:, 0:w], in0=ac[:, 0:w], in1=Px[:, 1, 0:w], op=ALU.add), [Px_b, ac_b], [ac_b])
                    for j in range(2):
                        kt = pairs[p][j]
                        s.op("pe", lambda E: E.matmul(O[m][0:dva, 0:w], lhsT=Vh[:, kt, 0:dva], rhs=Px[:, j, 0:w], start=(p == 0 and j == 0), stop=(p == npair - 1 and j == 1)),
                             [Vh_b, Px_b], [O_b[m]], track=(j == 1))
                    if not aug:
                        s.op("pe", lambda E: E.matmul(Z[m][0:dv, 0:w], lhsT=k.ones_b[:, 0:dv], rhs=Px[:, 0, 0:w], start=(p == 0), stop=False),
                             [k.ones_bb, Px_b], [Z_b[m]], track=True)
                if not aug:
                    s.op("dve", lambda E: E.tensor_copy(out=acb[:, 0:w], in_=ac[:, 0:w]), [ac_b], [acb_b])
                    s.op("pe", lambda E: E.matmul(Z[m][0:dv, 0:w], lhsT=k.ones_b[:, 0:dv], rhs=acb[:, 0:w], start=False, stop=True), [k.ones_bb, acb_b], [Z_b[m]], track=True)
            onx = on[oi % 2]
            oi += 1
            res = post(h, g, w, O, O_b, Z, Z_b, (r0, o0, o1, sq, onx, S[0], wk_b, S_b[0]))
            s.dma("pool", lambda E: E.dma_start(out=k.aT[h * dv:(h + 1) * dv, t0:t0 + w], in_=res[0:dv, 0:w]), [wk_b], [k.aT_b[g]])


class HTP:
    def __init__(self, k, st, l, pfx, v0=0):
        self.k = k
        sb, ps = k.sb, k.ps
        self.Ab = [sb(pfx + "_Ab%d" % c, [128, D], stack=st) for c in range(2)]
        self.Bb = [sb(pfx + "_Bb%d" % c, [128, D], stack=st) for c in range(2)]
        self.Ab_b, self.Bb_b = bufs(2), bufs(2)
        for c in range(2):
            load_bcast(k, "sp", self.Ab[c][:], self.Ab_b[c], l, v0, c)
            load_bcast(k, "sp", self.Bb[c][:], self.Bb_b[c], l, v0 + 1, c)
        self.xt = [sb(pfx + "_xt%d" % i, [128, D], stack=st) for i in range(2)]
        self.xt_b = bufs(2)
        self.ht = [sb(pfx + "_ht%d" % i, [128, D], stack=st) for i in range(2)]
        self.ht_b = bufs(2)
        self.junk = sb(pfx + "_junk", [128, D], stack=st)
        self.stat = [sb(pfx + "_stat%d" % i, [128, 2], stack=st) for i in range(2)]
        self.scr_b = bufs(2)
        self.hT = [sb(pfx + "_hT%d" % i, [128, 8, 512], BF16, stack=st) for i in range(2)]
        self.hT_b = bufs(2)
        self.tp = [ps(pfx + "_tp%d" % i, [128, 512], stack=st) for i in range(2)]
        self.tp_b = bufs(2)
        self.ti = 0

    def tile(self, t):
        k, s = self.k, self.k.s
        i = self.ti % 2
        self.ti += 1
        cond = 0 if t < 64 else 1
        x_, x_b, h_, h_b = self.xt[i], self.xt_b[i], self.ht[i], self.ht_b[i]
        s.dma("sp", lambda E: E.dma_start(out=x_[:], in_=k.xres[t * 128:(t + 1) * 128, :]), [k.xres_b[t]], [x_b])
        norm_tile(k, x_, x_b, h_, h_b, self.Ab[cond], self.Ab_b[cond], self.Bb[cond], self.Bb_b[cond], (self.junk, self.stat[i]), self.scr_b[i])
        return h_, h_b

    def group(self, g):
        k = self.k
        t0, w = group_range(g)
        hTg, hTg_b = self.hT[g % 2], self.hT_b[g % 2]
        for j in range(w // 128):
            h_, h_b = self.tile(t0 // 128 + j)
            transpose_tile(k, h_, h_b, self.tp, self.tp_b, hTg, hTg_b, j * 128)
        return hTg, hTg_b, t0, w


def rms_rows(k, src_list, n, gain, gain_b, dst, dst_b, wk, wk_b):
    s = k.s
    junk, st4 = wk
    for i, (ap, b, wd) in enumerate(src_list):
        s.op("act", lambda E, ap=ap, i=i, wd=wd: E.activation(out=junk[:, 0:wd], in_=ap, func=AF.Square, accum_out=st4[:, i:i + 1]), [b], [wk_b])
    if len(src_list) == 2:
        s.op("dve", lambda E: E.tensor_tensor(out=st4[:, 0:1], in0=st4[:, 0:1], in1=st4[:, 1:2], op=ALU.add), [wk_b], [wk_b])
    s.op("act", lambda E: E.activation(out=st4[:, 2:3], in_=st4[:, 0:1], func=AF.Ln, scale=float(1.0 / n), bias=k.epsc[:, 0:1]), [wk_b, k.epsc_b], [wk_b])
    s.op("act", lambda E: E.activation(out=st4[:, 2:3], in_=st4[:, 2:3], func=AF.Exp, scale=-0.5), [wk_b], [wk_b])
    c0 = 0
    for (ap, b, wd) in src_list:
        s.op("dve", lambda E, ap=ap, c0=c0, wd=wd: E.scalar_tensor_tensor(out=dst[:, c0:c0 + wd], in0=ap, scalar=st4[:, 2:3], in1=gain[:, c0:c0 + wd], op0=ALU.mult, op1=ALU.mult),
             [b, wk_b, gain_b], [dst_b])
        c0 += wd


def mixer_mla(k, l, keep_ctx):
    nc, s, I, IB = k.nc, k.s, k.I, k.IB
    with ExitStack() as st:
        Wdq, Wdq_b = load_weight_bf16(k, st, "m_wdq", I["mla_wdq"], IB["mla_wdq"], 8, 768, blk=256)
        Wdkv, Wdkv_b = load_weight_bf16(k, st, "m_wdkv", I["mla_wdkv"], IB["mla_wdkv"], 8, 288, blk=288, swap_cols=288, swap_unit=32)
        Wuq, Wuq_b = load_weight_bf16(k, st, "m_wuq", I["mla_wuq"], IB["mla_wuq"], 6, 1536, blk=512, swap_cols=1536, swap_unit=32)
        Wuk, Wuk_b = load_weight_bf16(k, st, "m_wuk", I["mla_wuk"], IB["mla_wuk"], 2, 1024, blk=512)
        Wuv, Wuv_b = load_weight_bf16(k, st, "m_wuv", I["mla_wuv"], IB["mla_wuv"], 2, 1024, blk=512)
        qnb = k.sb("m_qnb", [128, 768], stack=st)
        kvnb = k.sb("m_kvnb", [128, 256], stack=st)
        qnb_b, kvnb_b = Buf(), Buf()
        s.dma("sp", lambda E: E.dma_start(out=qnb[:], in_=I["mla_qnorm"].unsqueeze(0).broadcast_to([128, 768])), [], [qnb_b])
        s.dma("sp", lambda E: E.dma_start(out=kvnb[:], in_=I["mla_kvnorm"].unsqueeze(0).broadcast_to([128, 256])), [], [kvnb_b])
        htp = HTP(k, st, l, "m")
        qn = [k.sb("m_qn%d" % i, [128, 768], stack=st) for i in range(2)]
        qn_b = bufs(2)
        cn = [k.sb("m_cn%d" % i, [128, 256], stack=st) for i in range(2)]
        cn_b = bufs(2)
        junk = htp.junk
        st4 = [k.sb("m_st4%d" % i, [128, 4], stack=st) for i in range(2)]
        st4_b = bufs(2)
        qlT = [k.sb("m_qlT%d" % i, [128, 6, 512], BF16, stack=st) for i in range(2)]
        qlT_b = bufs(2)
        ckT = [k.sb("m_ckT%d" % i, [128, 2, 512], BF16, stack=st) for i in range(2)]
        ckT_b = bufs(2)
        rc = [k.sb("m_rc0", [96, 512], stack=st)] * 2
        rs = [k.sb("m_rs0", [96, 512], stack=st)] * 2
        rkc = [k.sb("m_rkc0", [32, 512], stack=st)] * 2
        rks = [k.sb("m_rks0", [32, 512], stack=st)] * 2
        rt_b = [Buf()] * 2
        t1 = [k.sb("m_t1%d" % i, [128, 512], stack=st) for i in range(2)]
        t2 = [k.sb("m_t2%d" % i, [128, 512], stack=st) for i in range(2)]
        t1_b, t2_b = bufs(2), bufs(2)
        ob = [k.sb("m_ob%d" % i, [128, 512], BF16, stack=st) for i in range(4)]
        ob_b = bufs(4)
        vo = [k.sb("m_vo0", [128, D], BF16, stack=st)] * 2
        vo_b = [Buf()] * 2
        pA0 = k.ps("m_pA0", [128, 512], stack=st)
        pA1 = k.ps("m_pA1", [128, 512], stack=st)
        pC = k.ps("m_pC", [128, 512], stack=st)
        pE = [k.ps("m_pE%d" % i, [128, 512], stack=st) for i in range(2)]
        pF = k.ps("m_pF", [128, 512], stack=st)
        pA0_b, pA1_b, pC_b, pF_b = Buf(), Buf(), Buf(), Buf()
        pE_b = bufs(2)
        oi = 0
        vi = 0
        ti = 0
        for g in range(k.NG):
            hTg, hTg_b, t0, w = htp.group(g)
            qlTg, qlTg_b = qlT[g % 2], qlT_b[g % 2]
            ckTg, ckTg_b = ckT[g % 2], ckT_b[g % 2]
            s.dma("sp", lambda E, g=g, t0=t0, w=w: E.dma_start(out=rc[g % 2][:, 0:w], in_=I["rope3c"][:, t0:t0 + w]), [IB["rope3c"]], [rt_b[g % 2]])
            s.dma("sp", lambda E, g=g, t0=t0, w=w: E.dma_start(out=rs[g % 2][:, 0:w], in_=I["rope3s"][:, t0:t0 + w]), [IB["rope3s"]], [rt_b[g % 2]])
            s.dma("sp", lambda E, g=g, t0=t0, w=w: E.dma_start(out=rkc[g % 2][:, 0:w], in_=I["rope3c"][64:96, t0:t0 + w]), [IB["rope3c"]], [rt_b[g % 2]])
            s.dma("sp", lambda E, g=g, t0=t0, w=w: E.dma_start(out=rks[g % 2][:, 0:w], in_=I["rope3s"][64:96, t0:t0 + w]), [IB["rope3s"]], [rt_b[g % 2]])
            for j in range(w // 128):
                i2 = ti % 2
                ti += 1
                for c in range(8):
                    s.op("pe", lambda E, c=c, j=j: E.matmul(pA0[:], lhsT=hTg[:, c, j * 128:(j + 1) * 128], rhs=Wdq[:, c, 0:512], start=(c == 0), stop=(c == 7)), [hTg_b, Wdq_b], [pA0_b], track=(c == 7))
                for c in range(8):
                    s.op("pe", lambda E, c=c, j=j: E.matmul(pA1[:, 0:256], lhsT=hTg[:, c, j * 128:(j + 1) * 128], rhs=Wdq[:, c, 512:768], start=(c == 0), stop=(c == 7)), [hTg_b, Wdq_b], [pA1_b], track=(c == 7))
                for c in range(8):
                    s.op("pe", lambda E, c=c, j=j: E.matmul(pC[:, 0:288], lhsT=hTg[:, c, j * 128:(j + 1) * 128], rhs=Wdkv[:, c, 0:288], start=(c == 0), stop=(c == 7)), [hTg_b, Wdkv_b], [pC_b], track=(c == 7))
                rms_rows(k, [(pA0[:], pA0_b, 512), (pA1[:, 0:256], pA1_b, 256)], 768, qnb, qnb_b, qn[i2], qn_b[i2], (junk, st4[i2]), st4_b[i2])
                transpose_tile(k, qn[i2], qn_b[i2], htp.tp, htp.tp_b, qlTg, qlTg_b, j * 128, nchunk=6)
                rms_rows(k, [(pC[:, 0:256], pC_b, 256)], 256, kvnb, kvnb_b, cn[i2], cn_b[i2], (junk, st4[i2]), st4_b[i2])
                transpose_tile(k, cn[i2], cn_b[i2], htp.tp, htp.tp_b, ckTg, ckTg_b, j * 128, nchunk=2)
            for c in range(8):
                s.op("pe", lambda E, c=c: E.matmul(pE[0][0:32, 0:w], lhsT=Wdkv[:, c, 256:288], rhs=hTg[:, c, 0:w], start=(c == 0), stop=(c == 7)), [hTg_b, Wdkv_b], [pE_b[0]], track=(c == 7))
            for c in range(8):
                s.op("pe", lambda E, c=c: E.matmul(pE[1][0:32, 0:w], lhsT=Wdkv[:, c, 288 + 256:288 + 288], rhs=hTg[:, c, 0:w], start=(c == 0), stop=(c == 7)), [hTg_b, Wdkv_b], [pE_b[1]], track=(c == 7))
            o_, o_b = ob[oi % 4], ob_b[oi % 4]
            oi += 1
            s.op("dve", lambda E: E.tensor_tensor(out=t1[0][0:32, 0:w], in0=pE[0][0:32, 0:w], in1=rkc[g % 2][:, 0:w], op=ALU.mult), [pE_b[0], rt_b[g % 2]], [t1_b[0]])
            s.op("dve", lambda E: E.tensor_tensor(out=t2[0][0:32, 0:w], in0=pE[1][0:32, 0:w], in1=rks[g % 2][:, 0:w], op=ALU.mult), [pE_b[1], rt_b[g % 2]], [t2_b[0]])
            s.op("pool", lambda E, o_=o_: E.tensor_tensor(out=o_[0:32, 0:w], in0=t1[0][0:32, 0:w], in1=t2[0][0:32, 0:w], op=ALU.add), [t1_b[0], t2_b[0]], [o_b])
            s.dma("pool", lambda E, o_=o_: E.dma_start(out=k.krT[:, t0:t0 + w], in_=o_[0:32, 0:w]), [o_b], [k.krT_b[g]])
            for hp in range(8):
                for c in range(2):
                    s.op("pe", lambda E, c=c, hp=hp: E.matmul(pF[:, 0:w], lhsT=Wuk[:, c, hp * 128:(hp + 1) * 128], rhs=ckTg[:, c, 0:w], start=(c == 0), stop=(c == 1)), [ckTg_b, Wuk_b], [pF_b], track=(c == 1))
                o_, o_b = ob[oi % 4], ob_b[oi % 4]
                oi += 1
                s.op("act", lambda E, o_=o_: E.activation(out=o_[:, 0:w], in_=pF[:, 0:w], func=AF.Copy), [pF_b], [o_b])
                s.dma("pool", lambda E, o_=o_, hp=hp: E.dma_start(out=k.kT[hp * 128:(hp + 1) * 128, t0:t0 + w], in_=o_[:, 0:w]), [o_b], [k.kT_b[g]])
            for h in range(16):
                for c in range(6):
                    s.op("pe", lambda E, c=c, h=h: E.matmul(pE[0][0:96, 0:w], lhsT=Wuq[:, c, h * 96:(h + 1) * 96], rhs=qlTg[:, c, 0:w], start=(c == 0), stop=(c == 5)), [qlTg_b, Wuq_b], [pE_b[0]], track=(c == 5))
                for c in range(6):
                    s.op("pe", lambda E, c=c, h=h: E.matmul(pE[1][0:96, 0:w], lhsT=Wuq[:, c, 1536 + h * 96:1536 + (h + 1) * 96], rhs=qlTg[:, c, 0:w], start=(c == 0), stop=(c == 5)), [qlTg_b, Wuq_b], [pE_b[1]], track=(c == 5))
                a1, a1_b = t1[h % 2], t1_b[h % 2]
                a2, a2_b = t2[h % 2], t2_b[h % 2]
                o_, o_b = ob[oi % 4], ob_b[oi % 4]
                oi += 1
                s.op("dve", lambda E, a1=a1: E.tensor_tensor(out=a1[0:96, 0:w], in0=pE[0][0:96, 0:w], in1=rc[g % 2][:, 0:w], op=ALU.mult), [pE_b[0], rt_b[g % 2]], [a1_b])
                s.op("dve", lambda E, a2=a2: E.tensor_tensor(out=a2[0:96, 0:w], in0=pE[1][0:96, 0:w], in1=rs[g % 2][:, 0:w], op=ALU.mult), [pE_b[1], rt_b[g % 2]], [a2_b])
                s.op("pool", lambda E, a1=a1, a2=a2, o_=o_: E.tensor_tensor(out=o_[0:96, 0:w], in0=a1[0:96, 0:w], in1=a2[0:96, 0:w], op=ALU.add), [a1_b, a2_b], [o_b])
                s.dma("pool", lambda E, o_=o_, h=h: E.dma_start(out=k.qT[h * 96:(h + 1) * 96, t0:t0 + w], in_=o_[0:96, 0:w]), [o_b], [k.qT_b[g]])
            for j in range(w // 128):
                v_, v_b = vo[vi % 2], vo_b[vi % 2]
                vi += 1
                for nb in range(2):
                    for c in range(2):
                        s.op("pe", lambda E, c=c, nb=nb, j=j: E.matmul(pF[:], lhsT=ckTg[:, c, j * 128:(j + 1) * 128], rhs=Wuv[:, c, nb * 512:(nb + 1) * 512], start=(c == 0), stop=(c == 1)), [ckTg_b, Wuv_b], [pF_b], track=(c == 1))
                    s.op("act", lambda E, nb=nb, v_=v_: E.activation(out=v_[:, nb * 512:(nb + 1) * 512], in_=pF[:], func=AF.Copy), [pF_b], [v_b])
                t = (t0 // 128) + j
                s.dma("pool", lambda E, t=t, v_=v_: E.dma_start(out=k.vv[t * 128:(t + 1) * 128, :], in_=v_[:]), [v_b], [k.vv_b[g]])
        s.barrier()
    with ExitStack() as st:
        def post(h, qb, w, O, O_b, Z, Z_b, wk):
            (r0, o0, o1, sq, on, pss, wk_b, pss_b) = wk
            s.op("act", lambda E: E.activation(out=o1[64:65, 0:w], in_=O[0][64:65, 0:w], func=AF.Copy), [O_b[0]], [wk_b])
            s.op("pe", lambda E: E.matmul(Z[0][0:64, 0:w], lhsT=k.ones_f[64:65, 0:64], rhs=o1[64:65, 0:w], start=True, stop=True), [wk_b, k.ones_bb], [Z_b[0]])
            s.op("dve", lambda E: E.reciprocal(out=r0[0:64, 0:w], in_=Z[0][0:64, 0:w]), [Z_b[0]], [wk_b])
            s.op("dve", lambda E: E.tensor_tensor(out=on[0:64, 0:w], in0=O[0][0:64, 0:w], in1=r0[0:64, 0:w], op=ALU.mult), [O_b[0], wk_b], [wk_b])
            return on
        attention(k, st, n_heads=16, maps=1, kp=96, dv=64, scale=float(96 ** -0.5), post=post, krows=64, qrows=96, extra=(k.krT, k.krT_b, 32), with_ctx_q=keep_ctx, aug=True)
        s.barrier()


def qkv_plain(k, st, l, pfx, wname, q_scale):
    s, I, IB = k.s, k.I, k.IB
    W, W_b = load_weight_bf16(k, st, pfx + "_w", I[wname], IB[wname], 8, 3 * D)
    htp = HTP(k, st, l, pfx)
    qo = [k.sb(pfx + "_qo%d" % i, [128, 512], BF16, stack=st) for i in range(4)]
    qo_b = bufs(4)
    vo = [k.sb(pfx + "_vo%d" % i, [128, D], BF16, stack=st) for i in range(2)]
    vo_b = bufs(2)
    pq = [k.ps(pfx + "_pq%d" % i, [128, 512], stack=st) for i in range(2)]
    pq_b = bufs(2)
    pv = [k.ps(pfx + "_pv%d" % i, [128, 512], stack=st) for i in range(2)]
    pv_b = bufs(2)
    oi = 0
    vi = 0
    for g in range(k.NG):
        hTg, hTg_b, t0, w = htp.group(g)
        for o in range(16):
            p, p_b = pq[o % 2], pq_b[o % 2]
            for c in range(8):
                s.op("pe", lambda E, c=c, o=o, p=p: E.matmul(p[:, 0:w], lhsT=W[:, c, o * 128:(o + 1) * 128], rhs=hTg[:, c, 0:w], start=(c == 0), stop=(c == 7)), [W_b, hTg_b], [p_b], track=(c == 7))
            q_, q_b = qo[oi % 4], qo_b[oi % 4]
            oi += 1
            if o < 8:
                s.op("act", lambda E, p=p, q_=q_: E.activation(out=q_[:, 0:w], in_=p[:, 0:w], func=AF.Copy, scale=float(q_scale)), [p_b], [q_b])
            else:
                s.op("dve", lambda E, p=p, q_=q_: E.tensor_copy(out=q_[:, 0:w], in_=p[:, 0:w]), [p_b], [q_b])
            dstT = k.qT if o < 8 else k.kT
            dst_b = (k.qT_b if o < 8 else k.kT_b)[g]
            oo = o % 8
            s.dma("pool", lambda E, dstT=dstT, oo=oo, q_=q_: E.dma_start(out=dstT[oo * 128:(oo + 1) * 128, t0:t0 + w], in_=q_[:, 0:w]), [q_b], [dst_b])
        for j in range(w // 128):
            v_, v_b = vo[vi % 2], vo_b[vi % 2]
            vi += 1
            for nb in range(2):
                p, p_b = pv[nb], pv_b[nb]
                for c in range(8):
                    s.op("pe", lambda E, c=c, nb=nb, p=p, j=j: E.matmul(p[:], lhsT=hTg[:, c, j * 128:(j + 1) * 128], rhs=W[:, c, 2 * D + nb * 512:2 * D + (nb + 1) * 512], start=(c == 0), stop=(c == 7)), [W_b, hTg_b], [p_b], track=(c == 7))
                s.op("act", lambda E, nb=nb, p=p, v_=v_: E.activation(out=v_[:, nb * 512:(nb + 1) * 512], in_=p[:], func=AF.Copy), [p_b], [v_b])
            t = (t0 // 128) + j
            s.dma("pool", lambda E, t=t, v_=v_: E.dma_start(out=k.vv[t * 128:(t + 1) * 128, :], in_=v_[:]), [v_b], [k.vv_b[g]])


def mixer_na(k, l, keep_ctx):
    s, I, IB = k.s, k.I, k.IB
    with ExitStack() as st:
        qkv_plain(k, st, l, "n", "na_w", 0.125)
        s.barrier()
    with ExitStack() as st:
        KT = [k.sb("n_KT%d" % i, [64, NT], BF16, stack=st) for i in range(2)]
        QT = [k.sb("n_QT%d" % i, [64, NT], BF16, stack=st) for i in range(2)]
        Ve = [k.sb("n_Ve%d" % i, [128, 66, 65], BF16, stack=st) for i in range(2)]
        Vo = [k.sb("n_Vo%d" % i, [128, 65, 65], BF16, stack=st) for i in range(2)]
        hb_b = bufs(2)
        for i in range(2):
            s.op("pool", lambda E: E.memset(Ve[i][:, :, 64:65], 1.0), [], [hb_b[i]])
            s.op("pool", lambda E: E.memset(Vo[i][:, :, 64:65], 1.0), [], [hb_b[i]])
        zr = k.sb("n_zr", [128, 512], stack=st)
        zr_b = Buf()
        bf = [k.sb("n_bf%d" % i, [128, 2048], stack=st) for i in range(2)]
        bf_b = bufs(2)
        bb = [k.sb("n_bb%d" % i, [128, 8, 4, 64], BF16, stack=st) for i in range(2)]
        bb_b = bufs(2)
        P = [k.sb("n_P%d" % i, [128, 512], BF16, stack=st) for i in range(3)]
        P_b = bufs(3)
        r0t = k.sb("n_r0", [64, 512], stack=st)
        on = [k.sb("n_on%d" % i, [64, 512], BF16, stack=st) for i in range(2)]
        on_b = bufs(2)
        r0_b = Buf()
        S = [k.ps("n_S%d" % i, [128, 512], stack=st) for i in range(2)]
        S_b = bufs(2)
        O = [k.ps("n_O%d" % i, [128, 512], stack=st) for i in range(2)]
        O_b = bufs(2)
        Z = [k.ps("n_Z%d" % i, [64, 512], stack=st) for i in range(2)]
        Z_b = bufs(2)
        si = 0
        pi = 0
        oi = 0
        for h in range(16):
            i2 = h % 2
            KTh, QTh, Veh, Voh, hb = KT[i2], QT[i2], Ve[i2], Vo[i2], hb_b[i2]
            s.dma("sp", lambda E, h=h, KTh=KTh: E.dma_start(out=KTh[:, :], in_=k.kT[h * 64:(h + 1) * 64, :]), list(k.kT_b), [hb])
            s.dma("sp", lambda E, h=h, QTh=QTh: E.dma_start(out=QTh[:, :], in_=k.qT[h * 64:(h + 1) * 64, :]), list(k.qT_b), [hb])
            for half in range(2):
                s.dma("sp", lambda E, h=h, Veh=Veh, half=half: E.dma_start(out=Veh[:, half * 33:(half + 1) * 33, 0:64], in_=k.vv[half * 33 * 128:(half + 1) * 33 * 128, h * 64:(h + 1) * 64].rearrange("(t p) d -> p t d", p=128)), list(k.vv_b), [hb])
            s.dma("sp", lambda E, h=h, Voh=Voh: E.dma_start(out=Voh[:, 0:33, 0:64], in_=k.vv[64:64 + 33 * 128, h * 64:(h + 1) * 64].rearrange("(t p) d -> p t d", p=128)), list(k.vv_b), [hb])
            s.dma("sp", lambda E, h=h, Voh=Voh: E.dma_start(out=Voh[:, 33:65, 0:64], in_=k.vv[64 + 33 * 128:64 + 65 * 128, h * 64:(h + 1) * 64].rearrange("(t p) d -> p t d", p=128)), list(k.vv_b), [hb])
            s.dma("sp", lambda E, h=h: E.dma_start(out=bf[i2][:], in_=I["na_bias"][h * 128:(h + 1) * 128, :]), [IB["na_bias"]], [bf_b[i2]])
            s.op("pool", lambda E: E.tensor_copy(out=bb[i2][:].rearrange("p a b c -> p (a b c)"), in_=bf[i2][:]), [bf_b[i2]], [bb_b[i2]])
            blocks = [("lat", rg) for rg in range(16)] + ([("ctx", 0)] if keep_ctx else [])
            for (kind, rg) in blocks:
                Ox, Ox_b, Zx, Zx_b = O[oi % 2], O_b[oi % 2], Z[oi % 2], Z_b[oi % 2]
                onx, onx_b = on[oi % 2], on_b[oi % 2]
                oi += 1
                if kind == "lat":
                    items = []
                    for i in range(8):
                        r = rg * 8 + i
                        r0 = min(max(r - 4, 0), 120)
                        items.append((r, r0, r - r0))
                    for i, (r, r0, v) in enumerate(items):
                        Sx, Sx_b = S[si % 2], S_b[si % 2]
                        si += 1
                        Px, Px_b = P[pi % 3], P_b[pi % 3]
                        pi += 1
                        for kt in range(4):
                            tok0 = r0 * 64 + kt * 128
                            s.op("pe", lambda E, Sx=Sx, kt=kt, tok0=tok0, r=r: E.matmul(Sx[:, kt * 64:(kt + 1) * 64], lhsT=KTh[:, tok0:tok0 + 128], rhs=QTh[:, r * 64:(r + 1) * 64], start=True, stop=False), [hb], [Sx_b], track=False)
                            s.op("pe", lambda E, Sx=Sx, kt=kt, v=v: E.matmul(Sx[:, kt * 64:(kt + 1) * 64], lhsT=k.identb[:], rhs=bb[i2][:, v, kt, :], start=False, stop=True), [k.identb_b, bb_b[i2]], [Sx_b], track=False)
                        for c in range(2):
                            s.op("pe", lambda E, Sx=Sx, c=c, r=r: E.matmul(Sx[:, (4 + c) * 64:(5 + c) * 64], lhsT=KTh[:, NL + c * 128:NL + (c + 1) * 128], rhs=QTh[:, r * 64:(r + 1) * 64], start=True, stop=True), [hb], [Sx_b], track=(c == 1))
                        s.op("act", lambda E, Sx=Sx, Px=Px: E.activation(out=Px[:, 0:384], in_=Sx[:, 0:384], func=AF.Exp), [Sx_b], [Px_b])
                        for kt in range(6):
                            if kt < 4:
                                vt = Veh[:, r0 // 2 + kt, :] if r0 % 2 == 0 else Voh[:, (r0 - 1) // 2 + kt, :]
                            else:
                                vt = Veh[:, 64 + (kt - 4), :]
                            s.op("pe", lambda E, vt=vt, Px=Px, kt=kt, i=i, Ox=Ox: E.matmul(Ox[0:65, i * 64:(i + 1) * 64], lhsT=vt, rhs=Px[:, kt * 64:(kt + 1) * 64], start=(kt == 0), stop=(kt == 5)), [hb, Px_b], [Ox_b], track=(kt == 5))
                    w = 512
                    c0 = rg * 512
                else:
                    Sx, Sx_b = S[si % 2], S_b[si % 2]
                    si += 1
                    Px, Px_b = P[pi % 3], P_b[pi % 3]
                    pi += 1
                    for c in range(2):
                        s.op("pe", lambda E, Sx=Sx, c=c: E.matmul(Sx[:, c * 256:(c + 1) * 256], lhsT=KTh[:, NL + c * 128:NL + (c + 1) * 128], rhs=QTh[:, NL:NT], start=True, stop=True), [hb], [Sx_b], track=(c == 1))
                    s.op("act", lambda E, Sx=Sx, Px=Px: E.activation(out=Px[:, :], in_=Sx[:, :], func=AF.Exp), [Sx_b], [Px_b])
                    for c in range(2):
                        s.op("pe", lambda E, Px=Px, c=c, Ox=Ox: E.matmul(Ox[0:65, 0:256], lhsT=Veh[:, 64 + c, :], rhs=Px[:, c * 256:(c + 1) * 256], start=(c == 0), stop=(c == 1)), [hb, Px_b], [Ox_b], track=(c == 1))
                    w = 256
                    c0 = NL
                s.op("act", lambda E: E.activation(out=zr[64:65, 0:w], in_=Ox[64:65, 0:w], func=AF.Copy), [Ox_b], [zr_b])
                s.op("pe", lambda E: E.matmul(Zx[0:64, 0:w], lhsT=k.ones_f[64:65, 0:64], rhs=zr[64:65, 0:w], start=True, stop=True), [zr_b, k.ones_bb], [Zx_b])
                s.op("dve", lambda E: E.reciprocal(out=r0t[:, 0:w], in_=Zx[0:64, 0:w]), [Zx_b], [r0_b])
                s.op("dve", lambda E: E.tensor_tensor(out=onx[:, 0:w], in0=Ox[0:64, 0:w], in1=r0t[:, 0:w], op=ALU.mult), [Ox_b, r0_b], [onx_b])
                g0 = c0 // 512
                s.dma("pool", lambda E, h=h, c0=c0, w=w, onx=onx: E.dma_start(out=k.aT[h * 64:(h + 1) * 64, c0:c0 + w], in_=onx[:, 0:w]), [onx_b], [k.aT_b[g0]])
        s.barrier()


def mixer_fourier(k, l, keep_ctx):
    s, I, IB = k.s, k.I, k.IB
    with ExitStack() as st:
        htp = HTP(k, st, l, "f")
        hb = [k.sb("f_hb%d" % i, [128, D], BF16, stack=st) for i in range(2)]
        hb_b = bufs(2)
        for t in range(NTILE):
            h_, h_b = htp.tile(t)
            s.op("act", lambda E, h_=h_, t=t: E.activation(out=hb[t % 2][:], in_=h_[:], func=AF.Copy), [h_b], [hb_b[t % 2]])
            s.dma("pool", lambda E, t=t: E.dma_start(out=k.h2tab[t * 128:(t + 1) * 128, :], in_=hb[t % 2][:]), [hb_b[t % 2]], [k.h2tab_b[t]])
        s.barrier()
    with ExitStack() as st:
        stg = k.sb("f_stg", [128, 2048], stack=st)
        stg_b = Buf()
        COS = k.sb("f_cos", [128, 64, 128], BF16, stack=st)
        SIN = k.sb("f_sin", [128, 64, 128], BF16, stack=st)
        F64 = k.sb("f_f64", [64, 4, 48], BF16, stack=st)
        CC = k.sb("f_cc", [128, 2, 256], BF16, stack=st)
        SC = k.sb("f_sc", [128, 2, 256], BF16, stack=st)
        NSC = k.sb("f_nsc", [128, 2, 256], BF16, stack=st)
        tab_b = Buf()
        for (dst, name) in [(COS, "fn_cos"), (SIN, "fn_sin")]:
            for q in range(4):
                s.dma("sp", lambda E, name=name, q=q: E.dma_start(out=stg[:], in_=I[name][:, q * 2048:(q + 1) * 2048]), [IB[name]], [stg_b])
                s.op("dve", lambda E, dst=dst, q=q: E.tensor_copy(out=dst[:].rearrange("p a b -> p (a b)")[:, q * 2048:(q + 1) * 2048], in_=stg[:]), [stg_b], [tab_b])
        s.dma("sp", lambda E: E.dma_start(out=stg[0:64, 0:192], in_=I["fn_f64"][:, :]), [], [stg_b])
        s.op("dve", lambda E: E.tensor_copy(out=F64[:].rearrange("p a b -> p (a b)"), in_=stg[0:64, 0:192]), [stg_b], [tab_b])
        for (dst, name) in [(CC, "fn_cc"), (SC, "fn_sc"), (NSC, "fn_nsc")]:
            s.dma("sp", lambda E, name=name: E.dma_start(out=stg[:, 0:512].rearrange("p (a b) -> p a b", a=2), in_=I[name].rearrange("(a p) l -> p a l", p=128)), [], [stg_b])
            s.op("dve", lambda E, dst=dst: E.tensor_copy(out=dst[:].rearrange("p a b -> p (a b)"), in_=stg[:, 0:512]), [stg_b], [tab_b])
        Xs = k.sb("f_Xs", [64, 128, 128], BF16, stack=st)
        Xs_b = Buf()
        A = k.sb("f_A", [128, 48, 128], BF16, stack=st)
        A_b = Buf()
        GTr = k.sb("f_GTr", [128, 2, NL], BF16, stack=st)
        GTi = k.sb("f_GTi", [128, 2, NL], BF16, stack=st)
        GT_b = Buf()
        hc = k.sb("f_hc", [128, 2, 256], BF16, stack=st)
        hc_b = Buf()
        yo = [k.sb("f_yo%d" % i, [128, 512], BF16, stack=st) for i in range(2)]
        yo_b = bufs(2)
        PA = [k.ps("f_PA%d" % i, [128, 512], stack=st) for i in range(2)]
        PA_b = bufs(2)
        PG = [k.ps("f_PG%d" % i, [128, 512], stack=st) for i in range(4)]
        PG_b = bufs(4)
        PY = [k.ps("f_PY%d" % i, [128, 512], stack=st) for i in range(2)]
        PY_b = bufs(2)
        ai = 0
        gi = 0
        yi = 0

        def channel_dft(gq, k0, kw, nk, scale, col0):
            nonlocal yi
            for lc in range(2):
                for kb in range(nk):
                    p, p_b = PY[yi % 2], PY_b[yi % 2]
                    y_, y_b = yo[yi % 2], yo_b[yi % 2]
                    yi += 1
                    ka = k0 + kb * kw
                    n = 0
                    for cc in range(2):
                        for (T, Gx) in [(CC, GTr), (SC, GTi)]:
                            s.op("pe", lambda E, T=T, Gx=Gx, cc=cc, lc=lc, ka=ka, p=p, n=n: E.matmul(p[:, 0:kw], lhsT=T[:, cc, lc * 128:(lc + 1) * 128], rhs=Gx[:, cc, ka:ka + kw], start=(n == 0), stop=(n == 3)), [tab_b, GT_b], [p_b], track=(n == 3))
                            n += 1
                    s.op("act", lambda E, p=p, y_=y_: E.activation(out=y_[:, 0:kw], in_=p[:, 0:kw], func=AF.Copy, scale=float(scale)), [p_b], [y_b])
                    g0 = (col0 + ka - k0) // 512
                    s.dma("pool", lambda E, y_=y_, lc=lc, ka=ka: E.dma_start(out=k.aT[gq * 256 + lc * 128:gq * 256 + (lc + 1) * 128, col0 + ka - k0:col0 + ka - k0 + kw], in_=y_[:, 0:kw]), [y_b], [k.aT_b[g0]])

        for gq in range(4):
            for cc in range(2):
                col = gq * 256 + cc * 128
                s.dma("sp", lambda E, col=col: E.dma_start(out=Xs[:, :, :], in_=k.h2tab[0:NL, col:col + 128].rearrange("(a b) c -> a b c", b=128)), list(k.h2tab_b), [Xs_b])
                for kb in range(4):
                    for c8 in range(16):
                        p, p_b = PA[ai % 2], PA_b[ai % 2]
                        for ci in range(8):
                            c = c8 * 8 + ci
                            s.op("pe", lambda E, p=p, ci=ci, c=c, kb=kb: E.matmul(p[:, ci * 48:(ci + 1) * 48], lhsT=Xs[:, :, c], rhs=F64[:, kb, :], start=True, stop=True), [Xs_b, tab_b], [p_b], track=(ci == 7))
                        eng = ["act", "dve"][ai % 2]
                        ai += 1
                        if eng == "act":
                            s.op("act", lambda E, p=p, c8=c8: E.activation(out=A[:, :, c8 * 8:(c8 + 1) * 8], in_=p[:, 0:384].rearrange("p (c j) -> p j c", j=48), func=AF.Copy), [p_b], [A_b])
                        else:
                            s.op("dve", lambda E, p=p, c8=c8: E.tensor_copy(out=A[:, :, c8 * 8:(c8 + 1) * 8], in_=p[:, 0:384].rearrange("p (c j) -> p j c", j=48)), [p_b], [A_b])
                    for quad in range(4):
                        pr, pr_b = PG[gi % 4], PG_b[gi % 4]
                        pim, pim_b = PG[(gi + 1) % 4], PG_b[(gi + 1) % 4]
                        gi += 2
                        for q in range(4):
                            k1l = quad * 4 + q
                            k1 = kb * 16 + k1l
                            s.op("pe", lambda E, pr=pr, q=q, k1l=k1l, k1=k1: E.matmul(pr[:, q * 128:(q + 1) * 128], lhsT=A[:, k1l, :], rhs=COS[:, k1, :], start=True, stop=False), [A_b, tab_b], [pr_b], track=False)
                            s.op("pe", lambda E, pr=pr, q=q, k1l=k1l, k1=k1: E.matmul(pr[:, q * 128:(q + 1) * 128], lhsT=A[:, 16 + k1l, :], rhs=SIN[:, k1, :], start=False, stop=True), [A_b, tab_b], [pr_b], track=(q == 3))
                            s.op("pe", lambda E, pim=pim, q=q, k1l=k1l, k1=k1: E.matmul(pim[:, q * 128:(q + 1) * 128], lhsT=A[:, 16 + k1l, :], rhs=COS[:, k1, :], start=True, stop=False), [A_b, tab_b], [pim_b], track=False)
                            s.op("pe", lambda E, pim=pim, q=q, k1l=k1l, k1=k1: E.matmul(pim[:, q * 128:(q + 1) * 128], lhsT=A[:, 32 + k1l, :], rhs=SIN[:, k1, :], start=False, stop=True), [A_b, tab_b], [pim_b], track=(q == 3))
                        k10 = kb * 16 + quad * 4
                        s.op("act", lambda E, pr=pr, cc=cc, k10=k10: E.activation(out=GTr[:, cc, :].rearrange("p (b a) -> p a b", a=64)[:, k10:k10 + 4, :], in_=pr[:].rearrange("p (q b) -> p q b", q=4), func=AF.Copy), [pr_b], [GT_b])
                        s.op("dve", lambda E, pim=pim, cc=cc, k10=k10: E.tensor_copy(out=GTi[:, cc, :].rearrange("p (b a) -> p a b", a=64)[:, k10:k10 + 4, :], in_=pim[:].rearrange("p (q b) -> p q b", q=4)), [pim_b], [GT_b])
            channel_dft(gq, 0, 512, 16, 1.0 / math.sqrt(NL * 256.0), 0)
            if keep_ctx:
                s.dma("sp", lambda E, gq=gq: E.dma_start(out=hc[:, :, :], in_=k.h2tab[NL:NT, gq * 256:(gq + 1) * 256].rearrange("(t p) c -> p t c", p=128)), list(k.h2tab_b), [hc_b])
                for cc in range(2):
                    pr, pr_b = PG[gi % 4], PG_b[gi % 4]
                    pim, pim_b = PG[(gi + 1) % 4], PG_b[(gi + 1) % 4]
                    gi += 2
                    for t in range(2):
                        s.op("pe", lambda E, pr=pr, t=t, cc=cc: E.matmul(pr[:, 0:256], lhsT=hc[:, t, cc * 128:(cc + 1) * 128], rhs=CC[:, t, :], start=(t == 0), stop=(t == 1)), [hc_b, tab_b], [pr_b], track=(t == 1))
                    for t in range(2):
                        s.op("pe", lambda E, pim=pim, t=t, cc=cc: E.matmul(pim[:, 0:256], lhsT=hc[:, t, cc * 128:(cc + 1) * 128], rhs=NSC[:, t, :], start=(t == 0), stop=(t == 1)), [hc_b, tab_b], [pim_b], track=(t == 1))
                    s.op("act", lambda E, pr=pr, cc=cc: E.activation(out=GTr[:, cc, 0:256], in_=pr[:, 0:256], func=AF.Copy), [pr_b], [GT_b])
                    s.op("dve", lambda E, pim=pim, cc=cc: E.tensor_copy(out=GTi[:, cc, 0:256], in_=pim[:, 0:256]), [pim_b], [GT_b])
                channel_dft(gq, 0, 256, 1, 1.0 / 256.0, NL)
        s.barrier()


def post_mixer_and_moe(k, l, keep_ctx):
    nc, s, I = k.nc, k.s, k.I
    wo_name = {0: "da_wo", 1: "fn_wo", 2: "na_wo", 3: "mla_wo"}[l % 4]
    wo_src, wo_src_b = I[wo_name], k.IB[wo_name]
    ntile = NTILE if keep_ctx else 64
    ngrp = k.NG if keep_ctx else 16
    conds = [0, 1] if keep_ctx else [0]
    es2 = ExitStack()
    aff = k.sb("r_aff", [128, NTILE, NEXP], stack=es2)
    aff_b = Buf()
    posi = k.sb("r_posi", [128, NTILE, NEXP], I32, stack=es2)
    posi_b = Buf()
    G2b = [k.sb("r_G2b%d" % c, [128, D], stack=es2) for c in range(2)]
    G2b_b = bufs(2)
    for c in conds:
        load_bcast(k, "sp", G2b[c][:], G2b_b[c], l, 5, c)
    with ExitStack() as st:
        Wo, Wo_b = load_weight_bf16(k, st, "o_w", wo_src, wo_src_b, 8, D)
        wr = k.sb("o_wr", [128, 8, NEXP], stack=st)
        wr_b = Buf()
        s.dma("sp", lambda E: E.dma_start(out=wr[:], in_=I["moe_wr"][l].rearrange("(c p) e -> p c e", p=128)), [], [wr_b])
        G1b = [k.sb("o_G1b%d" % c, [128, D], stack=st) for c in range(2)]
        A2b = [k.sb("o_A2b%d" % c, [128, D], stack=st) for c in range(2)]
        B2b = [k.sb("o_B2b%d" % c, [128, D], stack=st) for c in range(2)]
        G1b_b, A2b_b, B2b_b = bufs(2), bufs(2), bufs(2)
        for c in conds:
            load_bcast(k, "sp", G1b[c][:], G1b_b[c], l, 2, c)
            load_bcast(k, "sp", A2b[c][:], A2b_b[c], l, 3, c)
            load_bcast(k, "sp", B2b[c][:], B2b_b[c], l, 4, c)
        aTs = [k.sb("o_aT%d" % i, [128, 8, 512], BF16, stack=st) for i in range(2)]
        aTs_b = bufs(2)
        xt = [k.sb("o_xt%d" % i, [128, D], stack=st) for i in range(4)]
        xt_b = bufs(4)
        ht = [k.sb("o_ht%d" % i, [128, D], stack=st) for i in range(4)]
        ht_b = bufs(4)
        hb = [k.sb("o_hb%d" % i, [128, D], BF16, stack=st) for i in range(4)]
        hb_b = bufs(4)
        junk = k.sb("o_junk", [128, D], stack=st)
        stat = [k.sb("o_stat%d" % i, [128, 2], stack=st) for i in range(4)]
        scr_b = bufs(4)
        hT = [k.sb("o_hT%d" % i, [128, 8, 128], stack=st) for i in range(4)]
        hT_b = bufs(4)
        lg2 = [k.sb("o_lg%d" % i, [128, NEXP], stack=st) for i in range(2)]
        lgs2 = [k.sb("o_lgs%d" % i, [128, 2], stack=st) for i in range(2)]
        lg2_b = bufs(2)
        py = [k.ps("o_py%d" % i, [128, 512], stack=st) for i in range(2)]
        py_b = bufs(2)
        tp = [k.ps("o_tp%d" % i, [128, 512], stack=st) for i in range(2)]
        tp_b = bufs(2)
        pl = k.ps("o_pl", [128, NEXP], stack=st)
        pl_b = Buf()
        tiles = []
        for g in range(ngrp):
            t0, w = group_range(g)
            for j in range(w // 128):
                tiles.append((g, t0, w, j, t0 // 128 + j))

        def S1(ti):
            g, t0, w, j, t = tiles[ti]
            cond = 0 if t < 64 else 1
            a_, a_b = aTs[g % 2], aTs_b[g % 2]
            if j == 0:
                s.dma("sp", lambda E: E.dma_start(out=a_[:, :, 0:w], in_=k.aT[:, t0:t0 + w].rearrange("(c p) t -> p c t", p=128)), [k.aT_b[g]], [a_b])
            x_, x_b = xt[ti % 4], xt_b[ti % 4]
            h_, h_b = ht[ti % 4], ht_b[ti % 4]
            s.dma("sp", lambda E: E.dma_start(out=x_[:], in_=k.xres[t * 128:(t + 1) * 128, :]), [k.xres_b[t]], [x_b])
            for nb in range(2):
                p, p_b = py[nb], py_b[nb]
                for c in range(8):
                    s.op("pe", lambda E: E.matmul(p[:], lhsT=a_[:, c, j * 128:(j + 1) * 128], rhs=Wo[:, c, nb * 512:(nb + 1) * 512], start=(c == 0), stop=(c == 7)),
                         [a_b, Wo_b], [p_b], track=(c == 7))
                s.op("dve", lambda E: E.tensor_tensor(out=h_[:, nb * 512:(nb + 1) * 512], in0=p[:], in1=G1b[cond][:, nb * 512:(nb + 1) * 512], op=ALU.mult),
                     [p_b, G1b_b[cond]], [h_b])
            s.op("pool", lambda E: E.tensor_tensor(out=x_[:], in0=x_[:], in1=h_[:], op=ALU.add), [x_b, h_b], [x_b])
            s.dma("pool", lambda E: E.dma_start(out=k.xres[t * 128:(t + 1) * 128, :], in_=x_[:]), [x_b], [k.xres_b[t]])

        def S2(ti):
            g, t0, w, j, t = tiles[ti]
            cond = 0 if t < 64 else 1
            x_, x_b = xt[ti % 4], xt_b[ti % 4]
            h_, h_b = ht[ti % 4], ht_b[ti % 4]
            hb_, hb_bb = hb[ti % 4], hb_b[ti % 4]
            norm_tile(k, x_, x_b, h_, h_b, A2b[cond], A2b_b[cond], B2b[cond], B2b_b[cond], (junk, stat[ti % 4]), scr_b[ti % 4])
            s.op("act", lambda E: E.activation(out=hb_[:], in_=h_[:], func=AF.Copy), [h_b], [hb_bb])
            s.dma("pool", lambda E: E.dma_start(out=k.h2tab[t * 128:(t + 1) * 128, :], in_=hb_[:]), [hb_bb], [k.h2tab_b[t]])

        def S3(ti):
            g, t0, w, j, t = tiles[ti]
            h_, h_b = ht[ti % 4], ht_b[ti % 4]
            hT_, hT_bb = hT[ti % 4], hT_b[ti % 4]
            transpose_tile_f32(k, h_, h_b, tp, tp_b, hT_, hT_bb)
            for c in range(8):
                s.op("pe", lambda E: E.matmul(pl[:], lhsT=hT_[:, c, :], rhs=wr[:, c, :], start=(c == 0), stop=(c == 7)),
                     [hT_bb, wr_b], [pl_b], track=(c == 7))
            lg, lgs, lg_b = lg2[ti % 2], lgs2[ti % 2], lg2_b[ti % 2]
            s.op("act", lambda E: E.activation(out=lg[:], in_=pl[:], func=AF.Exp, accum_out=lgs[:, 0:1]), [pl_b], [lg_b])
            s.op("dve", lambda E: E.reciprocal(out=lgs[:, 1:2], in_=lgs[:, 0:1]), [lg_b], [lg_b])
            s.op("dve", lambda E: E.tensor_scalar(out=aff[:, t, :], in0=lg[:], scalar1=lgs[:, 1:2], scalar2=None, op0=ALU.mult), [lg_b], [aff_b])

        nt_ = len(tiles)
        for i in range(nt_ + 2):
            if i < nt_:
                S1(i)
            if 0 <= i - 1 < nt_:
                S2(i - 1)
            if 0 <= i - 2 < nt_:
                S3(i - 2)
        s.barrier()
    if k.stop == "postA%d" % l:
        es2.close()
        raise _Stop()
    routing(k, l, keep_ctx, aff, aff_b, posi, posi_b)
    if k.stop == "route%d" % l:
        es2.close()
        raise _Stop()
    experts(k, l, keep_ctx, G2b, G2b_b)
    es2.close()
    s.barrier()


def transpose_tile_f32(k, ht, ht_b, tp, tp_b, hT, hT_b):
    s = k.s
    for half in range(2):
        p, pb = tp[half], tp_b[half]
        for j in range(4):
            c = half * 4 + j
            s.op("pe", lambda E, c=c, j=j, p=p: E.transpose(out=p[:, j * 128:(j + 1) * 128], in_=ht[:, c * 128:(c + 1) * 128], identity=k.ident[:]),
                 [ht_b, k.ident_b], [pb], track=(j == 3))
        if half == 0:
            s.op("act", lambda E, p=p: E.activation(out=hT[:, 0:4, :], in_=p[:].rearrange("p (c t) -> p c t", c=4), func=AF.Copy), [pb], [hT_b])
        else:
            s.op("dve", lambda E, p=p: E.tensor_copy(out=hT[:, 4:8, :], in_=p[:].rearrange("p (c t) -> p c t", c=4)), [pb], [hT_b])


def routing(k, l, keep_ctx, aff, aff_b, posi, posi_b):
    nc, s, I = k.nc, k.s, k.I
    BIG = float(2 ** 20)
    with ExitStack() as st:
        affT = k.sb("g_affT", [NEXP, NT], stack=st)
        affT_b = Buf()
        msk = k.sb("g_msk", [NEXP, NT], stack=st)
        msk_b = Buf()
        cum = k.sb("g_cum", [NEXP, NT], stack=st)
        cum_b = Buf()
        onesr = k.sb("g_ones", [NEXP, NL], stack=st)
        onesr_b = Buf()
        sv = k.sb("g_sv", [NEXP, 8], stack=st)
        sv_b = Buf()
        posf = k.sb("g_posf", [128, NTILE, NEXP], stack=st)
        posf_b = Buf()
        metas = k.sb("g_metas", [128, NTILE, NEXP, 2], stack=st)
        metas_b = Buf()
        tp = [k.ps("g_tp%d" % i, [128, 512], stack=st) for i in range(2)]
        tp_b = bufs(2)
        s.op("pool", lambda E: E.memset(onesr[:], 1.0), [], [onesr_b])
        ebase = k.sb("g_ebase", [NEXP, 2], stack=st)
        ebase_b = Buf()
        s.dma("sp", lambda E: E.dma_start(out=ebase[:], in_=I["ebase"][:, :]), [], [ebase_b])
        for e in range(NEXP):
            s.dma("sp", lambda E, e=e: E.dma_start(out=k.meta[e * SLOTS:(e + 1) * SLOTS, :], in_=I["metainit"][e * SLOTS:(e + 1) * SLOTS, :]), [], [k.meta_b[e]])
        ntile = NTILE if keep_ctx else 64
        for t4 in range(0, ntile, 4):
            p, pb = tp[(t4 // 4) % 2], tp_b[(t4 // 4) % 2]
            n = min(4, ntile - t4)
            for j in range(n):
                s.op("pe", lambda E, t4=t4, j=j, p=p: E.transpose(out=p[0:NEXP, j * 128:(j + 1) * 128], in_=aff[:, t4 + j, :], identity=k.ident[:]),
                     [aff_b, k.ident_b], [pb], track=(j == n - 1))
            s.op("act", lambda E, t4=t4, n=n, p=p: E.activation(out=affT[:, t4 * 128:(t4 + n) * 128], in_=p[0:NEXP, 0:n * 128], func=AF.Copy), [pb], [affT_b])
        segs = [(0, NL, CAP_L, 0, 0)]
        if keep_ctx:
            segs.append((NL, NT, CAP_C, 4, CAP_L))
        for (a, b, cap, so, base) in segs:
            lo, mid, cntv, stp = (sv[:, so + i:so + i + 1] for i in range(4))
            s.op("dve", lambda E, lo=lo: E.memset(lo, 0.0), [], [sv_b])
            for it in range(30):
                wstep = float(2.0 ** -(it + 1))
                s.op("dve", lambda E, lo=lo, mid=mid, wstep=wstep: E.tensor_scalar(out=mid, in0=lo, scalar1=wstep, scalar2=None, op0=ALU.add), [sv_b], [sv_b])
                s.op("dve", lambda E, mid=mid, cntv=cntv, a=a, b=b: E.tensor_scalar(out=msk[:, a:b], in0=affT[:, a:b], scalar1=mid, scalar2=0.0, op0=ALU.is_ge, op1=ALU.add, accum_out=cntv),
                     [affT_b, sv_b], [msk_b, sv_b])
                s.op("dve", lambda E, cntv=cntv, stp=stp, cap=cap, wstep=wstep: E.tensor_scalar(out=stp, in0=cntv, scalar1=float(cap), scalar2=wstep, op0=ALU.is_ge, op1=ALU.mult), [sv_b], [sv_b])
                s.op("dve", lambda E, lo=lo, stp=stp: E.tensor_tensor(out=lo, in0=lo, in1=stp, op=ALU.add), [sv_b], [sv_b])
            s.op("dve", lambda E, lo=lo, a=a, b=b: E.tensor_scalar(out=msk[:, a:b], in0=affT[:, a:b], scalar1=lo, scalar2=None, op0=ALU.is_ge), [affT_b, sv_b, msk_b], [msk_b])
            s.op("dve", lambda E, a=a, b=b: E.tensor_tensor_scan(out=cum[:, a:b], data0=onesr[:, 0:b - a], data1=msk[:, a:b], initial=0.0, op0=ALU.mult, op1=ALU.add),
                 [msk_b, onesr_b], [cum_b])
            col = 0 if base == 0 else 1
            s.op("dve", lambda E, a=a, b=b, cap=cap: E.scalar_tensor_tensor(out=msk[:, a:b], in0=cum[:, a:b], scalar=float(cap), in1=msk[:, a:b], op0=ALU.is_le, op1=ALU.mult), [msk_b, cum_b], [msk_b])
            s.op("dve", lambda E, a=a, b=b, col=col: E.scalar_tensor_tensor(out=cum[:, a:b], in0=cum[:, a:b], scalar=ebase[:, col:col + 1], in1=msk[:, a:b], op0=ALU.add, op1=ALU.mult), [msk_b, cum_b, ebase_b], [cum_b])
            s.op("dve", lambda E, a=a, b=b: E.tensor_scalar(out=cum[:, a:b], in0=cum[:, a:b], scalar1=BIG, scalar2=None, op0=ALU.add), [cum_b], [cum_b])
        for t4 in range(0, ntile, 4):
            p, pb = tp[(t4 // 4) % 2], tp_b[(t4 // 4) % 2]
            n = min(4, ntile - t4)
            for j in range(n):
                s.op("pe", lambda E, t4=t4, j=j, p=p: E.transpose(out=p[:, j * NEXP:(j + 1) * NEXP], in_=cum[:, (t4 + j) * 128:(t4 + j + 1) * 128], identity=k.ident[0:NEXP, 0:NEXP]),
                     [cum_b, k.ident_b], [pb], track=(j == n - 1))
            s.op("act", lambda E, t4=t4, n=n, p=p: E.activation(out=posf[:, t4:t4 + n, :], in_=p[:, 0:n * NEXP].rearrange("p (t e) -> p t e", t=n), func=AF.Copy), [pb], [posf_b])
        s.op("dve", lambda E: E.tensor_copy(out=posi[:, 0:ntile, :], in_=posf[:, 0:ntile, :]), [posf_b], [posi_b])
        s.op("pool", lambda E: E.tensor_copy(out=metas[:, 0:ntile, :, 0], in_=k.tokid[:, 0:ntile].unsqueeze(2).broadcast_to([128, ntile, NEXP])), [k.tokid_b], [metas_b])
        s.op("pool", lambda E: E.tensor_copy(out=metas[:, 0:ntile, :, 1], in_=aff[:, 0:ntile, :]), [aff_b, metas_b], [metas_b])
        regs = {}

        def mkregs(E):
            regs["l"] = E.alloc_register("bnd_l%d" % l)
            E.reg_mov(regs["l"], NEXP * SLOTS - 1)
        s.raw("pool", mkregs)
        for t in range(ntile):
            rk = "l"
            for e in range(NEXP):
                s.dma("pool", lambda E, t=t, e=e, rk=rk: E.indirect_dma_start(
                    out=k.meta[:, :], out_offset=bass.IndirectOffsetOnAxis(ap=posi[:, t, e:e + 1], axis=0),
                    in_=metas[:, t, e, :], in_offset=None, bounds_check=Lazy(lambda: regs["l"]), oob_is_err=False),
                    [posi_b, metas_b], [k.meta_b[e]])

        def freeregs(E):
            E.free_register(regs["l"])
        s.raw("pool", freeregs)
        s.barrier()


def experts(k, l, keep_ctx, G2b, G2b_b):
    nc, s, I = k.nc, k.s, k.I
    nst = 9 if keep_ctx else 8
    nsl = nst * 128
    groups = [(0, 512), (512, 512)] + ([(1024, 128)] if keep_ctx else [])
    with ExitStack() as st:
        mt = [k.sb("e_mt%d" % i, [128, 9, 2], stack=st) for i in range(2)]
        mt_b = bufs(2)
        idx = [k.sb("e_idx%d" % i, [128, 9], I32, stack=st) for i in range(2)]
        idx_b = bufs(2)
        xs = [k.sb("e_xs%d" % i, [128, D], BF16, stack=st) for i in range(9)]
        xs_b = bufs(9)
        xsT2 = [k.sb("e_xsT%d" % i, [128, 8, SLOTS], BF16, stack=st) for i in range(2)]
        xsT2_b = bufs(2)
        aT = k.sb("e_aT", [128, 16, SLOTS], BF16, stack=st)
        aT_b = Buf()
        wstg = [k.sb("e_ws%d" % i, [128, 8, 512], stack=st) for i in range(2)]
        wstg_b = bufs(2)
        wbf = [k.sb("e_wb%d" % i, [128, 8, 512], BF16, stack=st) for i in range(6)]
        wbf_b = bufs(6)
        sg = [k.sb("e_sg%d" % i, [128, 512], stack=st) for i in range(2)]
        sg_b = bufs(2)
        yo = [k.sb("e_yo%d" % i, [128, D], stack=st) for i in range(2)]
        yo_b = bufs(2)
        tpb = [k.ps("e_tp%d" % i, [128, 512], BF16, stack=st) for i in range(2)]
        tpb_b = bufs(2)
        pg = [k.ps("e_pg%d" % i, [128, 512], stack=st) for i in range(2)]
        pg_b = bufs(2)
        pu = [k.ps("e_pu%d" % i, [128, 512], stack=st) for i in range(2)]
        pu_b = bufs(2)
        pyy = [k.ps("e_py%d" % i, [128, 512], stack=st) for i in range(2)]
        pyy_b = bufs(2)
        regs = {}

        def mkregs(E):
            regs["b"] = E.alloc_register("bnd_x%d" % l)
            E.reg_mov(regs["b"], NT)
        s.raw("pool", mkregs)
        wi = [0]
        ci = [0]

        def load_w(src_ap, src_b, kind):
            i = wi[0]
            wi[0] += 1
            stg, stg_b = wstg[i % 2], wstg_b[i % 2]
            wb, wb_b = wbf[i % 6], wbf_b[i % 6]
            if kind == "col":
                s.dma("sp", lambda E: E.dma_start(out=stg[:], in_=src_ap.rearrange("(c p) n -> p c n", p=128)), [src_b], [stg_b])
            else:
                s.dma("sp", lambda E: E.dma_start(out=stg[:].rearrange("p a b -> p (a b)").rearrange("p (c n) -> p c n", c=4), in_=src_ap.rearrange("(c p) n -> p c n", p=128)), [src_b], [stg_b])
            eng = ["pool", "dve", "act"][ci[0] % 3]
            ci[0] += 1
            if eng == "act":
                s.op("act", lambda E: E.activation(out=wb[:], in_=stg[:], func=AF.Copy), [stg_b], [wb_b])
            else:
                s.op(eng, lambda E: E.tensor_copy(out=wb[:], in_=stg[:]), [stg_b], [wb_b])
            return wb, wb_b

        xi = 0
        gi = 0
        yi = 0
        est = {}

        def gather(e):
            m_, m_b = mt[e % 2], mt_b[e % 2]
            ix, ix_b = idx[e % 2], idx_b[e % 2]
            xsT, xsT_b = xsT2[e % 2], xsT2_b[e % 2]
            s.dma("sp", lambda E: E.dma_start(out=m_[:, 0:nst, :], in_=k.meta[e * SLOTS:e * SLOTS + nsl, :].rearrange("(t p) c -> p t c", p=128)), [k.meta_b[e]], [m_b])
            s.op("dve", lambda E: E.tensor_copy(out=ix[:, 0:nst], in_=m_[:, 0:nst, 0]), [m_b], [ix_b])
            for stl in range(nst):
                x_, x_b = xs[stl], xs_b[stl]
                s.dma("pool", lambda E: E.indirect_dma_start(
                    out=x_[:, :], out_offset=None, in_=k.h2tab[:, :], in_offset=bass.IndirectOffsetOnAxis(ap=ix[:, stl:stl + 1], axis=0),
                    bounds_check=Lazy(lambda: regs["b"]), oob_is_err=False), list(k.h2tab_b) + [ix_b], [x_b])
            est[e] = (m_, m_b, ix, ix_b, xsT, xsT_b)

        def transposes(e):
            nonlocal xi
            (m_, m_b, ix, ix_b, xsT, xsT_b) = est[e]
            for stl in range(nst):
                x_, x_b = xs[stl], xs_b[stl]
                p, pb = tpb[xi % 2], tpb_b[xi % 2]
                xi += 1
                for half in range(2):
                    for j in range(4):
                        c = half * 4 + j
                        s.op("pe", lambda E: E.transpose(out=p[:, j * 128:(j + 1) * 128], in_=x_[:, c * 128:(c + 1) * 128], identity=k.identb[:]),
                             [x_b, k.identb_b], [pb], track=(j == 3))
                    if half == 0:
                        s.op("act", lambda E: E.activation(out=xsT[:, 0:4, stl * 128:(stl + 1) * 128], in_=p[:].rearrange("p (c t) -> p c t", c=4), func=AF.Copy), [pb], [xsT_b])
                    else:
                        s.op("dve", lambda E: E.tensor_copy(out=xsT[:, 4:8, stl * 128:(stl + 1) * 128], in_=p[:].rearrange("p (c t) -> p c t", c=4)), [pb], [xsT_b])

        def compute(e):
            nonlocal gi, yi
            if e + 1 < NEXP:
                gather(e + 1)
            (m_, m_b, ix, ix_b, xsT, xsT_b) = est[e]
            for fb in range(4):
                wg, wg_b = load_w(I["moe_wg%d" % l][e * D:(e + 1) * D, fb * 512:(fb + 1) * 512], k.IB["moe_wg%d" % l], "col")
                wu, wu_b = load_w(I["moe_wu%d" % l][e * D:(e + 1) * D, fb * 512:(fb + 1) * 512], k.IB["moe_wu%d" % l], "col")
                for f4 in range(4):
                    f = fb * 4 + f4
                    for (s0, sw) in groups:
                        g_, g_b = pg[gi % 2], pg_b[gi % 2]
                        u_, u_b = pu[gi % 2], pu_b[gi % 2]
                        sg_, sg_bb = sg[gi % 2], sg_b[gi % 2]
                        gi += 1
                        for c in range(8):
                            s.op("pe", lambda E, c=c, f4=f4, g_=g_, wg=wg, s0=s0, sw=sw: E.matmul(g_[:, 0:sw], lhsT=wg[:, c, f4 * 128:(f4 + 1) * 128], rhs=xsT[:, c, s0:s0 + sw], start=(c == 0), stop=(c == 7)),
                                 [wg_b, xsT_b], [g_b], track=(c == 7))
                        for c in range(8):
                            s.op("pe", lambda E, c=c, f4=f4, u_=u_, wu=wu, s0=s0, sw=sw: E.matmul(u_[:, 0:sw], lhsT=wu[:, c, f4 * 128:(f4 + 1) * 128], rhs=xsT[:, c, s0:s0 + sw], start=(c == 0), stop=(c == 7)),
                                 [wu_b, xsT_b], [u_b], track=(c == 7))
                        s.op("act", lambda E, g_=g_, sg_=sg_, sw=sw: E.activation(out=sg_[:, 0:sw], in_=g_[:, 0:sw], func=AF.Silu), [g_b], [sg_bb])
                        s.op("dve", lambda E, u_=u_, sg_=sg_, f=f, s0=s0, sw=sw: E.tensor_tensor(out=aT[:, f, s0:s0 + sw], in0=u_[:, 0:sw], in1=sg_[:, 0:sw], op=ALU.mult), [u_b, sg_bb], [aT_b])
            if e + 1 < NEXP:
                transposes(e + 1)
            wds = []
            for rb in range(4):
                wds.append(load_w(I["moe_wd%d" % l][e * EDIM + rb * 512:e * EDIM + (rb + 1) * 512, :], k.IB["moe_wd%d" % l], "row"))
            for stl in range(nst):
                cond = 0 if stl < 8 else 1
                y_, y_b = yo[yi % 2], yo_b[yi % 2]
                yi += 1
                for nb in range(2):
                    p, p_b = pyy[nb], pyy_b[nb]
                    for f in range(16):
                        wd, wd_b = wds[f // 4]
                        wdv = wd[:].rearrange("p a b -> p (a b)").rearrange("p (c n) -> p c n", c=4)
                        s.op("pe", lambda E, f=f, nb=nb, p=p, wdv=wdv, stl=stl: E.matmul(p[:], lhsT=aT[:, f, stl * 128:(stl + 1) * 128], rhs=wdv[:, f % 4, nb * 512:(nb + 1) * 512], start=(f == 0), stop=(f == 15)),
                             [aT_b, wd_b], [p_b], track=(f == 15))
                    s.op("dve", lambda E, nb=nb, p=p, y_=y_, m_=m_, stl=stl, cond=cond: E.scalar_tensor_tensor(out=y_[:, nb * 512:(nb + 1) * 512], in0=p[:], scalar=m_[:, stl, 1:2], in1=G2b[cond][:, nb * 512:(nb + 1) * 512], op0=ALU.mult, op1=ALU.mult),
                         [p_b, m_b, G2b_b[cond]], [y_b])
                s.dma("pool", lambda E, y_=y_, ix=ix, stl=stl: E.indirect_dma_start(
                    out=k.xres[:, :], out_offset=bass.IndirectOffsetOnAxis(ap=ix[:, stl:stl + 1], axis=0), in_=y_[:, :], in_offset=None,
                    bounds_check=Lazy(lambda: regs["b"]), oob_is_err=True, compute_op=ALU.add), [y_b, ix_b], list(k.xres_b))


        gather(0)
        transposes(0)
        for e in range(NEXP):
            compute(e)

        def freeregs(E):
            E.free_register(regs["b"])
        s.raw("pool", freeregs)
        s.barrier()


def final_norm(k):
    nc, s, I = k.nc, k.s, k.I
    with ExitStack() as st:
        nf = k.sb("f_nf", [128, D], stack=st)
        nf_b = Buf()
        s.dma("sp", lambda E: E.dma_start(out=nf[:], in_=I["norm_final"].unsqueeze(0).broadcast_to([128, D])), [], [nf_b])
        xt = [k.sb("f_xt%d" % i, [128, D], stack=st) for i in range(2)]
        xt_b = bufs(2)
        ot = [k.sb("f_ot%d" % i, [128, D], stack=st) for i in range(2)]
        ot_b = bufs(2)
        junk = k.sb("f_junk", [128, D], stack=st)
        stat = [k.sb("f_stat%d" % i, [128, 2], stack=st) for i in range(2)]
        scr_b = bufs(2)
        k.out_b = Buf()
        for t in range(64):
            x_, x_b = xt[t % 2], xt_b[t % 2]
            o_, o_b = ot[t % 2], ot_b[t % 2]
            sc = stat[t % 2]
            sb_ = scr_b[t % 2]
            s.dma("sp", lambda E, t=t, x_=x_: E.dma_start(out=x_[:], in_=k.xres[t * 128:(t + 1) * 128, :]), [k.xres_b[t]], [x_b])
            s.op("act", lambda E, x_=x_, sc=sc: E.activation(out=junk[:], in_=x_[:], func=AF.Square, accum_out=sc[:, 0:1]), [x_b], [sb_])
            s.op("act", lambda E, sc=sc: E.activation(out=sc[:, 1:2], in_=sc[:, 0:1], func=AF.Ln, scale=float(1.0 / D), bias=k.epsc[:, 0:1]), [sb_, k.epsc_b], [sb_])
            s.op("act", lambda E, sc=sc: E.activation(out=sc[:, 1:2], in_=sc[:, 1:2], func=AF.Exp, scale=-0.5), [sb_], [sb_])
            s.op("dve", lambda E, x_=x_, o_=o_, sc=sc: E.scalar_tensor_tensor(out=o_[:], in0=x_[:], scalar=sc[:, 1:2], in1=nf[:], op0=ALU.mult, op1=ALU.mult), [x_b, sb_, nf_b], [o_b])
            s.dma("pool", lambda E, t=t, o_=o_: E.dma_start(out=k.out[t * 128:(t + 1) * 128, :], in_=o_[:]), [o_b], [k.out_b])


def _rope_tables_T(rot_dim, reps_rows):
    t = np.arange(NL)
    rows = (t // GRID_W).astype(np.float32)
    cols = (t % GRID_W).astype(np.float32)
    n_freq = rot_dim // 4
    inv_freq = (np.float32(10000.0) ** (-np.arange(n_freq, dtype=np.float32) / np.float32(n_freq))).astype(np.float32)
    ang = np.concatenate([rows[:, None] * inv_freq, cols[:, None] * inv_freq], axis=-1).astype(np.float32)
    cos = np.cos(ang).astype(np.float32)
    sin = np.sin(ang).astype(np.float32)
    half = rot_dim // 2
    cT = np.ones((rot_dim, NT), np.float32)
    sT = np.zeros((rot_dim, NT), np.float32)
    cT[:half, :NL] = cos.T
    cT[half:, :NL] = cos.T
    sT[:half, :NL] = -sin.T
    sT[half:, :NL] = sin.T
    return cT, sT


def _swap_halves_cols(w, unit):
    din, dout = w.shape
    w4 = w.reshape(din, dout // unit, 2, unit // 2)
    return np.ascontiguousarray(w4[:, :, ::-1, :]).reshape(din, dout)


def _na_bias(rpb):
    NEG = np.float32(-30000.0)
    qc = np.arange(64)
    cs = np.clip(qc - 8, 0, 48)
    kc = np.arange(64)
    valid = (kc[:, None] >= cs[None, :]) & (kc[:, None] < cs[None, :] + 16)
    colidx = np.clip(kc[:, None] - qc[None, :] + 15, 0, 30)
    out = np.full((16, 128, 8, 4, 64), NEG, np.float32)
    for v in range(8):
        for kt in range(4):
            for half in range(2):
                kr = 2 * kt + half
                ridx = kr - v + 7
                vals = rpb[:, ridx][:, colidx]
                out[:, half * 64:(half + 1) * 64, v, kt, :] = np.where(valid[None], vals, NEG)
    return out


def _shard(a2d, r):
    n = a2d.shape[0] // 8
    return a2d[r * n:(r + 1) * n]


def make_shared(inp):
    f = lambda a: np.ascontiguousarray(np.asarray(a, dtype=np.float32))
    S = {}
    S["ada_b"] = f(inp["ada_b"])
    S["norm_mix"] = f(inp["norm_mix"])
    S["norm_ffn"] = f(inp["norm_ffn"])
    S["norm_final"] = f(inp["norm_final"])
    S["ident"] = np.eye(128, dtype=np.float32)
    S["tokid"] = f((np.arange(NTILE)[None, :] * 128 + np.arange(128)[:, None]))
    mi = np.zeros((NEXP * SLOTS, 2), np.float32)
    mi[:, 0] = DUMMY
    S["metainit"] = mi
    BIG = float(2 ** 20)
    eb = np.zeros((NEXP, 2), np.float32)
    eb[:, 0] = np.arange(NEXP) * SLOTS - 1 - BIG
    eb[:, 1] = np.arange(NEXP) * SLOTS + CAP_L - 1 - BIG
    S["ebase"] = eb
    S["moe_wr"] = f(inp["moe_w_router"])
    S["da_lam"] = f(np.stack([inp["da_lambda_q1"][0], inp["da_lambda_k1"][0], inp["da_lambda_q2"][0], inp["da_lambda_k2"][0]]))
    S["da_subln"] = f(np.asarray(inp["da_subln"][0]).reshape(128, 1))
    G = {}
    for l in range(4):
        G["ada_w%d" % l] = f(inp["ada_w"][l])
        G["moe_wg%d" % l] = f(inp["moe_w_gate"][l]).reshape(NEXP * D, EDIM)
        G["moe_wu%d" % l] = f(inp["moe_w_up"][l]).reshape(NEXP * D, EDIM)
        G["moe_wd%d" % l] = f(inp["moe_w_down"][l]).reshape(NEXP * EDIM, D)
    G["da_w"] = f(inp["da_w_qkv"][0])
    G["da_wo"] = f(inp["da_w_o"][0])
    G["fn_wo"] = f(inp["fn_w_o"][0])
    n2 = np.arange(128, dtype=np.float64)[:, None, None]
    k1 = np.arange(64, dtype=np.float64)[None, :, None]
    k2 = np.arange(128, dtype=np.float64)[None, None, :]
    th = 2.0 * np.pi * n2 * (k1 + 64.0 * k2) / 8192.0
    G["fn_cos"] = f(np.cos(th).reshape(128, 8192))
    G["fn_sin"] = f(np.sin(th).reshape(128, 8192))
    n1 = np.arange(64, dtype=np.float64)[:, None]
    kk = np.arange(64, dtype=np.float64)[None, :]
    c64 = np.cos(2.0 * np.pi * n1 * kk / 64.0)
    s64 = np.sin(2.0 * np.pi * n1 * kk / 64.0)
    f64t = np.zeros((64, 4, 48))
    for kb in range(4):
        f64t[:, kb, 0:16] = c64[:, kb * 16:(kb + 1) * 16]
        f64t[:, kb, 16:32] = -s64[:, kb * 16:(kb + 1) * 16]
        f64t[:, kb, 32:48] = -c64[:, kb * 16:(kb + 1) * 16]
    S["fn_f64"] = f(f64t.reshape(64, 192))
    cc_ = np.arange(256, dtype=np.float64)
    th2 = 2.0 * np.pi * cc_[:, None] * cc_[None, :] / 256.0
    S["fn_cc"] = f(np.cos(th2))
    S["fn_sc"] = f(np.sin(th2))
    S["fn_nsc"] = f(-np.sin(th2))
    G["na_w"] = f(inp["na_w_qkv"][0])
    G["na_wo"] = f(inp["na_w_o"][0])
    G["na_bias"] = _na_bias(np.asarray(inp["na_rpb"][0], np.float32)).reshape(NEXP * 128, 2048)
    S["mla_qnorm"] = f(inp["mla_q_norm"][0])
    S["mla_kvnorm"] = f(inp["mla_kv_norm"][0])
    G["mla_wdq"] = f(inp["mla_w_dq"][0])
    G["mla_wuq"] = f(inp["mla_w_uq"][0])
    G["mla_wdkv"] = f(inp["mla_w_dkv"][0])
    G["mla_wuk"] = f(inp["mla_w_uk"][0])
    G["mla_wuv"] = f(inp["mla_w_uv"][0])
    G["mla_wo"] = f(inp["mla_w_o"][0])
    c3, s3 = _rope_tables_T(32, None)
    G["rope3c"] = f(np.concatenate([np.ones((64, NT), np.float32), c3], axis=0))
    G["rope3s"] = f(np.concatenate([np.zeros((64, NT), np.float32), s3], axis=0))
    c0, s0 = _rope_tables_T(64, None)
    G["rope0c"] = f(np.concatenate([c0, c0], axis=0))
    G["rope0s"] = f(np.concatenate([s0, s0], axis=0))
    return S, G


def make_core_inputs(inp, S, G, core):
    b = core // 2
    m = dict(S)
    m["xin"] = np.ascontiguousarray(np.concatenate([np.asarray(inp["x"][b], np.float32), np.asarray(inp["ctx"][b], np.float32)], axis=0))
    cc = np.stack([np.asarray(inp["c"][b], np.float32), np.asarray(inp["c_ctx"], np.float32)], axis=-1)
    m["ccT"] = np.ascontiguousarray(cc.reshape(8, 128, 2).transpose(1, 0, 2))
    for name, a in G.items():
        m[name] = _shard(a, core)
    return m


_NC_CACHE = {}


def kernel(**inputs):
    if "nc" not in _NC_CACHE:
        _NC_CACHE["nc"] = build(n_layers=4, dbg=False, gather=False)
    nc = _NC_CACHE["nc"]
    S, G = make_shared(inputs)
    names = set(LAST_INPUT_NAMES)
    in_maps = []
    for b in range(4):
        m = make_core_inputs(inputs, S, G, 2 * b)
        m.update(G)
        in_maps.append({n: v for n, v in m.items() if n in names})
    res = run_bass_kernel_spmd(nc, in_maps, core_ids=list(range(4)))
    out = np.stack([np.asarray(res.results[b]["out"], dtype=np.float32) for b in range(4)], axis=0)
    return out
```

```python
import math
from contextlib import ExitStack
import numpy as np
import concourse.bass as bass
import concourse.mybir as mybir
from concourse.bass_utils import run_bass_kernel_spmd

F32 = mybir.dt.float32
BF16 = mybir.dt.bfloat16
I32 = mybir.dt.int32
AF = mybir.ActivationFunctionType
ALU = mybir.AluOpType
AX = mybir.AxisListType

D = 1024
NL = 8192
NC_ = 256
NT = NL + NC_
NTILE = NT // 128
DUMMY = NT
EPS = 1e-6
NEXP = 16
EDIM = 2048
CAP_L = 1024
CAP_C = 32
SLOTS = 1152
GRID_W = 64

ENGS = ["pe", "act", "dve", "pool", "sp"]


class Buf:
    __slots__ = ("w", "r")

    def __init__(self):
        self.w = None
        self.r = {}


def bufs(n):
    return [Buf() for _ in range(n)]


class Lazy:
    def __init__(self, f):
        self.f = f


class _Rec:
    def __init__(self):
        self.calls = []

    def __getattr__(self, name):
        def f(*args, **kw):
            self.calls.append((name, args, kw))
            return self
        return f


def _replay(E, call):
    name, args, kw = call
    args = [a.f() if isinstance(a, Lazy) else a for a in args]
    kw = {k_: (v.f() if isinstance(v, Lazy) else v) for k_, v in kw.items()}
    return getattr(E, name)(*args, **kw)


def _record(fn):
    r = _Rec()
    fn(r)
    assert len(r.calls) == 1, r.calls
    return r.calls[0]


class Sched:
    def __init__(self, nc, es, n_dma=None):
        self.nc = nc
        self.es = es
        self.ckeys = []
        self.prog = {e: [] for e in ENGS}
        self.cnt = {e: 0 for e in ENGS}
        self.waited = {e: {} for e in ENGS}
        self.sem = {}
        for e in ["pe", "act", "dve", "pool"]:
            self.sem[e] = es.enter_context(nc.semaphore("s_" + e))
        n_dma = n_dma or {"sp": 16, "pool": 16, "act": 4}
        self.dkeys = {}
        self.dnext = {}
        self.dval = {}
        for q, n in n_dma.items():
            ks = []
            for i in range(n):
                k = "d_%s_%d" % (q, i)
                self.sem[k] = es.enter_context(nc.semaphore(k))
                self.dval[k] = 0
                ks.append(k)
            self.dkeys[q] = ks
            self.dnext[q] = 0

    def _deps(self, reads, writes):
        deps = {}

        def add(k, v):
            if deps.get(k, 0) < v:
                deps[k] = v
        for b in reads:
            if b.w is not None:
                add(*b.w)
        for b in writes:
            if b.w is not None:
                add(*b.w)
            for k, v in b.r.items():
                add(k, v)
        return deps

    def _waits(self, eng, deps):
        for k, v in deps.items():
            if eng == "pe" and k == "pe":
                continue
            if self.waited[eng].get(k, 0) >= v:
                continue
            self.waited[eng][k] = v
            sem = self.sem[k]
            self.prog[eng].append(lambda E, sem=sem, v=v: E.wait_ge(sem, v))

    def _mark(self, ev, reads, writes):
        k, v = ev
        for b in reads:
            if b.r.get(k, 0) < v:
                b.r[k] = v
        for b in writes:
            b.w = ev
            b.r = {}

    def op(self, eng, fn, reads=(), writes=(), track=True):
        self._waits(eng, self._deps(reads, writes))
        if track:
            self.cnt[eng] += 1
            v = self.cnt[eng]
            sem = self.sem[eng]
            call = _record(fn)
            self.prog[eng].append(lambda E, call=call, sem=sem: _replay(E, call).then_inc(sem, 1))
        else:
            v = self.cnt[eng] + 1
            call = _record(fn)
            self.prog[eng].append(lambda E, call=call: _replay(E, call))
        self._mark((eng, v), reads, writes)

    def dma(self, q, fn, reads=(), writes=()):
        ks = self.dkeys[q]
        key = ks[self.dnext[q] % len(ks)]
        self.dnext[q] += 1
        deps = self._deps(reads, writes)
        if self.dval[key] > 0 and deps.get(key, 0) < self.dval[key]:
            deps[key] = self.dval[key]
        self._waits(q, deps)
        self.dval[key] += 16
        v = self.dval[key]
        sem = self.sem[key]
        call = _record(fn)
        self.prog[q].append(lambda E, call=call, sem=sem: _replay(E, call).then_inc(sem, 16))
        self._mark((key, v), reads, writes)

    def coll(self, fn, reads=(), writes=()):
        key = "cc_%d" % len(self.ckeys)
        self.ckeys.append(key)
        self.sem[key] = self.es.enter_context(self.nc.semaphore(key))
        self._waits("pool", self._deps(reads, writes))
        sem = self.sem[key]
        call = _record(fn)
        self.prog["pool"].append(lambda E, call=call, sem=sem: _replay(E, call).then_inc(sem, 1))
        self.dval[key] = 1
        self._mark((key, 1), reads, writes)

    def raw(self, eng, fn):
        self.prog[eng].append(lambda E, fn=fn: fn(E))

    def barrier(self):
        allev = {}
        for e in ["pe", "act", "dve", "pool"]:
            if self.cnt[e] > 0:
                allev[e] = self.cnt[e]
        for k, v in self.dval.items():
            if v > 0:
                allev[k] = v
        for e in ENGS:
            d = dict(allev)
            self._waits_all(e, d)

    def _waits_all(self, eng, deps):
        for k, v in deps.items():
            if self.waited[eng].get(k, 0) >= v:
                continue
            self.waited[eng][k] = v
            sem = self.sem[k]
            self.prog[eng].append(lambda E, sem=sem, v=v: E.wait_ge(sem, v))

    def emit(self):
        nc = self.nc
        with nc.Block() as block:
            @block.tensor
            def _(E):
                for f in self.prog["pe"]:
                    f(E)

            @block.scalar
            def _(E):
                for f in self.prog["act"]:
                    f(E)

            @block.vector
            def _(E):
                for f in self.prog["dve"]:
                    f(E)

            @block.gpsimd
            def _(E):
                for f in self.prog["pool"]:
                    f(E)

            @block.sync
            def _(E):
                for f in self.prog["sp"]:
                    f(E)


class K:
    pass


LAST_INPUT_NAMES = []


class _Stop(Exception):
    pass


def build(n_layers=4, dbg=False, stop=None, gather=True):
    nc = bass.Bass("TRN2", target_bir_lowering=False)
    del LAST_INPUT_NAMES[:]
    es = ExitStack()
    k = K()
    k.nc = nc
    k.dbg = dbg
    k.uid = 0
    k.stop = stop

    def chk(name):
        if stop == name:
            raise _Stop()
    k.chk = chk
    s = Sched(nc, es)
    k.s = s

    def din(name, shape, dt=F32):
        LAST_INPUT_NAMES.append(name)
        return nc.dram_tensor(name, list(shape), dt, kind="ExternalInput").ap()

    def dscr(name, shape, dt=F32):
        return nc.dram_tensor(name, list(shape), dt).ap()

    k.din = din
    k.dscr = dscr
    I = {}
    IB = {}

    def gin(name, rows, cols):
        assert rows % 8 == 0
        if not gather:
            I[name] = din(name, [rows, cols])
            IB[name] = Buf()
            return
        ext = din(name, [rows // 8, cols])
        loc = dscr(name + "_l", [rows // 8, cols])
        full = dscr(name + "_g", [rows, cols])
        lb, fb = Buf(), Buf()
        s.dma("pool", lambda E: E.dma_start(out=loc[:, :], in_=ext[:, :]), [], [lb])
        s.coll(lambda E: E.collective_compute("AllGather", ALU.bypass, replica_groups=[list(range(8))], ins=[loc[:, :]], outs=[full[:, :]]), [lb], [fb])
        I[name] = full
        IB[name] = fb

    k.gin = gin
    I["xin"] = din("xin", [NT, D])
    I["ccT"] = din("ccT", [128, 8, 2])
    I["ada_b"] = din("ada_b", [4, 6 * D])
    I["norm_mix"] = din("norm_mix", [4, D])
    I["norm_ffn"] = din("norm_ffn", [4, D])
    I["norm_final"] = din("norm_final", [D])
    I["ident"] = din("ident", [128, 128])
    I["tokid"] = din("tokid", [128, NTILE])
    I["metainit"] = din("metainit", [NEXP * SLOTS, 2])
    I["ebase"] = din("ebase", [NEXP, 2])
    I["moe_wr"] = din("moe_wr", [4, D, NEXP])
    I["da_lam"] = din("da_lam", [4, 64])
    I["da_subln"] = din("da_subln", [128, 1])
    for l in range(4):
        gin("ada_w%d" % l, D, 6 * D)
    gin("da_w", D, 3 * D)
    gin("da_wo", D, D)
    gin("rope0c", 128, NT)
    gin("rope0s", 128, NT)
    I["mla_qnorm"] = din("mla_qnorm", [768])
    I["mla_kvnorm"] = din("mla_kvnorm", [256])
    if n_layers >= 2:
        gin("fn_wo", D, D)
        gin("fn_cos", 128, NL)
        gin("fn_sin", 128, NL)
        I["fn_f64"] = din("fn_f64", [64, 192])
        I["fn_cc"] = din("fn_cc", [256, 256])
        I["fn_sc"] = din("fn_sc", [256, 256])
        I["fn_nsc"] = din("fn_nsc", [256, 256])
    if n_layers >= 3:
        gin("na_w", D, 3 * D)
        gin("na_wo", D, D)
        gin("na_bias", NEXP * 128, 2048)
    if n_layers >= 4:
        gin("mla_wdq", D, 768)
        gin("mla_wuq", 768, 1536)
        gin("mla_wdkv", D, 288)
        gin("mla_wuk", 256, D)
        gin("mla_wuv", 256, D)
        gin("mla_wo", D, D)
        gin("rope3c", 96, NT)
        gin("rope3s", 96, NT)
    for l in range(n_layers):
        gin("moe_wg%d" % l, NEXP * D, EDIM)
        gin("moe_wu%d" % l, NEXP * D, EDIM)
        gin("moe_wd%d" % l, NEXP * EDIM, D)
    k.IB = IB
    k.I = I
    out = nc.dram_tensor("out", [NL, D], F32, kind="ExternalOutput").ap()
    k.out = out
    if dbg:
        k.dbg_x = nc.dram_tensor("dbg_x", [NT, D], F32, kind="ExternalOutput").ap()
        k.dbg_t = {}
        for nm, shp in [("qT", [1536, NT]), ("kT", [D, NT]), ("vv", [NT, D]), ("aT", [D, NT])]:
            k.dbg_t[nm] = nc.dram_tensor("dbg_" + nm, shp, BF16, kind="ExternalOutput").ap()
    k.xres = dscr("xres", [NT + 1, D])
    k.xres_b = bufs(NTILE + 1)
    k.modv = dscr("modv", [4, 6, 2, D])
    k.modv_b = Buf()
    k.h2tab = dscr("h2tab", [NT + 1, D], BF16)
    k.h2tab_b = bufs(NTILE + 1)
    k.meta = dscr("meta", [NEXP * SLOTS, 2])
    k.meta_b = bufs(NEXP)
    k.qT = dscr("qT", [1536, NT], BF16)
    k.krT = dscr("krT", [32, NT], BF16)
    k.krT_b = bufs(17)
    k.kT = dscr("kT", [D, NT], BF16)
    k.vv = dscr("vv", [NT, D], BF16)
    k.aT = dscr("aT", [D, NT], BF16)
    NG = 17
    k.NG = NG
    k.qT_b = bufs(NG)
    k.kT_b = bufs(NG)
    k.vv_b = bufs(NG)
    k.aT_b = bufs(NG)

    def sb(name, shape, dt=F32, stack=es):
        k.uid += 1
        return stack.enter_context(nc.sbuf_tensor("sb%d_%s" % (k.uid, name), list(shape), dt))

    def ps(name, shape, dt=F32, stack=es):
        k.uid += 1
        return stack.enter_context(nc.psum_tensor("ps%d_%s" % (k.uid, name), list(shape), dt))

    k.sb = sb
    k.ps = ps
    k.ident = sb("ident", [128, 128])
    k.ident_b = Buf()
    k.identb = sb("identb", [128, 128], BF16)
    k.identb_b = Buf()
    k.ones_f = sb("ones_f", [128, 128])
    k.ones_b = sb("ones_b", [128, 128], BF16)
    k.ones_bb = Buf()
    k.tokid = sb("tokid", [128, NTILE])
    k.tokid_b = Buf()
    s.dma("sp", lambda E: E.dma_start(out=k.ident[:], in_=I["ident"][:, :]), [], [k.ident_b])
    s.dma("sp", lambda E: E.dma_start(out=k.tokid[:], in_=I["tokid"][:, :]), [], [k.tokid_b])
    s.op("dve", lambda E: E.tensor_copy(out=k.identb[:], in_=k.ident[:]), [k.ident_b], [k.identb_b])
    s.op("dve", lambda E: E.memset(k.ones_f[:], 1.0), [], [k.ones_bb])
    s.op("dve", lambda E: E.memset(k.ones_b[:], 1.0), [], [k.ones_bb])
    k.epsc = sb("epsc", [128, 1])
    k.epsc_b = Buf()
    s.op("dve", lambda E: E.memset(k.epsc[:], float(EPS)), [], [k.epsc_b])

    for t in range(NTILE):
        s.dma("sp", lambda E, t=t: E.dma_start(out=k.xres[t * 128:(t + 1) * 128, :], in_=I["xin"][t * 128:(t + 1) * 128, :]),
              [], [k.xres_b[t]])

    try:
        chk("init")
        phase_mod(k)
        if stop is not None and stop.startswith("mod"):
            raise _Stop()
        for l in range(n_layers):
            kind = l % 4
            if kind == 0:
                mixer_diff(k, l)
            elif kind == 1:
                mixer_fourier(k, l, keep_ctx=(l < 3))
            elif kind == 2:
                mixer_na(k, l, keep_ctx=(l < 3))
            elif kind == 3:
                mixer_mla(k, l, keep_ctx=(l < 3))
            chk("mixer%d" % l)
            post_mixer_and_moe(k, l, keep_ctx=(l < 3))
            chk("layer%d" % l)
    except _Stop:
        pass
    if dbg:
        s.barrier()
        for nm, src in [("qT", k.qT), ("kT", k.kT), ("vv", k.vv), ("aT", k.aT)]:
            rows = src.shape[0]
            for r0 in range(0, rows, 128):
                r1 = min(rows, r0 + 128)
                s.dma("sp", lambda E, nm=nm, src=src, r0=r0, r1=r1: E.dma_start(out=k.dbg_t[nm][r0:r1, :], in_=src[r0:r1, :]), [], [])
        for t in range(NTILE):
            s.dma("sp", lambda E, t=t: E.dma_start(out=k.dbg_x[t * 128:(t + 1) * 128, :], in_=k.xres[t * 128:(t + 1) * 128, :]),
                  [k.xres_b[t]], [])
    final_norm(k)
    s.barrier()
    s.emit()
    es.close()
    return nc


def phase_mod(k):
    nc, s, I = k.nc, k.s, k.I
    with ExitStack() as st:
        cc = k.sb("m_cc", [128, 8, 2], stack=st)
        sil = k.sb("m_sil", [128, 8, 2], stack=st)
        cc_b, sil_b = Buf(), Buf()
        wt = [k.sb("m_w%d" % i, [128, 8, 512], stack=st) for i in range(2)]
        wt_b = bufs(2)
        mrow = k.sb("m_row", [2, 6 * D], stack=st)
        mrow_b = Buf()
        adab = k.sb("m_adab", [2, 6 * D], stack=st)
        adab_b = Buf()
        nw = k.sb("m_nw", [2, 2, D], stack=st)
        nw_b = Buf()
        aa = k.sb("m_aa", [2, 2, D], stack=st)
        aa_b = Buf()
        pp = [k.ps("m_ps%d" % i, [2, 512], stack=st) for i in range(2)]
        pp_b = bufs(2)
        s.dma("sp", lambda E: E.dma_start(out=cc[:], in_=I["ccT"][:, :, :]), [], [cc_b])
        s.op("act", lambda E: E.activation(out=sil[:], in_=cc[:], func=AF.Silu), [cc_b], [sil_b])
        if k.stop == "mod_silu":
            s.barrier()
            return
        it = 0
        for l in range(4):
            s.dma("sp", lambda E, l=l: E.dma_start(out=adab[:], in_=I["ada_b"][l:l + 1, :].broadcast_to([2, 6 * D])), [], [adab_b])
            s.dma("sp", lambda E, l=l: E.dma_start(out=nw[:, 0, :], in_=I["norm_mix"][l:l + 1, :].broadcast_to([2, D])), [], [nw_b])
            s.dma("sp", lambda E, l=l: E.dma_start(out=nw[:, 1, :], in_=I["norm_ffn"][l:l + 1, :].broadcast_to([2, D])), [], [nw_b])
            for nb in range(12):
                w = wt[it % 2]
                wb = wt_b[it % 2]
                p = pp[it % 2]
                pb = pp_b[it % 2]
                it += 1
                s.dma("sp", lambda E, l=l, nb=nb, w=w: E.dma_start(
                    out=w[:], in_=I["ada_w%d" % l][:, nb * 512:(nb + 1) * 512].rearrange("(c p) n -> p c n", p=128)), [k.IB["ada_w%d" % l]], [wb])
                for c in range(8):
                    s.op("pe", lambda E, c=c, w=w, p=p: E.matmul(p[:], lhsT=sil[:, c, :], rhs=w[:, c, :], start=(c == 0), stop=(c == 7)),
                         [sil_b, wb], [pb], track=(c == 7))
                s.op("dve", lambda E, nb=nb, p=p: E.tensor_tensor(out=mrow[:, nb * 512:(nb + 1) * 512], in0=p[:], in1=adab[:, nb * 512:(nb + 1) * 512], op=ALU.add),
                     [pb, adab_b], [mrow_b])
                if k.stop == "mod_mm":
                    s.barrier()
                    return
            s.op("dve", lambda E: E.scalar_tensor_tensor(out=aa[:, 0, :], in0=mrow[:, D:2 * D], scalar=1.0, in1=nw[:, 0, :], op0=ALU.add, op1=ALU.mult),
                 [mrow_b, nw_b], [aa_b])
            s.op("dve", lambda E: E.scalar_tensor_tensor(out=aa[:, 1, :], in0=mrow[:, 4 * D:5 * D], scalar=1.0, in1=nw[:, 1, :], op0=ALU.add, op1=ALU.mult),
                 [mrow_b, nw_b], [aa_b])
            srcs = [aa[:, 0, :], mrow[:, 0:D], mrow[:, 2 * D:3 * D], aa[:, 1, :], mrow[:, 3 * D:4 * D], mrow[:, 5 * D:6 * D]]
            for v in range(6):
                s.dma("sp", lambda E, l=l, v=v, src=srcs[v]: E.dma_start(out=k.modv[l, v, :, :], in_=src), [mrow_b, aa_b], [k.modv_b])
            if k.stop == "mod_l0":
                s.barrier()
                return
        s.barrier()


def load_bcast(k, q, dst, dst_b, l, v, cond):
    k.s.dma(q, lambda E: E.dma_start(out=dst, in_=k.modv[l, v, cond:cond + 1, :].broadcast_to([128, D])), [k.modv_b], [dst_b])


def load_weight_bf16(k, st, name, src, src_b, C, ncols, blk=512, swap_cols=0, swap_unit=64):
    s = k.s
    dst = k.sb(name, [128, C, ncols + swap_cols], BF16, stack=st)
    dst_b = Buf()
    if getattr(st, "_wstg", None) is None:
        st._wstg = ([k.sb(name + "_st%d" % i, [128, 4096], stack=st) for i in range(2)], bufs(2), [0])
    stg_full, stg_b, stg_ctr = st._wstg
    stg = [t[:, 0:C * blk].rearrange("p (c n) -> p c n", c=C) for t in stg_full]
    nb = ncols // blk
    hu = swap_unit // 2
    for b in range(nb):
        t = stg[stg_ctr[0] % 2]
        tb = stg_b[stg_ctr[0] % 2]
        stg_ctr[0] += 1
        s.dma("sp", lambda E, b=b, t=t: E.dma_start(out=t, in_=src[:, b * blk:(b + 1) * blk].rearrange("(c p) n -> p c n", p=128)), [src_b], [tb])
        eng = ["pool", "dve"][b % 2]
        s.op(eng, lambda E, b=b, t=t: E.tensor_copy(out=dst[:, :, b * blk:(b + 1) * blk], in_=t), [tb], [dst_b])
        if (b + 1) * blk <= swap_cols:
            for c in range(C):
                for half in range(2):
                    eng2 = ["dve", "pool"][(c + half) % 2]
                    s.op(eng2, lambda E, b=b, t=t, c=c, half=half: E.tensor_copy(
                        out=dst[:, c, ncols + b * blk:ncols + (b + 1) * blk].rearrange("p (u two h) -> p u two h", two=2, h=hu)[:, :, half, :],
                        in_=t[:, c, :].rearrange("p (u two h) -> p u two h", two=2, h=hu)[:, :, 1 - half, :]), [tb], [dst_b])
    return dst, dst_b


def norm_tile(k, xt, xt_b, ht, ht_b, Ab, Ab_b, Bb, Bb_b, scr, scr_b):
    s = k.s
    junk, stat = scr
    s.op("act", lambda E: E.activation(out=junk[:], in_=xt[:], func=AF.Square, accum_out=stat[:, 0:1]), [xt_b], [scr_b])
    s.op("act", lambda E: E.activation(out=stat[:, 1:2], in_=stat[:, 0:1], func=AF.Ln, scale=float(1.0 / D), bias=k.epsc[:, 0:1]), [scr_b, k.epsc_b], [scr_b])
    s.op("act", lambda E: E.activation(out=stat[:, 1:2], in_=stat[:, 1:2], func=AF.Exp, scale=-0.5), [scr_b], [scr_b])
    s.op("dve", lambda E: E.scalar_tensor_tensor(out=ht[:], in0=xt[:], scalar=stat[:, 1:2], in1=Ab[:], op0=ALU.mult, op1=ALU.mult),
         [xt_b, scr_b, Ab_b], [ht_b])
    s.op("pool", lambda E: E.tensor_tensor(out=ht[:], in0=ht[:], in1=Bb[:], op=ALU.add), [ht_b, Bb_b], [ht_b])


def transpose_tile(k, ht, ht_b, tp, tp_b, hT, hT_b, col0, ncol=128, nchunk=8, evac=("act", "dve")):
    s = k.s
    for half in range((nchunk + 3) // 4):
        p = tp[half % len(tp)]
        pb = tp_b[half % len(tp)]
        cs = list(range(half * 4, min(nchunk, half * 4 + 4)))
        for j, c in enumerate(cs):
            s.op("pe", lambda E, c=c, j=j, p=p: E.transpose(out=p[:, j * 128:j * 128 + ncol], in_=ht[0:ncol, c * 128:(c + 1) * 128], identity=k.ident[0:ncol, 0:ncol]),
                 [ht_b, k.ident_b], [pb], track=(j == len(cs) - 1))
        eng = evac[half % len(evac)]
        n = len(cs)
        if eng == "act":
            s.op("act", lambda E, p=p, cs=cs, n=n: E.activation(out=hT[:, cs[0]:cs[0] + n, col0:col0 + ncol], in_=p[:, 0:n * 128].rearrange("p (c t) -> p c t", c=n)[:, :, 0:ncol], func=AF.Copy),
                 [pb], [hT_b])
        else:
            s.op("dve", lambda E, p=p, cs=cs, n=n: E.tensor_copy(out=hT[:, cs[0]:cs[0] + n, col0:col0 + ncol], in_=p[:, 0:n * 128].rearrange("p (c t) -> p c t", c=n)[:, :, 0:ncol]),
                 [pb], [hT_b])


def group_range(g):
    t0 = g * 512
    w = min(512, NT - t0)
    return t0, w


def mixer_diff(k, l):
    nc, s, I = k.nc, k.s, k.I
    lam_init = 0.8 - 0.6 * math.exp(-0.3 * l)
    with ExitStack() as st:
        W, W_b = load_weight_bf16(k, st, "d_w", I["da_w"], k.IB["da_w"], 8, 3 * D, swap_cols=2 * D, swap_unit=64)
        Ab = [k.sb("d_Ab%d" % c, [128, D], stack=st) for c in range(2)]
        Bb = [k.sb("d_Bb%d" % c, [128, D], stack=st) for c in range(2)]
        Ab_b, Bb_b = bufs(2), bufs(2)
        for c in range(2):
            load_bcast(k, "sp", Ab[c][:], Ab_b[c], l, 0, c)
            load_bcast(k, "sp", Bb[c][:], Bb_b[c], l, 1, c)
        xt = [k.sb("d_xt%d" % i, [128, D], stack=st) for i in range(2)]
        xt_b = bufs(2)
        ht = [k.sb("d_ht%d" % i, [128, D], stack=st) for i in range(2)]
        ht_b = bufs(2)
        junk = k.sb("d_junk", [128, D], stack=st)
        stat = [k.sb("d_stat%d" % i, [128, 2], stack=st) for i in range(2)]
        scr_b = bufs(2)
        hT = [k.sb("d_hT%d" % i, [128, 8, 512], BF16, stack=st) for i in range(2)]
        hT_b = bufs(2)
        rc = [k.sb("d_rc%d" % i, [128, 512], stack=st) for i in range(2)]
        rs = [k.sb("d_rs%d" % i, [128, 512], stack=st) for i in range(2)]
        rc_b, rs_b = bufs(2), bufs(2)
        t1 = [k.sb("d_t1%d" % i, [128, 512], stack=st) for i in range(2)]
        t2 = [k.sb("d_t2%d" % i, [128, 512], stack=st) for i in range(2)]
        t1_b, t2_b = bufs(2), bufs(2)
        qo = [k.sb("d_qo%d" % i, [128, 512], BF16, stack=st) for i in range(4)]
        qo_b = bufs(4)
        vo = [k.sb("d_vo%d" % i, [128, D], BF16, stack=st) for i in range(2)]
        vo_b = bufs(2)
        tp = [k.ps("d_tp%d" % i, [128, 512], stack=st) for i in range(2)]
        tp_b = bufs(2)
        pq = [k.ps("d_pq%d" % i, [128, 512], stack=st) for i in range(4)]
        pq_b = bufs(4)
        pv = [k.ps("d_pv%d" % i, [128, 512], stack=st) for i in range(2)]
        pv_b = bufs(2)
        ti = 0
        oi = 0
        vi = 0
        def prep(g):
            nonlocal ti
            t0, w = group_range(g)
            hTg, hTg_b = hT[g % 2], hT_b[g % 2]
            s.dma("sp", lambda E: E.dma_start(out=rc[g % 2][:, 0:w], in_=I["rope0c"][:, t0:t0 + w]), [k.IB["rope0c"]], [rc_b[g % 2]])
            s.dma("sp", lambda E: E.dma_start(out=rs[g % 2][:, 0:w], in_=I["rope0s"][:, t0:t0 + w]), [k.IB["rope0s"]], [rs_b[g % 2]])
            for j in range(w // 128):
                t = (t0 // 128) + j
                cond = 0 if t < 64 else 1
                x_, x_b = xt[ti % 2], xt_b[ti % 2]
                h_, h_b = ht[ti % 2], ht_b[ti % 2]
                s.dma("sp", lambda E: E.dma_start(out=x_[:], in_=k.xres[t * 128:(t + 1) * 128, :]), [k.xres_b[t]], [x_b])
                norm_tile(k, x_, x_b, h_, h_b, Ab[cond], Ab_b[cond], Bb[cond], Bb_b[cond], (junk, stat[ti % 2]), scr_b[ti % 2])
                transpose_tile(k, h_, h_b, tp, tp_b, hTg, hTg_b, j * 128)
                ti += 1

        prep(0)
        for g in range(k.NG):
            t0, w = group_range(g)
            hTg, hTg_b = hT[g % 2], hT_b[g % 2]
            if g + 1 < k.NG:
                prep(g + 1)
            for o in range(16):
                pa, pa_b = pq[(2 * o) % 4], pq_b[(2 * o) % 4]
                pb_, pb_b = pq[(2 * o + 1) % 4], pq_b[(2 * o + 1) % 4]
                for c in range(8):
                    s.op("pe", lambda E, c=c, o=o, pa=pa: E.matmul(pa[:, 0:w], lhsT=W[:, c, o * 128:(o + 1) * 128], rhs=hTg[:, c, 0:w], start=(c == 0), stop=(c == 7)),
                         [W_b, hTg_b], [pa_b], track=(c == 7))
                for c in range(8):
                    s.op("pe", lambda E, c=c, o=o, pb_=pb_: E.matmul(pb_[:, 0:w], lhsT=W[:, c, 3 * D + o * 128:3 * D + (o + 1) * 128], rhs=hTg[:, c, 0:w], start=(c == 0), stop=(c == 7)),
                         [W_b, hTg_b], [pb_b], track=(c == 7))
                a1, a1_b = t1[o % 2], t1_b[o % 2]
                a2, a2_b = t2[o % 2], t2_b[o % 2]
                q_, q_b = qo[oi % 4], qo_b[oi % 4]
                oi += 1
                s.op("dve", lambda E, pa=pa, a1=a1: E.tensor_tensor(out=a1[:, 0:w], in0=pa[:, 0:w], in1=rc[g % 2][:, 0:w], op=ALU.mult), [pa_b, rc_b[g % 2]], [a1_b])
                s.op("dve", lambda E, pb_=pb_, a2=a2: E.tensor_tensor(out=a2[:, 0:w], in0=pb_[:, 0:w], in1=rs[g % 2][:, 0:w], op=ALU.mult), [pb_b, rs_b[g % 2]], [a2_b])
                s.op("pool", lambda E, a1=a1, a2=a2, q_=q_: E.tensor_tensor(out=q_[:, 0:w], in0=a1[:, 0:w], in1=a2[:, 0:w], op=ALU.add), [a1_b, a2_b], [q_b])
                dstT = k.qT if o < 8 else k.kT
                dst_b = (k.qT_b if o < 8 else k.kT_b)[g]
                oo = o % 8
                s.dma("pool", lambda E, dstT=dstT, oo=oo, q_=q_: E.dma_start(out=dstT[oo * 128:(oo + 1) * 128, t0:t0 + w], in_=q_[:, 0:w]), [q_b], [dst_b])
            for j in range(w // 128):
                v_, v_b = vo[vi % 2], vo_b[vi % 2]
                vi += 1
                for nb in range(2):
                    p, p_b = pv[nb], pv_b[nb]
                    for c in range(8):
                        s.op("pe", lambda E, c=c, nb=nb, p=p, j=j: E.matmul(p[:], lhsT=hTg[:, c, j * 128:(j + 1) * 128], rhs=W[:, c, 2 * D + nb * 512:2 * D + (nb + 1) * 512], start=(c == 0), stop=(c == 7)),
                             [W_b, hTg_b], [p_b], track=(c == 7))
                    s.op("act", lambda E, nb=nb, p=p, v_=v_: E.activation(out=v_[:, nb * 512:(nb + 1) * 512], in_=p[:], func=AF.Copy), [p_b], [v_b])
                t = (t0 // 128) + j
                s.dma("pool", lambda E, t=t, v_=v_: E.dma_start(out=k.vv[t * 128:(t + 1) * 128, :], in_=v_[:]), [v_b], [k.vv_b[g]])
        s.barrier()
    k.chk("proj%d" % l)
    with ExitStack() as st:
        lamt = k.sb("a_lamt", [128, 4, 64], stack=st)
        lamt_b = Buf()
        lamp = k.sb("a_lamp", [128, 2, 64], stack=st)
        lamv = k.sb("a_lamv", [128, 4], stack=st)
        lam_b = Buf()
        gcol = k.sb("a_gcol", [128, 1], stack=st)
        gcol_b = Buf()
        s.dma("sp", lambda E: E.dma_start(out=lamt[:].rearrange("p a b -> p (a b)"), in_=I["da_lam"].rearrange("a b -> (a b)").unsqueeze(0).broadcast_to([128, 256])), [], [lamt_b])
        s.dma("sp", lambda E: E.dma_start(out=gcol[:], in_=I["da_subln"][:, :]), [], [gcol_b])
        s.op("dve", lambda E: E.tensor_tensor(out=lamp[:, 0, :], in0=lamt[:, 0, :], in1=lamt[:, 1, :], op=ALU.mult), [lamt_b], [lam_b])
        s.op("dve", lambda E: E.tensor_tensor(out=lamp[:, 1, :], in0=lamt[:, 2, :], in1=lamt[:, 3, :], op=ALU.mult), [lam_b, lamt_b], [lam_b])
        s.op("dve", lambda E: E.tensor_reduce(out=lamv[:, 0:2], in_=lamp[:], axis=AX.X, op=ALU.add), [lam_b], [lam_b])
        s.op("act", lambda E: E.activation(out=lamv[:, 0:2], in_=lamv[:, 0:2], func=AF.Exp), [lam_b], [lam_b])
        s.op("dve", lambda E: E.tensor_tensor(out=lamv[:, 2:3], in0=lamv[:, 1:2], in1=lamv[:, 0:1], op=ALU.subtract), [lam_b], [lam_b])
        s.op("dve", lambda E: E.tensor_scalar(out=lamv[:, 2:3], in0=lamv[:, 2:3], scalar1=float(-lam_init), scalar2=None, op0=ALU.add), [lam_b], [lam_b])
        s.op("dve", lambda E: E.tensor_scalar(out=gcol[:], in0=gcol[:], scalar1=float(1.0 - lam_init), scalar2=None, op0=ALU.mult), [gcol_b], [gcol_b])
        neg_lam = lamv[:, 2:3]

        def post(h, qb, w, O, O_b, Z, Z_b, wk):
            (r0, o0, o1, sq, on, pss, wk_b, pss_b) = wk
            s.op("dve", lambda E: E.reciprocal(out=r0[:, 0:w], in_=Z[0][:, 0:w]), [Z_b[0]], [wk_b])
            s.op("dve", lambda E: E.tensor_tensor(out=o0[:, 0:w], in0=O[0][:, 0:w], in1=r0[:, 0:w], op=ALU.mult), [O_b[0], wk_b], [wk_b])
            s.op("dve", lambda E: E.reciprocal(out=r0[:, 0:w], in_=Z[1][:, 0:w]), [Z_b[1], wk_b], [wk_b])
            s.op("dve", lambda E: E.tensor_tensor(out=o1[:, 0:w], in0=O[1][:, 0:w], in1=r0[:, 0:w], op=ALU.mult), [O_b[1], wk_b], [wk_b])
            s.op("dve", lambda E: E.scalar_tensor_tensor(out=o0[:, 0:w], in0=o1[:, 0:w], scalar=neg_lam, in1=o0[:, 0:w], op0=ALU.mult, op1=ALU.add), [wk_b, lam_b], [wk_b])
            s.op("pool", lambda E: E.tensor_tensor(out=sq[:, 0:w], in0=o0[:, 0:w], in1=o0[:, 0:w], op=ALU.mult), [wk_b], [wk_b])
            s.op("pe", lambda E: E.matmul(pss[:, 0:w], lhsT=k.ones_f[:], rhs=sq[:, 0:w], start=True, stop=True), [wk_b, k.ones_bb], [pss_b])
            s.op("act", lambda E: E.activation(out=r0[:, 0:w], in_=pss[:, 0:w], func=AF.Ln, scale=float(1.0 / 128.0), bias=k.epsc[:, 0:1]), [pss_b, wk_b, k.epsc_b], [wk_b])
            s.op("act", lambda E: E.activation(out=r0[:, 0:w], in_=r0[:, 0:w], func=AF.Exp, scale=-0.5), [wk_b], [wk_b])
            s.op("dve", lambda E: E.scalar_tensor_tensor(out=on[:, 0:w], in0=o0[:, 0:w], scalar=gcol[:, 0:1], in1=r0[:, 0:w], op0=ALU.mult, op1=ALU.mult), [wk_b, gcol_b], [wk_b])
            return on

        attention(k, st, n_heads=8, maps=2, kp=64, dv=128, scale=0.125, post=post)
        s.barrier()


def attention(k, st, n_heads, maps, kp, dv, scale, post, krows=None, qrows=None, extra=None, with_ctx_q=True, aug=False):
    nc, s = k.nc, k.s
    KP = maps * kp if maps > 1 else kp
    krows = krows or KP
    qrows = qrows or KP
    KT = [k.sb("a_KT%d" % i, [128, NT], BF16, stack=st) for i in range(2)]
    KT_b = bufs(2)
    dva = dv + 1 if aug else dv
    V = [k.sb("a_V%d" % i, [128, NTILE, dva], BF16, stack=st) for i in range(2)]
    V_b = bufs(2)
    if aug:
        for i in range(2):
            s.op("pool", lambda E: E.memset(V[i][:, :, dv:dv + 1], 1.0), [], [V_b[i]])
    Q = [k.sb("a_Q%d" % i, [128, 512], BF16, stack=st) for i in range(2)]
    Q_b = bufs(2)
    P = [k.sb("a_P%d" % i, [128, 2, 512], BF16, stack=st) for i in range(3)]
    P_b = bufs(3)
    acc = [k.sb("a_acc%d" % i, [128, 512], stack=st) for i in range(2)]
    acc_b = bufs(2)
    accb = [k.sb("a_accb%d" % i, [128, 512], BF16, stack=st) for i in range(2)]
    accb_b = bufs(2)
    r0 = k.sb("a_r0", [128, 512], stack=st)
    o0 = k.sb("a_o0", [128, 512], stack=st)
    o1 = k.sb("a_o1", [128, 512], stack=st)
    sq = k.sb("a_sq", [128, 512], stack=st)
    on = [k.sb("a_on%d" % i, [128, 512], BF16, stack=st) for i in range(2)]
    wk_b = Buf()
    S = [k.ps("a_S%d" % i, [128, 1024], stack=st) for i in range(2)]
    S_b = bufs(2)
    O = [k.ps("a_O%d" % i, [128, 512], stack=st) for i in range(2)]
    O_b = bufs(2)
    Z = [k.ps("a_Z%d" % i, [128, 512], stack=st) for i in range(2)]
    Z_b = bufs(2)
    qblocks = [(g,) + group_range(g) for g in range(k.NG if with_ctx_q else 16)]
    si = 0
    pi = 0
    qi = 0
    oi = 0
    ai = 0
    for h in range(n_heads):
        KTh, KTh_b = KT[h % 2], KT_b[h % 2]
        Vh, Vh_b = V[h % 2], V_b[h % 2]
        s.dma("sp", lambda E: E.dma_start(out=KTh[0:krows, :], in_=k.kT[h * krows:(h + 1) * krows, :]), list(k.kT_b), [KTh_b])
        if extra is not None:
            ex_ap, ex_b, ex_rows = extra
            s.dma("sp", lambda E: E.dma_start(out=KTh[krows:krows + ex_rows, :], in_=ex_ap[:, :]), list(ex_b), [KTh_b])
        for half in range(2):
            s.dma("sp", lambda E: E.dma_start(out=Vh[:, half * 33:(half + 1) * 33, 0:dv], in_=k.vv[half * 33 * 128:(half + 1) * 33 * 128, h * dv:(h + 1) * dv].rearrange("(t p) d -> p t d", p=128)),
                  list(k.vv_b), [Vh_b])
        for (g, t0, w) in qblocks:
            Qb, Qb_b = Q[qi % 2], Q_b[qi % 2]
            qi += 1
            s.dma("sp", lambda E: E.dma_start(out=Qb[0:qrows, 0:w], in_=k.qT[h * qrows:(h + 1) * qrows, t0:t0 + w]), [k.qT_b[g]], [Qb_b])
            ktiles = list(range(NTILE)) if t0 < NL else [64, 65]
            pairs = [(ktiles[2 * i], ktiles[2 * i + 1]) for i in range(len(ktiles) // 2)]
            npair = len(pairs)
            for m in range(maps):
                p0 = m * kp
                ac, ac_b = acc[ai % 2], acc_b[ai % 2]
                acb, acb_b = accb[ai % 2], accb_b[ai % 2]
                ai += 1
                slots = []

                def emit_S(p):
                    nonlocal si
                    Sx, Sx_b = S[si % 2], S_b[si % 2]
                    si += 1
                    for j in range(2):
                        kt = pairs[p][j]
                        s.op("pe", lambda E: E.matmul(Sx[:, j * 512:j * 512 + w], lhsT=KTh[p0:p0 + kp, kt * 128:(kt + 1) * 128], rhs=Qb[p0:p0 + kp, 0:w], start=True, stop=True),
                             [KTh_b, Qb_b], [Sx_b], track=(j == 1))
                    slots.append((Sx, Sx_b))

                emit_S(0)
                for p in range(npair):
                    if p + 1 < npair:
                        emit_S(p + 1)
                    Sx, Sx_b = slots[p]
                    Px, Px_b = P[pi % 3], P_b[pi % 3]
                    pi += 1
                    s.op("act", lambda E: E.activation(out=Px[:, :, 0:w], in_=Sx[:].rearrange("p (a b) -> p a b", a=2)[:, :, 0:w], func=AF.Exp, scale=float(scale)), [Sx_b], [Px_b])
                    if not aug:
                        if p == 0:
                            s.op("dve", lambda E: E.tensor_copy(out=ac[:, 0:w], in_=Px[:, 1, 0:w]), [Px_b], [ac_b])
                        else:
                            s.op("dve", lambda E: E.tensor_tensor(out=ac[:, 0:w], in0=ac[:, 0:w], in1=Px[:, 1, 0:w], op=ALU.add), [Px_b, ac_b], [ac_b])
                    for j in range(2):
                        kt = pairs[p][j]
                        s.op("pe", lambda E: E.matmul(O[m][0:dva, 0:w], lhsT=Vh[:, kt, 0:dva], rhs=Px[:, j, 0:w], start=(p == 0 and j == 0), stop=(p == npair - 1 and j == 1)),
                             [Vh_b, Px_b], [O_b[m]], track=(j == 1))
                    if not aug:
                        s.op("pe", lambda E: E.matmul(Z[m][0:dv, 0:w], lhsT=k.ones_b[:, 0:dv], rhs=Px[:, 0, 0:w], start=(p == 0), stop=False),
                             [k.ones_bb, Px_b], [Z_b[m]], track=True)
                if not aug:
                    s.op("dve", lambda E: E.tensor_copy(out=acb[:, 0:w], in_=ac[:, 0:w]), [ac_b], [acb_b])
                    s.op("pe", lambda E: E.matmul(Z[m][0:dv, 0:w], lhsT=k.ones_b[:, 0:dv], rhs=acb[:, 0:w], start=False, stop=True), [k.ones_bb, acb_b], [Z_b[m]], track=True)
            onx = on[oi % 2]
            oi += 1
            res = post(h, g, w, O, O_b, Z, Z_b, (r0, o0, o1, sq, onx, S[0], wk_b, S_b[0]))
            s.dma("pool", lambda E: E.dma_start(out=k.aT[h * dv:(h + 1) * dv, t0:t0 + w], in_=res[0:dv, 0:w]), [wk_b], [k.aT_b[g]])


class HTP:
    def __init__(self, k, st, l, pfx, v0=0):
        self.k = k
        sb, ps = k.sb, k.ps
        self.Ab = [sb(pfx + "_Ab%d" % c, [128, D], stack=st) for c in range(2)]
        self.Bb = [sb(pfx + "_Bb%d" % c, [128, D], stack=st) for c in range(2)]
        self.Ab_b, self.Bb_b = bufs(2), bufs(2)
        for c in range(2):
            load_bcast(k, "sp", self.Ab[c][:], self.Ab_b[c], l, v0, c)
            load_bcast(k, "sp", self.Bb[c][:], self.Bb_b[c], l, v0 + 1, c)
        self.xt = [sb(pfx + "_xt%d" % i, [128, D], stack=st) for i in range(2)]
        self.xt_b = bufs(2)
        self.ht = [sb(pfx + "_ht%d" % i, [128, D], stack=st) for i in range(2)]
        self.ht_b = bufs(2)
        self.junk = sb(pfx + "_junk", [128, D], stack=st)
        self.stat = [sb(pfx + "_stat%d" % i, [128, 2], stack=st) for i in range(2)]
        self.scr_b = bufs(2)
        self.hT = [sb(pfx + "_hT%d" % i, [128, 8, 512], BF16, stack=st) for i in range(2)]
        self.hT_b = bufs(2)
        self.tp = [ps(pfx + "_tp%d" % i, [128, 512], stack=st) for i in range(2)]
        self.tp_b = bufs(2)
        self.ti = 0

    def tile(self, t):
        k, s = self.k, self.k.s
        i = self.ti % 2
        self.ti += 1
        cond = 0 if t < 64 else 1
        x_, x_b, h_, h_b = self.xt[i], self.xt_b[i], self.ht[i], self.ht_b[i]
        s.dma("sp", lambda E: E.dma_start(out=x_[:], in_=k.xres[t * 128:(t + 1) * 128, :]), [k.xres_b[t]], [x_b])
        norm_tile(k, x_, x_b, h_, h_b, self.Ab[cond], self.Ab_b[cond], self.Bb[cond], self.Bb_b[cond], (self.junk, self.stat[i]), self.scr_b[i])
        return h_, h_b

    def group(self, g):
        k = self.k
        t0, w = group_range(g)
        hTg, hTg_b = self.hT[g % 2], self.hT_b[g % 2]
        for j in range(w // 128):
            h_, h_b = self.tile(t0 // 128 + j)
            transpose_tile(k, h_, h_b, self.tp, self.tp_b, hTg, hTg_b, j * 128)
        return hTg, hTg_b, t0, w


def rms_rows(k, src_list, n, gain, gain_b, dst, dst_b, wk, wk_b):
    s = k.s
    junk, st4 = wk
    for i, (ap, b, wd) in enumerate(src_list):
        s.op("act", lambda E, ap=ap, i=i, wd=wd: E.activation(out=junk[:, 0:wd], in_=ap, func=AF.Square, accum_out=st4[:, i:i + 1]), [b], [wk_b])
    if len(src_list) == 2:
        s.op("dve", lambda E: E.tensor_tensor(out=st4[:, 0:1], in0=st4[:, 0:1], in1=st4[:, 1:2], op=ALU.add), [wk_b], [wk_b])
    s.op("act", lambda E: E.activation(out=st4[:, 2:3], in_=st4[:, 0:1], func=AF.Ln, scale=float(1.0 / n), bias=k.epsc[:, 0:1]), [wk_b, k.epsc_b], [wk_b])
    s.op("act", lambda E: E.activation(out=st4[:, 2:3], in_=st4[:, 2:3], func=AF.Exp, scale=-0.5), [wk_b], [wk_b])
    c0 = 0
    for (ap, b, wd) in src_list:
        s.op("dve", lambda E, ap=ap, c0=c0, wd=wd: E.scalar_tensor_tensor(out=dst[:, c0:c0 + wd], in0=ap, scalar=st4[:, 2:3], in1=gain[:, c0:c0 + wd], op0=ALU.mult, op1=ALU.mult),
             [b, wk_b, gain_b], [dst_b])
        c0 += wd


def mixer_mla(k, l, keep_ctx):
    nc, s, I, IB = k.nc, k.s, k.I, k.IB
    with ExitStack() as st:
        Wdq, Wdq_b = load_weight_bf16(k, st, "m_wdq", I["mla_wdq"], IB["mla_wdq"], 8, 768, blk=256)
        Wdkv, Wdkv_b = load_weight_bf16(k, st, "m_wdkv", I["mla_wdkv"], IB["mla_wdkv"], 8, 288, blk=288, swap_cols=288, swap_unit=32)
        Wuq, Wuq_b = load_weight_bf16(k, st, "m_wuq", I["mla_wuq"], IB["mla_wuq"], 6, 1536, blk=512, swap_cols=1536, swap_unit=32)
        Wuk, Wuk_b = load_weight_bf16(k, st, "m_wuk", I["mla_wuk"], IB["mla_wuk"], 2, 1024, blk=512)
        Wuv, Wuv_b = load_weight_bf16(k, st, "m_wuv", I["mla_wuv"], IB["mla_wuv"], 2, 1024, blk=512)
        qnb = k.sb("m_qnb", [128, 768], stack=st)
        kvnb = k.sb("m_kvnb", [128, 256], stack=st)
        qnb_b, kvnb_b = Buf(), Buf()
        s.dma("sp", lambda E: E.dma_start(out=qnb[:], in_=I["mla_qnorm"].unsqueeze(0).broadcast_to([128, 768])), [], [qnb_b])
        s.dma("sp", lambda E: E.dma_start(out=kvnb[:], in_=I["mla_kvnorm"].unsqueeze(0).broadcast_to([128, 256])), [], [kvnb_b])
        htp = HTP(k, st, l, "m")
        qn = [k.sb("m_qn%d" % i, [128, 768], stack=st) for i in range(2)]
        qn_b = bufs(2)
        cn = [k.sb("m_cn%d" % i, [128, 256], stack=st) for i in range(2)]
        cn_b = bufs(2)
        junk = htp.junk
        st4 = [k.sb("m_st4%d" % i, [128, 4], stack=st) for i in range(2)]
        st4_b = bufs(2)
        qlT = [k.sb("m_qlT%d" % i, [128, 6, 512], BF16, stack=st) for i in range(2)]
        qlT_b = bufs(2)
        ckT = [k.sb("m_ckT%d" % i, [128, 2, 512], BF16, stack=st) for i in range(2)]
        ckT_b = bufs(2)
        rc = [k.sb("m_rc0", [96, 512], stack=st)] * 2
        rs = [k.sb("m_rs0", [96, 512], stack=st)] * 2
        rkc = [k.sb("m_rkc0", [32, 512], stack=st)] * 2
        rks = [k.sb("m_rks0", [32, 512], stack=st)] * 2
        rt_b = [Buf()] * 2
        t1 = [k.sb("m_t1%d" % i, [128, 512], stack=st) for i in range(2)]
        t2 = [k.sb("m_t2%d" % i, [128, 512], stack=st) for i in range(2)]
        t1_b, t2_b = bufs(2), bufs(2)
        ob = [k.sb("m_ob%d" % i, [128, 512], BF16, stack=st) for i in range(4)]
        ob_b = bufs(4)
        vo = [k.sb("m_vo0", [128, D], BF16, stack=st)] * 2
        vo_b = [Buf()] * 2
        pA0 = k.ps("m_pA0", [128, 512], stack=st)
        pA1 = k.ps("m_pA1", [128, 512], stack=st)
        pC = k.ps("m_pC", [128, 512], stack=st)
        pE = [k.ps("m_pE%d" % i, [128, 512], stack=st) for i in range(2)]
        pF = k.ps("m_pF", [128, 512], stack=st)
        pA0_b, pA1_b, pC_b, pF_b = Buf(), Buf(), Buf(), Buf()
        pE_b = bufs(2)
        oi = 0
        vi = 0
        ti = 0
        nxt = htp.group(0)
        for g in range(k.NG):
            hTg, hTg_b, t0, w = nxt
            if g + 1 < k.NG:
                nxt = htp.group(g + 1)
            qlTg, qlTg_b = qlT[g % 2], qlT_b[g % 2]
            ckTg, ckTg_b = ckT[g % 2], ckT_b[g % 2]
            s.dma("sp", lambda E, g=g, t0=t0, w=w: E.dma_start(out=rc[g % 2][:, 0:w], in_=I["rope3c"][:, t0:t0 + w]), [IB["rope3c"]], [rt_b[g % 2]])
            s.dma("sp", lambda E, g=g, t0=t0, w=w: E.dma_start(out=rs[g % 2][:, 0:w], in_=I["rope3s"][:, t0:t0 + w]), [IB["rope3s"]], [rt_b[g % 2]])
            s.dma("sp", lambda E, g=g, t0=t0, w=w: E.dma_start(out=rkc[g % 2][:, 0:w], in_=I["rope3c"][64:96, t0:t0 + w]), [IB["rope3c"]], [rt_b[g % 2]])
            s.dma("sp", lambda E, g=g, t0=t0, w=w: E.dma_start(out=rks[g % 2][:, 0:w], in_=I["rope3s"][64:96, t0:t0 + w]), [IB["rope3s"]], [rt_b[g % 2]])
            for j in range(w // 128):
                i2 = ti % 2
                ti += 1
                for c in range(8):
                    s.op("pe", lambda E, c=c, j=j: E.matmul(pA0[:], lhsT=hTg[:, c, j * 128:(j + 1) * 128], rhs=Wdq[:, c, 0:512], start=(c == 0), stop=(c == 7)), [hTg_b, Wdq_b], [pA0_b], track=(c == 7))
                for c in range(8):
                    s.op("pe", lambda E, c=c, j=j: E.matmul(pA1[:, 0:256], lhsT=hTg[:, c, j * 128:(j + 1) * 128], rhs=Wdq[:, c, 512:768], start=(c == 0), stop=(c == 7)), [hTg_b, Wdq_b], [pA1_b], track=(c == 7))
                for c in range(8):
                    s.op("pe", lambda E, c=c, j=j: E.matmul(pC[:, 0:288], lhsT=hTg[:, c, j * 128:(j + 1) * 128], rhs=Wdkv[:, c, 0:288], start=(c == 0), stop=(c == 7)), [hTg_b, Wdkv_b], [pC_b], track=(c == 7))
                rms_rows(k, [(pA0[:], pA0_b, 512), (pA1[:, 0:256], pA1_b, 256)], 768, qnb, qnb_b, qn[i2], qn_b[i2], (junk, st4[i2]), st4_b[i2])
                transpose_tile(k, qn[i2], qn_b[i2], htp.tp, htp.tp_b, qlTg, qlTg_b, j * 128, nchunk=6)
                rms_rows(k, [(pC[:, 0:256], pC_b, 256)], 256, kvnb, kvnb_b, cn[i2], cn_b[i2], (junk, st4[i2]), st4_b[i2])
                transpose_tile(k, cn[i2], cn_b[i2], htp.tp, htp.tp_b, ckTg, ckTg_b, j * 128, nchunk=2)
            for c in range(8):
                s.op("pe", lambda E, c=c: E.matmul(pE[0][0:32, 0:w], lhsT=Wdkv[:, c, 256:288], rhs=hTg[:, c, 0:w], start=(c == 0), stop=(c == 7)), [hTg_b, Wdkv_b], [pE_b[0]], track=(c == 7))
            for c in range(8):
                s.op("pe", lambda E, c=c: E.matmul(pE[1][0:32, 0:w], lhsT=Wdkv[:, c, 288 + 256:288 + 288], rhs=hTg[:, c, 0:w], start=(c == 0), stop=(c == 7)), [hTg_b, Wdkv_b], [pE_b[1]], track=(c == 7))
            o_, o_b = ob[oi % 4], ob_b[oi % 4]
            oi += 1
            s.op("dve", lambda E: E.tensor_tensor(out=t1[0][0:32, 0:w], in0=pE[0][0:32, 0:w], in1=rkc[g % 2][:, 0:w], op=ALU.mult), [pE_b[0], rt_b[g % 2]], [t1_b[0]])
            s.op("dve", lambda E: E.tensor_tensor(out=t2[0][0:32, 0:w], in0=pE[1][0:32, 0:w], in1=rks[g % 2][:, 0:w], op=ALU.mult), [pE_b[1], rt_b[g % 2]], [t2_b[0]])
            s.op("pool", lambda E, o_=o_: E.tensor_tensor(out=o_[0:32, 0:w], in0=t1[0][0:32, 0:w], in1=t2[0][0:32, 0:w], op=ALU.add), [t1_b[0], t2_b[0]], [o_b])
            s.dma("pool", lambda E, o_=o_: E.dma_start(out=k.krT[:, t0:t0 + w], in_=o_[0:32, 0:w]), [o_b], [k.krT_b[g]])
            for hp in range(8):
                for c in range(2):
                    s.op("pe", lambda E, c=c, hp=hp: E.matmul(pF[:, 0:w], lhsT=Wuk[:, c, hp * 128:(hp + 1) * 128], rhs=ckTg[:, c, 0:w], start=(c == 0), stop=(c == 1)), [ckTg_b, Wuk_b], [pF_b], track=(c == 1))
                o_, o_b = ob[oi % 4], ob_b[oi % 4]
                oi += 1
                s.op("act", lambda E, o_=o_: E.activation(out=o_[:, 0:w], in_=pF[:, 0:w], func=AF.Copy), [pF_b], [o_b])
                s.dma("pool", lambda E, o_=o_, hp=hp: E.dma_start(out=k.kT[hp * 128:(hp + 1) * 128, t0:t0 + w], in_=o_[:, 0:w]), [o_b], [k.kT_b[g]])
            for h in range(16):
                for c in range(6):
                    s.op("pe", lambda E, c=c, h=h: E.matmul(pE[0][0:96, 0:w], lhsT=Wuq[:, c, h * 96:(h + 1) * 96], rhs=qlTg[:, c, 0:w], start=(c == 0), stop=(c == 5)), [qlTg_b, Wuq_b], [pE_b[0]], track=(c == 5))
                for c in range(6):
                    s.op("pe", lambda E, c=c, h=h: E.matmul(pE[1][0:96, 0:w], lhsT=Wuq[:, c, 1536 + h * 96:1536 + (h + 1) * 96], rhs=qlTg[:, c, 0:w], start=(c == 0), stop=(c == 5)), [qlTg_b, Wuq_b], [pE_b[1]], track=(c == 5))
                a1, a1_b = t1[h % 2], t1_b[h % 2]
                a2, a2_b = t2[h % 2], t2_b[h % 2]
                o_, o_b = ob[oi % 4], ob_b[oi % 4]
                oi += 1
                s.op("dve", lambda E, a1=a1: E.tensor_tensor(out=a1[0:96, 0:w], in0=pE[0][0:96, 0:w], in1=rc[g % 2][:, 0:w], op=ALU.mult), [pE_b[0], rt_b[g % 2]], [a1_b])
                s.op("dve", lambda E, a2=a2: E.tensor_tensor(out=a2[0:96, 0:w], in0=pE[1][0:96, 0:w], in1=rs[g % 2][:, 0:w], op=ALU.mult), [pE_b[1], rt_b[g % 2]], [a2_b])
                s.op("pool", lambda E, a1=a1, a2=a2, o_=o_: E.tensor_tensor(out=o_[0:96, 0:w], in0=a1[0:96, 0:w], in1=a2[0:96, 0:w], op=ALU.add), [a1_b, a2_b], [o_b])
                s.dma("pool", lambda E, o_=o_, h=h: E.dma_start(out=k.qT[h * 96:(h + 1) * 96, t0:t0 + w], in_=o_[0:96, 0:w]), [o_b], [k.qT_b[g]])
            for j in range(w // 128):
                v_, v_b = vo[vi % 2], vo_b[vi % 2]
                vi += 1
                for nb in range(2):
                    for c in range(2):
                        s.op("pe", lambda E, c=c, nb=nb, j=j: E.matmul(pF[:], lhsT=ckTg[:, c, j * 128:(j + 1) * 128], rhs=Wuv[:, c, nb * 512:(nb + 1) * 512], start=(c == 0), stop=(c == 1)), [ckTg_b, Wuv_b], [pF_b], track=(c == 1))
                    s.op("act", lambda E, nb=nb, v_=v_: E.activation(out=v_[:, nb * 512:(nb + 1) * 512], in_=pF[:], func=AF.Copy), [pF_b], [v_b])
                t = (t0 // 128) + j
                s.dma("pool", lambda E, t=t, v_=v_: E.dma_start(out=k.vv[t * 128:(t + 1) * 128, :], in_=v_[:]), [v_b], [k.vv_b[g]])
        s.barrier()
    with ExitStack() as st:
        def post(h, qb, w, O, O_b, Z, Z_b, wk):
            (r0, o0, o1, sq, on, pss, wk_b, pss_b) = wk
            s.op("act", lambda E: E.activation(out=o1[64:65, 0:w], in_=O[0][64:65, 0:w], func=AF.Copy), [O_b[0]], [wk_b])
            s.op("pe", lambda E: E.matmul(Z[0][0:64, 0:w], lhsT=k.ones_f[64:65, 0:64], rhs=o1[64:65, 0:w], start=True, stop=True), [wk_b, k.ones_bb], [Z_b[0]])
            s.op("dve", lambda E: E.reciprocal(out=r0[0:64, 0:w], in_=Z[0][0:64, 0:w]), [Z_b[0]], [wk_b])
            s.op("dve", lambda E: E.tensor_tensor(out=on[0:64, 0:w], in0=O[0][0:64, 0:w], in1=r0[0:64, 0:w], op=ALU.mult), [O_b[0], wk_b], [wk_b])
            return on
        attention(k, st, n_heads=16, maps=1, kp=96, dv=64, scale=float(96 ** -0.5), post=post, krows=64, qrows=96, extra=(k.krT, k.krT_b, 32), with_ctx_q=keep_ctx, aug=True)
        s.barrier()


def qkv_plain(k, st, l, pfx, wname, q_scale):
    s, I, IB = k.s, k.I, k.IB
    W, W_b = load_weight_bf16(k, st, pfx + "_w", I[wname], IB[wname], 8, 3 * D)
    htp = HTP(k, st, l, pfx)
    qo = [k.sb(pfx + "_qo%d" % i, [128, 512], BF16, stack=st) for i in range(4)]
    qo_b = bufs(4)
    vo = [k.sb(pfx + "_vo%d" % i, [128, D], BF16, stack=st) for i in range(2)]
    vo_b = bufs(2)
    pq = [k.ps(pfx + "_pq%d" % i, [128, 512], stack=st) for i in range(2)]
    pq_b = bufs(2)
    pv = [k.ps(pfx + "_pv%d" % i, [128, 512], stack=st) for i in range(2)]
    pv_b = bufs(2)
    oi = 0
    vi = 0
    nxt = htp.group(0)
    for g in range(k.NG):
        hTg, hTg_b, t0, w = nxt
        if g + 1 < k.NG:
            nxt = htp.group(g + 1)
        for o in range(16):
            p, p_b = pq[o % 2], pq_b[o % 2]
            for c in range(8):
                s.op("pe", lambda E, c=c, o=o, p=p: E.matmul(p[:, 0:w], lhsT=W[:, c, o * 128:(o + 1) * 128], rhs=hTg[:, c, 0:w], start=(c == 0), stop=(c == 7)), [W_b, hTg_b], [p_b], track=(c == 7))
            q_, q_b = qo[oi % 4], qo_b[oi % 4]
            oi += 1
            if o < 8:
                s.op("act", lambda E, p=p, q_=q_: E.activation(out=q_[:, 0:w], in_=p[:, 0:w], func=AF.Copy, scale=float(q_scale)), [p_b], [q_b])
            else:
                s.op("dve", lambda E, p=p, q_=q_: E.tensor_copy(out=q_[:, 0:w], in_=p[:, 0:w]), [p_b], [q_b])
            dstT = k.qT if o < 8 else k.kT
            dst_b = (k.qT_b if o < 8 else k.kT_b)[g]
            oo = o % 8
            s.dma("pool", lambda E, dstT=dstT, oo=oo, q_=q_: E.dma_start(out=dstT[oo * 128:(oo + 1) * 128, t0:t0 + w], in_=q_[:, 0:w]), [q_b], [dst_b])
        for j in range(w // 128):
            v_, v_b = vo[vi % 2], vo_b[vi % 2]
            vi += 1
            for nb in range(2):
                p, p_b = pv[nb], pv_b[nb]
                for c in range(8):
                    s.op("pe", lambda E, c=c, nb=nb, p=p, j=j: E.matmul(p[:], lhsT=hTg[:, c, j * 128:(j + 1) * 128], rhs=W[:, c, 2 * D + nb * 512:2 * D + (nb + 1) * 512], start=(c == 0), stop=(c == 7)), [W_b, hTg_b], [p_b], track=(c == 7))
                s.op("act", lambda E, nb=nb, p=p, v_=v_: E.activation(out=v_[:, nb * 512:(nb + 1) * 512], in_=p[:], func=AF.Copy), [p_b], [v_b])
            t = (t0 // 128) + j
            s.dma("pool", lambda E, t=t, v_=v_: E.dma_start(out=k.vv[t * 128:(t + 1) * 128, :], in_=v_[:]), [v_b], [k.vv_b[g]])


def mixer_na(k, l, keep_ctx):
    s, I, IB = k.s, k.I, k.IB
    with ExitStack() as st:
        qkv_plain(k, st, l, "n", "na_w", 0.125)
        s.barrier()
    with ExitStack() as st:
        KT = [k.sb("n_KT%d" % i, [64, NT], BF16, stack=st) for i in range(2)]
        QT = [k.sb("n_QT%d" % i, [64, NT], BF16, stack=st) for i in range(2)]
        Ve = [k.sb("n_Ve%d" % i, [128, 66, 64], BF16, stack=st) for i in range(2)]
        Vo = [k.sb("n_Vo%d" % i, [128, 65, 64], BF16, stack=st) for i in range(2)]
        hb_b = bufs(2)
        bf = [k.sb("n_bf%d" % i, [128, 2048], stack=st) for i in range(2)]
        bf_b = bufs(2)
        bb = [k.sb("n_bb%d" % i, [128, 8, 4, 64], BF16, stack=st) for i in range(2)]
        bb_b = bufs(2)
        P = [k.sb("n_P%d" % i, [128, 512], BF16, stack=st) for i in range(3)]
        P_b = bufs(3)
        r0t = k.sb("n_r0", [64, 512], stack=st)
        on = [k.sb("n_on%d" % i, [64, 512], BF16, stack=st) for i in range(2)]
        on_b = bufs(2)
        r0_b = Buf()
        S = [k.ps("n_S%d" % i, [128, 512], stack=st) for i in range(2)]
        S_b = bufs(2)
        O = [k.ps("n_O%d" % i, [64, 512], stack=st) for i in range(2)]
        O_b = bufs(2)
        Z = [k.ps("n_Z%d" % i, [64, 512], stack=st) for i in range(2)]
        Z_b = bufs(2)
        si = 0
        pi = 0
        oi = 0
        for h in range(16):
            i2 = h % 2
            KTh, QTh, Veh, Voh, hb = KT[i2], QT[i2], Ve[i2], Vo[i2], hb_b[i2]
            s.dma("sp", lambda E, h=h, KTh=KTh: E.dma_start(out=KTh[:, :], in_=k.kT[h * 64:(h + 1) * 64, :]), list(k.kT_b), [hb])
            s.dma("sp", lambda E, h=h, QTh=QTh: E.dma_start(out=QTh[:, :], in_=k.qT[h * 64:(h + 1) * 64, :]), list(k.qT_b), [hb])
            for half in range(2):
                s.dma("sp", lambda E, h=h, Veh=Veh, half=half: E.dma_start(out=Veh[:, half * 33:(half + 1) * 33, :], in_=k.vv[half * 33 * 128:(half + 1) * 33 * 128, h * 64:(h + 1) * 64].rearrange("(t p) d -> p t d", p=128)), list(k.vv_b), [hb])
            s.dma("sp", lambda E, h=h, Voh=Voh: E.dma_start(out=Voh[:, 0:33, :], in_=k.vv[64:64 + 33 * 128, h * 64:(h + 1) * 64].rearrange("(t p) d -> p t d", p=128)), list(k.vv_b), [hb])
            s.dma("sp", lambda E, h=h, Voh=Voh: E.dma_start(out=Voh[:, 33:65, :], in_=k.vv[64 + 33 * 128:64 + 65 * 128, h * 64:(h + 1) * 64].rearrange("(t p) d -> p t d", p=128)), list(k.vv_b), [hb])
            s.dma("sp", lambda E, h=h: E.dma_start(out=bf[i2][:], in_=I["na_bias"][h * 128:(h + 1) * 128, :]), [IB["na_bias"]], [bf_b[i2]])
            s.op("pool", lambda E: E.tensor_copy(out=bb[i2][:].rearrange("p a b c -> p (a b c)"), in_=bf[i2][:]), [bf_b[i2]], [bb_b[i2]])
            blocks = [("lat", rg) for rg in range(16)] + ([("ctx", 0)] if keep_ctx else [])
            for (kind, rg) in blocks:
                Ox, Ox_b, Zx, Zx_b = O[oi % 2], O_b[oi % 2], Z[oi % 2], Z_b[oi % 2]
                onx, onx_b = on[oi % 2], on_b[oi % 2]
                oi += 1
                if kind == "lat":
                    items = []
                    for i in range(8):
                        r = rg * 8 + i
                        r0 = min(max(r - 4, 0), 120)
                        items.append((r, r0, r - r0))
                    for i, (r, r0, v) in enumerate(items):
                        Sx, Sx_b = S[si % 2], S_b[si % 2]
                        si += 1
                        Px, Px_b = P[pi % 3], P_b[pi % 3]
                        pi += 1
                        for kt in range(4):
                            tok0 = r0 * 64 + kt * 128
                            s.op("pe", lambda E, Sx=Sx, kt=kt, tok0=tok0, r=r: E.matmul(Sx[:, kt * 64:(kt + 1) * 64], lhsT=KTh[:, tok0:tok0 + 128], rhs=QTh[:, r * 64:(r + 1) * 64], start=True, stop=False), [hb], [Sx_b], track=False)
                            s.op("pe", lambda E, Sx=Sx, kt=kt, v=v: E.matmul(Sx[:, kt * 64:(kt + 1) * 64], lhsT=k.identb[:], rhs=bb[i2][:, v, kt, :], start=False, stop=True), [k.identb_b, bb_b[i2]], [Sx_b], track=False)
                        for c in range(2):
                            s.op("pe", lambda E, Sx=Sx, c=c, r=r: E.matmul(Sx[:, (4 + c) * 64:(5 + c) * 64], lhsT=KTh[:, NL + c * 128:NL + (c + 1) * 128], rhs=QTh[:, r * 64:(r + 1) * 64], start=True, stop=True), [hb], [Sx_b], track=(c == 1))
                        s.op("act", lambda E, Sx=Sx, Px=Px: E.activation(out=Px[:, 0:384], in_=Sx[:, 0:384], func=AF.Exp), [Sx_b], [Px_b])
                        for kt in range(6):
                            if kt < 4:
                                vt = Veh[:, r0 // 2 + kt, :] if r0 % 2 == 0 else Voh[:, (r0 - 1) // 2 + kt, :]
                            else:
                                vt = Veh[:, 64 + (kt - 4), :]
                            s.op("pe", lambda E, vt=vt, Px=Px, kt=kt, i=i, Ox=Ox: E.matmul(Ox[:, i * 64:(i + 1) * 64], lhsT=vt, rhs=Px[:, kt * 64:(kt + 1) * 64], start=(kt == 0), stop=(kt == 5)), [hb, Px_b], [Ox_b], track=False)
                            s.op("pe", lambda E, Px=Px, kt=kt, i=i, Zx=Zx: E.matmul(Zx[:, i * 64:(i + 1) * 64], lhsT=k.ones_b[:, 0:64], rhs=Px[:, kt * 64:(kt + 1) * 64], start=(kt == 0), stop=(kt == 5)), [k.ones_bb, Px_b], [Zx_b], track=(kt == 5))
                    w = 512
                    c0 = rg * 512
                else:
                    Sx, Sx_b = S[si % 2], S_b[si % 2]
                    si += 1
                    Px, Px_b = P[pi % 3], P_b[pi % 3]
                    pi += 1
                    for c in range(2):
                        s.op("pe", lambda E, Sx=Sx, c=c: E.matmul(Sx[:, c * 256:(c + 1) * 256], lhsT=KTh[:, NL + c * 128:NL + (c + 1) * 128], rhs=QTh[:, NL:NT], start=True, stop=True), [hb], [Sx_b], track=(c == 1))
                    s.op("act", lambda E, Sx=Sx, Px=Px: E.activation(out=Px[:, :], in_=Sx[:, :], func=AF.Exp), [Sx_b], [Px_b])
                    for c in range(2):
                        s.op("pe", lambda E, Px=Px, c=c, Ox=Ox: E.matmul(Ox[:, 0:256], lhsT=Veh[:, 64 + c, :], rhs=Px[:, c * 256:(c + 1) * 256], start=(c == 0), stop=(c == 1)), [hb, Px_b], [Ox_b], track=False)
                        s.op("pe", lambda E, Px=Px, c=c, Zx=Zx: E.matmul(Zx[:, 0:256], lhsT=k.ones_b[:, 0:64], rhs=Px[:, c * 256:(c + 1) * 256], start=(c == 0), stop=(c == 1)), [k.ones_bb, Px_b], [Zx_b], track=(c == 1))
                    w = 256
                    c0 = NL
                s.op("dve", lambda E, Zx=Zx, w=w: E.reciprocal(out=r0t[:, 0:w], in_=Zx[:, 0:w]), [Zx_b], [r0_b])
                s.op("dve", lambda E, Ox=Ox, onx=onx, w=w: E.tensor_tensor(out=onx[:, 0:w], in0=Ox[:, 0:w], in1=r0t[:, 0:w], op=ALU.mult), [Ox_b, r0_b], [onx_b])
                g0 = c0 // 512
                s.dma("pool", lambda E, h=h, c0=c0, w=w, onx=onx: E.dma_start(out=k.aT[h * 64:(h + 1) * 64, c0:c0 + w], in_=onx[:, 0:w]), [onx_b], [k.aT_b[g0]])
        s.barrier()


def mixer_fourier(k, l, keep_ctx):
    s, I, IB = k.s, k.I, k.IB
    with ExitStack() as st:
        htp = HTP(k, st, l, "f")
        hb = [k.sb("f_hb%d" % i, [128, D], BF16, stack=st) for i in range(2)]
        hb_b = bufs(2)
        for t in range(NTILE):
            h_, h_b = htp.tile(t)
            s.op("act", lambda E, h_=h_, t=t: E.activation(out=hb[t % 2][:], in_=h_[:], func=AF.Copy), [h_b], [hb_b[t % 2]])
            s.dma("pool", lambda E, t=t: E.dma_start(out=k.h2tab[t * 128:(t + 1) * 128, :], in_=hb[t % 2][:]), [hb_b[t % 2]], [k.h2tab_b[t]])
        s.barrier()
    with ExitStack() as st:
        stg = k.sb("f_stg", [128, 2048], stack=st)
        stg_b = Buf()
        COS = k.sb("f_cos", [128, 64, 128], BF16, stack=st)
        SIN = k.sb("f_sin", [128, 64, 128], BF16, stack=st)
        F64 = k.sb("f_f64", [64, 4, 48], BF16, stack=st)
        CC = k.sb("f_cc", [128, 2, 256], BF16, stack=st)
        SC = k.sb("f_sc", [128, 2, 256], BF16, stack=st)
        NSC = k.sb("f_nsc", [128, 2, 256], BF16, stack=st)
        tab_b = Buf()
        for (dst, name) in [(COS, "fn_cos"), (SIN, "fn_sin")]:
            for q in range(4):
                s.dma("sp", lambda E, name=name, q=q: E.dma_start(out=stg[:], in_=I[name][:, q * 2048:(q + 1) * 2048]), [IB[name]], [stg_b])
                s.op("dve", lambda E, dst=dst, q=q: E.tensor_copy(out=dst[:].rearrange("p a b -> p (a b)")[:, q * 2048:(q + 1) * 2048], in_=stg[:]), [stg_b], [tab_b])
        s.dma("sp", lambda E: E.dma_start(out=stg[0:64, 0:192], in_=I["fn_f64"][:, :]), [], [stg_b])
        s.op("dve", lambda E: E.tensor_copy(out=F64[:].rearrange("p a b -> p (a b)"), in_=stg[0:64, 0:192]), [stg_b], [tab_b])
        for (dst, name) in [(CC, "fn_cc"), (SC, "fn_sc"), (NSC, "fn_nsc")]:
            s.dma("sp", lambda E, name=name: E.dma_start(out=stg[:, 0:512].rearrange("p (a b) -> p a b", a=2), in_=I[name].rearrange("(a p) l -> p a l", p=128)), [], [stg_b])
            s.op("dve", lambda E, dst=dst: E.tensor_copy(out=dst[:].rearrange("p a b -> p (a b)"), in_=stg[:, 0:512]), [stg_b], [tab_b])
        Xs = k.sb("f_Xs", [64, 128, 128], BF16, stack=st)
        Xs_b = Buf()
        A = k.sb("f_A", [128, 48, 128], BF16, stack=st)
        A_b = Buf()
        GTr = k.sb("f_GTr", [128, 2, NL], BF16, stack=st)
        GTi = k.sb("f_GTi", [128, 2, NL], BF16, stack=st)
        GT_b = Buf()
        hc = k.sb("f_hc", [128, 2, 256], BF16, stack=st)
        hc_b = Buf()
        yo = [k.sb("f_yo%d" % i, [128, 512], BF16, stack=st) for i in range(2)]
        yo_b = bufs(2)
        PA = [k.ps("f_PA%d" % i, [128, 512], stack=st) for i in range(2)]
        PA_b = bufs(2)
        PG = [k.ps("f_PG%d" % i, [128, 512], stack=st) for i in range(4)]
        PG_b = bufs(4)
        PY = [k.ps("f_PY%d" % i, [128, 512], stack=st) for i in range(2)]
        PY_b = bufs(2)
        ai = 0
        gi = 0
        yi = 0

        def channel_dft(gq, k0, kw, nk, scale, col0):
            nonlocal yi
            for lc in range(2):
                for kb in range(nk):
                    p, p_b = PY[yi % 2], PY_b[yi % 2]
                    y_, y_b = yo[yi % 2], yo_b[yi % 2]
                    yi += 1
                    ka = k0 + kb * kw
                    n = 0
                    for cc in range(2):
                        for (T, Gx) in [(CC, GTr), (SC, GTi)]:
                            s.op("pe", lambda E, T=T, Gx=Gx, cc=cc, lc=lc, ka=ka, p=p, n=n: E.matmul(p[:, 0:kw], lhsT=T[:, cc, lc * 128:(lc + 1) * 128], rhs=Gx[:, cc, ka:ka + kw], start=(n == 0), stop=(n == 3)), [tab_b, GT_b], [p_b], track=(n == 3))
                            n += 1
                    s.op("act", lambda E, p=p, y_=y_: E.activation(out=y_[:, 0:kw], in_=p[:, 0:kw], func=AF.Copy, scale=float(scale)), [p_b], [y_b])
                    g0 = (col0 + ka - k0) // 512
                    s.dma("pool", lambda E, y_=y_, lc=lc, ka=ka: E.dma_start(out=k.aT[gq * 256 + lc * 128:gq * 256 + (lc + 1) * 128, col0 + ka - k0:col0 + ka - k0 + kw], in_=y_[:, 0:kw]), [y_b], [k.aT_b[g0]])

        for gq in range(4):
            for cc in range(2):
                col = gq * 256 + cc * 128
                s.dma("sp", lambda E, col=col: E.dma_start(out=Xs[:, :, :], in_=k.h2tab[0:NL, col:col + 128].rearrange("(a b) c -> a b c", b=128)), list(k.h2tab_b), [Xs_b])
                for kb in range(4):
                    for c8 in range(16):
                        p, p_b = PA[ai % 2], PA_b[ai % 2]
                        for ci in range(8):
                            c = c8 * 8 + ci
                            s.op("pe", lambda E, p=p, ci=ci, c=c, kb=kb: E.matmul(p[:, ci * 48:(ci + 1) * 48], lhsT=Xs[:, :, c], rhs=F64[:, kb, :], start=True, stop=True), [Xs_b, tab_b], [p_b], track=(ci == 7))
                        eng = ["act", "dve"][ai % 2]
                        ai += 1
                        if eng == "act":
                            s.op("act", lambda E, p=p, c8=c8: E.activation(out=A[:, :, c8 * 8:(c8 + 1) * 8], in_=p[:, 0:384].rearrange("p (c j) -> p j c", j=48), func=AF.Copy), [p_b], [A_b])
                        else:
                            s.op("dve", lambda E, p=p, c8=c8: E.tensor_copy(out=A[:, :, c8 * 8:(c8 + 1) * 8], in_=p[:, 0:384].rearrange("p (c j) -> p j c", j=48)), [p_b], [A_b])
                    for quad in range(4):
                        pr, pr_b = PG[gi % 4], PG_b[gi % 4]
                        pim, pim_b = PG[(gi + 1) % 4], PG_b[(gi + 1) % 4]
                        gi += 2
                        for q in range(4):
                            k1l = quad * 4 + q
                            k1 = kb * 16 + k1l
                            s.op("pe", lambda E, pr=pr, q=q, k1l=k1l, k1=k1: E.matmul(pr[:, q * 128:(q + 1) * 128], lhsT=A[:, k1l, :], rhs=COS[:, k1, :], start=True, stop=False), [A_b, tab_b], [pr_b], track=False)
                            s.op("pe", lambda E, pr=pr, q=q, k1l=k1l, k1=k1: E.matmul(pr[:, q * 128:(q + 1) * 128], lhsT=A[:, 16 + k1l, :], rhs=SIN[:, k1, :], start=False, stop=True), [A_b, tab_b], [pr_b], track=(q == 3))
                            s.op("pe", lambda E, pim=pim, q=q, k1l=k1l, k1=k1: E.matmul(pim[:, q * 128:(q + 1) * 128], lhsT=A[:, 16 + k1l, :], rhs=COS[:, k1, :], start=True, stop=False), [A_b, tab_b], [pim_b], track=False)
                            s.op("pe", lambda E, pim=pim, q=q, k1l=k1l, k1=k1: E.matmul(pim[:, q * 128:(q + 1) * 128], lhsT=A[:, 32 + k1l, :], rhs=SIN[:, k1, :], start=False, stop=True), [A_b, tab_b], [pim_b], track=(q == 3))
                        k10 = kb * 16 + quad * 4
                        s.op("act", lambda E, pr=pr, cc=cc, k10=k10: E.activation(out=GTr[:, cc, :].rearrange("p (b a) -> p a b", a=64)[:, k10:k10 + 4, :], in_=pr[:].rearrange("p (q b) -> p q b", q=4), func=AF.Copy), [pr_b], [GT_b])
                        s.op("dve", lambda E, pim=pim, cc=cc, k10=k10: E.tensor_copy(out=GTi[:, cc, :].rearrange("p (b a) -> p a b", a=64)[:, k10:k10 + 4, :], in_=pim[:].rearrange("p (q b) -> p q b", q=4)), [pim_b], [GT_b])
            channel_dft(gq, 0, 512, 16, 1.0 / math.sqrt(NL * 256.0), 0)
            if keep_ctx:
                s.dma("sp", lambda E, gq=gq: E.dma_start(out=hc[:, :, :], in_=k.h2tab[NL:NT, gq * 256:(gq + 1) * 256].rearrange("(t p) c -> p t c", p=128)), list(k.h2tab_b), [hc_b])
                for cc in range(2):
                    pr, pr_b = PG[gi % 4], PG_b[gi % 4]
                    pim, pim_b = PG[(gi + 1) % 4], PG_b[(gi + 1) % 4]
                    gi += 2
                    for t in range(2):
                        s.op("pe", lambda E, pr=pr, t=t, cc=cc: E.matmul(pr[:, 0:256], lhsT=hc[:, t, cc * 128:(cc + 1) * 128], rhs=CC[:, t, :], start=(t == 0), stop=(t == 1)), [hc_b, tab_b], [pr_b], track=(t == 1))
                    for t in range(2):
                        s.op("pe", lambda E, pim=pim, t=t, cc=cc: E.matmul(pim[:, 0:256], lhsT=hc[:, t, cc * 128:(cc + 1) * 128], rhs=NSC[:, t, :], start=(t == 0), stop=(t == 1)), [hc_b, tab_b], [pim_b], track=(t == 1))
                    s.op("act", lambda E, pr=pr, cc=cc: E.activation(out=GTr[:, cc, 0:256], in_=pr[:, 0:256], func=AF.Copy), [pr_b], [GT_b])
                    s.op("dve", lambda E, pim=pim, cc=cc: E.tensor_copy(out=GTi[:, cc, 0:256], in_=pim[:, 0:256]), [pim_b], [GT_b])
                channel_dft(gq, 0, 256, 1, 1.0 / 256.0, NL)
        s.barrier()


def post_mixer_and_moe(k, l, keep_ctx):
    nc, s, I = k.nc, k.s, k.I
    wo_name = {0: "da_wo", 1: "fn_wo", 2: "na_wo", 3: "mla_wo"}[l % 4]
    wo_src, wo_src_b = I[wo_name], k.IB[wo_name]
    ntile = NTILE if keep_ctx else 64
    ngrp = k.NG if keep_ctx else 16
    conds = [0, 1] if keep_ctx else [0]
    es2 = ExitStack()
    aff = k.sb("r_aff", [128, NTILE, NEXP], stack=es2)
    aff_b = Buf()
    posi = k.sb("r_posi", [128, NTILE, NEXP], I32, stack=es2)
    posi_b = Buf()
    G2b = [k.sb("r_G2b%d" % c, [128, D], stack=es2) for c in range(2)]
    G2b_b = bufs(2)
    for c in conds:
        load_bcast(k, "sp", G2b[c][:], G2b_b[c], l, 5, c)
    with ExitStack() as st:
        Wo, Wo_b = load_weight_bf16(k, st, "o_w", wo_src, wo_src_b, 8, D)
        wr = k.sb("o_wr", [128, 8, NEXP], stack=st)
        wr_b = Buf()
        s.dma("sp", lambda E: E.dma_start(out=wr[:], in_=I["moe_wr"][l].rearrange("(c p) e -> p c e", p=128)), [], [wr_b])
        G1b = [k.sb("o_G1b%d" % c, [128, D], stack=st) for c in range(2)]
        A2b = [k.sb("o_A2b%d" % c, [128, D], stack=st) for c in range(2)]
        B2b = [k.sb("o_B2b%d" % c, [128, D], stack=st) for c in range(2)]
        G1b_b, A2b_b, B2b_b = bufs(2), bufs(2), bufs(2)
        for c in conds:
            load_bcast(k, "sp", G1b[c][:], G1b_b[c], l, 2, c)
            load_bcast(k, "sp", A2b[c][:], A2b_b[c], l, 3, c)
            load_bcast(k, "sp", B2b[c][:], B2b_b[c], l, 4, c)
        aTs = [k.sb("o_aT%d" % i, [128, 8, 512], BF16, stack=st) for i in range(2)]
        aTs_b = bufs(2)
        xt = [k.sb("o_xt%d" % i, [128, D], stack=st) for i in range(4)]
        xt_b = bufs(4)
        ht = [k.sb("o_ht%d" % i, [128, D], stack=st) for i in range(4)]
        ht_b = bufs(4)
        hb = [k.sb("o_hb%d" % i, [128, D], BF16, stack=st) for i in range(4)]
        hb_b = bufs(4)
        junk = k.sb("o_junk", [128, D], stack=st)
        stat = [k.sb("o_stat%d" % i, [128, 2], stack=st) for i in range(4)]
        scr_b = bufs(4)
        hT = [k.sb("o_hT%d" % i, [128, 8, 128], stack=st) for i in range(4)]
        hT_b = bufs(4)
        lg2 = [k.sb("o_lg%d" % i, [128, NEXP], stack=st) for i in range(2)]
        lgs2 = [k.sb("o_lgs%d" % i, [128, 2], stack=st) for i in range(2)]
        lg2_b = bufs(2)
        py = [k.ps("o_py%d" % i, [128, 512], stack=st) for i in range(2)]
        py_b = bufs(2)
        tp = [k.ps("o_tp%d" % i, [128, 512], stack=st) for i in range(2)]
        tp_b = bufs(2)
        pl = k.ps("o_pl", [128, NEXP], stack=st)
        pl_b = Buf()
        tiles = []
        for g in range(ngrp):
            t0, w = group_range(g)
            for j in range(w // 128):
                tiles.append((g, t0, w, j, t0 // 128 + j))

        def S1(ti):
            g, t0, w, j, t = tiles[ti]
            cond = 0 if t < 64 else 1
            a_, a_b = aTs[g % 2], aTs_b[g % 2]
            if j == 0:
                s.dma("sp", lambda E: E.dma_start(out=a_[:, :, 0:w], in_=k.aT[:, t0:t0 + w].rearrange("(c p) t -> p c t", p=128)), [k.aT_b[g]], [a_b])
            x_, x_b = xt[ti % 4], xt_b[ti % 4]
            h_, h_b = ht[ti % 4], ht_b[ti % 4]
            s.dma("sp", lambda E: E.dma_start(out=x_[:], in_=k.xres[t * 128:(t + 1) * 128, :]), [k.xres_b[t]], [x_b])
            for nb in range(2):
                p, p_b = py[nb], py_b[nb]
                for c in range(8):
                    s.op("pe", lambda E: E.matmul(p[:], lhsT=a_[:, c, j * 128:(j + 1) * 128], rhs=Wo[:, c, nb * 512:(nb + 1) * 512], start=(c == 0), stop=(c == 7)),
                         [a_b, Wo_b], [p_b], track=(c == 7))
                s.op("dve", lambda E: E.tensor_tensor(out=h_[:, nb * 512:(nb + 1) * 512], in0=p[:], in1=G1b[cond][:, nb * 512:(nb + 1) * 512], op=ALU.mult),
                     [p_b, G1b_b[cond]], [h_b])
            s.op("pool", lambda E: E.tensor_tensor(out=x_[:], in0=x_[:], in1=h_[:], op=ALU.add), [x_b, h_b], [x_b])
            s.dma("pool", lambda E: E.dma_start(out=k.xres[t * 128:(t + 1) * 128, :], in_=x_[:]), [x_b], [k.xres_b[t]])

        def S2(ti):
            g, t0, w, j, t = tiles[ti]
            cond = 0 if t < 64 else 1
            x_, x_b = xt[ti % 4], xt_b[ti % 4]
            h_, h_b = ht[ti % 4], ht_b[ti % 4]
            hb_, hb_bb = hb[ti % 4], hb_b[ti % 4]
            norm_tile(k, x_, x_b, h_, h_b, A2b[cond], A2b_b[cond], B2b[cond], B2b_b[cond], (junk, stat[ti % 4]), scr_b[ti % 4])
            s.op("act", lambda E: E.activation(out=hb_[:], in_=h_[:], func=AF.Copy), [h_b], [hb_bb])
            s.dma("pool", lambda E: E.dma_start(out=k.h2tab[t * 128:(t + 1) * 128, :], in_=hb_[:]), [hb_bb], [k.h2tab_b[t]])

        def S3(ti):
            g, t0, w, j, t = tiles[ti]
            h_, h_b = ht[ti % 4], ht_b[ti % 4]
            hT_, hT_bb = hT[ti % 4], hT_b[ti % 4]
            transpose_tile_f32(k, h_, h_b, tp, tp_b, hT_, hT_bb)
            for c in range(8):
                s.op("pe", lambda E: E.matmul(pl[:], lhsT=hT_[:, c, :], rhs=wr[:, c, :], start=(c == 0), stop=(c == 7)),
                     [hT_bb, wr_b], [pl_b], track=(c == 7))
            lg, lgs, lg_b = lg2[ti % 2], lgs2[ti % 2], lg2_b[ti % 2]
            s.op("act", lambda E: E.activation(out=lg[:], in_=pl[:], func=AF.Exp, accum_out=lgs[:, 0:1]), [pl_b], [lg_b])
            s.op("dve", lambda E: E.reciprocal(out=lgs[:, 1:2], in_=lgs[:, 0:1]), [lg_b], [lg_b])
            s.op("dve", lambda E: E.tensor_scalar(out=aff[:, t, :], in0=lg[:], scalar1=lgs[:, 1:2], scalar2=None, op0=ALU.mult), [lg_b], [aff_b])

        nt_ = len(tiles)
        for i in range(nt_ + 2):
            if i < nt_:
                S1(i)
            if 0 <= i - 1 < nt_:
                S2(i - 1)
            if 0 <= i - 2 < nt_:
                S3(i - 2)
        s.barrier()
    if k.stop == "postA%d" % l:
        es2.close()
        raise _Stop()
    routing(k, l, keep_ctx, aff, aff_b, posi, posi_b)
    if k.stop == "route%d" % l:
        es2.close()
        raise _Stop()
    experts(k, l, keep_ctx, G2b, G2b_b)
    es2.close()
    s.barrier()


def transpose_tile_f32(k, ht, ht_b, tp, tp_b, hT, hT_b):
    s = k.s
    for half in range(2):
        p, pb = tp[half], tp_b[half]
        for j in range(4):
            c = half * 4 + j
            s.op("pe", lambda E, c=c, j=j, p=p: E.transpose(out=p[:, j * 128:(j + 1) * 128], in_=ht[:, c * 128:(c + 1) * 128], identity=k.ident[:]),
                 [ht_b, k.ident_b], [pb], track=(j == 3))
        if half == 0:
            s.op("act", lambda E, p=p: E.activation(out=hT[:, 0:4, :], in_=p[:].rearrange("p (c t) -> p c t", c=4), func=AF.Copy), [pb], [hT_b])
        else:
            s.op("dve", lambda E, p=p: E.tensor_copy(out=hT[:, 4:8, :], in_=p[:].rearrange("p (c t) -> p c t", c=4)), [pb], [hT_b])


def routing(k, l, keep_ctx, aff, aff_b, posi, posi_b):
    nc, s, I = k.nc, k.s, k.I
    BIG = float(2 ** 20)
    with ExitStack() as st:
        affT = k.sb("g_affT", [NEXP, NT], stack=st)
        affT_b = Buf()
        msk = k.sb("g_msk", [NEXP, NT], stack=st)
        msk_b = Buf()
        cum = k.sb("g_cum", [NEXP, NT], stack=st)
        cum_b = Buf()
        onesr = k.sb("g_ones", [NEXP, NL], stack=st)
        onesr_b = Buf()
        sv = k.sb("g_sv", [NEXP, 8], stack=st)
        sv_b = Buf()
        posf = k.sb("g_posf", [128, NTILE, NEXP], stack=st)
        posf_b = Buf()
        metas = k.sb("g_metas", [128, NTILE, NEXP, 2], stack=st)
        metas_b = Buf()
        tp = [k.ps("g_tp%d" % i, [128, 512], stack=st) for i in range(2)]
        tp_b = bufs(2)
        s.op("pool", lambda E: E.memset(onesr[:], 1.0), [], [onesr_b])
        ebase = k.sb("g_ebase", [NEXP, 2], stack=st)
        ebase_b = Buf()
        s.dma("sp", lambda E: E.dma_start(out=ebase[:], in_=I["ebase"][:, :]), [], [ebase_b])
        for e in range(NEXP):
            s.dma("sp", lambda E, e=e: E.dma_start(out=k.meta[e * SLOTS:(e + 1) * SLOTS, :], in_=I["metainit"][e * SLOTS:(e + 1) * SLOTS, :]), [], [k.meta_b[e]])
        ntile = NTILE if keep_ctx else 64
        for t4 in range(0, ntile, 4):
            p, pb = tp[(t4 // 4) % 2], tp_b[(t4 // 4) % 2]
            n = min(4, ntile - t4)
            for j in range(n):
                s.op("pe", lambda E, t4=t4, j=j, p=p: E.transpose(out=p[0:NEXP, j * 128:(j + 1) * 128], in_=aff[:, t4 + j, :], identity=k.ident[:]),
                     [aff_b, k.ident_b], [pb], track=(j == n - 1))
            s.op("act", lambda E, t4=t4, n=n, p=p: E.activation(out=affT[:, t4 * 128:(t4 + n) * 128], in_=p[0:NEXP, 0:n * 128], func=AF.Copy), [pb], [affT_b])
        segs = [(0, NL, CAP_L, 0, 0)]
        if keep_ctx:
            segs.append((NL, NT, CAP_C, 4, CAP_L))
        for (a, b, cap, so, base) in segs:
            lo, mid, cntv, stp = (sv[:, so + i:so + i + 1] for i in range(4))
            s.op("dve", lambda E, lo=lo: E.memset(lo, 0.0), [], [sv_b])
            for it in range(30):
                wstep = float(2.0 ** -(it + 1))
                s.op("dve", lambda E, lo=lo, mid=mid, wstep=wstep: E.tensor_scalar(out=mid, in0=lo, scalar1=wstep, scalar2=None, op0=ALU.add), [sv_b], [sv_b])
                s.op("dve", lambda E, mid=mid, cntv=cntv, a=a, b=b: E.tensor_scalar(out=msk[:, a:b], in0=affT[:, a:b], scalar1=mid, scalar2=0.0, op0=ALU.is_ge, op1=ALU.add, accum_out=cntv),
                     [affT_b, sv_b], [msk_b, sv_b])
                s.op("dve", lambda E, cntv=cntv, stp=stp, cap=cap, wstep=wstep: E.tensor_scalar(out=stp, in0=cntv, scalar1=float(cap), scalar2=wstep, op0=ALU.is_ge, op1=ALU.mult), [sv_b], [sv_b])
                s.op("dve", lambda E, lo=lo, stp=stp: E.tensor_tensor(out=lo, in0=lo, in1=stp, op=ALU.add), [sv_b], [sv_b])
            s.op("dve", lambda E, lo=lo, a=a, b=b: E.tensor_scalar(out=msk[:, a:b], in0=affT[:, a:b], scalar1=lo, scalar2=None, op0=ALU.is_ge), [affT_b, sv_b, msk_b], [msk_b])
            s.op("dve", lambda E, a=a, b=b: E.tensor_tensor_scan(out=cum[:, a:b], data0=onesr[:, 0:b - a], data1=msk[:, a:b], initial=0.0, op0=ALU.mult, op1=ALU.add),
                 [msk_b, onesr_b], [cum_b])
            col = 0 if base == 0 else 1
            s.op("dve", lambda E, a=a, b=b, cap=cap: E.scalar_tensor_tensor(out=msk[:, a:b], in0=cum[:, a:b], scalar=float(cap), in1=msk[:, a:b], op0=ALU.is_le, op1=ALU.mult), [msk_b, cum_b], [msk_b])
            s.op("dve", lambda E, a=a, b=b, col=col: E.scalar_tensor_tensor(out=cum[:, a:b], in0=cum[:, a:b], scalar=ebase[:, col:col + 1], in1=msk[:, a:b], op0=ALU.add, op1=ALU.mult), [msk_b, cum_b, ebase_b], [cum_b])
            s.op("dve", lambda E, a=a, b=b: E.tensor_scalar(out=cum[:, a:b], in0=cum[:, a:b], scalar1=BIG, scalar2=None, op0=ALU.add), [cum_b], [cum_b])
        for t4 in range(0, ntile, 4):
            p, pb = tp[(t4 // 4) % 2], tp_b[(t4 // 4) % 2]
            n = min(4, ntile - t4)
            for j in range(n):
                s.op("pe", lambda E, t4=t4, j=j, p=p: E.transpose(out=p[:, j * NEXP:(j + 1) * NEXP], in_=cum[:, (t4 + j) * 128:(t4 + j + 1) * 128], identity=k.ident[0:NEXP, 0:NEXP]),
                     [cum_b, k.ident_b], [pb], track=(j == n - 1))
            s.op("act", lambda E, t4=t4, n=n, p=p: E.activation(out=posf[:, t4:t4 + n, :], in_=p[:, 0:n * NEXP].rearrange("p (t e) -> p t e", t=n), func=AF.Copy), [pb], [posf_b])
        s.op("dve", lambda E: E.tensor_copy(out=posi[:, 0:ntile, :], in_=posf[:, 0:ntile, :]), [posf_b], [posi_b])
        s.op("pool", lambda E: E.tensor_copy(out=metas[:, 0:ntile, :, 0], in_=k.tokid[:, 0:ntile].unsqueeze(2).broadcast_to([128, ntile, NEXP])), [k.tokid_b], [metas_b])
        s.op("pool", lambda E: E.tensor_copy(out=metas[:, 0:ntile, :, 1], in_=aff[:, 0:ntile, :]), [aff_b, metas_b], [metas_b])
        regs = {}

        def mkregs(E):
            regs["l"] = E.alloc_register("bnd_l%d" % l)
            E.reg_mov(regs["l"], NEXP * SLOTS - 1)
        s.raw("pool", mkregs)
        for t in range(ntile):
            rk = "l"
            for e in range(NEXP):
                s.dma("pool", lambda E, t=t, e=e, rk=rk: E.indirect_dma_start(
                    out=k.meta[:, :], out_offset=bass.IndirectOffsetOnAxis(ap=posi[:, t, e:e + 1], axis=0),
                    in_=metas[:, t, e, :], in_offset=None, bounds_check=Lazy(lambda: regs["l"]), oob_is_err=False),
                    [posi_b, metas_b], [k.meta_b[e]])

        def freeregs(E):
            E.free_register(regs["l"])
        s.raw("pool", freeregs)
        s.barrier()


def experts(k, l, keep_ctx, G2b, G2b_b):
    nc, s, I = k.nc, k.s, k.I
    nst = 9 if keep_ctx else 8
    nsl = nst * 128
    groups = [(0, 512), (512, 512)] + ([(1024, 128)] if keep_ctx else [])
    with ExitStack() as st:
        mt = [k.sb("e_mt%d" % i, [128, 9, 2], stack=st) for i in range(2)]
        mt_b = bufs(2)
        idx = [k.sb("e_idx%d" % i, [128, 9], I32, stack=st) for i in range(2)]
        idx_b = bufs(2)
        xs = [k.sb("e_xs%d" % i, [128, D], BF16, stack=st) for i in range(9)]
        xs_b = bufs(9)
        xsT2 = [k.sb("e_xsT%d" % i, [128, 8, SLOTS], BF16, stack=st) for i in range(2)]
        xsT2_b = bufs(2)
        aT = k.sb("e_aT", [128, 16, SLOTS], BF16, stack=st)
        aT_b = Buf()
        wstg = [k.sb("e_ws%d" % i, [128, 8, 512], stack=st) for i in range(2)]
        wstg_b = bufs(2)
        wbf = [k.sb("e_wb%d" % i, [128, 8, 512], BF16, stack=st) for i in range(6)]
        wbf_b = bufs(6)
        sg = [k.sb("e_sg%d" % i, [128, 512], stack=st) for i in range(2)]
        sg_b = bufs(2)
        yo = [k.sb("e_yo%d" % i, [128, D], stack=st) for i in range(2)]
        yo_b = bufs(2)
        tpb = [k.ps("e_tp%d" % i, [128, 512], BF16, stack=st) for i in range(2)]
        tpb_b = bufs(2)
        pg = [k.ps("e_pg%d" % i, [128, 512], stack=st) for i in range(2)]
        pg_b = bufs(2)
        pu = [k.ps("e_pu%d" % i, [128, 512], stack=st) for i in range(2)]
        pu_b = bufs(2)
        pyy = [k.ps("e_py%d" % i, [128, 512], stack=st) for i in range(2)]
        pyy_b = bufs(2)
        regs = {}

        def mkregs(E):
            regs["b"] = E.alloc_register("bnd_x%d" % l)
            E.reg_mov(regs["b"], NT)
        s.raw("pool", mkregs)
        wi = [0]
        ci = [0]

        def load_w(src_ap, src_b, kind):
            i = wi[0]
            wi[0] += 1
            stg, stg_b = wstg[i % 2], wstg_b[i % 2]
            wb, wb_b = wbf[i % 6], wbf_b[i % 6]
            if kind == "col":
                s.dma("sp", lambda E: E.dma_start(out=stg[:], in_=src_ap.rearrange("(c p) n -> p c n", p=128)), [src_b], [stg_b])
            else:
                s.dma("sp", lambda E: E.dma_start(out=stg[:].rearrange("p a b -> p (a b)").rearrange("p (c n) -> p c n", c=4), in_=src_ap.rearrange("(c p) n -> p c n", p=128)), [src_b], [stg_b])
            eng = ["pool", "dve", "act"][ci[0] % 3]
            ci[0] += 1
            if eng == "act":
                s.op("act", lambda E: E.activation(out=wb[:], in_=stg[:], func=AF.Copy), [stg_b], [wb_b])
            else:
                s.op(eng, lambda E: E.tensor_copy(out=wb[:], in_=stg[:]), [stg_b], [wb_b])
            return wb, wb_b

        xi = 0
        gi = 0
        yi = 0
        est = {}

        def gather(e):
            m_, m_b = mt[e % 2], mt_b[e % 2]
            ix, ix_b = idx[e % 2], idx_b[e % 2]
            xsT, xsT_b = xsT2[e % 2], xsT2_b[e % 2]
            s.dma("sp", lambda E: E.dma_start(out=m_[:, 0:nst, :], in_=k.meta[e * SLOTS:e * SLOTS + nsl, :].rearrange("(t p) c -> p t c", p=128)), [k.meta_b[e]], [m_b])
            s.op("dve", lambda E: E.tensor_copy(out=ix[:, 0:nst], in_=m_[:, 0:nst, 0]), [m_b], [ix_b])
            for stl in range(nst):
                x_, x_b = xs[stl], xs_b[stl]
                s.dma("pool", lambda E: E.indirect_dma_start(
                    out=x_[:, :], out_offset=None, in_=k.h2tab[:, :], in_offset=bass.IndirectOffsetOnAxis(ap=ix[:, stl:stl + 1], axis=0),
                    bounds_check=Lazy(lambda: regs["b"]), oob_is_err=False), list(k.h2tab_b) + [ix_b], [x_b])
            est[e] = (m_, m_b, ix, ix_b, xsT, xsT_b)

        def transposes(e):
            nonlocal xi
            (m_, m_b, ix, ix_b, xsT, xsT_b) = est[e]
            for stl in range(nst):
                x_, x_b = xs[stl], xs_b[stl]
                p, pb = tpb[xi % 2], tpb_b[xi % 2]
                xi += 1
                for half in range(2):
                    for j in range(4):
                        c = half * 4 + j
                        s.op("pe", lambda E: E.transpose(out=p[:, j * 128:(j + 1) * 128], in_=x_[:, c * 128:(c + 1) * 128], identity=k.identb[:]),
                             [x_b, k.identb_b], [pb], track=(j == 3))
                    if half == 0:
                        s.op("act", lambda E: E.activation(out=xsT[:, 0:4, stl * 128:(stl + 1) * 128], in_=p[:].rearrange("p (c t) -> p c t", c=4), func=AF.Copy), [pb], [xsT_b])
                    else:
                        s.op("dve", lambda E: E.tensor_copy(out=xsT[:, 4:8, stl * 128:(stl + 1) * 128], in_=p[:].rearrange("p (c t) -> p c t", c=4)), [pb], [xsT_b])

        def compute(e):
            nonlocal gi, yi
            if e + 1 < NEXP:
                gather(e + 1)
            (m_, m_b, ix, ix_b, xsT, xsT_b) = est[e]
            for fb in range(4):
                wg, wg_b = load_w(I["moe_wg%d" % l][e * D:(e + 1) * D, fb * 512:(fb + 1) * 512], k.IB["moe_wg%d" % l], "col")
                wu, wu_b = load_w(I["moe_wu%d" % l][e * D:(e + 1) * D, fb * 512:(fb + 1) * 512], k.IB["moe_wu%d" % l], "col")
                for f4 in range(4):
                    f = fb * 4 + f4
                    for (s0, sw) in groups:
                        g_, g_b = pg[gi % 2], pg_b[gi % 2]
                        u_, u_b = pu[gi % 2], pu_b[gi % 2]
                        sg_, sg_bb = sg[gi % 2], sg_b[gi % 2]
                        gi += 1
                        for c in range(8):
                            s.op("pe", lambda E, c=c, f4=f4, g_=g_, wg=wg, s0=s0, sw=sw: E.matmul(g_[:, 0:sw], lhsT=wg[:, c, f4 * 128:(f4 + 1) * 128], rhs=xsT[:, c, s0:s0 + sw], start=(c == 0), stop=(c == 7)),
                                 [wg_b, xsT_b], [g_b], track=(c == 7))
                        for c in range(8):
                            s.op("pe", lambda E, c=c, f4=f4, u_=u_, wu=wu, s0=s0, sw=sw: E.matmul(u_[:, 0:sw], lhsT=wu[:, c, f4 * 128:(f4 + 1) * 128], rhs=xsT[:, c, s0:s0 + sw], start=(c == 0), stop=(c == 7)),
                                 [wu_b, xsT_b], [u_b], track=(c == 7))
                        s.op("act", lambda E, g_=g_, sg_=sg_, sw=sw: E.activation(out=sg_[:, 0:sw], in_=g_[:, 0:sw], func=AF.Silu), [g_b], [sg_bb])
                        s.op("dve", lambda E, u_=u_, sg_=sg_, f=f, s0=s0, sw=sw: E.tensor_tensor(out=aT[:, f, s0:s0 + sw], in0=u_[:, 0:sw], in1=sg_[:, 0:sw], op=ALU.mult), [u_b, sg_bb], [aT_b])
            if e + 1 < NEXP:
                transposes(e + 1)
            wds = []
            for rb in range(4):
                wds.append(load_w(I["moe_wd%d" % l][e * EDIM + rb * 512:e * EDIM + (rb + 1) * 512, :], k.IB["moe_wd%d" % l], "row"))
            for stl in range(nst):
                cond = 0 if stl < 8 else 1
                y_, y_b = yo[yi % 2], yo_b[yi % 2]
                yi += 1
                for nb in range(2):
                    p, p_b = pyy[nb], pyy_b[nb]
                    for f in range(16):
                        wd, wd_b = wds[f // 4]
                        wdv = wd[:].rearrange("p a b -> p (a b)").rearrange("p (c n) -> p c n", c=4)
                        s.op("pe", lambda E, f=f, nb=nb, p=p, wdv=wdv, stl=stl: E.matmul(p[:], lhsT=aT[:, f, stl * 128:(stl + 1) * 128], rhs=wdv[:, f % 4, nb * 512:(nb + 1) * 512], start=(f == 0), stop=(f == 15)),
                             [aT_b, wd_b], [p_b], track=(f == 15))
                    s.op("dve", lambda E, nb=nb, p=p, y_=y_, m_=m_, stl=stl, cond=cond: E.scalar_tensor_tensor(out=y_[:, nb * 512:(nb + 1) * 512], in0=p[:], scalar=m_[:, stl, 1:2], in1=G2b[cond][:, nb * 512:(nb + 1) * 512], op0=ALU.mult, op1=ALU.mult),
                         [p_b, m_b, G2b_b[cond]], [y_b])
                s.dma("pool", lambda E, y_=y_, ix=ix, stl=stl: E.indirect_dma_start(
                    out=k.xres[:, :], out_offset=bass.IndirectOffsetOnAxis(ap=ix[:, stl:stl + 1], axis=0), in_=y_[:, :], in_offset=None,
                    bounds_check=Lazy(lambda: regs["b"]), oob_is_err=True, compute_op=ALU.add), [y_b, ix_b], list(k.xres_b))


        gather(0)
        transposes(0)
        for e in range(NEXP):
            compute(e)

        def freeregs(E):
            E.free_register(regs["b"])
        s.raw("pool", freeregs)
        s.barrier()


def final_norm(k):
    nc, s, I = k.nc, k.s, k.I
    with ExitStack() as st:
        nf = k.sb("f_nf", [128, D], stack=st)
        nf_b = Buf()
        s.dma("sp", lambda E: E.dma_start(out=nf[:], in_=I["norm_final"].unsqueeze(0).broadcast_to([128, D])), [], [nf_b])
        xt = [k.sb("f_xt%d" % i, [128, D], stack=st) for i in range(2)]
        xt_b = bufs(2)
        ot = [k.sb("f_ot%d" % i, [128, D], stack=st) for i in range(2)]
        ot_b = bufs(2)
        junk = k.sb("f_junk", [128, D], stack=st)
        stat = [k.sb("f_stat%d" % i, [128, 2], stack=st) for i in range(2)]
        scr_b = bufs(2)
        k.out_b = Buf()
        for t in range(64):
            x_, x_b = xt[t % 2], xt_b[t % 2]
            o_, o_b = ot[t % 2], ot_b[t % 2]
            sc = stat[t % 2]
            sb_ = scr_b[t % 2]
            s.dma("sp", lambda E, t=t, x_=x_: E.dma_start(out=x_[:], in_=k.xres[t * 128:(t + 1) * 128, :]), [k.xres_b[t]], [x_b])
            s.op("act", lambda E, x_=x_, sc=sc: E.activation(out=junk[:], in_=x_[:], func=AF.Square, accum_out=sc[:, 0:1]), [x_b], [sb_])
            s.op("act", lambda E, sc=sc: E.activation(out=sc[:, 1:2], in_=sc[:, 0:1], func=AF.Ln, scale=float(1.0 / D), bias=k.epsc[:, 0:1]), [sb_, k.epsc_b], [sb_])
            s.op("act", lambda E, sc=sc: E.activation(out=sc[:, 1:2], in_=sc[:, 1:2], func=AF.Exp, scale=-0.5), [sb_], [sb_])
            s.op("dve", lambda E, x_=x_, o_=o_, sc=sc: E.scalar_tensor_tensor(out=o_[:], in0=x_[:], scalar=sc[:, 1:2], in1=nf[:], op0=ALU.mult, op1=ALU.mult), [x_b, sb_, nf_b], [o_b])
            s.dma("pool", lambda E, t=t, o_=o_: E.dma_start(out=k.out[t * 128:(t + 1) * 128, :], in_=o_[:]), [o_b], [k.out_b])


def _rope_tables_T(rot_dim, reps_rows):
    t = np.arange(NL)
    rows = (t // GRID_W).astype(np.float32)
    cols = (t % GRID_W).astype(np.float32)
    n_freq = rot_dim // 4
    inv_freq = (np.float32(10000.0) ** (-np.arange(n_freq, dtype=np.float32) / np.float32(n_freq))).astype(np.float32)
    ang = np.concatenate([rows[:, None] * inv_freq, cols[:, None] * inv_freq], axis=-1).astype(np.float32)
    cos = np.cos(ang).astype(np.float32)
    sin = np.sin(ang).astype(np.float32)
    half = rot_dim // 2
    cT = np.ones((rot_dim, NT), np.float32)
    sT = np.zeros((rot_dim, NT), np.float32)
    cT[:half, :NL] = cos.T
    cT[half:, :NL] = cos.T
    sT[:half, :NL] = -sin.T
    sT[half:, :NL] = sin.T
    return cT, sT


def _swap_halves_cols(w, unit):
    din, dout = w.shape
    w4 = w.reshape(din, dout // unit, 2, unit // 2)
    return np.ascontiguousarray(w4[:, :, ::-1, :]).reshape(din, dout)


def _na_bias(rpb):
    NEG = np.float32(-30000.0)
    qc = np.arange(64)
    cs = np.clip(qc - 8, 0, 48)
    kc = np.arange(64)
    valid = (kc[:, None] >= cs[None, :]) & (kc[:, None] < cs[None, :] + 16)
    colidx = np.clip(kc[:, None] - qc[None, :] + 15, 0, 30)
    out = np.full((16, 128, 8, 4, 64), NEG, np.float32)
    for v in range(8):
        for kt in range(4):
            for half in range(2):
                kr = 2 * kt + half
                ridx = kr - v + 7
                vals = rpb[:, ridx][:, colidx]
                out[:, half * 64:(half + 1) * 64, v, kt, :] = np.where(valid[None], vals, NEG)
    return out


def _shard(a2d, r):
    n = a2d.shape[0] // 8
    return a2d[r * n:(r + 1) * n]


def make_shared(inp):
    f = lambda a: np.ascontiguousarray(np.asarray(a, dtype=np.float32))
    S = {}
    S["ada_b"] = f(inp["ada_b"])
    S["norm_mix"] = f(inp["norm_mix"])
    S["norm_ffn"] = f(inp["norm_ffn"])
    S["norm_final"] = f(inp["norm_final"])
    S["ident"] = np.eye(128, dtype=np.float32)
    S["tokid"] = f((np.arange(NTILE)[None, :] * 128 + np.arange(128)[:, None]))
    mi = np.zeros((NEXP * SLOTS, 2), np.float32)
    mi[:, 0] = DUMMY
    S["metainit"] = mi
    BIG = float(2 ** 20)
    eb = np.zeros((NEXP, 2), np.float32)
    eb[:, 0] = np.arange(NEXP) * SLOTS - 1 - BIG
    eb[:, 1] = np.arange(NEXP) * SLOTS + CAP_L - 1 - BIG
    S["ebase"] = eb
    S["moe_wr"] = f(inp["moe_w_router"])
    S["da_lam"] = f(np.stack([inp["da_lambda_q1"][0], inp["da_lambda_k1"][0], inp["da_lambda_q2"][0], inp["da_lambda_k2"][0]]))
    S["da_subln"] = f(np.asarray(inp["da_subln"][0]).reshape(128, 1))
    G = {}
    for l in range(4):
        G["ada_w%d" % l] = f(inp["ada_w"][l])
        G["moe_wg%d" % l] = f(inp["moe_w_gate"][l]).reshape(NEXP * D, EDIM)
        G["moe_wu%d" % l] = f(inp["moe_w_up"][l]).reshape(NEXP * D, EDIM)
        G["moe_wd%d" % l] = f(inp["moe_w_down"][l]).reshape(NEXP * EDIM, D)
    G["da_w"] = f(inp["da_w_qkv"][0])
    G["da_wo"] = f(inp["da_w_o"][0])
    G["fn_wo"] = f(inp["fn_w_o"][0])
    n2 = np.arange(128, dtype=np.float64)[:, None, None]
    k1 = np.arange(64, dtype=np.float64)[None, :, None]
    k2 = np.arange(128, dtype=np.float64)[None, None, :]
    th = 2.0 * np.pi * n2 * (k1 + 64.0 * k2) / 8192.0
    G["fn_cos"] = f(np.cos(th).reshape(128, 8192))
    G["fn_sin"] = f(np.sin(th).reshape(128, 8192))
    n1 = np.arange(64, dtype=np.float64)[:, None]
    kk = np.arange(64, dtype=np.float64)[None, :]
    c64 = np.cos(2.0 * np.pi * n1 * kk / 64.0)
    s64 = np.sin(2.0 * np.pi * n1 * kk / 64.0)
    f64t = np.zeros((64, 4, 48))
    for kb in range(4):
        f64t[:, kb, 0:16] = c64[:, kb * 16:(kb + 1) * 16]
        f64t[:, kb, 16:32] = -s64[:, kb * 16:(kb + 1) * 16]
        f64t[:, kb, 32:48] = -c64[:, kb * 16:(kb + 1) * 16]
    S["fn_f64"] = f(f64t.reshape(64, 192))
    cc_ = np.arange(256, dtype=np.float64)
    th2 = 2.0 * np.pi * cc_[:, None] * cc_[None, :] / 256.0
    S["fn_cc"] = f(np.cos(th2))
    S["fn_sc"] = f(np.sin(th2))
    S["fn_nsc"] = f(-np.sin(th2))
    G["na_w"] = f(inp["na_w_qkv"][0])
    G["na_wo"] = f(inp["na_w_o"][0])
    G["na_bias"] = _na_bias(np.asarray(inp["na_rpb"][0], np.float32)).reshape(NEXP * 128, 2048)
    S["mla_qnorm"] = f(inp["mla_q_norm"][0])
    S["mla_kvnorm"] = f(inp["mla_kv_norm"][0])
    G["mla_wdq"] = f(inp["mla_w_dq"][0])
    G["mla_wuq"] = f(inp["mla_w_uq"][0])
    G["mla_wdkv"] = f(inp["mla_w_dkv"][0])
    G["mla_wuk"] = f(inp["mla_w_uk"][0])
    G["mla_wuv"] = f(inp["mla_w_uv"][0])
    G["mla_wo"] = f(inp["mla_w_o"][0])
    c3, s3 = _rope_tables_T(32, None)
    G["rope3c"] = f(np.concatenate([np.ones((64, NT), np.float32), c3], axis=0))
    G["rope3s"] = f(np.concatenate([np.zeros((64, NT), np.float32), s3], axis=0))
    c0, s0 = _rope_tables_T(64, None)
    G["rope0c"] = f(np.concatenate([c0, c0], axis=0))
    G["rope0s"] = f(np.concatenate([s0, s0], axis=0))
    return S, G


def make_core_inputs(inp, S, G, core):
    b = core // 2
    m = dict(S)
    m["xin"] = np.ascontiguousarray(np.concatenate([np.asarray(inp["x"][b], np.float32), np.asarray(inp["ctx"][b], np.float32)], axis=0))
    cc = np.stack([np.asarray(inp["c"][b], np.float32), np.asarray(inp["c_ctx"], np.float32)], axis=-1)
    m["ccT"] = np.ascontiguousarray(cc.reshape(8, 128, 2).transpose(1, 0, 2))
    for name, a in G.items():
        m[name] = _shard(a, core)
    return m


_NC_CACHE = {}


def kernel(**inputs):
    if "nc" not in _NC_CACHE:
        _NC_CACHE["nc"] = build(n_layers=4, dbg=False, gather=False)
    nc = _NC_CACHE["nc"]
    S, G = make_shared(inputs)
    names = set(LAST_INPUT_NAMES)
    in_maps = []
    for b in range(4):
        m = make_core_inputs(inputs, S, G, 2 * b)
        m.update(G)
        in_maps.append({n: v for n, v in m.items() if n in names})
    res = run_bass_kernel_spmd(nc, in_maps, core_ids=list(range(4)))
    out = np.stack([np.asarray(res.results[b]["out"], dtype=np.float32) for b in range(4)], axis=0)
    return out
```
